# Optimizing a Trainium2 kernel written in Bass

```python
import math
import jax, jax.numpy as jnp
from jax import lax
import numpy as np

D_MODEL = 2048
BATCH = 2
SEQ = 8192
DEPTH = 1

CONV_WIDTH = D_MODEL // 2
CONV_KERNEL = 31
N_HEADS = 16
HEAD_DIM = D_MODEL // N_HEADS
ROPE_THETA = 500000.0
ROT_FRACTION = 4
IDX_HEADS = 16
IDX_DIM = 64
TOPK_MAX = 256
Q_BLOCK = 128
IDX_SCALE = IDX_DIM ** -0.5 * IDX_HEADS ** -0.5
ATTN_SCALE = HEAD_DIM ** -0.5
NEG_BIG = -1e30
N_GROUPS = 4
EXPERTS_PER_GROUP = 8
N_EXPERTS = N_GROUPS * EXPERTS_PER_GROUP
TOP_EXPERTS = 2
EXPERT_FF = 512
MOE_TOKEN_BLOCK = 128
EPS = 1e-6

IN_WIDTHS = (
    2 * CONV_WIDTH,
    N_HEADS * HEAD_DIM,
    HEAD_DIM,
    HEAD_DIM,
    IDX_HEADS * IDX_DIM,
    IDX_DIM,
    IDX_HEADS,
    D_MODEL,
    D_MODEL,
)
IN_TOTAL = sum(IN_WIDTHS)

kernel_name = "hybrid_conformer_dsa_hmoe"


def rmsnorm(x, g):
    xf = x.astype(jnp.float32)
    y = xf * lax.rsqrt(jnp.mean(xf * xf, axis=-1, keepdims=True) + EPS)
    return (y * g.astype(jnp.float32)).astype(x.dtype)


def layernorm(x, g, b):
    xf = x.astype(jnp.float32)
    mu = jnp.mean(xf, axis=-1, keepdims=True)
    var = jnp.mean(jnp.square(xf - mu), axis=-1, keepdims=True)
    y = (xf - mu) * lax.rsqrt(var + EPS)
    return (y * g.astype(jnp.float32) + b.astype(jnp.float32)).astype(x.dtype)


def rope_tables(positions, rot_dim):
    inv = ROPE_THETA ** (-jnp.arange(0, rot_dim, 2, dtype=jnp.float32) / rot_dim)
    ang = positions.astype(jnp.float32)[..., None] * inv
    return jnp.cos(ang), jnp.sin(ang)


def apply_partial_rope(x, cos, sin):
    half = cos.shape[-1]
    xr = x[..., :2 * half].astype(jnp.float32)
    x1, x2 = xr[..., :half], xr[..., half:]
    c, s = cos[:, :, None, :], sin[:, :, None, :]
    rot = jnp.concatenate([x1 * c - x2 * s, x1 * s + x2 * c], axis=-1).astype(x.dtype)
    return jnp.concatenate([rot, x[..., 2 * half:]], axis=-1)


def split_columns(z):
    points, acc = [], 0
    for w in IN_WIDTHS[:-1]:
        acc += w
        points.append(acc)
    return jnp.split(z, points, axis=-1)


def conformer_conv(u, dw_w, dw_b, ln_g, ln_b, w_pw):
    a, gate = jnp.split(u, 2, axis=-1)
    y = a * jax.nn.sigmoid(gate)
    y = lax.conv_general_dilated(
        y, dw_w[:, None, :].astype(y.dtype), window_strides=(1,),
        padding=[(CONV_KERNEL - 1, 0)],
        dimension_numbers=('NWC', 'WIO', 'NWC'),
        feature_group_count=CONV_WIDTH) + dw_b
    y = jax.nn.silu(layernorm(y, ln_g, ln_b))
    return y @ w_pw


def indexed_sparse_attention(q, k, v, q_idx, k_idx, w_idx):
    B, T = q.shape[0], q.shape[1]
    n_sel = min(TOPK_MAX, T // 4)
    n_blocks = T // Q_BLOCK
    key_pos = jnp.arange(T)

    def block(i):
        start = i * Q_BLOCK
        qb = lax.dynamic_slice_in_dim(q, start, Q_BLOCK, axis=1)
        qib = lax.dynamic_slice_in_dim(q_idx, start, Q_BLOCK, axis=1)
        wb = lax.dynamic_slice_in_dim(w_idx, start, Q_BLOCK, axis=1)
        q_pos = start + jnp.arange(Q_BLOCK)
        causal = key_pos[None, :] <= q_pos[:, None]
        s = jnp.einsum('bqhd,bkd->bqhk', qib, k_idx).astype(jnp.float32)
        score = jnp.einsum('bqh,bqhk->bqk', wb.astype(jnp.float32), jax.nn.relu(s)) * IDX_SCALE
        score = jnp.where(causal[None], score, -jnp.inf)
        _, sel = lax.top_k(score, n_sel)
        k_sel = jax.vmap(lambda kb, ib: kb[ib])(k, sel)
        v_sel = jax.vmap(lambda vb, ib: vb[ib])(v, sel)
        valid = sel <= q_pos[None, :, None]
        logits = jnp.einsum('bqhd,bqkd->bqhk', qb, k_sel).astype(jnp.float32) * ATTN_SCALE
        logits = jnp.where(valid[:, :, None, :], logits, NEG_BIG)
        p = jax.nn.softmax(logits, axis=-1).astype(v.dtype)
        return jnp.einsum('bqhk,bqkd->bqhd', p, v_sel)

    out = lax.map(block, jnp.arange(n_blocks))
    return jnp.moveaxis(out, 0, 1).reshape(B, T, N_HEADS * HEAD_DIM)


def hierarchical_moe(hn, w_rg, b_rg, w_re, b_re, wg, wu, wd):
    B, T, D = hn.shape
    N = B * T
    xf = hn.reshape(N, D)
    g_logits = (xf @ w_rg).astype(jnp.float32) + b_rg.astype(jnp.float32)
    g_prob = jax.nn.softmax(g_logits, axis=-1)
    _, g_idx = lax.top_k(g_logits, 1)
    p_g = jnp.take_along_axis(g_prob, g_idx, axis=-1)
    e_logits = ((xf @ w_re).astype(jnp.float32) + b_re.astype(jnp.float32)).reshape(
        N, N_GROUPS, EXPERTS_PER_GROUP)
    e_in = jnp.take_along_axis(e_logits, g_idx[:, :, None], axis=1)[:, 0]
    e_top, e_idx = lax.top_k(e_in, TOP_EXPERTS)
    e_w = jax.nn.softmax(e_top, axis=-1)
    within = jnp.sum(jax.nn.one_hot(e_idx, EXPERTS_PER_GROUP, dtype=jnp.float32)
                     * e_w[..., None], axis=1)
    comb = (jax.nn.one_hot(g_idx[:, 0], N_GROUPS, dtype=jnp.float32) * p_g)[:, :, None] \
        * within[:, None, :]
    comb = comb.reshape(N, N_EXPERTS).astype(hn.dtype)

    def block(args):
        xb, cb = args
        gate = jnp.einsum('nd,edf->nef', xb, wg)
        up = jnp.einsum('nd,edf->nef', xb, wu)
        act = jax.nn.silu(gate) * up * cb[:, :, None]
        return jnp.einsum('nef,efd->nd', act, wd)

    nb = N // MOE_TOKEN_BLOCK
    y = lax.map(block, (xf.reshape(nb, MOE_TOKEN_BLOCK, D),
                        comb.reshape(nb, MOE_TOKEN_BLOCK, N_EXPERTS)))
    return y.reshape(B, T, D)


def setup_inputs(seed: int = 0) -> dict:
    key = jax.random.key(seed)
    ks = jax.random.split(key, 24)
    f32 = jnp.float32

    def nrm(k, shape, fan_in):
        return jax.random.normal(k, shape, f32) * (fan_in ** -0.5)

    def gain(k, shape):
        return 1.0 + 0.02 * jax.random.normal(k, shape, f32)

    L = DEPTH
    x = jax.random.normal(ks[0], (BATCH, SEQ, D_MODEL), f32)
    offset = jax.random.randint(ks[1], (BATCH, 1), 0, 4096, dtype=jnp.int32)
    positions = (jnp.arange(SEQ, dtype=jnp.int32)[None, :] + offset).astype(jnp.int32)
    return {
        "x": x,
        "positions": positions,
        "attn_norm_g": gain(ks[2], (L, D_MODEL)),
        "w_in": nrm(ks[3], (L, D_MODEL, IN_TOTAL), D_MODEL),
        "conv_dw_w": nrm(ks[4], (L, CONV_KERNEL, CONV_WIDTH), CONV_KERNEL),
        "conv_dw_b": 0.02 * jax.random.normal(ks[5], (L, CONV_WIDTH), f32),
        "conv_ln_g": gain(ks[6], (L, CONV_WIDTH)),
        "conv_ln_b": 0.02 * jax.random.normal(ks[7], (L, CONV_WIDTH), f32),
        "w_conv_out": nrm(ks[8], (L, CONV_WIDTH, D_MODEL), CONV_WIDTH),
        "q_norm_g": gain(ks[9], (L, HEAD_DIM)),
        "k_norm_g": gain(ks[10], (L, HEAD_DIM)),
        "w_attn_o": nrm(ks[11], (L, N_HEADS * HEAD_DIM, D_MODEL), N_HEADS * HEAD_DIM),
        "w_out": nrm(ks[12], (L, D_MODEL, D_MODEL), D_MODEL),
        "ffn_norm_g": gain(ks[13], (L, D_MODEL)),
        "w_router_group": nrm(ks[14], (L, D_MODEL, N_GROUPS), D_MODEL),
        "b_router_group": 0.01 * jax.random.normal(ks[15], (L, N_GROUPS), f32),
        "w_router_expert": nrm(ks[16], (L, D_MODEL, N_EXPERTS), D_MODEL),
        "b_router_expert": 0.01 * jax.random.normal(ks[17], (L, N_EXPERTS), f32),
        "w_exp_gate": nrm(ks[18], (L, N_EXPERTS, D_MODEL, EXPERT_FF), D_MODEL),
        "w_exp_up": nrm(ks[19], (L, N_EXPERTS, D_MODEL, EXPERT_FF), D_MODEL),
        "w_exp_down": nrm(ks[20], (L, N_EXPERTS, EXPERT_FF, D_MODEL), EXPERT_FF),
    }


def reference(x, positions, attn_norm_g, w_in, conv_dw_w, conv_dw_b, conv_ln_g, conv_ln_b,
              w_conv_out, q_norm_g, k_norm_g, w_attn_o, w_out, ffn_norm_g,
              w_router_group, b_router_group, w_router_expert, b_router_expert,
              w_exp_gate, w_exp_up, w_exp_down):
    B, T, _ = x.shape
    cos_a, sin_a = rope_tables(positions, HEAD_DIM // ROT_FRACTION)
    cos_i, sin_i = rope_tables(positions, IDX_DIM // ROT_FRACTION)
    h = x
    for l in range(DEPTH):
        hn = rmsnorm(h, attn_norm_g[l])
        z = hn @ w_in[l]
        u_conv, q, k, v, qi, ki, wi, g_conv, g_attn = split_columns(z)

        y_conv = conformer_conv(u_conv, conv_dw_w[l], conv_dw_b[l], conv_ln_g[l],
                                conv_ln_b[l], w_conv_out[l])

        q = apply_partial_rope(rmsnorm(q.reshape(B, T, N_HEADS, HEAD_DIM), q_norm_g[l]),
                               cos_a, sin_a)
        k = apply_partial_rope(rmsnorm(k.reshape(B, T, 1, HEAD_DIM), k_norm_g[l]),
                               cos_a, sin_a)[:, :, 0]
        qi = apply_partial_rope(qi.reshape(B, T, IDX_HEADS, IDX_DIM), cos_i, sin_i)
        ki = apply_partial_rope(ki.reshape(B, T, 1, IDX_DIM), cos_i, sin_i)[:, :, 0]
        y_attn = indexed_sparse_attention(q, k, v, qi, ki, wi) @ w_attn_o[l]

        mix = jax.nn.sigmoid(g_conv) * y_conv + jax.nn.sigmoid(g_attn) * y_attn
        h = h + mix @ w_out[l]

        hn2 = rmsnorm(h, ffn_norm_g[l])
        h = h + hierarchical_moe(hn2, w_router_group[l], b_router_group[l],
                                 w_router_expert[l], b_router_expert[l],
                                 w_exp_gate[l], w_exp_up[l], w_exp_down[l])
    return h
```

```python
import math
from contextlib import ExitStack

import numpy as np
import concourse.bass as bass
import concourse.mybir as mybir
from concourse.bass_utils import run_bass_kernel_spmd

F32 = mybir.dt.float32
BF16 = mybir.dt.bfloat16
I32 = mybir.dt.int32
AF = mybir.ActivationFunctionType
ALU = mybir.AluOpType
AX = mybir.AxisListType

D = 2048
SEQ = 8192
NT = SEQ // 128
NB = 16
EPS = 1e-6
IN_TOTAL = 9552
C_U, C_Q, C_K, C_V, C_QI, C_KI, C_WI, C_GC, C_GA = 0, 2048, 4096, 4224, 4352, 5376, 5440, 5456, 7504
NEG = -1.0e30
EPOCH = 30000
NSLOT = 8
NSEL = 256
NBIS = 16
ATTN_SCALE = 128 ** -0.5
NE = 32
FF = 512


def L(name, *a, **k):
    return lambda e: getattr(e, name)(*a, **k)


class Reg:
    __slots__ = ("w", "r", "name", "psum")

    def __init__(self, name="", psum=False):
        self.w = None
        self.r = []
        self.name = name
        self.psum = psum


class KB:
    def __init__(self, nc, es):
        self.nc = nc
        self.es = es
        self.names = ("pe", "act", "dve", "pool", "sp")
        self.sem = {}
        self.cnt = {e: 0 for e in ("pe", "act", "dve", "pool")}
        self.seen = {e: {} for e in self.names}
        self.dn = {q: 0 for q in ("sp", "act", "pool")}
        for q in self.dn:
            for i in range(NSLOT):
                self.sem[("d", q, i)] = es.enter_context(nc.semaphore(f"d_{q}{i}"))
        self.nins = 0
        self.prog = {e: [] for e in self.names}

    def emit(self):
        with self.nc.Block() as block:
            def run(en):
                def body(e):
                    for it in self.prog[en]:
                        if it[0] == "w":
                            e.wait_ge(it[1], it[2])
                        elif it[0] == "i":
                            it[1](e).then_inc(it[2], 1)
                        else:
                            e.dma_start(out=it[1], in_=it[2], **it[4]).then_inc(it[3], 16)
                return body
            block.sync(run("sp"))
            block.tensor(run("pe"))
            block.scalar(run("act"))
            block.vector(run("dve"))
            block.gpsimd(run("pool"))

    def _semfor(self, key):
        if key not in self.sem:
            self.sem[key] = self.es.enter_context(self.nc.semaphore(f"s_{key[0]}{key[1]}"))
        return self.sem[key]

    def _wait(self, en, evs):
        best = {}
        for k, v in evs:
            if best.get(k, 0) < v:
                best[k] = v
        for k, v in best.items():
            if self.seen[en].get(k, 0) < v:
                self.prog[en].append(("w", self._semfor(k), v))
                self.seen[en][k] = v

    def _deps(self, en, r, w, is_dma=False):
        evs = []
        for t in r:
            if t.w is not None:
                if not (en == "pe" and t.w[0][0] == "pe"):
                    evs.append(t.w)
            if t.psum:
                for ev in t.r:
                    if ev[0][0] != en:
                        evs.append(ev)
        for t in w:
            if t.w is not None:
                if not (en == "pe" and t.w[0][0] == "pe"):
                    evs.append(t.w)
            for ev in t.r:
                if en == "pe" and ev[0][0] == "pe":
                    continue
                evs.append(ev)
        return evs

    def _commit(self, ev, r, w):
        for t in r:
            t.r.append(ev)
            if len(t.r) > 48:
                best = {}
                for k, v in t.r:
                    if best.get(k, 0) < v:
                        best[k] = v
                t.r = list(best.items())
        for t in w:
            t.w = ev
            t.r = []

    def op(self, en, fn, r=(), w=()):
        self._wait(en, self._deps(en, r, w))
        c = self.cnt[en]
        key = (en, c // EPOCH)
        val = c % EPOCH + 1
        self.prog[en].append(("i", fn, self._semfor(key)))
        self.cnt[en] = c + 1
        self._commit((key, val), r, w)
        self.nins += 1

    def dma(self, q, out, in_, r=(), w=(), **kw):
        i = self.dn[q]
        slot, use = i % NSLOT, i // NSLOT
        key = ("d", q, slot)
        evs = self._deps(q, r, w, is_dma=True)
        if use > 0:
            evs.append((key, 16 * use))
        self._wait(q, evs)
        self.prog[q].append(("d", out, in_, self.sem[key], kw))
        self.dn[q] = i + 1
        self._commit((key, 16 * (use + 1)), r, w)
        self.nins += 1

    def all_events(self):
        evs = []
        for en, c in self.cnt.items():
            if c > 0:
                evs.append(((en, (c - 1) // EPOCH), (c - 1) % EPOCH + 1))
        for q, i in self.dn.items():
            for back in range(1, min(i, NSLOT) + 1):
                ii = i - back
                evs.append((("d", q, ii % NSLOT), 16 * (ii // NSLOT + 1)))
        return evs

    def barrier(self):
        evs = self.all_events()
        for en in self.names:
            self._wait(en, evs)

    def finish(self):
        self._wait("sp", self.all_events())


def build_nc(stage="full"):
    nc = bass.Bass("TRN2", target_bir_lowering=False)
    es = ExitStack()
    with es:
        _build(nc, es, stage)
    return nc


def _build(nc, es, stage):
    kb = KB(nc, es)
    dbg = stage != "full"

    def dram(name, shape, dt=F32, kind="ExternalInput"):
        return nc.dram_tensor(name, list(shape), dt, kind=kind).ap()

    def scratch(name, shape, dt, want_dbg):
        kind = "ExternalOutput" if (dbg and want_dbg) else "Internal"
        return nc.dram_tensor(name, list(shape), dt, kind=kind).ap()

    def mk_sb(stack):
        def sb(name, shape, dt=F32):
            return stack.enter_context(nc.sbuf_tensor(name, list(shape), dt))
        return sb

    sb = mk_sb(es)

    xb = dram("xb", [SEQ, D])
    pos_t = dram("pos_t", [128, NT], I32)
    pos_o = dram("pos_o", [128, NB], I32)
    consts = dram("consts", [128, 512])
    gA_d = dram("gA", [128, D])
    g2_d = dram("g2", [128, D])
    w_in = dram("w_in", [D, IN_TOTAL])
    ident_d = dram("ident", [128, 128])
    xo = dram("xo", [NB, 160, D])
    cmask_d = dram("cmask", [128, 512])
    cvec_d = dram("cvec", [128, 8 * 34])
    w_pw = dram("w_conv_out", [1024, D])
    w_ao = dram("w_attn_o", [D, D])
    w_o = dram("w_out", [D, D])
    wr_d = dram("wr", [D, 36])
    w_eg = dram("w_exp_gate", [NE, D, FF])
    w_eu = dram("w_exp_up", [NE, D, FF])
    w_ed = dram("w_exp_down", [NE, FF, D])
    out_d = dram("out", [NB, 128, D], kind="ExternalOutput")

    s_qT = scratch("s_qT", [NB, 128, 2048], BF16, stage.startswith("B1"))
    s_qiT = scratch("s_qiT", [NB, 128, 1024], BF16, stage.startswith("B1"))
    s_wi = scratch("s_wi", [NB, 128, 16], F32, stage.startswith("B1"))
    s_sT = scratch("s_sT", [NB, 128, 1024], BF16, stage.startswith("B1"))
    s_xT = scratch("s_xT", [NB, 128, 2048], BF16, stage.startswith("B1"))
    s_AT = scratch("s_AT", [NB, 128, 2048], BF16, stage in ("B2", "B2s"))
    s_hT = scratch("s_hT", [NB, 128, 2048], BF16, stage in ("B3", "B3s"))
    s_comb = scratch("s_comb", [128, NB * NE], F32, stage in ("B3", "B3s"))

    w_in_v = w_in.rearrange("(kc p) n -> p kc n", p=128)

    pall = es.enter_context(nc.psum_tensor("pall", [128, 4096], F32))
    pallh = pall.bitcast(BF16)
    r_pb = [Reg(f"pb{i}", psum=True) for i in range(8)]

    def PF(bank, c0=0, c1=512):
        return pall[:, bank * 512 + c0: bank * 512 + c1]

    def PH(bank, c0=0, c1=1024):
        return pallh[:, bank * 1024 + c0: bank * 1024 + c1]

    cst = sb("cst", [128, 512])
    r_cst = Reg("cst")
    kb.dma("sp", cst[:], consts, w=[r_cst])
    gqs = cst[:, 0:128]
    gk = cst[:, 128:256]
    invA = cst[:, 256:272]
    invI = cst[:, 272:280]
    brt = cst[:, 288:324]
    identf = sb("identf", [128, 128])
    identb = sb("identb", [128, 128], BF16)
    r_id = Reg("ident")
    kb.dma("sp", identf[:], ident_d, w=[r_id])
    kb.op("dve", L("tensor_copy", out=identb[:], in_=identf[:]), r=[r_id], w=[r_id])
    epst = sb("epst", [128, 1])
    r_eps = Reg("eps")
    kb.op("dve", L("memset", epst[:], EPS), w=[r_eps])
    gbuf = sb("gbuf", [128, D])
    r_gbuf = Reg("gbuf")

    def rope_tables(cos_t, sin_t, name, posf_ap, ncol, inv_ap, half, r_in, tmp_stack):
        tsb = mk_sb(tmp_stack)
        u = tsb(name + "_u", [128, ncol, half])
        ki = tsb(name + "_ki", [128, ncol, half], I32)
        kf = tsb(name + "_kf", [128, ncol, half])
        d = tsb(name + "_d", [128, ncol, half])
        m1 = tsb(name + "_m1", [128, ncol, half])
        rr = Reg(name)
        kb.op("dve", L("tensor_tensor", out=u[:], in0=posf_ap.unsqueeze(2).broadcast_to([128, ncol, half]),
                       in1=inv_ap.unsqueeze(1).broadcast_to([128, ncol, half]), op=ALU.mult),
              r=[r_in, r_cst], w=[rr])
        kb.op("dve", L("tensor_scalar", out=u[:], in0=u[:], scalar1=1.0 / (2 * math.pi), scalar2=None,
                       op0=ALU.mult), r=[rr], w=[rr])
        for shift, dst in ((0.0, sin_t), (0.25, cos_t)):
            src = u
            if shift != 0.0:
                kb.op("dve", L("tensor_scalar", out=d[:], in0=u[:], scalar1=shift, scalar2=None, op0=ALU.add),
                      r=[rr], w=[rr])
                src = d
            kb.op("dve", L("tensor_copy", out=ki[:], in_=src[:]), r=[rr], w=[rr])
            kb.op("dve", L("tensor_copy", out=kf[:], in_=ki[:]), r=[rr], w=[rr])
            kb.op("dve", L("tensor_tensor", out=d[:], in0=src[:], in1=kf[:], op=ALU.subtract), r=[rr], w=[rr])
            kb.op("dve", L("tensor_scalar", out=m1[:], in0=d[:], scalar1=0.5, scalar2=None, op0=ALU.is_gt),
                  r=[rr], w=[rr])
            kb.op("dve", L("tensor_tensor", out=d[:], in0=d[:], in1=m1[:], op=ALU.subtract), r=[rr], w=[rr])
            kb.op("dve", L("tensor_scalar", out=m1[:], in0=d[:], scalar1=-0.5, scalar2=None, op0=ALU.is_lt),
                  r=[rr], w=[rr])
            kb.op("dve", L("tensor_tensor", out=d[:], in0=d[:], in1=m1[:], op=ALU.add), r=[rr], w=[rr])
            kb.op("act", L("activation", out=dst[:], in_=d[:], func=AF.Sin, scale=2 * math.pi * (1 - 1e-6)),
                  r=[rr], w=[rr])
        return cos_t, sin_t, rr

    def rope(dst, src, half, cos_ap, sin_ap, t4, r_src, r_dst, r_tab, r_t4):
        def sl(ap, a, b):
            return ap[:, a:b] if len(ap.shape) == 2 else ap[:, :, a:b]
        x1, x2 = sl(src, 0, half), sl(src, half, 2 * half)
        ts = [t4[i] for i in range(4)]
        kb.op("dve", L("tensor_tensor", out=ts[0], in0=x1, in1=cos_ap, op=ALU.mult), r=[r_src, r_tab], w=[r_t4])
        kb.op("dve", L("tensor_tensor", out=ts[1], in0=x2, in1=sin_ap, op=ALU.mult), r=[r_src, r_tab], w=[r_t4])
        kb.op("dve", L("tensor_tensor", out=ts[2], in0=x1, in1=sin_ap, op=ALU.mult), r=[r_src, r_tab], w=[r_t4])
        kb.op("dve", L("tensor_tensor", out=ts[3], in0=x2, in1=cos_ap, op=ALU.mult), r=[r_src, r_tab], w=[r_t4])
        kb.op("dve", L("tensor_tensor", out=sl(dst, 0, half), in0=ts[0], in1=ts[1], op=ALU.subtract),
              r=[r_t4], w=[r_dst])
        kb.op("dve", L("tensor_tensor", out=sl(dst, half, 2 * half), in0=ts[2], in1=ts[3], op=ALU.add),
              r=[r_t4], w=[r_dst])

    def rstd_from_ss(st, c_ss, n, r_st):
        kb.op("act", L("activation", out=st[:, c_ss + 1:c_ss + 2], in_=st[:, c_ss:c_ss + 1], func=AF.Sqrt,
                       scale=1.0 / n, bias=epst[0:st.shape[0], :]), r=[r_st, r_eps], w=[r_st])
        kb.op("dve", L("reciprocal", out=st[:, c_ss + 2:c_ss + 3], in_=st[:, c_ss + 1:c_ss + 2]),
              r=[r_st], w=[r_st])

    posoi = sb("posoi", [128, NB], I32)
    posof = sb("posof", [128, NB])
    r_poso = Reg("poso")
    kb.dma("sp", posoi[:], pos_o, w=[r_poso])
    kb.op("dve", L("tensor_copy", out=posof[:], in_=posoi[:]), r=[r_poso], w=[r_poso])
    cosAo, sinAo = sb("rao_cos", [128, NB, 16]), sb("rao_sin", [128, NB, 16])
    cosIo, sinIo = sb("rio_cos", [128, NB, 8]), sb("rio_sin", [128, NB, 8])
    with ExitStack() as tmps:
        _, _, r_ropeAo = rope_tables(cosAo, sinAo, "rao", posof[:], NB, invA, 16, r_poso, tmps)
        _, _, r_ropeIo = rope_tables(cosIo, sinIo, "rio", posof[:], NB, invI, 8, r_poso, tmps)
        kb.barrier()

    ngroups = 4 if stage not in ("B1s", "B2s", "B3s", "Ms") and not stage.startswith("B1c") else 1
    cut = int(stage[3:]) if stage.startswith("B1c") else 99
    if stage in ("smoke", "A"):
        ngroups = 0
    with ExitStack() as lb:
        lsb = mk_sb(lb)
        kb.dma("sp", gbuf[:], gA_d, w=[r_gbuf])
        cw = lsb("cw", [128, 8, 34])
        r_cw = Reg("cw")
        kb.dma("sp", cw[:].rearrange("p a b -> p (a b)"), cvec_d, w=[r_cw])
        onesf = lsb("onesf", [128, 128])
        r_ones = Reg("ones")
        kb.op("dve", L("memset", onesf[:], 1.0 / 1024.0), w=[r_ones])
        xT_g = lsb("xT_g", [128, 16, 640], BF16)
        r_xTg = [Reg(f"xTg{i}") for i in range(4)]
        xm = [lsb(f"xm{i}", [128, D]) for i in range(2)]
        r_xm = [Reg(f"xm{i}") for i in range(2)]
        xhl1 = lsb("xhl", [32, D])
        xhl = [xhl1, xhl1]
        r_xhl1 = Reg("xhl")
        r_xhl = [r_xhl1, r_xhl1]
        xs = [lsb(f"xsb{i}", [128, D], BF16) for i in range(2)]
        r_xs = [Reg(f"xsb{i}") for i in range(2)]
        xsh1 = lsb("xsh", [32, D], BF16)
        xsh = [xsh1, xsh1]
        r_xsh1 = Reg("xsh")
        r_xsh = [r_xsh1, r_xsh1]
        st = [lsb(f"stb{i}", [128, 8]) for i in range(2)]
        r_st = [Reg(f"stb{i}") for i in range(2)]
        yT = lsb("yT", [128, 8, 640])
        r_yT = [Reg(f"yT{i}") for i in range(8)]
        cacc = lsb("cacc", [128, 8, 512])
        r_cacc = [Reg(f"cacc{i}") for i in range(8)]
        sT = lsb("sT", [128, 8, 512], BF16)
        r_sT = Reg("sT")
        wu = [lsb(f"wu{i}", [128, 16, 256], BF16) for i in range(2)]
        r_wu = [Reg(f"wu{i}") for i in range(2)]
        wbuf = [lsb(f"wbuf{i}", [128, 16, 512], BF16) for i in range(2)]
        r_wbuf = [Reg(f"wbuf{i}") for i in range(2)]
        sg = [lsb(f"sg{i}", [128, 640]) for i in range(2)]
        r_sg = [Reg(f"sg{i}") for i in range(2)]
        lnm = lsb("lnm", [128, 3, 512])
        r_lnm = Reg("lnm")
        qsq = lsb("qsq", [128, 512])
        r_qsq = Reg("qsq")
        stq = [lsb(f"stq{i}", [128, 12]) for i in range(2)]
        r_stq = [Reg(f"stq{i}") for i in range(2)]
        qn = [lsb(f"qn{i}", [128, 4, 128]) for i in range(2)]
        r_qn = [Reg(f"qn{i}") for i in range(2)]
        qb = [lsb(f"qb{i}", [128, 512], BF16) for i in range(2)]
        r_qb = [Reg(f"qb{i}") for i in range(2)]
        t4q = [lsb(f"t4q{i}", [128, 4, 8, 16]) for i in range(2)]
        r_t4q = [Reg(f"t4q{i}") for i in range(2)]
        qTs = [lsb(f"qTs{i}", [128, 4, 128], BF16) for i in range(2)]
        r_qTs = [Reg(f"qTs{i}") for i in range(2)]
        wis = lsb("wis", [128, 4, 16])
        r_wis = Reg("wis")
        wwi = lsb("wwi", [128, 16, 16], BF16)
        r_wwi = Reg("wwi")
        kb.dma("pool", wwi[:], w_in_v[:, :, C_WI:C_WI + 16], w=[r_wwi])

        wu_n = [0]
        wb_n = [0]

        def load_wbuf(c0):
            i = wb_n[0] % 2
            wb_n[0] += 1
            kb.dma("pool", wbuf[i][:], w_in_v[:, :, c0:c0 + 512], w=[r_wbuf[i]])
            return i

        for gi in range(ngroups):
            for bl in range(4):
                m = 4 * gi + bl
                b2 = bl % 2
                bT = 2 * b2
                kb.dma("sp", xm[b2][:], xo[m, 32:160, :], w=[r_xm[b2]])
                kb.dma("sp", xhl[b2][:], xo[m, 0:32, :], w=[r_xhl[b2]])
                kb.op("act", L("activation", out=xs[b2][:], in_=xm[b2][:], func=AF.Square,
                               accum_out=st[b2][:, 0:1]), r=[r_xm[b2]], w=[r_xs[b2], r_st[b2]])
                rstd_from_ss(st[b2], 0, D, r_st[b2])
                kb.op("dve", L("scalar_tensor_tensor", out=xs[b2][:], in0=xm[b2][:], scalar=st[b2][:, 2:3],
                               in1=gbuf[:], op0=ALU.mult, op1=ALU.mult),
                      r=[r_xm[b2], r_st[b2], r_gbuf], w=[r_xs[b2]])
                for kc in range(16):
                    kb.op("pe", L("transpose", out=PH(bT, kc * 128, (kc + 1) * 128),
                                  in_=xs[b2][:, kc * 128:(kc + 1) * 128], identity=identb[:]),
                          r=[r_xs[b2], r_id], w=[r_pb[bT + kc // 8]])
                kb.op("act", L("copy", out=xT_g[:, :, bl * 128:(bl + 1) * 128],
                               in_=PH(bT, 0, 2048).rearrange("p (a b) -> p a b", a=16)),
                      r=[r_pb[bT], r_pb[bT + 1]], w=[r_xTg[bl]])
                kb.dma("sp", s_xT[m].rearrange("p (a b) -> p a b", a=16), xT_g[:, :, bl * 128:(bl + 1) * 128],
                       r=[r_xTg[bl]])
                bH = 4 + b2
                kb.op("act", L("activation", out=xsh[b2][:], in_=xhl[b2][:], func=AF.Square,
                               accum_out=st[b2][0:32, 3:4]), r=[r_xhl[b2]], w=[r_xsh[b2], r_st[b2]])
                rstd_from_ss(st[b2][0:32, :], 3, D, r_st[b2])
                kb.op("dve", L("scalar_tensor_tensor", out=xsh[b2][:], in0=xhl[b2][:], scalar=st[b2][0:32, 5:6],
                               in1=gbuf[0:32, :], op0=ALU.mult, op1=ALU.mult),
                      r=[r_xhl[b2], r_st[b2], r_gbuf], w=[r_xsh[b2]])
                for kc in range(16):
                    kb.op("pe", L("transpose", out=PH(bH, kc * 32, (kc + 1) * 32),
                                  in_=xsh[b2][:, kc * 128:(kc + 1) * 128], identity=identb[0:32, 0:32]),
                          r=[r_xsh[b2], r_id], w=[r_pb[bH]])
                kb.op("act", L("copy", out=xT_g[:, :, 512 + bl * 32:512 + (bl + 1) * 32],
                               in_=PH(bH, 0, 512).rearrange("p (a b) -> p a b", a=16)),
                      r=[r_pb[bH]], w=[r_xTg[bl]])

            if cut <= 1:
                break
            for cc in range(8):
                i = wu_n[0] % 2
                wu_n[0] += 1
                kb.dma("pool", wu[i][:, :, 0:128], w_in_v[:, :, C_U + cc * 128:C_U + (cc + 1) * 128], w=[r_wu[i]])
                kb.dma("pool", wu[i][:, :, 128:256], w_in_v[:, :, C_U + 1024 + cc * 128:C_U + 1024 + (cc + 1) * 128],
                       w=[r_wu[i]])
                b3 = 3 * (cc % 2)
                bA, bG, bHh = b3, b3 + 1, b3 + 2
                for (bank, c0, c1, wc, x0, x1) in ((bA, 0, 512, 0, 0, 512), (bG, 0, 512, 128, 0, 512),
                                                   (bHh, 0, 128, 0, 512, 640), (bHh, 128, 256, 128, 512, 640)):
                    for kc in range(16):
                        kb.op("pe", L("matmul", out=PF(bank, c0, c1), lhsT=wu[i][:, kc, wc:wc + 128],
                                      rhs=xT_g[:, kc, x0:x1], start=(kc == 0), stop=(kc == 15)),
                              r=[r_wu[i]] + r_xTg, w=[r_pb[bank]])
                s2 = cc % 2
                kb.op("act", L("activation", out=sg[s2][:, 0:512], in_=PF(bG), func=AF.Sigmoid),
                      r=[r_pb[bG]], w=[r_sg[s2]])
                kb.op("act", L("activation", out=sg[s2][:, 512:640], in_=PF(bHh, 128, 256), func=AF.Sigmoid),
                      r=[r_pb[bHh]], w=[r_sg[s2]])
                yv = yT[:, cc, :].rearrange("p (b t) -> p b t", b=4)
                kb.op("dve", L("tensor_tensor", out=yv[:, :, 32:160],
                               in0=PF(bA).rearrange("p (b t) -> p b t", b=4),
                               in1=sg[s2][:, 0:512].rearrange("p (b t) -> p b t", b=4), op=ALU.mult),
                      r=[r_pb[bA], r_sg[s2]], w=[r_yT[cc]])
                kb.op("dve", L("tensor_tensor", out=yv[:, :, 0:32],
                               in0=PF(bHh, 0, 128).rearrange("p (b t) -> p b t", b=4),
                               in1=sg[s2][:, 512:640].rearrange("p (b t) -> p b t", b=4), op=ALU.mult),
                      r=[r_pb[bHh], r_sg[s2]], w=[r_yT[cc]])

            if cut <= 2:
                break
            for cc in range(8):
                yv = yT[:, cc, :].rearrange("p (b t) -> p b t", b=4)
                av = cacc[:, cc, :].rearrange("p (b t) -> p b t", b=4)
                kb.op("dve", L("tensor_scalar", out=av, in0=yv[:, :, 2:130], scalar1=cw[:, cc, 0:1],
                               scalar2=cw[:, cc, 31:32], op0=ALU.mult, op1=ALU.add),
                      r=[r_yT[cc], r_cw], w=[r_cacc[cc]])
                for k in range(1, 31):
                    kb.op("dve", L("scalar_tensor_tensor", out=av, in0=yv[:, :, 2 + k:130 + k],
                                   scalar=cw[:, cc, k:k + 1], in1=av, op0=ALU.mult, op1=ALU.add),
                          r=[r_yT[cc], r_cw, r_cacc[cc]], w=[r_cacc[cc]])
            bM, bS = 6, 7
            for cc in range(8):
                kb.op("pe", L("matmul", out=PF(bM), lhsT=onesf[:], rhs=cacc[:, cc, :], start=(cc == 0),
                              stop=(cc == 7)), r=[r_ones, r_cacc[cc]], w=[r_pb[bM]])
            sqv = yT[:].rearrange("p a b -> p (a b)")[:, 0:4096].rearrange("p (a b) -> p a b", a=8)
            kb.op("act", L("activation", out=sqv, in_=cacc[:], func=AF.Square), r=r_cacc, w=r_yT)
            for cc in range(8):
                kb.op("pe", L("matmul", out=PF(bS), lhsT=onesf[:], rhs=sqv[:, cc, :], start=(cc == 0),
                              stop=(cc == 7)), r=[r_ones] + r_yT, w=[r_pb[bS]])
            kb.op("act", L("copy", out=lnm[:, 0, :], in_=PF(bM)), r=[r_pb[bM]], w=[r_lnm])
            kb.op("dve", L("tensor_tensor", out=lnm[:, 1, :], in0=lnm[:, 0, :], in1=lnm[:, 0, :], op=ALU.mult),
                  r=[r_lnm], w=[r_lnm])
            kb.op("dve", L("tensor_tensor", out=lnm[:, 1, :], in0=PF(bS), in1=lnm[:, 1, :], op=ALU.subtract),
                  r=[r_pb[bS], r_lnm], w=[r_lnm])
            kb.op("act", L("activation", out=lnm[:, 2, :], in_=lnm[:, 1, :], func=AF.Sqrt, bias=epst[:]),
                  r=[r_lnm, r_eps], w=[r_lnm])
            kb.op("dve", L("reciprocal", out=lnm[:, 1, :], in_=lnm[:, 2, :]), r=[r_lnm], w=[r_lnm])
            for cc in range(8):
                kb.op("dve", L("tensor_tensor", out=cacc[:, cc, :], in0=cacc[:, cc, :], in1=lnm[:, 0, :],
                               op=ALU.subtract), r=[r_cacc[cc], r_lnm], w=[r_cacc[cc]])
                kb.op("dve", L("tensor_tensor", out=cacc[:, cc, :], in0=cacc[:, cc, :], in1=lnm[:, 1, :],
                               op=ALU.mult), r=[r_cacc[cc], r_lnm], w=[r_cacc[cc]])
                kb.op("act", L("activation", out=sT[:, cc, :], in_=cacc[:, cc, :], func=AF.Silu,
                               scale=cw[:, cc, 32:33], bias=cw[:, cc, 33:34]),
                      r=[r_cacc[cc], r_cw], w=[r_sT])
            for bl in range(4):
                m = 4 * gi + bl
                kb.dma("sp", s_sT[m].rearrange("p (a b) -> p a b", a=8), sT[:, :, bl * 128:(bl + 1) * 128],
                       r=[r_sT])

            if cut <= 3:
                break
            it = 0
            for qc in range(4):
                wi_ = load_wbuf(C_Q + qc * 512)
                for bl in range(4):
                    m = 4 * gi + bl
                    p2 = it % 2
                    it += 1
                    bQ = p2
                    bT = 2 + p2
                    for kc in range(16):
                        kb.op("pe", L("matmul", out=PF(bQ), lhsT=xT_g[:, kc, bl * 128:(bl + 1) * 128],
                                      rhs=wbuf[wi_][:, kc, :], start=(kc == 0), stop=(kc == 15)),
                              r=[r_xTg[bl], r_wbuf[wi_]], w=[r_pb[bQ]])
                    kb.op("act", L("activation", out=qsq[:], in_=PF(bQ), func=AF.Square),
                          r=[r_pb[bQ]], w=[r_qsq])
                    kb.op("dve", L("tensor_reduce", out=stq[p2][:, 0:4],
                                   in_=qsq[:].rearrange("p (h d) -> p h d", h=4), axis=AX.X, op=ALU.add),
                          r=[r_qsq], w=[r_stq[p2]])
                    kb.op("act", L("activation", out=stq[p2][:, 4:8], in_=stq[p2][:, 0:4], func=AF.Sqrt,
                                   scale=1.0 / 128, bias=epst[:]), r=[r_stq[p2], r_eps], w=[r_stq[p2]])
                    kb.op("dve", L("reciprocal", out=stq[p2][:, 8:12], in_=stq[p2][:, 4:8]),
                          r=[r_stq[p2]], w=[r_stq[p2]])
                    kb.op("dve", L("tensor_tensor", out=qn[p2][:],
                                   in0=PF(bQ).rearrange("p (h d) -> p h d", h=4),
                                   in1=stq[p2][:, 8:12].unsqueeze(2).broadcast_to([128, 4, 128]), op=ALU.mult),
                          r=[r_pb[bQ], r_stq[p2]], w=[r_qn[p2]])
                    kb.op("dve", L("tensor_tensor", out=qn[p2][:], in0=qn[p2][:],
                                   in1=gqs.unsqueeze(1).broadcast_to([128, 4, 128]), op=ALU.mult),
                          r=[r_qn[p2], r_cst], w=[r_qn[p2]])
                    qbv = qb[p2][:].rearrange("p (h d) -> p h d", h=4)
                    kb.op("act", L("copy", out=qbv[:, :, 32:128], in_=qn[p2][:, :, 32:128]),
                          r=[r_qn[p2]], w=[r_qb[p2]])
                    t4 = [t4q[p2][:, i, 0:4, :] for i in range(4)]
                    rope(qbv, qn[p2][:], 16, cosAo[:, m, :].unsqueeze(1).broadcast_to([128, 4, 16]),
                         sinAo[:, m, :].unsqueeze(1).broadcast_to([128, 4, 16]), t4,
                         r_qn[p2], r_qb[p2], r_ropeAo, r_t4q[p2])
                    for h in range(4):
                        kb.op("pe", L("transpose", out=PH(bT, h * 128, (h + 1) * 128),
                                      in_=qb[p2][:, h * 128:(h + 1) * 128], identity=identb[:]),
                              r=[r_qb[p2], r_id], w=[r_pb[bT]])
                    kb.op("act", L("copy", out=qTs[p2][:],
                                   in_=PH(bT, 0, 512).rearrange("p (a b) -> p a b", a=4)),
                          r=[r_pb[bT]], w=[r_qTs[p2]])
                    kb.dma("sp", s_qT[m].rearrange("p (a b) -> p a b", a=16)[:, 4 * qc:4 * qc + 4, :], qTs[p2][:],
                           r=[r_qTs[p2]])

            if cut <= 4:
                break
            for c2 in range(2):
                wi_ = load_wbuf(C_QI + c2 * 512)
                for bl in range(4):
                    m = 4 * gi + bl
                    p2 = it % 2
                    it += 1
                    bQ = p2
                    bT = 2 + p2
                    for kc in range(16):
                        kb.op("pe", L("matmul", out=PF(bQ), lhsT=xT_g[:, kc, bl * 128:(bl + 1) * 128],
                                      rhs=wbuf[wi_][:, kc, :], start=(kc == 0), stop=(kc == 15)),
                              r=[r_xTg[bl], r_wbuf[wi_]], w=[r_pb[bQ]])
                    kb.op("act", L("copy", out=qn[p2][:].rearrange("p a b -> p (a b)"), in_=PF(bQ)),
                          r=[r_pb[bQ]], w=[r_qn[p2]])
                    import os
                    SUB = int(os.environ.get("SUB", "9"))
                    if SUB <= 1:
                        continue
                    pv = qn[p2][:].rearrange("p a b -> p (a b)").rearrange("p (h d) -> p h d", h=8)
                    qbv = qb[p2][:].rearrange("p (h d) -> p h d", h=8)
                    kb.op("act", L("copy", out=qbv[:, :, 16:64], in_=pv[:, :, 16:64]),
                          r=[r_qn[p2]], w=[r_qb[p2]])
                    if SUB <= 2:
                        continue
                    t4 = [t4q[p2][:, i, :, 0:8] for i in range(4)]
                    rope(qbv, pv, 8, cosIo[:, m, :].unsqueeze(1).broadcast_to([128, 8, 8]),
                         sinIo[:, m, :].unsqueeze(1).broadcast_to([128, 8, 8]), t4,
                         r_qn[p2], r_qb[p2], r_ropeIo, r_t4q[p2])
                    if SUB <= 3:
                        continue
                    for h in range(4):
                        kb.op("pe", L("transpose", out=PH(bT, h * 128, (h + 1) * 128),
                                      in_=qb[p2][:, h * 128:(h + 1) * 128], identity=identb[:]),
                              r=[r_qb[p2], r_id], w=[r_pb[bT]])
                    kb.op("act", L("copy", out=qTs[p2][:],
                                   in_=PH(bT, 0, 512).rearrange("p (a b) -> p a b", a=4)),
                          r=[r_pb[bT]], w=[r_qTs[p2]])
                    if SUB <= 4:
                        continue
                    kb.dma("sp", s_qiT[m].rearrange("p (a b) -> p a b", a=8)[:, 4 * c2:4 * c2 + 4, :], qTs[p2][:],
                           r=[r_qTs[p2]])
            if cut <= 5:
                break
            for bl in range(4):
                m = 4 * gi + bl
                bQ = 4 + bl % 2
                for kc in range(16):
                    kb.op("pe", L("matmul", out=PF(bQ, 0, 16), lhsT=xT_g[:, kc, bl * 128:(bl + 1) * 128],
                                  rhs=wwi[:, kc, :], start=(kc == 0), stop=(kc == 15)),
                          r=[r_xTg[bl], r_wwi], w=[r_pb[bQ]])
                kb.op("act", L("copy", out=wis[:, bl, :], in_=PF(bQ, 0, 16)), r=[r_pb[bQ]], w=[r_wis])
                kb.dma("sp", s_wi[m], wis[:, bl, :], r=[r_wis])
        kb.barrier()

    if stage in ("B1", "B1s") or stage.startswith("B1c"):
        kb.finish()
        kb.emit()
        print("instructions:", kb.nins)
        return

    kvs = ExitStack()
    with kvs:
        ksb = mk_sb(kvs)
        KT = ksb("KT", [128, SEQ], BF16)
        Vt = ksb("Vt", [128, NT, 129], BF16)
        KIT = ksb("KIT", [128, SEQ], BF16)
        r_KT = [Reg(f"KT{n}") for n in range(NT)]
        r_V = [Reg(f"V{n}") for n in range(NT)]
        r_KIT = [Reg(f"KIT{n}") for n in range(NT)]
        r_vone = Reg("vone")
        kb.op("pool", L("memset", Vt[:, :, 128:129], 1.0), w=[r_vone])

        ntile_a = NT if stage not in ("smoke", "B1s", "B2s", "B3s", "Ms") else (2 if stage not in ("B2s", "B3s", "Ms") else 16)
        with ExitStack() as la:
            lsb = mk_sb(la)
            kb.dma("sp", gbuf[:], gA_d, w=[r_gbuf])
            posi = lsb("posi", [128, NT], I32)
            posf = lsb("posf", [128, NT])
            r_pos = Reg("pos")
            kb.dma("sp", posi[:], pos_t, w=[r_pos])
            kb.op("dve", L("tensor_copy", out=posf[:], in_=posi[:]), r=[r_pos], w=[r_pos])
            cosA, sinA = lsb("ra_cos", [128, NT, 16]), lsb("ra_sin", [128, NT, 16])
            cosI, sinI = lsb("ri_cos", [128, NT, 8]), lsb("ri_sin", [128, NT, 8])
            with ExitStack() as tmps:
                _, _, r_ropeA = rope_tables(cosA, sinA, "ra", posf[:], NT, invA, 16, r_pos, tmps)
                _, _, r_ropeI = rope_tables(cosI, sinI, "ri", posf[:], NT, invI, 8, r_pos, tmps)
                kb.barrier()
            wkv = lsb("wkv", [128, 16, 320], BF16)
            r_wkv = Reg("wkv")
            for (c0, c1, o0) in ((C_K, C_K + 256, 0), (C_KI, C_KI + 64, 256)):
                kb.dma("pool", wkv[:, :, o0:o0 + (c1 - c0)], w_in_v[:, :, c0:c1], w=[r_wkv])
            xin = [lsb(f"xin{i}", [128, D]) for i in range(2)]
            r_xin = [Reg(f"xin{i}") for i in range(2)]
            junk = lsb("junk", [128, D], BF16)
            r_junk = Reg("junk")
            xs = [lsb(f"xs{i}", [128, D], BF16) for i in range(2)]
            r_xs = [Reg(f"xs{i}") for i in range(2)]
            xT = [lsb(f"xT{i}", [128, 16, 128], BF16) for i in range(2)]
            r_xT = [Reg(f"xT{i}") for i in range(2)]
            st = [lsb(f"st{i}", [128, 8]) for i in range(2)]
            r_st = [Reg(f"st{i}") for i in range(2)]
            kn = [lsb(f"kn{i}", [128, 192]) for i in range(2)]
            kfin = [lsb(f"kfin{i}", [128, 256], BF16) for i in range(2)]
            tmp = [lsb(f"tmp{i}", [128, 4, 16]) for i in range(2)]
            r_kn = [Reg(f"kn{i}") for i in range(2)]
            r_kf = [Reg(f"kf{i}") for i in range(2)]
            r_tmp = [Reg(f"tmp{i}") for i in range(2)]

            for n in range(ntile_a):
                b2 = n % 2
                bT = 2 * b2
                bZ = 4 + b2
                bK = 6 + b2
                kb.dma("sp", xin[b2][:], xb[n * 128:(n + 1) * 128, :], w=[r_xin[b2]])
                kb.op("act", L("activation", out=junk[:], in_=xin[b2][:], func=AF.Square, accum_out=st[b2][:, 0:1]),
                      r=[r_xin[b2]], w=[r_junk, r_st[b2]])
                rstd_from_ss(st[b2], 0, D, r_st[b2])
                kb.op("dve", L("scalar_tensor_tensor", out=xs[b2][:], in0=xin[b2][:], scalar=st[b2][:, 2:3],
                               in1=gbuf[:], op0=ALU.mult, op1=ALU.mult),
                      r=[r_xin[b2], r_st[b2], r_gbuf], w=[r_xs[b2]])
                for kc in range(16):
                    kb.op("pe", L("transpose", out=PH(bT, kc * 128, (kc + 1) * 128),
                                  in_=xs[b2][:, kc * 128:(kc + 1) * 128], identity=identb[:]),
                          r=[r_xs[b2], r_id], w=[r_pb[bT + kc // 8]])
                kb.op("act", L("copy", out=xT[b2][:].rearrange("p a b -> p (a b)"), in_=PH(bT, 0, 2048)),
                      r=[r_pb[bT], r_pb[bT + 1]], w=[r_xT[b2]])
                for kc in range(16):
                    kb.op("pe", L("matmul", out=PF(bZ, 0, 320), lhsT=xT[b2][:, kc, :], rhs=wkv[:, kc, :],
                                  start=(kc == 0), stop=(kc == 15)),
                          r=[r_xT[b2], r_wkv], w=[r_pb[bZ]])
                kb.op("act", L("copy", out=Vt[:, n, 0:128], in_=PF(bZ, 128, 256)), r=[r_pb[bZ]], w=[r_V[n]])
                kb.op("act", L("activation", out=kn[b2][:, 0:128], in_=PF(bZ, 0, 128), func=AF.Square,
                               accum_out=st[b2][:, 3:4]), r=[r_pb[bZ]], w=[r_kn[b2], r_st[b2]])
                rstd_from_ss(st[b2], 3, 128, r_st[b2])
                kb.op("dve", L("scalar_tensor_tensor", out=kn[b2][:, 0:128], in0=PF(bZ, 0, 128),
                               scalar=st[b2][:, 5:6], in1=gk, op0=ALU.mult, op1=ALU.mult),
                      r=[r_pb[bZ], r_st[b2], r_cst], w=[r_kn[b2]])
                kb.op("dve", L("tensor_copy", out=kn[b2][:, 128:192], in_=PF(bZ, 256, 320)),
                      r=[r_pb[bZ]], w=[r_kn[b2]])
                kb.op("dve", L("tensor_copy", out=kfin[b2][:, 32:128], in_=kn[b2][:, 32:128]),
                      r=[r_kn[b2]], w=[r_kf[b2]])
                kb.op("dve", L("tensor_copy", out=kfin[b2][:, 144:192], in_=kn[b2][:, 144:192]),
                      r=[r_kn[b2]], w=[r_kf[b2]])
                t4a = [tmp[b2][:, i, 0:16] for i in range(4)]
                t4i = [tmp[b2][:, i, 0:8] for i in range(4)]
                rope(kfin[b2][:, 0:128], kn[b2][:, 0:128], 16, cosA[:, n, :], sinA[:, n, :], t4a,
                     r_kn[b2], r_kf[b2], r_ropeA, r_tmp[b2])
                rope(kfin[b2][:, 128:192], kn[b2][:, 128:192], 8, cosI[:, n, :], sinI[:, n, :], t4i,
                     r_kn[b2], r_kf[b2], r_ropeI, r_tmp[b2])
                kb.op("dve", L("tensor_copy", out=kfin[b2][:, 192:256], in_=kfin[b2][:, 128:192]),
                      r=[r_kf[b2]], w=[r_kf[b2]])
                kb.op("pe", L("transpose", out=PH(bK, 0, 128), in_=kfin[b2][:, 0:128], identity=identb[:]),
                      r=[r_kf[b2], r_id], w=[r_pb[bK]])
                kb.op("pe", L("transpose", out=PH(bK, 128, 256), in_=kfin[b2][:, 128:256], identity=identb[:]),
                      r=[r_kf[b2], r_id], w=[r_pb[bK]])
                kb.op("act", L("copy", out=KT[:, n * 128:(n + 1) * 128], in_=PH(bK, 0, 128)),
                      r=[r_pb[bK]], w=[r_KT[n]])
                kb.op("act", L("copy", out=KIT[:, n * 128:(n + 1) * 128], in_=PH(bK, 128, 256)),
                      r=[r_pb[bK]], w=[r_KIT[n]])
            kb.barrier()

        if stage in ("smoke", "A"):
            o_kt = dram("o_kt", [128, SEQ], BF16, kind="ExternalOutput")
            o_v = dram("o_v", [128, NT * 129], BF16, kind="ExternalOutput")
            o_kit = dram("o_kit", [128, SEQ], BF16, kind="ExternalOutput")
            regs = r_KT[:ntile_a] + r_V[:ntile_a] + r_KIT[:ntile_a] + [r_vone]
            ro = Reg("out")
            T_ = ntile_a * 128
            kb.dma("sp", o_kt[:, 0:T_], KT[:, 0:T_], r=regs, w=[ro])
            kb.dma("sp", o_v[:, 0:ntile_a * 129], Vt[:, 0:ntile_a, :].rearrange("p a b -> p (a b)"), r=regs, w=[ro])
            kb.dma("sp", o_kit[:, 0:T_], KIT[:, 0:T_], r=regs, w=[ro])
            kb.finish()
            kb.emit()
            print("instructions:", kb.nins)
            return


        blocks_b2 = list(range(NB))
        if stage == "B2s":
            blocks_b2 = [0, 1]
        if stage in ("B3s", "Ms"):
            blocks_b2 = [0, 1, 2, 3]
        with ExitStack() as l2:
            lsb = mk_sb(l2)
            cmask = lsb("cmask_sb", [128, 512])
            r_cmask = Reg("cmask")
            kb.dma("sp", cmask[:], cmask_d, w=[r_cmask])
            sc = lsb("sc", [128, SEQ])
            r_sc = [Reg(f"sc{i}") for i in range(16)]
            selb = lsb("selb", [128, SEQ], BF16)
            r_sel = Reg("sel")
            selT = lsb("selT", [128, NT, 128], BF16)
            r_selT = [Reg(f"selT{i}") for i in range(4)]
            qTb = [lsb(f"qTb{i}", [128, 16, 128], BF16) for i in range(2)]
            r_qTb = [Reg(f"qTb{i}") for i in range(2)]
            qiTb = [lsb(f"qiTb{i}", [128, 8, 128], BF16) for i in range(2)]
            r_qiTb = [Reg(f"qiTb{i}") for i in range(2)]
            wib = [lsb(f"wib{i}", [128, 16]) for i in range(2)]
            r_wib = [Reg(f"wib{i}") for i in range(2)]
            rl = [lsb(f"rl{i}", [128, 512]) for i in range(2)]
            r_rl = [Reg(f"rl{i}") for i in range(2)]
            Eb = [lsb(f"Eb{i}", [128, 512], BF16) for i in range(2)]
            r_Eb = [Reg(f"Eb{i}") for i in range(2)]
            Pb = [lsb(f"Pb{i}", [128, 4, 128], BF16) for i in range(2)]
            r_Pb = [Reg(f"Pb{i}") for i in range(2)]
            bis = lsb("bis", [128, 8])
            r_bis = Reg("bis")
            bisA = lsb("bisA", [128, 2])
            r_mid, r_cnt, r_cnta = Reg("mid"), Reg("cnt"), Reg("cnta")
            r_selD, r_selA = Reg("selD"), Reg("selA")
            rden = lsb("rden", [128, 16])
            r_rden = Reg("rden")
            Ab = lsb("Ab", [128, 2048], BF16)
            r_Ab = Reg("Ab")
            ATb = lsb("ATb", [128, 2048], BF16)
            r_ATb = Reg("ATb")
            cnt_ib = [0]
            cnt_ia = [0]

            def load_q(bi, m):
                q2 = bi % 2
                kb.dma("sp", qTb[q2][:].rearrange("p a b -> p (a b)"), s_qT[m], w=[r_qTb[q2]])
                kb.dma("sp", qiTb[q2][:].rearrange("p a b -> p (a b)"), s_qiT[m], w=[r_qiTb[q2]])
                kb.dma("sp", wib[q2][:], s_wi[m], w=[r_wib[q2]])

            def gen_indexer(bi, m):
                q2 = bi % 2
                NCH = m + 1
                for ch in range(NCH):
                    scc = sc[:, ch * 512:(ch + 1) * 512]
                    for h in range(16):
                        ib = cnt_ib[0]
                        cnt_ib[0] += 1
                        bank = 5 + ib % 3
                        i2 = ib % 2
                        hf = h % 2
                        kb.op("pe", L("matmul", out=PF(bank), lhsT=qiTb[q2][hf * 64:(hf + 1) * 64, h // 2, :],
                                      rhs=KIT[hf * 64:(hf + 1) * 64, ch * 512:(ch + 1) * 512], start=True, stop=True),
                              r=[r_qiTb[q2]] + r_KIT[4 * ch:4 * ch + 4], w=[r_pb[bank]])
                        kb.op("act", L("activation", out=rl[i2][:], in_=PF(bank), func=AF.Relu),
                              r=[r_pb[bank]], w=[r_rl[i2]])
                        if h == 0:
                            kb.op("dve", L("tensor_scalar", out=scc, in0=rl[i2][:], scalar1=wib[q2][:, 0:1],
                                           scalar2=None, op0=ALU.mult), r=[r_rl[i2], r_wib[q2]], w=[r_sc[ch]])
                        else:
                            kb.op("dve", L("scalar_tensor_tensor", out=scc, in0=rl[i2][:],
                                           scalar=wib[q2][:, h:h + 1], in1=scc, op0=ALU.mult, op1=ALU.add),
                                  r=[r_rl[i2], r_wib[q2], r_sc[ch]], w=[r_sc[ch]])
                        yield

            def topk_and_mask(bi, m):
                NCH = m + 1
                S = 512 * NCH
                NKT = 4 * NCH
                rs = r_sc[0:NCH]
                kb.op("dve", L("tensor_reduce", out=bis[:, 0:1], in_=sc[:, 0:S], axis=AX.X, op=ALU.min),
                      r=rs, w=[r_bis])
                lc = sc[:, S - 512:S]
                kb.op("dve", L("tensor_tensor", out=lc, in0=lc, in1=cmask[:], op=ALU.add),
                      r=[r_sc[NCH - 1], r_cmask], w=[r_sc[NCH - 1]])
                kb.op("dve", L("tensor_reduce", out=bis[:, 1:2], in_=sc[:, 0:S], axis=AX.X, op=ALU.max),
                      r=rs, w=[r_bis])
                kb.op("dve", L("tensor_tensor", out=bis[:, 2:3], in0=bis[:, 1:2], in1=bis[:, 0:1], op=ALU.subtract),
                      r=[r_bis], w=[r_bis])
                kb.op("dve", L("tensor_scalar", out=bis[:, 2:3], in0=bis[:, 2:3], scalar1=1.0001, scalar2=1e-6,
                               op0=ALU.mult, op1=ALU.add), r=[r_bis], w=[r_bis])
                kb.op("dve", L("tensor_copy", out=bis[:, 3:4], in_=bis[:, 0:1]), r=[r_bis], w=[r_bis])
                nd = max(1, int(round(0.45 * NCH)))
                Sd = 512 * nd
                n_act = S - Sd
                rsd = r_sc[0:nd]
                rsa = r_sc[nd:NCH]
                for itb in range(1, NBIS + 1):
                    f = 2.0 ** (-itb)
                    kb.op("dve", L("scalar_tensor_tensor", out=bis[:, 4:5], in0=bis[:, 2:3], scalar=f,
                                   in1=bis[:, 3:4], op0=ALU.mult, op1=ALU.add), r=[r_bis], w=[r_mid])
                    if n_act > 0:
                        kb.op("act", L("activation", out=selb[:, Sd:S], in_=sc[:, Sd:S], func=AF.Sign, scale=-1.0,
                                       bias=bis[:, 4:5], accum_out=bisA[:, 0:1]),
                              r=rsa + [r_mid], w=[r_selA, r_cnta])
                    kb.op("dve", L("tensor_scalar", out=selb[:, 0:Sd], in0=sc[:, 0:Sd], scalar1=bis[:, 4:5],
                                   scalar2=0.0, op0=ALU.is_ge, op1=ALU.add, accum_out=bis[:, 5:6]),
                          r=rsd + [r_mid], w=[r_selD, r_cnt])
                    if n_act > 0:
                        kb.op("dve", L("scalar_tensor_tensor", out=bis[:, 5:6], in0=bisA[:, 0:1], scalar=-0.5,
                                       in1=bis[:, 5:6], op0=ALU.mult, op1=ALU.add), r=[r_cnta, r_cnt], w=[r_cnt])
                    kb.op("dve", L("tensor_scalar", out=bis[:, 6:7], in0=bis[:, 5:6],
                                   scalar1=NSEL - 0.5 - 0.5 * n_act, scalar2=f, op0=ALU.is_ge, op1=ALU.mult),
                          r=[r_cnt], w=[r_bis])
                    kb.op("dve", L("scalar_tensor_tensor", out=bis[:, 3:4], in0=bis[:, 6:7], scalar=bis[:, 2:3],
                                   in1=bis[:, 3:4], op0=ALU.mult, op1=ALU.add), r=[r_bis], w=[r_bis])
                kb.op("dve", L("tensor_scalar", out=selb[:, 0:S], in0=sc[:, 0:S], scalar1=bis[:, 3:4],
                               scalar2=None, op0=ALU.is_ge), r=rs + [r_bis], w=[r_selD, r_selA])
                for g in range((NKT + 15) // 16):
                    n_in = min(16, NKT - 16 * g)
                    bT = 2 * (g % 2)
                    for k2 in range(n_in):
                        kt = 16 * g + k2
                        kb.op("pe", L("transpose", out=PH(bT, k2 * 128, (k2 + 1) * 128),
                                      in_=selb[:, kt * 128:(kt + 1) * 128], identity=identb[:]),
                              r=[r_selD, r_selA, r_id], w=[r_pb[bT + k2 // 8]])
                    kb.op("act", L("copy", out=selT[:, 16 * g:16 * g + n_in, :],
                                   in_=PH(bT, 0, n_in * 128).rearrange("p (a b) -> p a b", a=n_in)),
                          r=[r_pb[bT], r_pb[bT + 1]], w=[r_selT[g]])

            def gen_attention(bi, m):
                q2 = bi % 2
                NCH = m + 1
                NKT = 4 * NCH
                its = [(ps_, kt, hgl) for ps_ in range(2) for kt in range(NKT) for hgl in range(2)]
                ia0 = cnt_ia[0]
                cnt_ia[0] += len(its)

                def issue_qk(idx):
                    ps_, kt, hgl = its[idx]
                    hg = 2 * ps_ + hgl
                    lb = (ia0 + idx) % 2
                    kb.op("pe", L("matmul", out=PF(lb), lhsT=KT[:, kt * 128:(kt + 1) * 128],
                                  rhs=qTb[q2][:, 4 * hg:4 * hg + 4, :], start=True, stop=True),
                          r=[r_KT[kt], r_qTb[q2]], w=[r_pb[lb]])

                def issue_rest(idx):
                    ps_, kt, hgl = its[idx]
                    lb = (ia0 + idx) % 2
                    kb.op("act", L("activation", out=Eb[lb][:], in_=PF(lb), func=AF.Exp, scale=ATTN_SCALE),
                          r=[r_pb[lb]], w=[r_Eb[lb]])
                    kb.op("dve", L("tensor_tensor", out=Pb[lb][:],
                                   in0=Eb[lb][:].rearrange("p (h t) -> p h t", h=4),
                                   in1=selT[:, kt, :].unsqueeze(1).broadcast_to([128, 4, 128]), op=ALU.mult),
                          r=[r_Eb[lb], r_selT[kt // 16]], w=[r_Pb[lb]])
                    for h4 in range(4):
                        h8 = 4 * hgl + h4
                        pbk = 2 + h8 // 3
                        o0 = (h8 % 3) * 129
                        kb.op("pe", L("matmul", out=PF(pbk, o0, o0 + 129), lhsT=Pb[lb][:, h4, :],
                                      rhs=Vt[:, kt, :], start=(kt == 0 and h8 % 3 == 0),
                                      stop=(kt == NKT - 1 and (h8 % 3 == 2 or h8 == 7))),
                              r=[r_Pb[lb], r_V[kt], r_vone], w=[r_pb[pbk]])

                def normalise(ps_):
                    for pbk in range(2, 5):
                        h0 = 3 * (pbk - 2)
                        nh = min(3, 8 - h0)
                        hh = 8 * ps_ + h0
                        pv3 = PF(pbk, 0, nh * 129).rearrange("p (h c) -> p h c", c=129)
                        kb.op("dve", L("reciprocal", out=rden[:, hh:hh + nh].unsqueeze(2), in_=pv3[:, :, 128:129]),
                              r=[r_pb[pbk]], w=[r_rden])
                        kb.op("dve", L("tensor_tensor",
                                       out=Ab[:, hh * 128:(hh + nh) * 128].rearrange("p (h d) -> p h d", h=nh),
                                       in0=pv3[:, :, 0:128],
                                       in1=rden[:, hh:hh + nh].unsqueeze(2).broadcast_to([128, nh, 128]), op=ALU.mult),
                              r=[r_pb[pbk], r_rden], w=[r_Ab])

                issue_qk(0)
                for idx in range(len(its)):
                    if idx + 1 < len(its) and its[idx + 1][0] == its[idx][0]:
                        issue_qk(idx + 1)
                    issue_rest(idx)
                    if idx + 1 < len(its) and its[idx + 1][0] != its[idx][0]:
                        normalise(0)
                        issue_qk(idx + 1)
                    yield
                normalise(1)
                for h in range(16):
                    kb.op("pe", L("transpose", out=PH(0, h * 128, (h + 1) * 128), in_=Ab[:, h * 128:(h + 1) * 128],
                                  identity=identb[:]), r=[r_Ab, r_id], w=[r_pb[h // 8]])
                kb.op("act", L("copy", out=ATb[:], in_=PH(0, 0, 2048)), r=[r_pb[0], r_pb[1]], w=[r_ATb])
                kb.dma("sp", s_AT[m], ATb[:], r=[r_ATb])

            def run_interleaved(gens):
                gens = [g for g in gens if g is not None]
                while gens:
                    nxt = []
                    for g in gens:
                        try:
                            next(g)
                            nxt.append(g)
                        except StopIteration:
                            pass
                    gens = nxt

            nb2 = len(blocks_b2)
            load_q(0, blocks_b2[0])
            run_interleaved([gen_indexer(0, blocks_b2[0])])
            topk_and_mask(0, blocks_b2[0])
            for bi, m in enumerate(blocks_b2):
                nxt_g = None
                if bi + 1 < nb2:
                    load_q(bi + 1, blocks_b2[bi + 1])
                    nxt_g = gen_indexer(bi + 1, blocks_b2[bi + 1])
                run_interleaved([gen_attention(bi, m), nxt_g])
                if bi + 1 < nb2:
                    topk_and_mask(bi + 1, blocks_b2[bi + 1])
            kb.barrier()

    if stage in ("B2", "B2s"):
        kb.finish()
        kb.emit()
        print("instructions:", kb.nins)
        return

    comb_all = sb("comb_all", [128, NB, NE])
    r_comb = Reg("comb")
    kb.op("pool", L("memset", comb_all[:], 0.0), w=[r_comb])
    ng3 = 4 if stage not in ("B3s", "Ms") else 1
    with ExitStack() as l3:
        lsb = mk_sb(l3)
        kb.dma("sp", gbuf[:], g2_d, w=[r_gbuf])
        wrf = lsb("wrf", [128, 16, 36])
        r_wrf = Reg("wrf")
        kb.dma("sp", wrf[:], wr_d.rearrange("(kc p) n -> p kc n", p=128), w=[r_wrf])
        xTm = [lsb(f"xTm{i}", [128, 16, 128], BF16) for i in range(4)]
        r_xTm = [Reg(f"xTm{i}") for i in range(4)]
        sTm = [lsb(f"sTm{i}", [128, 8, 128], BF16) for i in range(4)]
        r_sTm = [Reg(f"sTm{i}") for i in range(4)]
        ATm = [lsb(f"ATm{i}", [128, 16, 128], BF16) for i in range(4)]
        r_ATm = [Reg(f"ATm{i}") for i in range(4)]
        xh = [lsb(f"xh{i}", [128, D]) for i in range(4)]
        r_xh = [Reg(f"xh{i}") for i in range(4)]
        wb3 = [lsb(f"wb3{i}", [128, 16, 512], BF16) for i in range(2)]
        r_wb3 = [Reg(f"wb3{i}") for i in range(2)]
        sgc = [lsb(f"sgc{i}", [128, 512], BF16) for i in range(4)]
        r_sgc = [Reg(f"sgc{i}") for i in range(4)]
        sga = [lsb(f"sga{i}", [128, 512], BF16) for i in range(4)]
        r_sga = [Reg(f"sga{i}") for i in range(4)]
        t1 = [lsb(f"t1{i}", [128, 512]) for i in range(4)]
        r_t1 = [Reg(f"t1{i}") for i in range(4)]
        t2 = [lsb(f"t2{i}", [128, 512]) for i in range(2)]
        r_t2 = [Reg(f"t2{i}") for i in range(2)]
        mix = [lsb(f"mix{i}", [128, D], BF16) for i in range(4)]
        r_mix = [Reg(f"mix{i}") for i in range(4)]
        mixT = [lsb(f"mixT{i}", [128, 16, 128], BF16) for i in range(4)]
        r_mixT = [Reg(f"mixT{i}") for i in range(4)]
        hn2f = lsb("hn2f", [128, D])
        r_hn2f = Reg("hn2f")
        hTf = lsb("hTf", [128, 16, 128])
        r_hTf = Reg("hTf")
        hTb = lsb("hTb", [128, 16, 128], BF16)
        r_hTb = Reg("hTb")
        st3 = lsb("st3", [128, 8])
        r_st3 = Reg("st3")
        lg = lsb("lg", [128, 36])
        rt = lsb("rt", [128, 96])
        r_rt = Reg("rt")
        w3n = [0]
        pq = [0]

        def load_w3(src_ap, nk):
            i = w3n[0] % 2
            w3n[0] += 1
            kb.dma("pool", wb3[i][:, 0:nk, :], src_ap, w=[r_wb3[i]])
            return i

        w_pw_v = w_pw.rearrange("(kc p) n -> p kc n", p=128)
        w_ao_v = w_ao.rearrange("(kc p) n -> p kc n", p=128)
        w_o_v = w_o.rearrange("(kc p) n -> p kc n", p=128)

        def proj(bl, lhs_fn, nk, wi_, regs):
            bank = pq[0] % 4
            pq[0] += 1
            for kc in range(nk):
                kb.op("pe", L("matmul", out=PF(bank), lhsT=lhs_fn(kc), rhs=wb3[wi_][:, kc, :],
                              start=(kc == 0), stop=(kc == nk - 1)), r=regs + [r_wb3[wi_]], w=[r_pb[bank]])
            return bank

        import os
        CUT3 = int(os.environ.get("CUT3", "99"))
        for gi in range(ng3):
            for bl in range(4):
                m = 4 * gi + bl
                kb.dma("sp", xTm[bl][:].rearrange("p a b -> p (a b)"), s_xT[m], w=[r_xTm[bl]])
                kb.dma("sp", sTm[bl][:].rearrange("p a b -> p (a b)"), s_sT[m], w=[r_sTm[bl]])
                kb.dma("sp", ATm[bl][:].rearrange("p a b -> p (a b)"), s_AT[m], w=[r_ATm[bl]])
                kb.dma("sp", xh[bl][:], xo[m, 32:160, :], w=[r_xh[bl]])
            for c in range(4):
                cs = slice(c * 512, (c + 1) * 512)
                wi_ = load_w3(w_in_v[:, :, C_GC + c * 512:C_GC + (c + 1) * 512], 16)
                for bl in range(4):
                    bank = proj(bl, lambda kc: xTm[bl][:, kc, :], 16, wi_, [r_xTm[bl]])
                    kb.op("act", L("activation", out=sgc[bl][:], in_=PF(bank), func=AF.Sigmoid),
                          r=[r_pb[bank]], w=[r_sgc[bl]])
                wi_ = load_w3(w_pw_v[:, :, cs], 8)
                for bl in range(4):
                    bank = proj(bl, lambda kc: sTm[bl][:, kc, :], 8, wi_, [r_sTm[bl]])
                    kb.op("dve", L("tensor_tensor", out=t1[bl][:], in0=PF(bank), in1=sgc[bl][:], op=ALU.mult),
                          r=[r_pb[bank], r_sgc[bl]], w=[r_t1[bl]])
                wi_ = load_w3(w_in_v[:, :, C_GA + c * 512:C_GA + (c + 1) * 512], 16)
                for bl in range(4):
                    bank = proj(bl, lambda kc: xTm[bl][:, kc, :], 16, wi_, [r_xTm[bl]])
                    kb.op("act", L("activation", out=sga[bl][:], in_=PF(bank), func=AF.Sigmoid),
                          r=[r_pb[bank]], w=[r_sga[bl]])
                wi_ = load_w3(w_ao_v[:, :, cs], 16)
                for bl in range(4):
                    bank = proj(bl, lambda kc: ATm[bl][:, kc, :], 16, wi_, [r_ATm[bl]])
                    kb.op("dve", L("tensor_tensor", out=t2[bl % 2][:], in0=PF(bank), in1=sga[bl][:], op=ALU.mult),
                          r=[r_pb[bank], r_sga[bl]], w=[r_t2[bl % 2]])
                    kb.op("dve", L("tensor_tensor", out=mix[bl][:, cs], in0=t1[bl][:], in1=t2[bl % 2][:], op=ALU.add),
                          r=[r_t1[bl], r_t2[bl % 2]], w=[r_mix[bl]])
            if CUT3 <= 1:
                break
            for bl in range(4):
                bT = 4 + 2 * (bl % 2)
                for kc in range(16):
                    kb.op("pe", L("transpose", out=PH(bT, kc * 128, (kc + 1) * 128),
                                  in_=mix[bl][:, kc * 128:(kc + 1) * 128], identity=identb[:]),
                          r=[r_mix[bl], r_id], w=[r_pb[bT + kc // 8]])
                kb.op("act", L("copy", out=mixT[bl][:].rearrange("p a b -> p (a b)"), in_=PH(bT, 0, 2048)),
                      r=[r_pb[bT], r_pb[bT + 1]], w=[r_mixT[bl]])
            for c in range(4):
                cs = slice(c * 512, (c + 1) * 512)
                wi_ = load_w3(w_o_v[:, :, cs], 16)
                for bl in range(4):
                    bank = proj(bl, lambda kc: mixT[bl][:, kc, :], 16, wi_, [r_mixT[bl]])
                    kb.op("dve", L("tensor_tensor", out=xh[bl][:, cs], in0=PF(bank), in1=xh[bl][:, cs], op=ALU.add),
                          r=[r_pb[bank], r_xh[bl]], w=[r_xh[bl]])
            if CUT3 <= 2:
                break
            for bl in range(4):
                m = 4 * gi + bl
                kb.dma("sp", out_d[m], xh[bl][:], r=[r_xh[bl]])
                kb.op("act", L("activation", out=hn2f[:], in_=xh[bl][:], func=AF.Square, accum_out=st3[:, 0:1]),
                      r=[r_xh[bl]], w=[r_hn2f, r_st3])
                rstd_from_ss(st3, 0, D, r_st3)
                kb.op("dve", L("scalar_tensor_tensor", out=hn2f[:], in0=xh[bl][:], scalar=st3[:, 2:3], in1=gbuf[:],
                               op0=ALU.mult, op1=ALU.mult), r=[r_xh[bl], r_st3, r_gbuf], w=[r_hn2f])
                if CUT3 <= 3:
                    continue
                for kc in range(16):
                    kb.op("pe", L("matmul", out=PF(4 + kc // 4, (kc % 4) * 128, (kc % 4 + 1) * 128),
                                  lhsT=hn2f[:, kc * 128:(kc + 1) * 128], rhs=identf[:], start=True, stop=True),
                          r=[r_hn2f, r_id], w=[r_pb[4 + kc // 4]])
                SUB3 = int(os.environ.get("SUB3", "9"))
                if SUB3 <= 1:
                    continue
                for q4 in range(4):
                    kb.op("act", L("copy", out=hTb[:, 4 * q4:4 * q4 + 4, :].rearrange("p a b -> p (a b)"),
                                   in_=PF(4 + q4)), r=[r_pb[4 + q4]], w=[r_hTb])
                    if SUB3 <= 2:
                        continue
                    kb.op("dve", L("tensor_copy", out=hTf[:, 4 * q4:4 * q4 + 4, :].rearrange("p a b -> p (a b)"),
                                   in_=PF(4 + q4)), r=[r_pb[4 + q4]], w=[r_hTf])
                if SUB3 <= 3:
                    continue
                kb.dma("sp", s_hT[m], hTb[:].rearrange("p a b -> p (a b)"), r=[r_hTb])
                if CUT3 <= 4:
                    continue
                bank = pq[0] % 4
                pq[0] += 1
                for kc in range(16):
                    kb.op("pe", L("matmul", out=PF(bank, 0, 36), lhsT=hTf[:, kc, :], rhs=wrf[:, kc, :],
                                  start=(kc == 0), stop=(kc == 15)), r=[r_hTf, r_wrf], w=[r_pb[bank]])
                if CUT3 <= 5:
                    continue
                R = [r_rt]
                kb.op("dve", L("tensor_tensor", out=lg[:], in0=PF(bank, 0, 36), in1=brt, op=ALU.add),
                      r=[r_pb[bank], r_cst], w=R)
                gl = lg[:, 0:4]
                el = lg[:, 4:36].rearrange("p (g e) -> p g e", g=4)
                gmax, ngmax, sumg, pg = rt[:, 0:1], rt[:, 1:2], rt[:, 2:3], rt[:, 3:4]
                ohg, eg = rt[:, 4:8], rt[:, 8:12]
                tmp48 = rt[:, 12:44].rearrange("p (g e) -> p g e", g=4)
                e_in, mx8, oh1, oh2 = rt[:, 44:52], rt[:, 52:60], rt[:, 60:68], rt[:, 68:76]
                dd, ed, w1, w2 = rt[:, 76:77], rt[:, 77:78], rt[:, 78:79], rt[:, 79:80]
                wi1, wpg = rt[:, 80:88], rt[:, 88:96]
                kb.op("dve", L("tensor_reduce", out=gmax, in_=gl, axis=AX.X, op=ALU.max), r=R, w=R)
                kb.op("dve", L("tensor_scalar", out=ohg, in0=gl, scalar1=gmax, scalar2=None, op0=ALU.is_equal),
                      r=R, w=R)
                kb.op("dve", L("tensor_scalar", out=ngmax, in0=gmax, scalar1=-1.0, scalar2=None, op0=ALU.mult),
                      r=R, w=R)
                kb.op("act", L("activation", out=eg, in_=gl, func=AF.Exp, bias=ngmax, accum_out=sumg), r=R, w=R)
                kb.op("dve", L("reciprocal", out=pg, in_=sumg), r=R, w=R)
                kb.op("dve", L("tensor_tensor", out=tmp48, in0=el, in1=ohg.unsqueeze(2).broadcast_to([128, 4, 8]),
                               op=ALU.mult), r=R, w=R)
                kb.op("dve", L("tensor_reduce", out=e_in, in_=tmp48.rearrange("p g e -> p e g"), axis=AX.X,
                               op=ALU.add), r=R, w=R)
                if CUT3 <= 6:
                    continue
                kb.op("dve", L("max", out=mx8, in_=e_in), r=R, w=R)
                if CUT3 <= 7:
                    continue
                kb.op("dve", L("tensor_scalar", out=oh1, in0=e_in, scalar1=mx8[:, 0:1], scalar2=None,
                               op0=ALU.is_equal), r=R, w=R)
                kb.op("dve", L("tensor_scalar", out=oh2, in0=e_in, scalar1=mx8[:, 1:2], scalar2=None,
                               op0=ALU.is_equal), r=R, w=R)
                kb.op("dve", L("tensor_tensor", out=dd, in0=mx8[:, 1:2], in1=mx8[:, 0:1], op=ALU.subtract), r=R, w=R)
                kb.op("act", L("activation", out=ed, in_=dd, func=AF.Exp), r=R, w=R)
                kb.op("dve", L("tensor_scalar", out=w1, in0=ed, scalar1=1.0, scalar2=None, op0=ALU.add), r=R, w=R)
                kb.op("dve", L("reciprocal", out=w1, in_=w1), r=R, w=R)
                kb.op("dve", L("tensor_tensor", out=w2, in0=ed, in1=w1, op=ALU.mult), r=R, w=R)
                kb.op("dve", L("tensor_scalar", out=wi1, in0=oh1, scalar1=w1, scalar2=None, op0=ALU.mult), r=R, w=R)
                kb.op("dve", L("scalar_tensor_tensor", out=wi1, in0=oh2, scalar=w2, in1=wi1, op0=ALU.mult,
                               op1=ALU.add), r=R, w=R)
                kb.op("dve", L("tensor_scalar", out=wpg, in0=wi1, scalar1=pg, scalar2=None, op0=ALU.mult), r=R, w=R)
                kb.op("dve", L("tensor_tensor", out=comb_all[:, m, :].rearrange("p (g e) -> p g e", g=4),
                               in0=ohg.unsqueeze(2).broadcast_to([128, 4, 8]),
                               in1=wpg.unsqueeze(1).broadcast_to([128, 4, 8]), op=ALU.mult), r=R, w=[r_comb])
        if stage in ("B3", "B3s"):
            kb.dma("sp", s_comb, comb_all[:].rearrange("p a b -> p (a b)"), r=[r_comb])
        kb.barrier()

    if stage in ("B3", "B3s"):
        kb.finish()
        kb.emit()
        print("instructions:", kb.nins)
        return

    with ExitStack() as l4:
        lsb = mk_sb(l4)
        hT = lsb("hT", [128, 16, 1024], BF16)
        r_hT = [Reg(f"hT{i}") for i in range(8)]
        acc = lsb("acc", [128, 8, D])
        r_acc = [[Reg(f"acc{i}_{k}") for k in range(2)] for i in range(8)]
        wg = [lsb(f"wg{i}", [128, 16, 256], BF16) for i in range(2)]
        r_wg = [Reg(f"wg{i}") for i in range(2)]
        wu_ = [lsb(f"wup{i}", [128, 16, 256], BF16) for i in range(2)]
        r_wu_ = [Reg(f"wup{i}") for i in range(2)]
        wd = [lsb(f"wd{i}", [128, 4, D], BF16) for i in range(2)]
        r_wd = [Reg(f"wd{i}") for i in range(2)]
        actT = lsb("actT", [128, 4, 1024], BF16)
        r_actT = [Reg(f"actT{i}") for i in range(4)]
        sgm = [lsb(f"sgm{i}", [128, 512], BF16) for i in range(2)]
        r_sgm = [Reg(f"sgm{i}") for i in range(2)]
        npass = 2
        nblk = 8
        ne_run = NE
        if stage == "Ms":
            npass, nblk, ne_run = 1, 4, 2
        ntch = nblk // 4
        hw_n = 0
        gq_n = 0
        dq_n = 0
        for p in range(npass):
            for blk in range(nblk):
                m = 8 * p + blk
                kb.dma("sp", hT[:, :, blk * 128:(blk + 1) * 128], s_hT[m].rearrange("p (a b) -> p a b", a=16),
                       w=[r_hT[blk]])
                kb.dma("sp", acc[:, blk, :], out_d[m], w=r_acc[blk])
            for e in range(ne_run):
                d2 = e % 2
                kb.dma("pool", wd[d2][:], w_ed[e].rearrange("(fc p) n -> p fc n", p=128), w=[r_wd[d2]])
                for half in range(2):
                    h2 = hw_n % 2
                    hw_n += 1
                    kb.dma("pool", wg[h2][:], w_eg[e].rearrange("(kc p) f -> p kc f", p=128)[:, :, half * 256:(half + 1) * 256],
                           w=[r_wg[h2]])
                    kb.dma("pool", wu_[h2][:], w_eu[e].rearrange("(kc p) f -> p kc f", p=128)[:, :, half * 256:(half + 1) * 256],
                           w=[r_wu_[h2]])
                    for fcl in range(2):
                        fc = 2 * half + fcl
                        for tch in range(ntch):
                            g2_ = gq_n % 2
                            gq_n += 1
                            bG, bU = g2_, 2 + g2_
                            for kc in range(16):
                                kb.op("pe", L("matmul", out=PF(bG), lhsT=wg[h2][:, kc, fcl * 128:(fcl + 1) * 128],
                                              rhs=hT[:, kc, tch * 512:(tch + 1) * 512], start=(kc == 0), stop=(kc == 15)),
                                      r=[r_wg[h2]] + r_hT[4 * tch:4 * tch + 4], w=[r_pb[bG]])
                            for kc in range(16):
                                kb.op("pe", L("matmul", out=PF(bU), lhsT=wu_[h2][:, kc, fcl * 128:(fcl + 1) * 128],
                                              rhs=hT[:, kc, tch * 512:(tch + 1) * 512], start=(kc == 0), stop=(kc == 15)),
                                      r=[r_wu_[h2]] + r_hT[4 * tch:4 * tch + 4], w=[r_pb[bU]])
                            kb.op("act", L("activation", out=sgm[g2_][:], in_=PF(bG), func=AF.Silu),
                                  r=[r_pb[bG]], w=[r_sgm[g2_]])
                            kb.op("dve", L("tensor_tensor", out=actT[:, fc, tch * 512:(tch + 1) * 512], in0=PF(bU),
                                           in1=sgm[g2_][:], op=ALU.mult), r=[r_pb[bU], r_sgm[g2_]], w=[r_actT[fc]])
                for blk in range(nblk):
                    m = 8 * p + blk
                    for nh in range(2):
                        bD = 4 + 2 * (dq_n % 2)
                        dq_n += 1
                        for sub in range(2):
                            c0 = nh * 1024 + sub * 512
                            for fc in range(4):
                                kb.op("pe", L("matmul", out=PF(bD + sub), lhsT=actT[:, fc, blk * 128:(blk + 1) * 128],
                                              rhs=wd[d2][:, fc, c0:c0 + 512], start=(fc == 0), stop=(fc == 3)),
                                      r=[r_actT[fc], r_wd[d2]], w=[r_pb[bD + sub]])
                        av = acc[:, blk, nh * 1024:(nh + 1) * 1024]
                        kb.op("dve", L("scalar_tensor_tensor", out=av, in0=PF(bD, 0, 1024),
                                       scalar=comb_all[:, m, e:e + 1], in1=av, op0=ALU.mult, op1=ALU.add),
                              r=[r_pb[bD], r_pb[bD + 1], r_comb, r_acc[blk][nh]], w=[r_acc[blk][nh]])
            for blk in range(nblk):
                m = 8 * p + blk
                kb.dma("sp", out_d[m], acc[:, blk, :], r=r_acc[blk])
        kb.barrier()
    kb.finish()
    kb.emit()
    print("instructions:", kb.nins)
    return

    raise NotImplementedError


def host_consts(inputs):
    theta = 500000.0
    invA = theta ** (-np.arange(0, 32, 2, dtype=np.float32) / np.float32(32))
    invI = theta ** (-np.arange(0, 16, 2, dtype=np.float32) / np.float32(16))
    c = np.zeros((128, 512), np.float32)
    c[:, 0:128] = np.asarray(inputs["q_norm_g"]).reshape(1, 128)
    c[:, 128:256] = np.asarray(inputs["k_norm_g"]).reshape(1, 128)
    c[:, 256:272] = invA.astype(np.float32)[None, :]
    c[:, 272:280] = invI.astype(np.float32)[None, :]
    c[:, 288:292] = np.asarray(inputs["b_router_group"]).reshape(1, 4)
    c[:, 292:324] = np.asarray(inputs["b_router_expert"]).reshape(1, 32)
    return c


def make_in_maps(inputs, stage="full"):
    x = np.asarray(inputs["x"])
    pos = np.asarray(inputs["positions"])
    consts = host_consts(inputs)
    ident = np.eye(128, dtype=np.float32)
    w_in = np.ascontiguousarray(np.asarray(inputs["w_in"])[0])
    gA = np.ascontiguousarray(np.broadcast_to(np.asarray(inputs["attn_norm_g"]).reshape(1, D), (128, D)))
    g2 = np.ascontiguousarray(np.broadcast_to(np.asarray(inputs["ffn_norm_g"]).reshape(1, D), (128, D)))
    cv = np.zeros((128, 8, 34), np.float32)
    dw = np.asarray(inputs["conv_dw_w"])[0]
    cv[:, :, 0:31] = dw.T.reshape(8, 128, 31).transpose(1, 0, 2)
    cv[:, :, 31] = np.asarray(inputs["conv_dw_b"])[0].reshape(8, 128).T
    cv[:, :, 32] = np.asarray(inputs["conv_ln_g"])[0].reshape(8, 128).T
    cv[:, :, 33] = np.asarray(inputs["conv_ln_b"])[0].reshape(8, 128).T
    wr = np.ascontiguousarray(np.concatenate([np.asarray(inputs["w_router_group"])[0],
                                              np.asarray(inputs["w_router_expert"])[0]], axis=1))
    shared = {
        "consts": consts, "gA": gA, "g2": g2, "w_in": w_in, "ident": ident,
        "cvec": np.ascontiguousarray(cv.reshape(128, 8 * 34)),
        "w_conv_out": np.ascontiguousarray(np.asarray(inputs["w_conv_out"])[0]),
        "w_attn_o": np.ascontiguousarray(np.asarray(inputs["w_attn_o"])[0]),
        "w_out": np.ascontiguousarray(np.asarray(inputs["w_out"])[0]),
        "wr": wr,
        "w_exp_gate": np.ascontiguousarray(np.asarray(inputs["w_exp_gate"])[0]),
        "w_exp_up": np.ascontiguousarray(np.asarray(inputs["w_exp_up"])[0]),
        "w_exp_down": np.ascontiguousarray(np.asarray(inputs["w_exp_down"])[0]),
    }
    maps = []
    for c in range(8):
        b, j = c // 4, c % 4
        xpad = np.concatenate([np.zeros((32, D), np.float32), x[b]], axis=0)
        xo_ = np.stack([xpad[128 * (4 * m + j):128 * (4 * m + j) + 160] for m in range(NB)])
        pos_o = np.stack([pos[b, 128 * (4 * m + j):128 * (4 * m + j) + 128] for m in range(NB)], axis=1)
        cidx = np.arange(512)[None, :]
        prow = np.arange(128)[:, None]
        cmask = np.where(cidx <= 128 * j + prow, 0.0, NEG).astype(np.float32)
        m_ = dict(shared)
        m_.update({
            "xb": np.ascontiguousarray(x[b]),
            "pos_t": np.ascontiguousarray(pos[b].reshape(NT, 128).T),
            "pos_o": np.ascontiguousarray(pos_o.astype(np.int32)),
            "xo": np.ascontiguousarray(xo_),
            "cmask": cmask,
        })
        maps.append(m_)
    return maps


def kernel(**inputs):
    nc = build_nc("full")
    maps = make_in_maps(inputs)
    res = run_bass_kernel_spmd(nc, maps, core_ids=list(range(8)))
    out = np.zeros((2, SEQ, D), np.float32)
    for c in range(8):
        b, j = c // 4, c % 4
        o = np.asarray(res.results[c]["out"])
        for m in range(NB):
            i = 4 * m + j
            out[b, 128 * i:128 * (i + 1)] = o[m]
    return out
```

```python
import math
from contextlib import ExitStack

import numpy as np
import concourse.bass as bass
import concourse.mybir as mybir
from concourse.bass_utils import run_bass_kernel_spmd

F32 = mybir.dt.float32
BF16 = mybir.dt.bfloat16
I32 = mybir.dt.int32
AF = mybir.ActivationFunctionType
ALU = mybir.AluOpType
AX = mybir.AxisListType

D = 2048
SEQ = 8192
NT = SEQ // 128
NB = 16
EPS = 1e-6
IN_TOTAL = 9552
C_U, C_Q, C_K, C_V, C_QI, C_KI, C_WI, C_GC, C_GA = 0, 2048, 4096, 4224, 4352, 5376, 5440, 5456, 7504
NEG = -1.0e30
EPOCH = 30000
NSLOT = 8
NSEL = 256
NBIS = 16
ATTN_SCALE = 128 ** -0.5
NE = 32
FF = 512


def L(name, *a, **k):
    return lambda e: getattr(e, name)(*a, **k)


class Reg:
    __slots__ = ("w", "r", "name", "psum")

    def __init__(self, name="", psum=False):
        self.w = None
        self.r = []
        self.name = name
        self.psum = psum


class KB:
    def __init__(self, nc, es):
        self.nc = nc
        self.es = es
        self.names = ("pe", "act", "dve", "pool", "sp")
        self.sem = {}
        self.cnt = {e: 0 for e in ("pe", "act", "dve", "pool")}
        self.seen = {e: {} for e in self.names}
        self.dn = {q: 0 for q in ("sp", "act", "pool")}
        for q in self.dn:
            for i in range(NSLOT):
                self.sem[("d", q, i)] = es.enter_context(nc.semaphore(f"d_{q}{i}"))
        self.nins = 0
        self.prog = {e: [] for e in self.names}

    def emit(self):
        with self.nc.Block() as block:
            def run(en):
                def body(e):
                    for it in self.prog[en]:
                        if it[0] == "w":
                            e.wait_ge(it[1], it[2])
                        elif it[0] == "i":
                            it[1](e).then_inc(it[2], 1)
                        else:
                            e.dma_start(out=it[1], in_=it[2], **it[4]).then_inc(it[3], 16)
                return body
            block.sync(run("sp"))
            block.tensor(run("pe"))
            block.scalar(run("act"))
            block.vector(run("dve"))
            block.gpsimd(run("pool"))

    def _semfor(self, key):
        if key not in self.sem:
            self.sem[key] = self.es.enter_context(self.nc.semaphore(f"s_{key[0]}{key[1]}"))
        return self.sem[key]

    def _wait(self, en, evs):
        best = {}
        for k, v in evs:
            if best.get(k, 0) < v:
                best[k] = v
        for k, v in best.items():
            if self.seen[en].get(k, 0) < v:
                self.prog[en].append(("w", self._semfor(k), v))
                self.seen[en][k] = v

    def _deps(self, en, r, w, is_dma=False):
        evs = []
        for t in r:
            if t.w is not None:
                if not (en == "pe" and t.w[0][0] == "pe"):
                    evs.append(t.w)
            if t.psum:
                for ev in t.r:
                    if ev[0][0] != en:
                        evs.append(ev)
        for t in w:
            if t.w is not None:
                if not (en == "pe" and t.w[0][0] == "pe"):
                    evs.append(t.w)
            for ev in t.r:
                if en == "pe" and ev[0][0] == "pe":
                    continue
                evs.append(ev)
        return evs

    def _commit(self, ev, r, w):
        for t in r:
            t.r.append(ev)
            if len(t.r) > 48:
                best = {}
                for k, v in t.r:
                    if best.get(k, 0) < v:
                        best[k] = v
                t.r = list(best.items())
        for t in w:
            t.w = ev
            t.r = []

    def op(self, en, fn, r=(), w=()):
        self._wait(en, self._deps(en, r, w))
        c = self.cnt[en]
        key = (en, c // EPOCH)
        val = c % EPOCH + 1
        self.prog[en].append(("i", fn, self._semfor(key)))
        self.cnt[en] = c + 1
        self._commit((key, val), r, w)
        self.nins += 1

    def dma(self, q, out, in_, r=(), w=(), **kw):
        i = self.dn[q]
        slot, use = i % NSLOT, i // NSLOT
        key = ("d", q, slot)
        evs = self._deps(q, r, w, is_dma=True)
        if use > 0:
            evs.append((key, 16 * use))
        self._wait(q, evs)
        self.prog[q].append(("d", out, in_, self.sem[key], kw))
        self.dn[q] = i + 1
        self._commit((key, 16 * (use + 1)), r, w)
        self.nins += 1

    def all_events(self):
        evs = []
        for en, c in self.cnt.items():
            if c > 0:
                evs.append(((en, (c - 1) // EPOCH), (c - 1) % EPOCH + 1))
        for q, i in self.dn.items():
            for back in range(1, min(i, NSLOT) + 1):
                ii = i - back
                evs.append((("d", q, ii % NSLOT), 16 * (ii // NSLOT + 1)))
        return evs

    def barrier(self):
        evs = self.all_events()
        for en in self.names:
            self._wait(en, evs)

    def finish(self):
        self._wait("sp", self.all_events())


def build_nc(stage="full"):
    nc = bass.Bass("TRN2", target_bir_lowering=False)
    es = ExitStack()
    with es:
        _build(nc, es, stage)
    return nc


def _build(nc, es, stage):
    kb = KB(nc, es)
    dbg = stage != "full"

    def dram(name, shape, dt=F32, kind="ExternalInput"):
        return nc.dram_tensor(name, list(shape), dt, kind=kind).ap()

    def scratch(name, shape, dt, want_dbg):
        kind = "ExternalOutput" if (dbg and want_dbg) else "Internal"
        return nc.dram_tensor(name, list(shape), dt, kind=kind).ap()

    def mk_sb(stack):
        def sb(name, shape, dt=F32):
            return stack.enter_context(nc.sbuf_tensor(name, list(shape), dt))
        return sb

    sb = mk_sb(es)

    xb = dram("xb", [SEQ, D])
    pos_t = dram("pos_t", [128, NT], I32)
    pos_o = dram("pos_o", [128, NB], I32)
    consts = dram("consts", [128, 512])
    gA_d = dram("gA", [128, D])
    g2_d = dram("g2", [128, D])
    w_in = dram("w_in", [D, IN_TOTAL])
    ident_d = dram("ident", [128, 128])
    xo = dram("xo", [NB, 160, D])
    cmask_d = dram("cmask", [128, 512])
    cvec_d = dram("cvec", [128, 8 * 34])
    w_pw = dram("w_conv_out", [1024, D])
    w_ao = dram("w_attn_o", [D, D])
    w_o = dram("w_out", [D, D])
    wr_d = dram("wr", [D, 36])
    w_eg = dram("w_exp_gate", [NE, D, FF])
    w_eu = dram("w_exp_up", [NE, D, FF])
    w_ed = dram("w_exp_down", [NE, FF, D])
    out_d = dram("out", [NB, 128, D], kind="ExternalOutput")

    s_qT = scratch("s_qT", [NB, 128, 2048], BF16, stage.startswith("B1"))
    s_qiT = scratch("s_qiT", [NB, 128, 1024], BF16, stage.startswith("B1"))
    s_wi = scratch("s_wi", [NB, 128, 16], F32, stage.startswith("B1"))
    s_sT = scratch("s_sT", [NB, 128, 1024], BF16, stage.startswith("B1"))
    s_xT = scratch("s_xT", [NB, 128, 2048], BF16, stage.startswith("B1"))
    s_AT = scratch("s_AT", [NB, 128, 2048], BF16, stage in ("B2", "B2s"))
    s_hT = scratch("s_hT", [NB, 128, 2048], BF16, stage in ("B3", "B3s"))
    s_comb = scratch("s_comb", [128, NB * NE], F32, stage in ("B3", "B3s"))

    w_in_v = w_in.rearrange("(kc p) n -> p kc n", p=128)

    pall = es.enter_context(nc.psum_tensor("pall", [128, 4096], F32))
    pallh = pall.bitcast(BF16)
    r_pb = [Reg(f"pb{i}", psum=True) for i in range(8)]

    def PF(bank, c0=0, c1=512):
        return pall[:, bank * 512 + c0: bank * 512 + c1]

    def PH(bank, c0=0, c1=1024):
        return pallh[:, bank * 1024 + c0: bank * 1024 + c1]

    cst = sb("cst", [128, 512])
    r_cst = Reg("cst")
    kb.dma("sp", cst[:], consts, w=[r_cst])
    gqs = cst[:, 0:128]
    gk = cst[:, 128:256]
    invA = cst[:, 256:272]
    invI = cst[:, 272:280]
    brt = cst[:, 288:324]
    identf = sb("identf", [128, 128])
    identb = sb("identb", [128, 128], BF16)
    r_id = Reg("ident")
    kb.dma("sp", identf[:], ident_d, w=[r_id])
    kb.op("dve", L("tensor_copy", out=identb[:], in_=identf[:]), r=[r_id], w=[r_id])
    epst = sb("epst", [128, 1])
    r_eps = Reg("eps")
    kb.op("dve", L("memset", epst[:], EPS), w=[r_eps])
    gbuf = sb("gbuf", [128, D])
    r_gbuf = Reg("gbuf")

    def rope_tables(cos_t, sin_t, name, posf_ap, ncol, inv_ap, half, r_in, tmp_stack):
        tsb = mk_sb(tmp_stack)
        u = tsb(name + "_u", [128, ncol, half])
        ki = tsb(name + "_ki", [128, ncol, half], I32)
        kf = tsb(name + "_kf", [128, ncol, half])
        d = tsb(name + "_d", [128, ncol, half])
        m1 = tsb(name + "_m1", [128, ncol, half])
        rr = Reg(name)
        kb.op("dve", L("tensor_tensor", out=u[:], in0=posf_ap.unsqueeze(2).broadcast_to([128, ncol, half]),
                       in1=inv_ap.unsqueeze(1).broadcast_to([128, ncol, half]), op=ALU.mult),
              r=[r_in, r_cst], w=[rr])
        kb.op("dve", L("tensor_scalar", out=u[:], in0=u[:], scalar1=1.0 / (2 * math.pi), scalar2=None,
                       op0=ALU.mult), r=[rr], w=[rr])
        for shift, dst in ((0.0, sin_t), (0.25, cos_t)):
            src = u
            if shift != 0.0:
                kb.op("dve", L("tensor_scalar", out=d[:], in0=u[:], scalar1=shift, scalar2=None, op0=ALU.add),
                      r=[rr], w=[rr])
                src = d
            kb.op("dve", L("tensor_copy", out=ki[:], in_=src[:]), r=[rr], w=[rr])
            kb.op("dve", L("tensor_copy", out=kf[:], in_=ki[:]), r=[rr], w=[rr])
            kb.op("dve", L("tensor_tensor", out=d[:], in0=src[:], in1=kf[:], op=ALU.subtract), r=[rr], w=[rr])
            kb.op("dve", L("tensor_scalar", out=m1[:], in0=d[:], scalar1=0.5, scalar2=None, op0=ALU.is_gt),
                  r=[rr], w=[rr])
            kb.op("dve", L("tensor_tensor", out=d[:], in0=d[:], in1=m1[:], op=ALU.subtract), r=[rr], w=[rr])
            kb.op("dve", L("tensor_scalar", out=m1[:], in0=d[:], scalar1=-0.5, scalar2=None, op0=ALU.is_lt),
                  r=[rr], w=[rr])
            kb.op("dve", L("tensor_tensor", out=d[:], in0=d[:], in1=m1[:], op=ALU.add), r=[rr], w=[rr])
            kb.op("act", L("activation", out=dst[:], in_=d[:], func=AF.Sin, scale=2 * math.pi * (1 - 1e-6)),
                  r=[rr], w=[rr])
        return cos_t, sin_t, rr

    def rope(dst, src, half, cos_ap, sin_ap, t4, r_src, r_dst, r_tab, r_t4):
        def sl(ap, a, b):
            return ap[:, a:b] if len(ap.shape) == 2 else ap[:, :, a:b]
        x1, x2 = sl(src, 0, half), sl(src, half, 2 * half)
        ts = [t4[i] for i in range(4)]
        kb.op("dve", L("tensor_tensor", out=ts[0], in0=x1, in1=cos_ap, op=ALU.mult), r=[r_src, r_tab], w=[r_t4])
        kb.op("dve", L("tensor_tensor", out=ts[1], in0=x2, in1=sin_ap, op=ALU.mult), r=[r_src, r_tab], w=[r_t4])
        kb.op("dve", L("tensor_tensor", out=ts[2], in0=x1, in1=sin_ap, op=ALU.mult), r=[r_src, r_tab], w=[r_t4])
        kb.op("dve", L("tensor_tensor", out=ts[3], in0=x2, in1=cos_ap, op=ALU.mult), r=[r_src, r_tab], w=[r_t4])
        kb.op("dve", L("tensor_tensor", out=sl(dst, 0, half), in0=ts[0], in1=ts[1], op=ALU.subtract),
              r=[r_t4], w=[r_dst])
        kb.op("dve", L("tensor_tensor", out=sl(dst, half, 2 * half), in0=ts[2], in1=ts[3], op=ALU.add),
              r=[r_t4], w=[r_dst])

    def rstd_from_ss(st, c_ss, n, r_st):
        kb.op("act", L("activation", out=st[:, c_ss + 1:c_ss + 2], in_=st[:, c_ss:c_ss + 1], func=AF.Sqrt,
                       scale=1.0 / n, bias=epst[0:st.shape[0], :]), r=[r_st, r_eps], w=[r_st])
        kb.op("dve", L("reciprocal", out=st[:, c_ss + 2:c_ss + 3], in_=st[:, c_ss + 1:c_ss + 2]),
              r=[r_st], w=[r_st])

    posoi = sb("posoi", [128, NB], I32)
    posof = sb("posof", [128, NB])
    r_poso = Reg("poso")
    kb.dma("sp", posoi[:], pos_o, w=[r_poso])
    kb.op("dve", L("tensor_copy", out=posof[:], in_=posoi[:]), r=[r_poso], w=[r_poso])
    cosAo, sinAo = sb("rao_cos", [128, NB, 16]), sb("rao_sin", [128, NB, 16])
    cosIo, sinIo = sb("rio_cos", [128, NB, 8]), sb("rio_sin", [128, NB, 8])
    with ExitStack() as tmps:
        _, _, r_ropeAo = rope_tables(cosAo, sinAo, "rao", posof[:], NB, invA, 16, r_poso, tmps)
        _, _, r_ropeIo = rope_tables(cosIo, sinIo, "rio", posof[:], NB, invI, 8, r_poso, tmps)
        kb.barrier()

    ngroups = 4 if stage not in ("B1s", "B2s", "B3s", "Ms") and not stage.startswith("B1c") else 1
    cut = int(stage[3:]) if stage.startswith("B1c") else 99
    if stage in ("smoke", "A"):
        ngroups = 0
    with ExitStack() as lb:
        lsb = mk_sb(lb)
        kb.dma("sp", gbuf[:], gA_d, w=[r_gbuf])
        cw = lsb("cw", [128, 8, 34])
        r_cw = Reg("cw")
        kb.dma("sp", cw[:].rearrange("p a b -> p (a b)"), cvec_d, w=[r_cw])
        onesf = lsb("onesf", [128, 128])
        r_ones = Reg("ones")
        kb.op("dve", L("memset", onesf[:], 1.0 / 1024.0), w=[r_ones])
        xT_g = lsb("xT_g", [128, 16, 640], BF16)
        r_xTg = [Reg(f"xTg{i}") for i in range(4)]
        xm = [lsb(f"xm{i}", [128, D]) for i in range(2)]
        r_xm = [Reg(f"xm{i}") for i in range(2)]
        xhl1 = lsb("xhl", [32, D])
        xhl = [xhl1, xhl1]
        r_xhl1 = Reg("xhl")
        r_xhl = [r_xhl1, r_xhl1]
        xs = [lsb(f"xsb{i}", [128, D], BF16) for i in range(2)]
        r_xs = [Reg(f"xsb{i}") for i in range(2)]
        xsh1 = lsb("xsh", [32, D], BF16)
        xsh = [xsh1, xsh1]
        r_xsh1 = Reg("xsh")
        r_xsh = [r_xsh1, r_xsh1]
        st = [lsb(f"stb{i}", [128, 8]) for i in range(2)]
        r_st = [Reg(f"stb{i}") for i in range(2)]
        yT = lsb("yT", [128, 8, 640])
        r_yT = [Reg(f"yT{i}") for i in range(8)]
        cacc = lsb("cacc", [128, 8, 512])
        r_cacc = [Reg(f"cacc{i}") for i in range(8)]
        sT = lsb("sT", [128, 8, 512], BF16)
        r_sT = Reg("sT")
        wu = [lsb(f"wu{i}", [128, 16, 256], BF16) for i in range(2)]
        r_wu = [Reg(f"wu{i}") for i in range(2)]
        wbuf = [lsb(f"wbuf{i}", [128, 16, 512], BF16) for i in range(2)]
        r_wbuf = [Reg(f"wbuf{i}") for i in range(2)]
        sg = [lsb(f"sg{i}", [128, 640]) for i in range(2)]
        r_sg = [Reg(f"sg{i}") for i in range(2)]
        lnm = lsb("lnm", [128, 3, 512])
        r_lnm = Reg("lnm")
        qsq = lsb("qsq", [128, 512])
        r_qsq = Reg("qsq")
        stq = [lsb(f"stq{i}", [128, 12]) for i in range(2)]
        r_stq = [Reg(f"stq{i}") for i in range(2)]
        qn = [lsb(f"qn{i}", [128, 4, 128]) for i in range(2)]
        r_qn = [Reg(f"qn{i}") for i in range(2)]
        qb = [lsb(f"qb{i}", [128, 512], BF16) for i in range(2)]
        r_qb = [Reg(f"qb{i}") for i in range(2)]
        t4q = [lsb(f"t4q{i}", [128, 4, 8, 16]) for i in range(2)]
        r_t4q = [Reg(f"t4q{i}") for i in range(2)]
        qTs = [lsb(f"qTs{i}", [128, 4, 128], BF16) for i in range(2)]
        r_qTs = [Reg(f"qTs{i}") for i in range(2)]
        wis = lsb("wis", [128, 4, 16])
        r_wis = Reg("wis")
        wwi = lsb("wwi", [128, 16, 16], BF16)
        r_wwi = Reg("wwi")
        kb.dma("pool", wwi[:], w_in_v[:, :, C_WI:C_WI + 16], w=[r_wwi])

        wu_n = [0]
        wb_n = [0]

        def load_wbuf(c0):
            i = wb_n[0] % 2
            wb_n[0] += 1
            kb.dma("pool", wbuf[i][:], w_in_v[:, :, c0:c0 + 512], w=[r_wbuf[i]])
            return i

        for gi in range(ngroups):
            for bl in range(4):
                m = 4 * gi + bl
                b2 = bl % 2
                bT = 2 * b2
                kb.dma("sp", xm[b2][:], xo[m, 32:160, :], w=[r_xm[b2]])
                kb.dma("sp", xhl[b2][:], xo[m, 0:32, :], w=[r_xhl[b2]])
                kb.op("act", L("activation", out=xs[b2][:], in_=xm[b2][:], func=AF.Square,
                               accum_out=st[b2][:, 0:1]), r=[r_xm[b2]], w=[r_xs[b2], r_st[b2]])
                rstd_from_ss(st[b2], 0, D, r_st[b2])
                kb.op("dve", L("scalar_tensor_tensor", out=xs[b2][:], in0=xm[b2][:], scalar=st[b2][:, 2:3],
                               in1=gbuf[:], op0=ALU.mult, op1=ALU.mult),
                      r=[r_xm[b2], r_st[b2], r_gbuf], w=[r_xs[b2]])
                for kc in range(16):
                    kb.op("pe", L("transpose", out=PH(bT, kc * 128, (kc + 1) * 128),
                                  in_=xs[b2][:, kc * 128:(kc + 1) * 128], identity=identb[:]),
                          r=[r_xs[b2], r_id], w=[r_pb[bT + kc // 8]])
                kb.op("act", L("copy", out=xT_g[:, :, bl * 128:(bl + 1) * 128],
                               in_=PH(bT, 0, 2048).rearrange("p (a b) -> p a b", a=16)),
                      r=[r_pb[bT], r_pb[bT + 1]], w=[r_xTg[bl]])
                kb.dma("sp", s_xT[m].rearrange("p (a b) -> p a b", a=16), xT_g[:, :, bl * 128:(bl + 1) * 128],
                       r=[r_xTg[bl]])
                bH = 4 + b2
                kb.op("act", L("activation", out=xsh[b2][:], in_=xhl[b2][:], func=AF.Square,
                               accum_out=st[b2][0:32, 3:4]), r=[r_xhl[b2]], w=[r_xsh[b2], r_st[b2]])
                rstd_from_ss(st[b2][0:32, :], 3, D, r_st[b2])
                kb.op("dve", L("scalar_tensor_tensor", out=xsh[b2][:], in0=xhl[b2][:], scalar=st[b2][0:32, 5:6],
                               in1=gbuf[0:32, :], op0=ALU.mult, op1=ALU.mult),
                      r=[r_xhl[b2], r_st[b2], r_gbuf], w=[r_xsh[b2]])
                for kc in range(16):
                    kb.op("pe", L("transpose", out=PH(bH, kc * 32, (kc + 1) * 32),
                                  in_=xsh[b2][:, kc * 128:(kc + 1) * 128], identity=identb[0:32, 0:32]),
                          r=[r_xsh[b2], r_id], w=[r_pb[bH]])
                kb.op("act", L("copy", out=xT_g[:, :, 512 + bl * 32:512 + (bl + 1) * 32],
                               in_=PH(bH, 0, 512).rearrange("p (a b) -> p a b", a=16)),
                      r=[r_pb[bH]], w=[r_xTg[bl]])

            if cut <= 1:
                break
            for cc in range(8):
                i = wu_n[0] % 2
                wu_n[0] += 1
                kb.dma("pool", wu[i][:, :, 0:128], w_in_v[:, :, C_U + cc * 128:C_U + (cc + 1) * 128], w=[r_wu[i]])
                kb.dma("pool", wu[i][:, :, 128:256], w_in_v[:, :, C_U + 1024 + cc * 128:C_U + 1024 + (cc + 1) * 128],
                       w=[r_wu[i]])
                b3 = 3 * (cc % 2)
                bA, bG, bHh = b3, b3 + 1, b3 + 2
                for (bank, c0, c1, wc, x0, x1) in ((bA, 0, 512, 0, 0, 512), (bG, 0, 512, 128, 0, 512),
                                                   (bHh, 0, 128, 0, 512, 640), (bHh, 128, 256, 128, 512, 640)):
                    for kc in range(16):
                        kb.op("pe", L("matmul", out=PF(bank, c0, c1), lhsT=wu[i][:, kc, wc:wc + 128],
                                      rhs=xT_g[:, kc, x0:x1], start=(kc == 0), stop=(kc == 15)),
                              r=[r_wu[i]] + r_xTg, w=[r_pb[bank]])
                s2 = cc % 2
                kb.op("act", L("activation", out=sg[s2][:, 0:512], in_=PF(bG), func=AF.Sigmoid),
                      r=[r_pb[bG]], w=[r_sg[s2]])
                kb.op("act", L("activation", out=sg[s2][:, 512:640], in_=PF(bHh, 128, 256), func=AF.Sigmoid),
                      r=[r_pb[bHh]], w=[r_sg[s2]])
                yv = yT[:, cc, :].rearrange("p (b t) -> p b t", b=4)
                kb.op("dve", L("tensor_tensor", out=yv[:, :, 32:160],
                               in0=PF(bA).rearrange("p (b t) -> p b t", b=4),
                               in1=sg[s2][:, 0:512].rearrange("p (b t) -> p b t", b=4), op=ALU.mult),
                      r=[r_pb[bA], r_sg[s2]], w=[r_yT[cc]])
                kb.op("dve", L("tensor_tensor", out=yv[:, :, 0:32],
                               in0=PF(bHh, 0, 128).rearrange("p (b t) -> p b t", b=4),
                               in1=sg[s2][:, 512:640].rearrange("p (b t) -> p b t", b=4), op=ALU.mult),
                      r=[r_pb[bHh], r_sg[s2]], w=[r_yT[cc]])

            if cut <= 2:
                break
            for cc in range(8):
                yv = yT[:, cc, :].rearrange("p (b t) -> p b t", b=4)
                av = cacc[:, cc, :].rearrange("p (b t) -> p b t", b=4)
                kb.op("dve", L("tensor_scalar", out=av, in0=yv[:, :, 2:130], scalar1=cw[:, cc, 0:1],
                               scalar2=cw[:, cc, 31:32], op0=ALU.mult, op1=ALU.add),
                      r=[r_yT[cc], r_cw], w=[r_cacc[cc]])
                for k in range(1, 31):
                    kb.op("dve", L("scalar_tensor_tensor", out=av, in0=yv[:, :, 2 + k:130 + k],
                                   scalar=cw[:, cc, k:k + 1], in1=av, op0=ALU.mult, op1=ALU.add),
                          r=[r_yT[cc], r_cw, r_cacc[cc]], w=[r_cacc[cc]])
            bM, bS = 6, 7
            for cc in range(8):
                kb.op("pe", L("matmul", out=PF(bM), lhsT=onesf[:], rhs=cacc[:, cc, :], start=(cc == 0),
                              stop=(cc == 7)), r=[r_ones, r_cacc[cc]], w=[r_pb[bM]])
            sqv = yT[:].rearrange("p a b -> p (a b)")[:, 0:4096].rearrange("p (a b) -> p a b", a=8)
            kb.op("act", L("activation", out=sqv, in_=cacc[:], func=AF.Square), r=r_cacc, w=r_yT)
            for cc in range(8):
                kb.op("pe", L("matmul", out=PF(bS), lhsT=onesf[:], rhs=sqv[:, cc, :], start=(cc == 0),
                              stop=(cc == 7)), r=[r_ones] + r_yT, w=[r_pb[bS]])
            kb.op("act", L("copy", out=lnm[:, 0, :], in_=PF(bM)), r=[r_pb[bM]], w=[r_lnm])
            kb.op("dve", L("tensor_tensor", out=lnm[:, 1, :], in0=lnm[:, 0, :], in1=lnm[:, 0, :], op=ALU.mult),
                  r=[r_lnm], w=[r_lnm])
            kb.op("dve", L("tensor_tensor", out=lnm[:, 1, :], in0=PF(bS), in1=lnm[:, 1, :], op=ALU.subtract),
                  r=[r_pb[bS], r_lnm], w=[r_lnm])
            kb.op("act", L("activation", out=lnm[:, 2, :], in_=lnm[:, 1, :], func=AF.Sqrt, bias=epst[:]),
                  r=[r_lnm, r_eps], w=[r_lnm])
            kb.op("dve", L("reciprocal", out=lnm[:, 1, :], in_=lnm[:, 2, :]), r=[r_lnm], w=[r_lnm])
            for cc in range(8):
                kb.op("dve", L("tensor_tensor", out=cacc[:, cc, :], in0=cacc[:, cc, :], in1=lnm[:, 0, :],
                               op=ALU.subtract), r=[r_cacc[cc], r_lnm], w=[r_cacc[cc]])
                kb.op("dve", L("tensor_tensor", out=cacc[:, cc, :], in0=cacc[:, cc, :], in1=lnm[:, 1, :],
                               op=ALU.mult), r=[r_cacc[cc], r_lnm], w=[r_cacc[cc]])
                kb.op("act", L("activation", out=sT[:, cc, :], in_=cacc[:, cc, :], func=AF.Silu,
                               scale=cw[:, cc, 32:33], bias=cw[:, cc, 33:34]),
                      r=[r_cacc[cc], r_cw], w=[r_sT])
            for bl in range(4):
                m = 4 * gi + bl
                kb.dma("sp", s_sT[m].rearrange("p (a b) -> p a b", a=8), sT[:, :, bl * 128:(bl + 1) * 128],
                       r=[r_sT])

            if cut <= 3:
                break
            it = 0
            for qc in range(4):
                wi_ = load_wbuf(C_Q + qc * 512)
                for bl in range(4):
                    m = 4 * gi + bl
                    p2 = it % 2
                    it += 1
                    bQ = p2
                    bT = 2 + p2
                    for kc in range(16):
                        kb.op("pe", L("matmul", out=PF(bQ), lhsT=xT_g[:, kc, bl * 128:(bl + 1) * 128],
                                      rhs=wbuf[wi_][:, kc, :], start=(kc == 0), stop=(kc == 15)),
                              r=[r_xTg[bl], r_wbuf[wi_]], w=[r_pb[bQ]])
                    kb.op("act", L("activation", out=qsq[:], in_=PF(bQ), func=AF.Square),
                          r=[r_pb[bQ]], w=[r_qsq])
                    kb.op("dve", L("tensor_reduce", out=stq[p2][:, 0:4],
                                   in_=qsq[:].rearrange("p (h d) -> p h d", h=4), axis=AX.X, op=ALU.add),
                          r=[r_qsq], w=[r_stq[p2]])
                    kb.op("act", L("activation", out=stq[p2][:, 4:8], in_=stq[p2][:, 0:4], func=AF.Sqrt,
                                   scale=1.0 / 128, bias=epst[:]), r=[r_stq[p2], r_eps], w=[r_stq[p2]])
                    kb.op("dve", L("reciprocal", out=stq[p2][:, 8:12], in_=stq[p2][:, 4:8]),
                          r=[r_stq[p2]], w=[r_stq[p2]])
                    kb.op("dve", L("tensor_tensor", out=qn[p2][:],
                                   in0=PF(bQ).rearrange("p (h d) -> p h d", h=4),
                                   in1=stq[p2][:, 8:12].unsqueeze(2).broadcast_to([128, 4, 128]), op=ALU.mult),
                          r=[r_pb[bQ], r_stq[p2]], w=[r_qn[p2]])
                    kb.op("dve", L("tensor_tensor", out=qn[p2][:], in0=qn[p2][:],
                                   in1=gqs.unsqueeze(1).broadcast_to([128, 4, 128]), op=ALU.mult),
                          r=[r_qn[p2], r_cst], w=[r_qn[p2]])
                    qbv = qb[p2][:].rearrange("p (h d) -> p h d", h=4)
                    kb.op("act", L("copy", out=qbv[:, :, 32:128], in_=qn[p2][:, :, 32:128]),
                          r=[r_qn[p2]], w=[r_qb[p2]])
                    t4 = [t4q[p2][:, i, 0:4, :] for i in range(4)]
                    rope(qbv, qn[p2][:], 16, cosAo[:, m, :].unsqueeze(1).broadcast_to([128, 4, 16]),
                         sinAo[:, m, :].unsqueeze(1).broadcast_to([128, 4, 16]), t4,
                         r_qn[p2], r_qb[p2], r_ropeAo, r_t4q[p2])
                    for h in range(4):
                        kb.op("pe", L("transpose", out=PH(bT, h * 128, (h + 1) * 128),
                                      in_=qb[p2][:, h * 128:(h + 1) * 128], identity=identb[:]),
                              r=[r_qb[p2], r_id], w=[r_pb[bT]])
                    kb.op("act", L("copy", out=qTs[p2][:],
                                   in_=PH(bT, 0, 512).rearrange("p (a b) -> p a b", a=4)),
                          r=[r_pb[bT]], w=[r_qTs[p2]])
                    kb.dma("sp", s_qT[m].rearrange("p (a b) -> p a b", a=16)[:, 4 * qc:4 * qc + 4, :], qTs[p2][:],
                           r=[r_qTs[p2]])

            if cut <= 4:
                break
            for c2 in range(2):
                wi_ = load_wbuf(C_QI + c2 * 512)
                for bl in range(4):
                    m = 4 * gi + bl
                    p2 = it % 2
                    it += 1
                    bQ = p2
                    bT = 2 + p2
                    for kc in range(16):
                        kb.op("pe", L("matmul", out=PF(bQ), lhsT=xT_g[:, kc, bl * 128:(bl + 1) * 128],
                                      rhs=wbuf[wi_][:, kc, :], start=(kc == 0), stop=(kc == 15)),
                              r=[r_xTg[bl], r_wbuf[wi_]], w=[r_pb[bQ]])
                    kb.op("act", L("copy", out=qn[p2][:].rearrange("p a b -> p (a b)"), in_=PF(bQ)),
                          r=[r_pb[bQ]], w=[r_qn[p2]])
                    import os
                    SUB = int(os.environ.get("SUB", "9"))
                    if SUB <= 1:
                        continue
                    pv = qn[p2][:].rearrange("p a b -> p (a b)").rearrange("p (h d) -> p h d", h=8)
                    qbv = qb[p2][:].rearrange("p (h d) -> p h d", h=8)
                    kb.op("act", L("copy", out=qbv[:, :, 16:64], in_=pv[:, :, 16:64]),
                          r=[r_qn[p2]], w=[r_qb[p2]])
                    if SUB <= 2:
                        continue
                    t4 = [t4q[p2][:, i, :, 0:8] for i in range(4)]
                    rope(qbv, pv, 8, cosIo[:, m, :].unsqueeze(1).broadcast_to([128, 8, 8]),
                         sinIo[:, m, :].unsqueeze(1).broadcast_to([128, 8, 8]), t4,
                         r_qn[p2], r_qb[p2], r_ropeIo, r_t4q[p2])
                    if SUB <= 3:
                        continue
                    for h in range(4):
                        kb.op("pe", L("transpose", out=PH(bT, h * 128, (h + 1) * 128),
                                      in_=qb[p2][:, h * 128:(h + 1) * 128], identity=identb[:]),
                              r=[r_qb[p2], r_id], w=[r_pb[bT]])
                    kb.op("act", L("copy", out=qTs[p2][:],
                                   in_=PH(bT, 0, 512).rearrange("p (a b) -> p a b", a=4)),
                          r=[r_pb[bT]], w=[r_qTs[p2]])
                    if SUB <= 4:
                        continue
                    kb.dma("sp", s_qiT[m].rearrange("p (a b) -> p a b", a=8)[:, 4 * c2:4 * c2 + 4, :], qTs[p2][:],
                           r=[r_qTs[p2]])
            if cut <= 5:
                break
            for bl in range(4):
                m = 4 * gi + bl
                bQ = 4 + bl % 2
                for kc in range(16):
                    kb.op("pe", L("matmul", out=PF(bQ, 0, 16), lhsT=xT_g[:, kc, bl * 128:(bl + 1) * 128],
                                  rhs=wwi[:, kc, :], start=(kc == 0), stop=(kc == 15)),
                          r=[r_xTg[bl], r_wwi], w=[r_pb[bQ]])
                kb.op("act", L("copy", out=wis[:, bl, :], in_=PF(bQ, 0, 16)), r=[r_pb[bQ]], w=[r_wis])
                kb.dma("sp", s_wi[m], wis[:, bl, :], r=[r_wis])
        kb.barrier()

    if stage in ("B1", "B1s") or stage.startswith("B1c"):
        kb.finish()
        kb.emit()
        print("instructions:", kb.nins)
        return

    kvs = ExitStack()
    with kvs:
        ksb = mk_sb(kvs)
        KT = ksb("KT", [128, SEQ], BF16)
        Vt = ksb("Vt", [128, NT, 129], BF16)
        KIT = ksb("KIT", [128, SEQ], BF16)
        r_KT = [Reg(f"KT{n}") for n in range(NT)]
        r_V = [Reg(f"V{n}") for n in range(NT)]
        r_KIT = [Reg(f"KIT{n}") for n in range(NT)]
        r_vone = Reg("vone")
        kb.op("pool", L("memset", Vt[:, :, 128:129], 1.0), w=[r_vone])

        ntile_a = NT if stage not in ("smoke", "B1s", "B2s", "B3s", "Ms") else (2 if stage not in ("B2s", "B3s", "Ms") else 16)
        with ExitStack() as la:
            lsb = mk_sb(la)
            kb.dma("sp", gbuf[:], gA_d, w=[r_gbuf])
            posi = lsb("posi", [128, NT], I32)
            posf = lsb("posf", [128, NT])
            r_pos = Reg("pos")
            kb.dma("sp", posi[:], pos_t, w=[r_pos])
            kb.op("dve", L("tensor_copy", out=posf[:], in_=posi[:]), r=[r_pos], w=[r_pos])
            cosA, sinA = lsb("ra_cos", [128, NT, 16]), lsb("ra_sin", [128, NT, 16])
            cosI, sinI = lsb("ri_cos", [128, NT, 8]), lsb("ri_sin", [128, NT, 8])
            with ExitStack() as tmps:
                _, _, r_ropeA = rope_tables(cosA, sinA, "ra", posf[:], NT, invA, 16, r_pos, tmps)
                _, _, r_ropeI = rope_tables(cosI, sinI, "ri", posf[:], NT, invI, 8, r_pos, tmps)
                kb.barrier()
            wkv = lsb("wkv", [128, 16, 320], BF16)
            r_wkv = Reg("wkv")
            for (c0, c1, o0) in ((C_K, C_K + 256, 0), (C_KI, C_KI + 64, 256)):
                kb.dma("pool", wkv[:, :, o0:o0 + (c1 - c0)], w_in_v[:, :, c0:c1], w=[r_wkv])
            xin = [lsb(f"xin{i}", [128, D]) for i in range(2)]
            r_xin = [Reg(f"xin{i}") for i in range(2)]
            junk = lsb("junk", [128, D], BF16)
            r_junk = Reg("junk")
            xs = [lsb(f"xs{i}", [128, D], BF16) for i in range(2)]
            r_xs = [Reg(f"xs{i}") for i in range(2)]
            xT = [lsb(f"xT{i}", [128, 16, 128], BF16) for i in range(2)]
            r_xT = [Reg(f"xT{i}") for i in range(2)]
            st = [lsb(f"st{i}", [128, 8]) for i in range(2)]
            r_st = [Reg(f"st{i}") for i in range(2)]
            kn = [lsb(f"kn{i}", [128, 192]) for i in range(2)]
            kfin = [lsb(f"kfin{i}", [128, 256], BF16) for i in range(2)]
            tmp = [lsb(f"tmp{i}", [128, 4, 16]) for i in range(2)]
            r_kn = [Reg(f"kn{i}") for i in range(2)]
            r_kf = [Reg(f"kf{i}") for i in range(2)]
            r_tmp = [Reg(f"tmp{i}") for i in range(2)]

            for n in range(ntile_a):
                b2 = n % 2
                bT = 2 * b2
                bZ = 4 + b2
                bK = 6 + b2
                kb.dma("sp", xin[b2][:], xb[n * 128:(n + 1) * 128, :], w=[r_xin[b2]])
                kb.op("act", L("activation", out=junk[:], in_=xin[b2][:], func=AF.Square, accum_out=st[b2][:, 0:1]),
                      r=[r_xin[b2]], w=[r_junk, r_st[b2]])
                rstd_from_ss(st[b2], 0, D, r_st[b2])
                kb.op("dve", L("scalar_tensor_tensor", out=xs[b2][:], in0=xin[b2][:], scalar=st[b2][:, 2:3],
                               in1=gbuf[:], op0=ALU.mult, op1=ALU.mult),
                      r=[r_xin[b2], r_st[b2], r_gbuf], w=[r_xs[b2]])
                for kc in range(16):
                    kb.op("pe", L("transpose", out=PH(bT, kc * 128, (kc + 1) * 128),
                                  in_=xs[b2][:, kc * 128:(kc + 1) * 128], identity=identb[:]),
                          r=[r_xs[b2], r_id], w=[r_pb[bT + kc // 8]])
                kb.op("act", L("copy", out=xT[b2][:].rearrange("p a b -> p (a b)"), in_=PH(bT, 0, 2048)),
                      r=[r_pb[bT], r_pb[bT + 1]], w=[r_xT[b2]])
                for kc in range(16):
                    kb.op("pe", L("matmul", out=PF(bZ, 0, 320), lhsT=xT[b2][:, kc, :], rhs=wkv[:, kc, :],
                                  start=(kc == 0), stop=(kc == 15)),
                          r=[r_xT[b2], r_wkv], w=[r_pb[bZ]])
                kb.op("act", L("copy", out=Vt[:, n, 0:128], in_=PF(bZ, 128, 256)), r=[r_pb[bZ]], w=[r_V[n]])
                kb.op("act", L("activation", out=kn[b2][:, 0:128], in_=PF(bZ, 0, 128), func=AF.Square,
                               accum_out=st[b2][:, 3:4]), r=[r_pb[bZ]], w=[r_kn[b2], r_st[b2]])
                rstd_from_ss(st[b2], 3, 128, r_st[b2])
                kb.op("dve", L("scalar_tensor_tensor", out=kn[b2][:, 0:128], in0=PF(bZ, 0, 128),
                               scalar=st[b2][:, 5:6], in1=gk, op0=ALU.mult, op1=ALU.mult),
                      r=[r_pb[bZ], r_st[b2], r_cst], w=[r_kn[b2]])
                kb.op("dve", L("tensor_copy", out=kn[b2][:, 128:192], in_=PF(bZ, 256, 320)),
                      r=[r_pb[bZ]], w=[r_kn[b2]])
                kb.op("dve", L("tensor_copy", out=kfin[b2][:, 32:128], in_=kn[b2][:, 32:128]),
                      r=[r_kn[b2]], w=[r_kf[b2]])
                kb.op("dve", L("tensor_copy", out=kfin[b2][:, 144:192], in_=kn[b2][:, 144:192]),
                      r=[r_kn[b2]], w=[r_kf[b2]])
                t4a = [tmp[b2][:, i, 0:16] for i in range(4)]
                t4i = [tmp[b2][:, i, 0:8] for i in range(4)]
                rope(kfin[b2][:, 0:128], kn[b2][:, 0:128], 16, cosA[:, n, :], sinA[:, n, :], t4a,
                     r_kn[b2], r_kf[b2], r_ropeA, r_tmp[b2])
                rope(kfin[b2][:, 128:192], kn[b2][:, 128:192], 8, cosI[:, n, :], sinI[:, n, :], t4i,
                     r_kn[b2], r_kf[b2], r_ropeI, r_tmp[b2])
                kb.op("dve", L("tensor_copy", out=kfin[b2][:, 192:256], in_=kfin[b2][:, 128:192]),
                      r=[r_kf[b2]], w=[r_kf[b2]])
                kb.op("pe", L("transpose", out=PH(bK, 0, 128), in_=kfin[b2][:, 0:128], identity=identb[:]),
                      r=[r_kf[b2], r_id], w=[r_pb[bK]])
                kb.op("pe", L("transpose", out=PH(bK, 128, 256), in_=kfin[b2][:, 128:256], identity=identb[:]),
                      r=[r_kf[b2], r_id], w=[r_pb[bK]])
                kb.op("act", L("copy", out=KT[:, n * 128:(n + 1) * 128], in_=PH(bK, 0, 128)),
                      r=[r_pb[bK]], w=[r_KT[n]])
                kb.op("act", L("copy", out=KIT[:, n * 128:(n + 1) * 128], in_=PH(bK, 128, 256)),
                      r=[r_pb[bK]], w=[r_KIT[n]])
            kb.barrier()

        if stage in ("smoke", "A"):
            o_kt = dram("o_kt", [128, SEQ], BF16, kind="ExternalOutput")
            o_v = dram("o_v", [128, NT * 129], BF16, kind="ExternalOutput")
            o_kit = dram("o_kit", [128, SEQ], BF16, kind="ExternalOutput")
            regs = r_KT[:ntile_a] + r_V[:ntile_a] + r_KIT[:ntile_a] + [r_vone]
            ro = Reg("out")
            T_ = ntile_a * 128
            kb.dma("sp", o_kt[:, 0:T_], KT[:, 0:T_], r=regs, w=[ro])
            kb.dma("sp", o_v[:, 0:ntile_a * 129], Vt[:, 0:ntile_a, :].rearrange("p a b -> p (a b)"), r=regs, w=[ro])
            kb.dma("sp", o_kit[:, 0:T_], KIT[:, 0:T_], r=regs, w=[ro])
            kb.finish()
            kb.emit()
            print("instructions:", kb.nins)
            return


        blocks_b2 = list(range(NB))
        if stage == "B2s":
            blocks_b2 = [0, 1]
        if stage in ("B3s", "Ms"):
            blocks_b2 = [0, 1, 2, 3]
        with ExitStack() as l2:
            lsb = mk_sb(l2)
            cmask = lsb("cmask_sb", [128, 512])
            r_cmask = Reg("cmask")
            kb.dma("sp", cmask[:], cmask_d, w=[r_cmask])
            sc = lsb("sc", [128, SEQ])
            r_sc = [Reg(f"sc{i}") for i in range(16)]
            selb = lsb("selb", [128, SEQ], BF16)
            r_sel = Reg("sel")
            selT = lsb("selT", [128, NT, 128], BF16)
            r_selT = [Reg(f"selT{i}") for i in range(4)]
            qTb = [lsb(f"qTb{i}", [128, 16, 128], BF16) for i in range(2)]
            r_qTb = [Reg(f"qTb{i}") for i in range(2)]
            qiTb = [lsb(f"qiTb{i}", [128, 8, 128], BF16) for i in range(2)]
            r_qiTb = [Reg(f"qiTb{i}") for i in range(2)]
            wib = [lsb(f"wib{i}", [128, 16]) for i in range(2)]
            r_wib = [Reg(f"wib{i}") for i in range(2)]
            rl = [lsb(f"rl{i}", [128, 512]) for i in range(2)]
            r_rl = [Reg(f"rl{i}") for i in range(2)]
            Eb = [lsb(f"Eb{i}", [128, 512], BF16) for i in range(2)]
            r_Eb = [Reg(f"Eb{i}") for i in range(2)]
            Pb = [lsb(f"Pb{i}", [128, 4, 128], BF16) for i in range(2)]
            r_Pb = [Reg(f"Pb{i}") for i in range(2)]
            bis = lsb("bis", [128, 8])
            r_bis = Reg("bis")
            bisA = lsb("bisA", [128, 2])
            r_mid, r_cnt, r_cnta = Reg("mid"), Reg("cnt"), Reg("cnta")
            r_selD, r_selA = Reg("selD"), Reg("selA")
            rden = lsb("rden", [128, 16])
            r_rden = Reg("rden")
            Ab = lsb("Ab", [128, 2048], BF16)
            r_Ab = Reg("Ab")
            ATb = lsb("ATb", [128, 2048], BF16)
            r_ATb = Reg("ATb")
            cnt_ib = [0]
            cnt_ia = [0]

            def load_q(bi, m):
                q2 = bi % 2
                kb.dma("sp", qTb[q2][:].rearrange("p a b -> p (a b)"), s_qT[m], w=[r_qTb[q2]])
                kb.dma("sp", qiTb[q2][:].rearrange("p a b -> p (a b)"), s_qiT[m], w=[r_qiTb[q2]])
                kb.dma("sp", wib[q2][:], s_wi[m], w=[r_wib[q2]])

            def gen_indexer(bi, m):
                q2 = bi % 2
                NCH = m + 1
                for ch in range(NCH):
                    scc = sc[:, ch * 512:(ch + 1) * 512]
                    for h in range(16):
                        ib = cnt_ib[0]
                        cnt_ib[0] += 1
                        bank = 5 + ib % 3
                        i2 = ib % 2
                        hf = h % 2
                        kb.op("pe", L("matmul", out=PF(bank), lhsT=qiTb[q2][hf * 64:(hf + 1) * 64, h // 2, :],
                                      rhs=KIT[hf * 64:(hf + 1) * 64, ch * 512:(ch + 1) * 512], start=True, stop=True),
                              r=[r_qiTb[q2]] + r_KIT[4 * ch:4 * ch + 4], w=[r_pb[bank]])
                        kb.op("act", L("activation", out=rl[i2][:], in_=PF(bank), func=AF.Relu),
                              r=[r_pb[bank]], w=[r_rl[i2]])
                        if h == 0:
                            kb.op("dve", L("tensor_scalar", out=scc, in0=rl[i2][:], scalar1=wib[q2][:, 0:1],
                                           scalar2=None, op0=ALU.mult), r=[r_rl[i2], r_wib[q2]], w=[r_sc[ch]])
                        else:
                            kb.op("dve", L("scalar_tensor_tensor", out=scc, in0=rl[i2][:],
                                           scalar=wib[q2][:, h:h + 1], in1=scc, op0=ALU.mult, op1=ALU.add),
                                  r=[r_rl[i2], r_wib[q2], r_sc[ch]], w=[r_sc[ch]])
                        yield

            def topk_and_mask(bi, m):
                NCH = m + 1
                S = 512 * NCH
                NKT = 4 * NCH
                rs = r_sc[0:NCH]
                kb.op("dve", L("tensor_reduce", out=bis[:, 0:1], in_=sc[:, 0:S], axis=AX.X, op=ALU.min),
                      r=rs, w=[r_bis])
                lc = sc[:, S - 512:S]
                kb.op("dve", L("tensor_tensor", out=lc, in0=lc, in1=cmask[:], op=ALU.add),
                      r=[r_sc[NCH - 1], r_cmask], w=[r_sc[NCH - 1]])
                kb.op("dve", L("tensor_reduce", out=bis[:, 1:2], in_=sc[:, 0:S], axis=AX.X, op=ALU.max),
                      r=rs, w=[r_bis])
                kb.op("dve", L("tensor_tensor", out=bis[:, 2:3], in0=bis[:, 1:2], in1=bis[:, 0:1], op=ALU.subtract),
                      r=[r_bis], w=[r_bis])
                kb.op("dve", L("tensor_scalar", out=bis[:, 2:3], in0=bis[:, 2:3], scalar1=1.0001, scalar2=1e-6,
                               op0=ALU.mult, op1=ALU.add), r=[r_bis], w=[r_bis])
                kb.op("dve", L("tensor_copy", out=bis[:, 3:4], in_=bis[:, 0:1]), r=[r_bis], w=[r_bis])
                nd = max(1, int(round(0.45 * NCH)))
                Sd = 512 * nd
                n_act = S - Sd
                rsd = r_sc[0:nd]
                rsa = r_sc[nd:NCH]
                for itb in range(1, NBIS + 1):
                    f = 2.0 ** (-itb)
                    kb.op("dve", L("scalar_tensor_tensor", out=bis[:, 4:5], in0=bis[:, 2:3], scalar=f,
                                   in1=bis[:, 3:4], op0=ALU.mult, op1=ALU.add), r=[r_bis], w=[r_mid])
                    if n_act > 0:
                        kb.op("act", L("activation", out=selb[:, Sd:S], in_=sc[:, Sd:S], func=AF.Sign, scale=-1.0,
                                       bias=bis[:, 4:5], accum_out=bisA[:, 0:1]),
                              r=rsa + [r_mid], w=[r_selA, r_cnta])
                    kb.op("dve", L("tensor_scalar", out=selb[:, 0:Sd], in0=sc[:, 0:Sd], scalar1=bis[:, 4:5],
                                   scalar2=0.0, op0=ALU.is_ge, op1=ALU.add, accum_out=bis[:, 5:6]),
                          r=rsd + [r_mid], w=[r_selD, r_cnt])
                    if n_act > 0:
                        kb.op("dve", L("scalar_tensor_tensor", out=bis[:, 5:6], in0=bisA[:, 0:1], scalar=-0.5,
                                       in1=bis[:, 5:6], op0=ALU.mult, op1=ALU.add), r=[r_cnta, r_cnt], w=[r_cnt])
                    kb.op("dve", L("tensor_scalar", out=bis[:, 6:7], in0=bis[:, 5:6],
                                   scalar1=NSEL - 0.5 - 0.5 * n_act, scalar2=f, op0=ALU.is_ge, op1=ALU.mult),
                          r=[r_cnt], w=[r_bis])
                    kb.op("dve", L("scalar_tensor_tensor", out=bis[:, 3:4], in0=bis[:, 6:7], scalar=bis[:, 2:3],
                                   in1=bis[:, 3:4], op0=ALU.mult, op1=ALU.add), r=[r_bis], w=[r_bis])
                kb.op("dve", L("tensor_scalar", out=selb[:, 0:S], in0=sc[:, 0:S], scalar1=bis[:, 3:4],
                               scalar2=None, op0=ALU.is_ge), r=rs + [r_bis], w=[r_selD, r_selA])
                for g in range((NKT + 15) // 16):
                    n_in = min(16, NKT - 16 * g)
                    bT = 2 * (g % 2)
                    for k2 in range(n_in):
                        kt = 16 * g + k2
                        kb.op("pe", L("transpose", out=PH(bT, k2 * 128, (k2 + 1) * 128),
                                      in_=selb[:, kt * 128:(kt + 1) * 128], identity=identb[:]),
                              r=[r_selD, r_selA, r_id], w=[r_pb[bT + k2 // 8]])
                    kb.op("act", L("copy", out=selT[:, 16 * g:16 * g + n_in, :],
                                   in_=PH(bT, 0, n_in * 128).rearrange("p (a b) -> p a b", a=n_in)),
                          r=[r_pb[bT], r_pb[bT + 1]], w=[r_selT[g]])

            def gen_attention(bi, m):
                q2 = bi % 2
                NCH = m + 1
                NKT = 4 * NCH
                its = [(ps_, kt, hgl) for ps_ in range(2) for kt in range(NKT) for hgl in range(2)]
                ia0 = cnt_ia[0]
                cnt_ia[0] += len(its)

                def issue_qk(idx):
                    ps_, kt, hgl = its[idx]
                    hg = 2 * ps_ + hgl
                    lb = (ia0 + idx) % 2
                    kb.op("pe", L("matmul", out=PF(lb), lhsT=KT[:, kt * 128:(kt + 1) * 128],
                                  rhs=qTb[q2][:, 4 * hg:4 * hg + 4, :], start=True, stop=True),
                          r=[r_KT[kt], r_qTb[q2]], w=[r_pb[lb]])

                def issue_mid(idx):
                    ps_, kt, hgl = its[idx]
                    lb = (ia0 + idx) % 2
                    kb.op("act", L("activation", out=Eb[lb][:], in_=PF(lb), func=AF.Exp, scale=ATTN_SCALE),
                          r=[r_pb[lb]], w=[r_Eb[lb]])
                    kb.op("dve", L("tensor_tensor", out=Pb[lb][:],
                                   in0=Eb[lb][:].rearrange("p (h t) -> p h t", h=4),
                                   in1=selT[:, kt, :].unsqueeze(1).broadcast_to([128, 4, 128]), op=ALU.mult),
                          r=[r_Eb[lb], r_selT[kt // 16]], w=[r_Pb[lb]])

                def issue_pv(idx):
                    ps_, kt, hgl = its[idx]
                    lb = (ia0 + idx) % 2
                    for h4 in range(4):
                        h8 = 4 * hgl + h4
                        pbk = 2 + h8 // 3
                        o0 = (h8 % 3) * 129
                        kb.op("pe", L("matmul", out=PF(pbk, o0, o0 + 129), lhsT=Pb[lb][:, h4, :],
                                      rhs=Vt[:, kt, :], start=(kt == 0 and h8 % 3 == 0),
                                      stop=(kt == NKT - 1 and (h8 % 3 == 2 or h8 == 7))),
                              r=[r_Pb[lb], r_V[kt], r_vone], w=[r_pb[pbk]])

                def normalise(ps_):
                    for pbk in range(2, 5):
                        h0 = 3 * (pbk - 2)
                        nh = min(3, 8 - h0)
                        hh = 8 * ps_ + h0
                        pv3 = PF(pbk, 0, nh * 129).rearrange("p (h c) -> p h c", c=129)
                        kb.op("dve", L("reciprocal", out=rden[:, hh:hh + nh].unsqueeze(2), in_=pv3[:, :, 128:129]),
                              r=[r_pb[pbk]], w=[r_rden])
                        kb.op("dve", L("tensor_tensor",
                                       out=Ab[:, hh * 128:(hh + nh) * 128].rearrange("p (h d) -> p h d", h=nh),
                                       in0=pv3[:, :, 0:128],
                                       in1=rden[:, hh:hh + nh].unsqueeze(2).broadcast_to([128, nh, 128]), op=ALU.mult),
                              r=[r_pb[pbk], r_rden], w=[r_Ab])

                issue_qk(0)
                for idx in range(len(its)):
                    if idx + 1 < len(its) and its[idx + 1][0] == its[idx][0]:
                        issue_qk(idx + 1)
                    issue_mid(idx)
                    yield "mid"
                    issue_pv(idx)
                    if idx + 1 < len(its) and its[idx + 1][0] != its[idx][0]:
                        normalise(0)
                        issue_qk(idx + 1)
                    yield "end"
                normalise(1)
                for h in range(16):
                    kb.op("pe", L("transpose", out=PH(0, h * 128, (h + 1) * 128), in_=Ab[:, h * 128:(h + 1) * 128],
                                  identity=identb[:]), r=[r_Ab, r_id], w=[r_pb[h // 8]])
                kb.op("act", L("copy", out=ATb[:], in_=PH(0, 0, 2048)), r=[r_pb[0], r_pb[1]], w=[r_ATb])
                kb.dma("sp", s_AT[m], ATb[:], r=[r_ATb])

            def run_interleaved(gens):
                att, idxg = gens[0], (gens[1] if len(gens) > 1 else None)
                while att is not None:
                    try:
                        tag = next(att)
                    except StopIteration:
                        att = None
                        break
                    if tag == "mid" and idxg is not None:
                        try:
                            next(idxg)
                        except StopIteration:
                            idxg = None
                if idxg is not None:
                    for _ in idxg:
                        pass

            nb2 = len(blocks_b2)
            load_q(0, blocks_b2[0])
            run_interleaved([None, gen_indexer(0, blocks_b2[0])])
            topk_and_mask(0, blocks_b2[0])
            for bi, m in enumerate(blocks_b2):
                nxt_g = None
                if bi + 1 < nb2:
                    load_q(bi + 1, blocks_b2[bi + 1])
                    nxt_g = gen_indexer(bi + 1, blocks_b2[bi + 1])
                run_interleaved([gen_attention(bi, m), nxt_g])
                if bi + 1 < nb2:
                    topk_and_mask(bi + 1, blocks_b2[bi + 1])
            kb.barrier()

    if stage in ("B2", "B2s"):
        kb.finish()
        kb.emit()
        print("instructions:", kb.nins)
        return

    comb_all = sb("comb_all", [128, NB, NE])
    r_comb = Reg("comb")
    kb.op("pool", L("memset", comb_all[:], 0.0), w=[r_comb])
    ng3 = 4 if stage not in ("B3s", "Ms") else 1
    with ExitStack() as l3:
        lsb = mk_sb(l3)
        kb.dma("sp", gbuf[:], g2_d, w=[r_gbuf])
        wrf = lsb("wrf", [128, 16, 36])
        r_wrf = Reg("wrf")
        kb.dma("sp", wrf[:], wr_d.rearrange("(kc p) n -> p kc n", p=128), w=[r_wrf])
        xTm = [lsb(f"xTm{i}", [128, 16, 128], BF16) for i in range(4)]
        r_xTm = [Reg(f"xTm{i}") for i in range(4)]
        sTm = [lsb(f"sTm{i}", [128, 8, 128], BF16) for i in range(4)]
        r_sTm = [Reg(f"sTm{i}") for i in range(4)]
        ATm = [lsb(f"ATm{i}", [128, 16, 128], BF16) for i in range(4)]
        r_ATm = [Reg(f"ATm{i}") for i in range(4)]
        xh = [lsb(f"xh{i}", [128, D]) for i in range(4)]
        r_xh = [Reg(f"xh{i}") for i in range(4)]
        wb3 = [lsb(f"wb3{i}", [128, 16, 512], BF16) for i in range(2)]
        r_wb3 = [Reg(f"wb3{i}") for i in range(2)]
        sgc = [lsb(f"sgc{i}", [128, 512], BF16) for i in range(4)]
        r_sgc = [Reg(f"sgc{i}") for i in range(4)]
        sga = [lsb(f"sga{i}", [128, 512], BF16) for i in range(4)]
        r_sga = [Reg(f"sga{i}") for i in range(4)]
        t1 = [lsb(f"t1{i}", [128, 512]) for i in range(4)]
        r_t1 = [Reg(f"t1{i}") for i in range(4)]
        t2 = [lsb(f"t2{i}", [128, 512]) for i in range(2)]
        r_t2 = [Reg(f"t2{i}") for i in range(2)]
        mix = [lsb(f"mix{i}", [128, D], BF16) for i in range(4)]
        r_mix = [Reg(f"mix{i}") for i in range(4)]
        mixT = [lsb(f"mixT{i}", [128, 16, 128], BF16) for i in range(4)]
        r_mixT = [Reg(f"mixT{i}") for i in range(4)]
        hn2f = lsb("hn2f", [128, D])
        r_hn2f = Reg("hn2f")
        hTf = lsb("hTf", [128, 16, 128])
        r_hTf = Reg("hTf")
        hTb = lsb("hTb", [128, 16, 128], BF16)
        r_hTb = Reg("hTb")
        st3 = lsb("st3", [128, 8])
        r_st3 = Reg("st3")
        lg = lsb("lg", [128, 36])
        rt = lsb("rt", [128, 96])
        r_rt = Reg("rt")
        w3n = [0]
        pq = [0]

        def load_w3(src_ap, nk):
            i = w3n[0] % 2
            w3n[0] += 1
            kb.dma("pool", wb3[i][:, 0:nk, :], src_ap, w=[r_wb3[i]])
            return i

        w_pw_v = w_pw.rearrange("(kc p) n -> p kc n", p=128)
        w_ao_v = w_ao.rearrange("(kc p) n -> p kc n", p=128)
        w_o_v = w_o.rearrange("(kc p) n -> p kc n", p=128)

        def proj(bl, lhs_fn, nk, wi_, regs):
            bank = pq[0] % 4
            pq[0] += 1
            for kc in range(nk):
                kb.op("pe", L("matmul", out=PF(bank), lhsT=lhs_fn(kc), rhs=wb3[wi_][:, kc, :],
                              start=(kc == 0), stop=(kc == nk - 1)), r=regs + [r_wb3[wi_]], w=[r_pb[bank]])
            return bank

        import os
        CUT3 = int(os.environ.get("CUT3", "99"))
        for gi in range(ng3):
            for bl in range(4):
                m = 4 * gi + bl
                kb.dma("sp", xTm[bl][:].rearrange("p a b -> p (a b)"), s_xT[m], w=[r_xTm[bl]])
                kb.dma("sp", sTm[bl][:].rearrange("p a b -> p (a b)"), s_sT[m], w=[r_sTm[bl]])
                kb.dma("sp", ATm[bl][:].rearrange("p a b -> p (a b)"), s_AT[m], w=[r_ATm[bl]])
                kb.dma("sp", xh[bl][:], xo[m, 32:160, :], w=[r_xh[bl]])
            for c in range(4):
                cs = slice(c * 512, (c + 1) * 512)
                wi_ = load_w3(w_in_v[:, :, C_GC + c * 512:C_GC + (c + 1) * 512], 16)
                for bl in range(4):
                    bank = proj(bl, lambda kc: xTm[bl][:, kc, :], 16, wi_, [r_xTm[bl]])
                    kb.op("act", L("activation", out=sgc[bl][:], in_=PF(bank), func=AF.Sigmoid),
                          r=[r_pb[bank]], w=[r_sgc[bl]])
                wi_ = load_w3(w_pw_v[:, :, cs], 8)
                for bl in range(4):
                    bank = proj(bl, lambda kc: sTm[bl][:, kc, :], 8, wi_, [r_sTm[bl]])
                    kb.op("dve", L("tensor_tensor", out=t1[bl][:], in0=PF(bank), in1=sgc[bl][:], op=ALU.mult),
                          r=[r_pb[bank], r_sgc[bl]], w=[r_t1[bl]])
                wi_ = load_w3(w_in_v[:, :, C_GA + c * 512:C_GA + (c + 1) * 512], 16)
                for bl in range(4):
                    bank = proj(bl, lambda kc: xTm[bl][:, kc, :], 16, wi_, [r_xTm[bl]])
                    kb.op("act", L("activation", out=sga[bl][:], in_=PF(bank), func=AF.Sigmoid),
                          r=[r_pb[bank]], w=[r_sga[bl]])
                wi_ = load_w3(w_ao_v[:, :, cs], 16)
                for bl in range(4):
                    bank = proj(bl, lambda kc: ATm[bl][:, kc, :], 16, wi_, [r_ATm[bl]])
                    kb.op("dve", L("tensor_tensor", out=t2[bl % 2][:], in0=PF(bank), in1=sga[bl][:], op=ALU.mult),
                          r=[r_pb[bank], r_sga[bl]], w=[r_t2[bl % 2]])
                    kb.op("dve", L("tensor_tensor", out=mix[bl][:, cs], in0=t1[bl][:], in1=t2[bl % 2][:], op=ALU.add),
                          r=[r_t1[bl], r_t2[bl % 2]], w=[r_mix[bl]])
            if CUT3 <= 1:
                break
            for bl in range(4):
                bT = 4 + 2 * (bl % 2)
                for kc in range(16):
                    kb.op("pe", L("transpose", out=PH(bT, kc * 128, (kc + 1) * 128),
                                  in_=mix[bl][:, kc * 128:(kc + 1) * 128], identity=identb[:]),
                          r=[r_mix[bl], r_id], w=[r_pb[bT + kc // 8]])
                kb.op("act", L("copy", out=mixT[bl][:].rearrange("p a b -> p (a b)"), in_=PH(bT, 0, 2048)),
                      r=[r_pb[bT], r_pb[bT + 1]], w=[r_mixT[bl]])
            for c in range(4):
                cs = slice(c * 512, (c + 1) * 512)
                wi_ = load_w3(w_o_v[:, :, cs], 16)
                for bl in range(4):
                    bank = proj(bl, lambda kc: mixT[bl][:, kc, :], 16, wi_, [r_mixT[bl]])
                    kb.op("dve", L("tensor_tensor", out=xh[bl][:, cs], in0=PF(bank), in1=xh[bl][:, cs], op=ALU.add),
                          r=[r_pb[bank], r_xh[bl]], w=[r_xh[bl]])
            if CUT3 <= 2:
                break
            for bl in range(4):
                m = 4 * gi + bl
                kb.dma("sp", out_d[m], xh[bl][:], r=[r_xh[bl]])
                kb.op("act", L("activation", out=hn2f[:], in_=xh[bl][:], func=AF.Square, accum_out=st3[:, 0:1]),
                      r=[r_xh[bl]], w=[r_hn2f, r_st3])
                rstd_from_ss(st3, 0, D, r_st3)
                kb.op("dve", L("scalar_tensor_tensor", out=hn2f[:], in0=xh[bl][:], scalar=st3[:, 2:3], in1=gbuf[:],
                               op0=ALU.mult, op1=ALU.mult), r=[r_xh[bl], r_st3, r_gbuf], w=[r_hn2f])
                if CUT3 <= 3:
                    continue
                for kc in range(16):
                    kb.op("pe", L("matmul", out=PF(4 + kc // 4, (kc % 4) * 128, (kc % 4 + 1) * 128),
                                  lhsT=hn2f[:, kc * 128:(kc + 1) * 128], rhs=identf[:], start=True, stop=True),
                          r=[r_hn2f, r_id], w=[r_pb[4 + kc // 4]])
                SUB3 = int(os.environ.get("SUB3", "9"))
                if SUB3 <= 1:
                    continue
                for q4 in range(4):
                    kb.op("act", L("copy", out=hTb[:, 4 * q4:4 * q4 + 4, :].rearrange("p a b -> p (a b)"),
                                   in_=PF(4 + q4)), r=[r_pb[4 + q4]], w=[r_hTb])
                    if SUB3 <= 2:
                        continue
                    kb.op("dve", L("tensor_copy", out=hTf[:, 4 * q4:4 * q4 + 4, :].rearrange("p a b -> p (a b)"),
                                   in_=PF(4 + q4)), r=[r_pb[4 + q4]], w=[r_hTf])
                if SUB3 <= 3:
                    continue
                kb.dma("sp", s_hT[m], hTb[:].rearrange("p a b -> p (a b)"), r=[r_hTb])
                if CUT3 <= 4:
                    continue
                bank = pq[0] % 4
                pq[0] += 1
                for kc in range(16):
                    kb.op("pe", L("matmul", out=PF(bank, 0, 36), lhsT=hTf[:, kc, :], rhs=wrf[:, kc, :],
                                  start=(kc == 0), stop=(kc == 15)), r=[r_hTf, r_wrf], w=[r_pb[bank]])
                if CUT3 <= 5:
                    continue
                R = [r_rt]
                kb.op("dve", L("tensor_tensor", out=lg[:], in0=PF(bank, 0, 36), in1=brt, op=ALU.add),
                      r=[r_pb[bank], r_cst], w=R)
                gl = lg[:, 0:4]
                el = lg[:, 4:36].rearrange("p (g e) -> p g e", g=4)
                gmax, ngmax, sumg, pg = rt[:, 0:1], rt[:, 1:2], rt[:, 2:3], rt[:, 3:4]
                ohg, eg = rt[:, 4:8], rt[:, 8:12]
                tmp48 = rt[:, 12:44].rearrange("p (g e) -> p g e", g=4)
                e_in, mx8, oh1, oh2 = rt[:, 44:52], rt[:, 52:60], rt[:, 60:68], rt[:, 68:76]
                dd, ed, w1, w2 = rt[:, 76:77], rt[:, 77:78], rt[:, 78:79], rt[:, 79:80]
                wi1, wpg = rt[:, 80:88], rt[:, 88:96]
                kb.op("dve", L("tensor_reduce", out=gmax, in_=gl, axis=AX.X, op=ALU.max), r=R, w=R)
                kb.op("dve", L("tensor_scalar", out=ohg, in0=gl, scalar1=gmax, scalar2=None, op0=ALU.is_equal),
                      r=R, w=R)
                kb.op("dve", L("tensor_scalar", out=ngmax, in0=gmax, scalar1=-1.0, scalar2=None, op0=ALU.mult),
                      r=R, w=R)
                kb.op("act", L("activation", out=eg, in_=gl, func=AF.Exp, bias=ngmax, accum_out=sumg), r=R, w=R)
                kb.op("dve", L("reciprocal", out=pg, in_=sumg), r=R, w=R)
                kb.op("dve", L("tensor_tensor", out=tmp48, in0=el, in1=ohg.unsqueeze(2).broadcast_to([128, 4, 8]),
                               op=ALU.mult), r=R, w=R)
                kb.op("dve", L("tensor_reduce", out=e_in, in_=tmp48.rearrange("p g e -> p e g"), axis=AX.X,
                               op=ALU.add), r=R, w=R)
                if CUT3 <= 6:
                    continue
                kb.op("dve", L("max", out=mx8, in_=e_in), r=R, w=R)
                if CUT3 <= 7:
                    continue
                kb.op("dve", L("tensor_scalar", out=oh1, in0=e_in, scalar1=mx8[:, 0:1], scalar2=None,
                               op0=ALU.is_equal), r=R, w=R)
                kb.op("dve", L("tensor_scalar", out=oh2, in0=e_in, scalar1=mx8[:, 1:2], scalar2=None,
                               op0=ALU.is_equal), r=R, w=R)
                kb.op("dve", L("tensor_tensor", out=dd, in0=mx8[:, 1:2], in1=mx8[:, 0:1], op=ALU.subtract), r=R, w=R)
                kb.op("act", L("activation", out=ed, in_=dd, func=AF.Exp), r=R, w=R)
                kb.op("dve", L("tensor_scalar", out=w1, in0=ed, scalar1=1.0, scalar2=None, op0=ALU.add), r=R, w=R)
                kb.op("dve", L("reciprocal", out=w1, in_=w1), r=R, w=R)
                kb.op("dve", L("tensor_tensor", out=w2, in0=ed, in1=w1, op=ALU.mult), r=R, w=R)
                kb.op("dve", L("tensor_scalar", out=wi1, in0=oh1, scalar1=w1, scalar2=None, op0=ALU.mult), r=R, w=R)
                kb.op("dve", L("scalar_tensor_tensor", out=wi1, in0=oh2, scalar=w2, in1=wi1, op0=ALU.mult,
                               op1=ALU.add), r=R, w=R)
                kb.op("dve", L("tensor_scalar", out=wpg, in0=wi1, scalar1=pg, scalar2=None, op0=ALU.mult), r=R, w=R)
                kb.op("dve", L("tensor_tensor", out=comb_all[:, m, :].rearrange("p (g e) -> p g e", g=4),
                               in0=ohg.unsqueeze(2).broadcast_to([128, 4, 8]),
                               in1=wpg.unsqueeze(1).broadcast_to([128, 4, 8]), op=ALU.mult), r=R, w=[r_comb])
        if stage in ("B3", "B3s"):
            kb.dma("sp", s_comb, comb_all[:].rearrange("p a b -> p (a b)"), r=[r_comb])
        kb.barrier()

    if stage in ("B3", "B3s"):
        kb.finish()
        kb.emit()
        print("instructions:", kb.nins)
        return

    with ExitStack() as l4:
        lsb = mk_sb(l4)
        hT = lsb("hT", [128, 16, 1024], BF16)
        r_hT = [Reg(f"hT{i}") for i in range(8)]
        acc = lsb("acc", [128, 8, D])
        r_acc = [[Reg(f"acc{i}_{k}") for k in range(2)] for i in range(8)]
        wg = [lsb(f"wg{i}", [128, 16, 256], BF16) for i in range(2)]
        r_wg = [Reg(f"wg{i}") for i in range(2)]
        wu_ = [lsb(f"wup{i}", [128, 16, 256], BF16) for i in range(2)]
        r_wu_ = [Reg(f"wup{i}") for i in range(2)]
        wd = [lsb(f"wd{i}", [128, 4, D], BF16) for i in range(2)]
        r_wd = [Reg(f"wd{i}") for i in range(2)]
        actT = lsb("actT", [128, 4, 1024], BF16)
        r_actT = [Reg(f"actT{i}") for i in range(4)]
        sgm = [lsb(f"sgm{i}", [128, 512], BF16) for i in range(2)]
        r_sgm = [Reg(f"sgm{i}") for i in range(2)]
        npass = 2
        nblk = 8
        ne_run = NE
        if stage == "Ms":
            npass, nblk, ne_run = 1, 4, 2
        ntch = nblk // 4
        hw_n = 0
        gq_n = 0
        dq_n = 0
        for p in range(npass):
            for blk in range(nblk):
                m = 8 * p + blk
                kb.dma("sp", hT[:, :, blk * 128:(blk + 1) * 128], s_hT[m].rearrange("p (a b) -> p a b", a=16),
                       w=[r_hT[blk]])
                kb.dma("sp", acc[:, blk, :], out_d[m], w=r_acc[blk])
            for e in range(ne_run):
                d2 = e % 2
                kb.dma("pool", wd[d2][:], w_ed[e].rearrange("(fc p) n -> p fc n", p=128), w=[r_wd[d2]])
                for half in range(2):
                    h2 = hw_n % 2
                    hw_n += 1
                    kb.dma("pool", wg[h2][:], w_eg[e].rearrange("(kc p) f -> p kc f", p=128)[:, :, half * 256:(half + 1) * 256],
                           w=[r_wg[h2]])
                    kb.dma("pool", wu_[h2][:], w_eu[e].rearrange("(kc p) f -> p kc f", p=128)[:, :, half * 256:(half + 1) * 256],
                           w=[r_wu_[h2]])
                    for fcl in range(2):
                        fc = 2 * half + fcl
                        for tch in range(ntch):
                            g2_ = gq_n % 2
                            gq_n += 1
                            bG, bU = g2_, 2 + g2_
                            for kc in range(16):
                                kb.op("pe", L("matmul", out=PF(bG), lhsT=wg[h2][:, kc, fcl * 128:(fcl + 1) * 128],
                                              rhs=hT[:, kc, tch * 512:(tch + 1) * 512], start=(kc == 0), stop=(kc == 15)),
                                      r=[r_wg[h2]] + r_hT[4 * tch:4 * tch + 4], w=[r_pb[bG]])
                            for kc in range(16):
                                kb.op("pe", L("matmul", out=PF(bU), lhsT=wu_[h2][:, kc, fcl * 128:(fcl + 1) * 128],
                                              rhs=hT[:, kc, tch * 512:(tch + 1) * 512], start=(kc == 0), stop=(kc == 15)),
                                      r=[r_wu_[h2]] + r_hT[4 * tch:4 * tch + 4], w=[r_pb[bU]])
                            kb.op("act", L("activation", out=sgm[g2_][:], in_=PF(bG), func=AF.Silu),
                                  r=[r_pb[bG]], w=[r_sgm[g2_]])
                            kb.op("dve", L("tensor_tensor", out=actT[:, fc, tch * 512:(tch + 1) * 512], in0=PF(bU),
                                           in1=sgm[g2_][:], op=ALU.mult), r=[r_pb[bU], r_sgm[g2_]], w=[r_actT[fc]])
                for blk in range(nblk):
                    m = 8 * p + blk
                    for nh in range(2):
                        bD = 4 + 2 * (dq_n % 2)
                        dq_n += 1
                        for sub in range(2):
                            c0 = nh * 1024 + sub * 512
                            for fc in range(4):
                                kb.op("pe", L("matmul", out=PF(bD + sub), lhsT=actT[:, fc, blk * 128:(blk + 1) * 128],
                                              rhs=wd[d2][:, fc, c0:c0 + 512], start=(fc == 0), stop=(fc == 3)),
                                      r=[r_actT[fc], r_wd[d2]], w=[r_pb[bD + sub]])
                        av = acc[:, blk, nh * 1024:(nh + 1) * 1024]
                        kb.op("dve", L("scalar_tensor_tensor", out=av, in0=PF(bD, 0, 1024),
                                       scalar=comb_all[:, m, e:e + 1], in1=av, op0=ALU.mult, op1=ALU.add),
                              r=[r_pb[bD], r_pb[bD + 1], r_comb, r_acc[blk][nh]], w=[r_acc[blk][nh]])
            for blk in range(nblk):
                m = 8 * p + blk
                kb.dma("sp", out_d[m], acc[:, blk, :], r=r_acc[blk])
        kb.barrier()
    kb.finish()
    kb.emit()
    print("instructions:", kb.nins)
    return

    raise NotImplementedError


def host_consts(inputs):
    theta = 500000.0
    invA = theta ** (-np.arange(0, 32, 2, dtype=np.float32) / np.float32(32))
    invI = theta ** (-np.arange(0, 16, 2, dtype=np.float32) / np.float32(16))
    c = np.zeros((128, 512), np.float32)
    c[:, 0:128] = np.asarray(inputs["q_norm_g"]).reshape(1, 128)
    c[:, 128:256] = np.asarray(inputs["k_norm_g"]).reshape(1, 128)
    c[:, 256:272] = invA.astype(np.float32)[None, :]
    c[:, 272:280] = invI.astype(np.float32)[None, :]
    c[:, 288:292] = np.asarray(inputs["b_router_group"]).reshape(1, 4)
    c[:, 292:324] = np.asarray(inputs["b_router_expert"]).reshape(1, 32)
    return c


def make_in_maps(inputs, stage="full"):
    x = np.asarray(inputs["x"])
    pos = np.asarray(inputs["positions"])
    consts = host_consts(inputs)
    ident = np.eye(128, dtype=np.float32)
    w_in = np.ascontiguousarray(np.asarray(inputs["w_in"])[0])
    gA = np.ascontiguousarray(np.broadcast_to(np.asarray(inputs["attn_norm_g"]).reshape(1, D), (128, D)))
    g2 = np.ascontiguousarray(np.broadcast_to(np.asarray(inputs["ffn_norm_g"]).reshape(1, D), (128, D)))
    cv = np.zeros((128, 8, 34), np.float32)
    dw = np.asarray(inputs["conv_dw_w"])[0]
    cv[:, :, 0:31] = dw.T.reshape(8, 128, 31).transpose(1, 0, 2)
    cv[:, :, 31] = np.asarray(inputs["conv_dw_b"])[0].reshape(8, 128).T
    cv[:, :, 32] = np.asarray(inputs["conv_ln_g"])[0].reshape(8, 128).T
    cv[:, :, 33] = np.asarray(inputs["conv_ln_b"])[0].reshape(8, 128).T
    wr = np.ascontiguousarray(np.concatenate([np.asarray(inputs["w_router_group"])[0],
                                              np.asarray(inputs["w_router_expert"])[0]], axis=1))
    shared = {
        "consts": consts, "gA": gA, "g2": g2, "w_in": w_in, "ident": ident,
        "cvec": np.ascontiguousarray(cv.reshape(128, 8 * 34)),
        "w_conv_out": np.ascontiguousarray(np.asarray(inputs["w_conv_out"])[0]),
        "w_attn_o": np.ascontiguousarray(np.asarray(inputs["w_attn_o"])[0]),
        "w_out": np.ascontiguousarray(np.asarray(inputs["w_out"])[0]),
        "wr": wr,
        "w_exp_gate": np.ascontiguousarray(np.asarray(inputs["w_exp_gate"])[0]),
        "w_exp_up": np.ascontiguousarray(np.asarray(inputs["w_exp_up"])[0]),
        "w_exp_down": np.ascontiguousarray(np.asarray(inputs["w_exp_down"])[0]),
    }
    maps = []
    for c in range(8):
        b, j = c // 4, c % 4
        xpad = np.concatenate([np.zeros((32, D), np.float32), x[b]], axis=0)
        xo_ = np.stack([xpad[128 * (4 * m + j):128 * (4 * m + j) + 160] for m in range(NB)])
        pos_o = np.stack([pos[b, 128 * (4 * m + j):128 * (4 * m + j) + 128] for m in range(NB)], axis=1)
        cidx = np.arange(512)[None, :]
        prow = np.arange(128)[:, None]
        cmask = np.where(cidx <= 128 * j + prow, 0.0, NEG).astype(np.float32)
        m_ = dict(shared)
        m_.update({
            "xb": np.ascontiguousarray(x[b]),
            "pos_t": np.ascontiguousarray(pos[b].reshape(NT, 128).T),
            "pos_o": np.ascontiguousarray(pos_o.astype(np.int32)),
            "xo": np.ascontiguousarray(xo_),
            "cmask": cmask,
        })
        maps.append(m_)
    return maps


def kernel(**inputs):
    nc = build_nc("full")
    maps = make_in_maps(inputs)
    res = run_bass_kernel_spmd(nc, maps, core_ids=list(range(8)))
    out = np.zeros((2, SEQ, D), np.float32)
    for c in range(8):
        b, j = c // 4, c % 4
        o = np.asarray(res.results[c]["out"])
        for m in range(NB):
            i = 4 * m + j
            out[b, 128 * i:128 * (i + 1)] = o[m]
    return out
```

```python
import math
from contextlib import ExitStack

import numpy as np
import concourse.bass as bass
import concourse.mybir as mybir
from concourse.bass_utils import run_bass_kernel_spmd

F32 = mybir.dt.float32
BF16 = mybir.dt.bfloat16
I32 = mybir.dt.int32
AF = mybir.ActivationFunctionType
ALU = mybir.AluOpType
AX = mybir.AxisListType

D = 2048
SEQ = 8192
NT = SEQ // 128
NB = 16
EPS = 1e-6
IN_TOTAL = 9552
C_U, C_Q, C_K, C_V, C_QI, C_KI, C_WI, C_GC, C_GA = 0, 2048, 4096, 4224, 4352, 5376, 5440, 5456, 7504
NEG = -1.0e30
EPOCH = 30000
NSLOT = 8
NSEL = 256
NBIS = 16
ATTN_SCALE = 128 ** -0.5
NE = 32
FF = 512


def L(name, *a, **k):
    return lambda e: getattr(e, name)(*a, **k)


class Reg:
    __slots__ = ("w", "r", "name", "psum")

    def __init__(self, name="", psum=False):
        self.w = None
        self.r = []
        self.name = name
        self.psum = psum


class KB:
    def __init__(self, nc, es):
        self.nc = nc
        self.es = es
        self.names = ("pe", "act", "dve", "pool", "sp")
        self.sem = {}
        self.cnt = {e: 0 for e in ("pe", "act", "dve", "pool")}
        self.seen = {e: {} for e in self.names}
        self.dn = {q: 0 for q in ("sp", "act", "pool")}
        for q in self.dn:
            for i in range(NSLOT):
                self.sem[("d", q, i)] = es.enter_context(nc.semaphore(f"d_{q}{i}"))
        self.nins = 0
        self.prog = {e: [] for e in self.names}

    def emit(self):
        with self.nc.Block() as block:
            def run(en):
                def body(e):
                    for it in self.prog[en]:
                        if it[0] == "w":
                            e.wait_ge(it[1], it[2])
                        elif it[0] == "i":
                            it[1](e).then_inc(it[2], 1)
                        else:
                            e.dma_start(out=it[1], in_=it[2], **it[4]).then_inc(it[3], 16)
                return body
            block.sync(run("sp"))
            block.tensor(run("pe"))
            block.scalar(run("act"))
            block.vector(run("dve"))
            block.gpsimd(run("pool"))

    def _semfor(self, key):
        if key not in self.sem:
            self.sem[key] = self.es.enter_context(self.nc.semaphore(f"s_{key[0]}{key[1]}"))
        return self.sem[key]

    def _wait(self, en, evs):
        best = {}
        for k, v in evs:
            if best.get(k, 0) < v:
                best[k] = v
        for k, v in best.items():
            if self.seen[en].get(k, 0) < v:
                self.prog[en].append(("w", self._semfor(k), v))
                self.seen[en][k] = v

    def _deps(self, en, r, w, is_dma=False):
        evs = []
        for t in r:
            if t.w is not None:
                if not (en == "pe" and t.w[0][0] == "pe"):
                    evs.append(t.w)
            if t.psum:
                for ev in t.r:
                    if ev[0][0] != en:
                        evs.append(ev)
        for t in w:
            if t.w is not None:
                if not (en == "pe" and t.w[0][0] == "pe"):
                    evs.append(t.w)
            for ev in t.r:
                if en == "pe" and ev[0][0] == "pe":
                    continue
                evs.append(ev)
        return evs

    def _commit(self, ev, r, w):
        for t in r:
            t.r.append(ev)
            if len(t.r) > 48:
                best = {}
                for k, v in t.r:
                    if best.get(k, 0) < v:
                        best[k] = v
                t.r = list(best.items())
        for t in w:
            t.w = ev
            t.r = []

    def op(self, en, fn, r=(), w=()):
        self._wait(en, self._deps(en, r, w))
        c = self.cnt[en]
        key = (en, c // EPOCH)
        val = c % EPOCH + 1
        self.prog[en].append(("i", fn, self._semfor(key)))
        self.cnt[en] = c + 1
        self._commit((key, val), r, w)
        self.nins += 1

    def dma(self, q, out, in_, r=(), w=(), **kw):
        i = self.dn[q]
        slot, use = i % NSLOT, i // NSLOT
        key = ("d", q, slot)
        evs = self._deps(q, r, w, is_dma=True)
        if use > 0:
            evs.append((key, 16 * use))
        self._wait(q, evs)
        self.prog[q].append(("d", out, in_, self.sem[key], kw))
        self.dn[q] = i + 1
        self._commit((key, 16 * (use + 1)), r, w)
        self.nins += 1

    def all_events(self):
        evs = []
        for en, c in self.cnt.items():
            if c > 0:
                evs.append(((en, (c - 1) // EPOCH), (c - 1) % EPOCH + 1))
        for q, i in self.dn.items():
            for back in range(1, min(i, NSLOT) + 1):
                ii = i - back
                evs.append((("d", q, ii % NSLOT), 16 * (ii // NSLOT + 1)))
        return evs

    def barrier(self):
        evs = self.all_events()
        for en in self.names:
            self._wait(en, evs)

    def finish(self):
        self._wait("sp", self.all_events())


def build_nc(stage="full"):
    nc = bass.Bass("TRN2", target_bir_lowering=False)
    es = ExitStack()
    with es:
        _build(nc, es, stage)
    return nc


def _build(nc, es, stage):
    kb = KB(nc, es)
    dbg = stage != "full"

    def dram(name, shape, dt=F32, kind="ExternalInput"):
        return nc.dram_tensor(name, list(shape), dt, kind=kind).ap()

    def scratch(name, shape, dt, want_dbg):
        kind = "ExternalOutput" if (dbg and want_dbg) else "Internal"
        return nc.dram_tensor(name, list(shape), dt, kind=kind).ap()

    def mk_sb(stack):
        def sb(name, shape, dt=F32):
            return stack.enter_context(nc.sbuf_tensor(name, list(shape), dt))
        return sb

    sb = mk_sb(es)

    xb = dram("xb", [SEQ, D])
    pos_t = dram("pos_t", [128, NT], I32)
    pos_o = dram("pos_o", [128, NB], I32)
    consts = dram("consts", [128, 512])
    gA_d = dram("gA", [128, D])
    g2_d = dram("g2", [128, D])
    w_in = dram("w_in", [D, IN_TOTAL])
    ident_d = dram("ident", [128, 128])
    xo = dram("xo", [NB, 160, D])
    cmask_d = dram("cmask", [128, 512])
    cvec_d = dram("cvec", [128, 8 * 34])
    w_pw = dram("w_conv_out", [1024, D])
    w_ao = dram("w_attn_o", [D, D])
    w_o = dram("w_out", [D, D])
    wr_d = dram("wr", [D, 36])
    w_eg = dram("w_exp_gate", [NE, D, FF])
    w_eu = dram("w_exp_up", [NE, D, FF])
    w_ed = dram("w_exp_down", [NE, FF, D])
    out_d = dram("out", [NB, 128, D], kind="ExternalOutput")

    s_qT = scratch("s_qT", [NB, 128, 2048], BF16, stage.startswith("B1"))
    s_qiT = scratch("s_qiT", [NB, 128, 1024], BF16, stage.startswith("B1"))
    s_wi = scratch("s_wi", [NB, 128, 16], F32, stage.startswith("B1"))
    s_sT = scratch("s_sT", [NB, 128, 1024], BF16, stage.startswith("B1"))
    s_xT = scratch("s_xT", [NB, 128, 2048], BF16, stage.startswith("B1"))
    s_AT = scratch("s_AT", [NB, 128, 2048], BF16, stage in ("B2", "B2s"))
    s_hT = scratch("s_hT", [NB, 128, 2048], BF16, stage in ("B3", "B3s"))
    s_comb = scratch("s_comb", [128, NB * NE], F32, stage in ("B3", "B3s"))

    w_in_v = w_in.rearrange("(kc p) n -> p kc n", p=128)

    pall = es.enter_context(nc.psum_tensor("pall", [128, 4096], F32))
    pallh = pall.bitcast(BF16)
    r_pb = [Reg(f"pb{i}", psum=True) for i in range(8)]

    def PF(bank, c0=0, c1=512):
        return pall[:, bank * 512 + c0: bank * 512 + c1]

    def PH(bank, c0=0, c1=1024):
        return pallh[:, bank * 1024 + c0: bank * 1024 + c1]

    cst = sb("cst", [128, 512])
    r_cst = Reg("cst")
    kb.dma("sp", cst[:], consts, w=[r_cst])
    gqs = cst[:, 0:128]
    gk = cst[:, 128:256]
    invA = cst[:, 256:272]
    invI = cst[:, 272:280]
    brt = cst[:, 288:324]
    identf = sb("identf", [128, 128])
    identb = sb("identb", [128, 128], BF16)
    r_id = Reg("ident")
    kb.dma("sp", identf[:], ident_d, w=[r_id])
    kb.op("dve", L("tensor_copy", out=identb[:], in_=identf[:]), r=[r_id], w=[r_id])
    epst = sb("epst", [128, 1])
    r_eps = Reg("eps")
    kb.op("dve", L("memset", epst[:], EPS), w=[r_eps])
    gbuf = sb("gbuf", [128, D])
    r_gbuf = Reg("gbuf")

    def rope_tables(cos_t, sin_t, name, posf_ap, ncol, inv_ap, half, r_in, tmp_stack):
        tsb = mk_sb(tmp_stack)
        u = tsb(name + "_u", [128, ncol, half])
        ki = tsb(name + "_ki", [128, ncol, half], I32)
        kf = tsb(name + "_kf", [128, ncol, half])
        d = tsb(name + "_d", [128, ncol, half])
        m1 = tsb(name + "_m1", [128, ncol, half])
        rr = Reg(name)
        kb.op("dve", L("tensor_tensor", out=u[:], in0=posf_ap.unsqueeze(2).broadcast_to([128, ncol, half]),
                       in1=inv_ap.unsqueeze(1).broadcast_to([128, ncol, half]), op=ALU.mult),
              r=[r_in, r_cst], w=[rr])
        kb.op("dve", L("tensor_scalar", out=u[:], in0=u[:], scalar1=1.0 / (2 * math.pi), scalar2=None,
                       op0=ALU.mult), r=[rr], w=[rr])
        for shift, dst in ((0.0, sin_t), (0.25, cos_t)):
            src = u
            if shift != 0.0:
                kb.op("dve", L("tensor_scalar", out=d[:], in0=u[:], scalar1=shift, scalar2=None, op0=ALU.add),
                      r=[rr], w=[rr])
                src = d
            kb.op("dve", L("tensor_copy", out=ki[:], in_=src[:]), r=[rr], w=[rr])
            kb.op("dve", L("tensor_copy", out=kf[:], in_=ki[:]), r=[rr], w=[rr])
            kb.op("dve", L("tensor_tensor", out=d[:], in0=src[:], in1=kf[:], op=ALU.subtract), r=[rr], w=[rr])
            kb.op("dve", L("tensor_scalar", out=m1[:], in0=d[:], scalar1=0.5, scalar2=None, op0=ALU.is_gt),
                  r=[rr], w=[rr])
            kb.op("dve", L("tensor_tensor", out=d[:], in0=d[:], in1=m1[:], op=ALU.subtract), r=[rr], w=[rr])
            kb.op("dve", L("tensor_scalar", out=m1[:], in0=d[:], scalar1=-0.5, scalar2=None, op0=ALU.is_lt),
                  r=[rr], w=[rr])
            kb.op("dve", L("tensor_tensor", out=d[:], in0=d[:], in1=m1[:], op=ALU.add), r=[rr], w=[rr])
            kb.op("act", L("activation", out=dst[:], in_=d[:], func=AF.Sin, scale=2 * math.pi * (1 - 1e-6)),
                  r=[rr], w=[rr])
        return cos_t, sin_t, rr

    def rope(dst, src, half, cos_ap, sin_ap, t4, r_src, r_dst, r_tab, r_t4):
        def sl(ap, a, b):
            return ap[:, a:b] if len(ap.shape) == 2 else ap[:, :, a:b]
        x1, x2 = sl(src, 0, half), sl(src, half, 2 * half)
        ts = [t4[i] for i in range(4)]
        kb.op("dve", L("tensor_tensor", out=ts[0], in0=x1, in1=cos_ap, op=ALU.mult), r=[r_src, r_tab], w=[r_t4])
        kb.op("dve", L("tensor_tensor", out=ts[1], in0=x2, in1=sin_ap, op=ALU.mult), r=[r_src, r_tab], w=[r_t4])
        kb.op("dve", L("tensor_tensor", out=ts[2], in0=x1, in1=sin_ap, op=ALU.mult), r=[r_src, r_tab], w=[r_t4])
        kb.op("dve", L("tensor_tensor", out=ts[3], in0=x2, in1=cos_ap, op=ALU.mult), r=[r_src, r_tab], w=[r_t4])
        kb.op("dve", L("tensor_tensor", out=sl(dst, 0, half), in0=ts[0], in1=ts[1], op=ALU.subtract),
              r=[r_t4], w=[r_dst])
        kb.op("dve", L("tensor_tensor", out=sl(dst, half, 2 * half), in0=ts[2], in1=ts[3], op=ALU.add),
              r=[r_t4], w=[r_dst])

    def rstd_from_ss(st, c_ss, n, r_st):
        kb.op("act", L("activation", out=st[:, c_ss + 1:c_ss + 2], in_=st[:, c_ss:c_ss + 1], func=AF.Sqrt,
                       scale=1.0 / n, bias=epst[0:st.shape[0], :]), r=[r_st, r_eps], w=[r_st])
        kb.op("dve", L("reciprocal", out=st[:, c_ss + 2:c_ss + 3], in_=st[:, c_ss + 1:c_ss + 2]),
              r=[r_st], w=[r_st])

    posoi = sb("posoi", [128, NB], I32)
    posof = sb("posof", [128, NB])
    r_poso = Reg("poso")
    kb.dma("sp", posoi[:], pos_o, w=[r_poso])
    kb.op("dve", L("tensor_copy", out=posof[:], in_=posoi[:]), r=[r_poso], w=[r_poso])
    cosAo, sinAo = sb("rao_cos", [128, NB, 16]), sb("rao_sin", [128, NB, 16])
    cosIo, sinIo = sb("rio_cos", [128, NB, 8]), sb("rio_sin", [128, NB, 8])
    with ExitStack() as tmps:
        _, _, r_ropeAo = rope_tables(cosAo, sinAo, "rao", posof[:], NB, invA, 16, r_poso, tmps)
        _, _, r_ropeIo = rope_tables(cosIo, sinIo, "rio", posof[:], NB, invI, 8, r_poso, tmps)
        kb.barrier()

    ngroups = 4 if stage not in ("B1s", "B2s", "B3s", "Ms") and not stage.startswith("B1c") else 1
    cut = int(stage[3:]) if stage.startswith("B1c") else 99
    if stage in ("smoke", "A"):
        ngroups = 0
    with ExitStack() as lb:
        lsb = mk_sb(lb)
        kb.dma("sp", gbuf[:], gA_d, w=[r_gbuf])
        cw = lsb("cw", [128, 8, 34])
        r_cw = Reg("cw")
        kb.dma("sp", cw[:].rearrange("p a b -> p (a b)"), cvec_d, w=[r_cw])
        onesf = lsb("onesf", [128, 128])
        r_ones = Reg("ones")
        kb.op("dve", L("memset", onesf[:], 1.0 / 1024.0), w=[r_ones])
        xT_g = lsb("xT_g", [128, 16, 640], BF16)
        r_xTg = [Reg(f"xTg{i}") for i in range(4)]
        xm = [lsb(f"xm{i}", [128, D]) for i in range(2)]
        r_xm = [Reg(f"xm{i}") for i in range(2)]
        xhl1 = lsb("xhl", [32, D])
        xhl = [xhl1, xhl1]
        r_xhl1 = Reg("xhl")
        r_xhl = [r_xhl1, r_xhl1]
        xs = [lsb(f"xsb{i}", [128, D], BF16) for i in range(2)]
        r_xs = [Reg(f"xsb{i}") for i in range(2)]
        xsh1 = lsb("xsh", [32, D], BF16)
        xsh = [xsh1, xsh1]
        r_xsh1 = Reg("xsh")
        r_xsh = [r_xsh1, r_xsh1]
        st = [lsb(f"stb{i}", [128, 8]) for i in range(2)]
        r_st = [Reg(f"stb{i}") for i in range(2)]
        yT = lsb("yT", [128, 8, 640])
        r_yT = [Reg(f"yT{i}") for i in range(8)]
        cacc = lsb("cacc", [128, 8, 512])
        r_cacc = [Reg(f"cacc{i}") for i in range(8)]
        sT = lsb("sT", [128, 8, 512], BF16)
        r_sT = Reg("sT")
        wu = [lsb(f"wu{i}", [128, 16, 256], BF16) for i in range(2)]
        r_wu = [Reg(f"wu{i}") for i in range(2)]
        wbuf = [lsb(f"wbuf{i}", [128, 16, 512], BF16) for i in range(2)]
        r_wbuf = [Reg(f"wbuf{i}") for i in range(2)]
        sg = [lsb(f"sg{i}", [128, 640]) for i in range(2)]
        r_sg = [Reg(f"sg{i}") for i in range(2)]
        lnm = lsb("lnm", [128, 3, 512])
        r_lnm = Reg("lnm")
        qsq = lsb("qsq", [128, 512])
        r_qsq = Reg("qsq")
        stq = [lsb(f"stq{i}", [128, 12]) for i in range(2)]
        r_stq = [Reg(f"stq{i}") for i in range(2)]
        qn = [lsb(f"qn{i}", [128, 4, 128]) for i in range(2)]
        r_qn = [Reg(f"qn{i}") for i in range(2)]
        qb = [lsb(f"qb{i}", [128, 512], BF16) for i in range(2)]
        r_qb = [Reg(f"qb{i}") for i in range(2)]
        t4q = [lsb(f"t4q{i}", [128, 4, 8, 16]) for i in range(2)]
        r_t4q = [Reg(f"t4q{i}") for i in range(2)]
        qTs = [lsb(f"qTs{i}", [128, 4, 128], BF16) for i in range(2)]
        r_qTs = [Reg(f"qTs{i}") for i in range(2)]
        wis = lsb("wis", [128, 4, 16])
        r_wis = Reg("wis")
        wwi = lsb("wwi", [128, 16, 16], BF16)
        r_wwi = Reg("wwi")
        kb.dma("pool", wwi[:], w_in_v[:, :, C_WI:C_WI + 16], w=[r_wwi])

        wu_n = [0]
        wb_n = [0]

        def load_wbuf(c0):
            i = wb_n[0] % 2
            wb_n[0] += 1
            kb.dma("pool", wbuf[i][:], w_in_v[:, :, c0:c0 + 512], w=[r_wbuf[i]])
            return i

        for gi in range(ngroups):
            for bl in range(4):
                m = 4 * gi + bl
                b2 = bl % 2
                bT = 2 * b2
                kb.dma("sp", xm[b2][:], xo[m, 32:160, :], w=[r_xm[b2]])
                kb.dma("sp", xhl[b2][:], xo[m, 0:32, :], w=[r_xhl[b2]])
                kb.op("act", L("activation", out=xs[b2][:], in_=xm[b2][:], func=AF.Square,
                               accum_out=st[b2][:, 0:1]), r=[r_xm[b2]], w=[r_xs[b2], r_st[b2]])
                rstd_from_ss(st[b2], 0, D, r_st[b2])
                kb.op("dve", L("scalar_tensor_tensor", out=xs[b2][:], in0=xm[b2][:], scalar=st[b2][:, 2:3],
                               in1=gbuf[:], op0=ALU.mult, op1=ALU.mult),
                      r=[r_xm[b2], r_st[b2], r_gbuf], w=[r_xs[b2]])
                for kc in range(16):
                    kb.op("pe", L("transpose", out=PH(bT, kc * 128, (kc + 1) * 128),
                                  in_=xs[b2][:, kc * 128:(kc + 1) * 128], identity=identb[:]),
                          r=[r_xs[b2], r_id], w=[r_pb[bT + kc // 8]])
                kb.op("act", L("copy", out=xT_g[:, :, bl * 128:(bl + 1) * 128],
                               in_=PH(bT, 0, 2048).rearrange("p (a b) -> p a b", a=16)),
                      r=[r_pb[bT], r_pb[bT + 1]], w=[r_xTg[bl]])
                kb.dma("sp", s_xT[m].rearrange("p (a b) -> p a b", a=16), xT_g[:, :, bl * 128:(bl + 1) * 128],
                       r=[r_xTg[bl]])
                bH = 4 + b2
                kb.op("act", L("activation", out=xsh[b2][:], in_=xhl[b2][:], func=AF.Square,
                               accum_out=st[b2][0:32, 3:4]), r=[r_xhl[b2]], w=[r_xsh[b2], r_st[b2]])
                rstd_from_ss(st[b2][0:32, :], 3, D, r_st[b2])
                kb.op("dve", L("scalar_tensor_tensor", out=xsh[b2][:], in0=xhl[b2][:], scalar=st[b2][0:32, 5:6],
                               in1=gbuf[0:32, :], op0=ALU.mult, op1=ALU.mult),
                      r=[r_xhl[b2], r_st[b2], r_gbuf], w=[r_xsh[b2]])
                for kc in range(16):
                    kb.op("pe", L("transpose", out=PH(bH, kc * 32, (kc + 1) * 32),
                                  in_=xsh[b2][:, kc * 128:(kc + 1) * 128], identity=identb[0:32, 0:32]),
                          r=[r_xsh[b2], r_id], w=[r_pb[bH]])
                kb.op("act", L("copy", out=xT_g[:, :, 512 + bl * 32:512 + (bl + 1) * 32],
                               in_=PH(bH, 0, 512).rearrange("p (a b) -> p a b", a=16)),
                      r=[r_pb[bH]], w=[r_xTg[bl]])

            if cut <= 1:
                break
            for cc in range(8):
                i = wu_n[0] % 2
                wu_n[0] += 1
                kb.dma("pool", wu[i][:, :, 0:128], w_in_v[:, :, C_U + cc * 128:C_U + (cc + 1) * 128], w=[r_wu[i]])
                kb.dma("pool", wu[i][:, :, 128:256], w_in_v[:, :, C_U + 1024 + cc * 128:C_U + 1024 + (cc + 1) * 128],
                       w=[r_wu[i]])
                b3 = 3 * (cc % 2)
                bA, bG, bHh = b3, b3 + 1, b3 + 2
                for (bank, c0, c1, wc, x0, x1) in ((bA, 0, 512, 0, 0, 512), (bG, 0, 512, 128, 0, 512),
                                                   (bHh, 0, 128, 0, 512, 640), (bHh, 128, 256, 128, 512, 640)):
                    for kc in range(16):
                        kb.op("pe", L("matmul", out=PF(bank, c0, c1), lhsT=wu[i][:, kc, wc:wc + 128],
                                      rhs=xT_g[:, kc, x0:x1], start=(kc == 0), stop=(kc == 15)),
                              r=[r_wu[i]] + r_xTg, w=[r_pb[bank]])
                s2 = cc % 2
                kb.op("act", L("activation", out=sg[s2][:, 0:512], in_=PF(bG), func=AF.Sigmoid),
                      r=[r_pb[bG]], w=[r_sg[s2]])
                kb.op("act", L("activation", out=sg[s2][:, 512:640], in_=PF(bHh, 128, 256), func=AF.Sigmoid),
                      r=[r_pb[bHh]], w=[r_sg[s2]])
                yv = yT[:, cc, :].rearrange("p (b t) -> p b t", b=4)
                kb.op("dve", L("tensor_tensor", out=yv[:, :, 32:160],
                               in0=PF(bA).rearrange("p (b t) -> p b t", b=4),
                               in1=sg[s2][:, 0:512].rearrange("p (b t) -> p b t", b=4), op=ALU.mult),
                      r=[r_pb[bA], r_sg[s2]], w=[r_yT[cc]])
                kb.op("dve", L("tensor_tensor", out=yv[:, :, 0:32],
                               in0=PF(bHh, 0, 128).rearrange("p (b t) -> p b t", b=4),
                               in1=sg[s2][:, 512:640].rearrange("p (b t) -> p b t", b=4), op=ALU.mult),
                      r=[r_pb[bHh], r_sg[s2]], w=[r_yT[cc]])

            if cut <= 2:
                break
            for cc in range(8):
                yv = yT[:, cc, :].rearrange("p (b t) -> p b t", b=4)
                av = cacc[:, cc, :].rearrange("p (b t) -> p b t", b=4)
                kb.op("dve", L("tensor_scalar", out=av, in0=yv[:, :, 2:130], scalar1=cw[:, cc, 0:1],
                               scalar2=cw[:, cc, 31:32], op0=ALU.mult, op1=ALU.add),
                      r=[r_yT[cc], r_cw], w=[r_cacc[cc]])
                for k in range(1, 31):
                    kb.op("dve", L("scalar_tensor_tensor", out=av, in0=yv[:, :, 2 + k:130 + k],
                                   scalar=cw[:, cc, k:k + 1], in1=av, op0=ALU.mult, op1=ALU.add),
                          r=[r_yT[cc], r_cw, r_cacc[cc]], w=[r_cacc[cc]])
            bM, bS = 6, 7
            for cc in range(8):
                kb.op("pe", L("matmul", out=PF(bM), lhsT=onesf[:], rhs=cacc[:, cc, :], start=(cc == 0),
                              stop=(cc == 7)), r=[r_ones, r_cacc[cc]], w=[r_pb[bM]])
            sqv = yT[:].rearrange("p a b -> p (a b)")[:, 0:4096].rearrange("p (a b) -> p a b", a=8)
            kb.op("act", L("activation", out=sqv, in_=cacc[:], func=AF.Square), r=r_cacc, w=r_yT)
            for cc in range(8):
                kb.op("pe", L("matmul", out=PF(bS), lhsT=onesf[:], rhs=sqv[:, cc, :], start=(cc == 0),
                              stop=(cc == 7)), r=[r_ones] + r_yT, w=[r_pb[bS]])
            kb.op("act", L("copy", out=lnm[:, 0, :], in_=PF(bM)), r=[r_pb[bM]], w=[r_lnm])
            kb.op("dve", L("tensor_tensor", out=lnm[:, 1, :], in0=lnm[:, 0, :], in1=lnm[:, 0, :], op=ALU.mult),
                  r=[r_lnm], w=[r_lnm])
            kb.op("dve", L("tensor_tensor", out=lnm[:, 1, :], in0=PF(bS), in1=lnm[:, 1, :], op=ALU.subtract),
                  r=[r_pb[bS], r_lnm], w=[r_lnm])
            kb.op("act", L("activation", out=lnm[:, 2, :], in_=lnm[:, 1, :], func=AF.Sqrt, bias=epst[:]),
                  r=[r_lnm, r_eps], w=[r_lnm])
            kb.op("dve", L("reciprocal", out=lnm[:, 1, :], in_=lnm[:, 2, :]), r=[r_lnm], w=[r_lnm])
            for cc in range(8):
                kb.op("dve", L("tensor_tensor", out=cacc[:, cc, :], in0=cacc[:, cc, :], in1=lnm[:, 0, :],
                               op=ALU.subtract), r=[r_cacc[cc], r_lnm], w=[r_cacc[cc]])
                kb.op("dve", L("tensor_tensor", out=cacc[:, cc, :], in0=cacc[:, cc, :], in1=lnm[:, 1, :],
                               op=ALU.mult), r=[r_cacc[cc], r_lnm], w=[r_cacc[cc]])
                kb.op("act", L("activation", out=sT[:, cc, :], in_=cacc[:, cc, :], func=AF.Silu,
                               scale=cw[:, cc, 32:33], bias=cw[:, cc, 33:34]),
                      r=[r_cacc[cc], r_cw], w=[r_sT])
            for bl in range(4):
                m = 4 * gi + bl
                kb.dma("sp", s_sT[m].rearrange("p (a b) -> p a b", a=8), sT[:, :, bl * 128:(bl + 1) * 128],
                       r=[r_sT])

            if cut <= 3:
                break
            it = 0
            for qc in range(4):
                wi_ = load_wbuf(C_Q + qc * 512)
                for bl in range(4):
                    m = 4 * gi + bl
                    p2 = it % 2
                    it += 1
                    bQ = p2
                    bT = 2 + p2
                    for kc in range(16):
                        kb.op("pe", L("matmul", out=PF(bQ), lhsT=xT_g[:, kc, bl * 128:(bl + 1) * 128],
                                      rhs=wbuf[wi_][:, kc, :], start=(kc == 0), stop=(kc == 15)),
                              r=[r_xTg[bl], r_wbuf[wi_]], w=[r_pb[bQ]])
                    kb.op("act", L("activation", out=qsq[:], in_=PF(bQ), func=AF.Square),
                          r=[r_pb[bQ]], w=[r_qsq])
                    kb.op("dve", L("tensor_reduce", out=stq[p2][:, 0:4],
                                   in_=qsq[:].rearrange("p (h d) -> p h d", h=4), axis=AX.X, op=ALU.add),
                          r=[r_qsq], w=[r_stq[p2]])
                    kb.op("act", L("activation", out=stq[p2][:, 4:8], in_=stq[p2][:, 0:4], func=AF.Sqrt,
                                   scale=1.0 / 128, bias=epst[:]), r=[r_stq[p2], r_eps], w=[r_stq[p2]])
                    kb.op("dve", L("reciprocal", out=stq[p2][:, 8:12], in_=stq[p2][:, 4:8]),
                          r=[r_stq[p2]], w=[r_stq[p2]])
                    kb.op("dve", L("tensor_tensor", out=qn[p2][:],
                                   in0=PF(bQ).rearrange("p (h d) -> p h d", h=4),
                                   in1=stq[p2][:, 8:12].unsqueeze(2).broadcast_to([128, 4, 128]), op=ALU.mult),
                          r=[r_pb[bQ], r_stq[p2]], w=[r_qn[p2]])
                    kb.op("dve", L("tensor_tensor", out=qn[p2][:], in0=qn[p2][:],
                                   in1=gqs.unsqueeze(1).broadcast_to([128, 4, 128]), op=ALU.mult),
                          r=[r_qn[p2], r_cst], w=[r_qn[p2]])
                    qbv = qb[p2][:].rearrange("p (h d) -> p h d", h=4)
                    kb.op("act", L("copy", out=qbv[:, :, 32:128], in_=qn[p2][:, :, 32:128]),
                          r=[r_qn[p2]], w=[r_qb[p2]])
                    t4 = [t4q[p2][:, i, 0:4, :] for i in range(4)]
                    rope(qbv, qn[p2][:], 16, cosAo[:, m, :].unsqueeze(1).broadcast_to([128, 4, 16]),
                         sinAo[:, m, :].unsqueeze(1).broadcast_to([128, 4, 16]), t4,
                         r_qn[p2], r_qb[p2], r_ropeAo, r_t4q[p2])
                    for h in range(4):
                        kb.op("pe", L("transpose", out=PH(bT, h * 128, (h + 1) * 128),
                                      in_=qb[p2][:, h * 128:(h + 1) * 128], identity=identb[:]),
                              r=[r_qb[p2], r_id], w=[r_pb[bT]])
                    kb.op("act", L("copy", out=qTs[p2][:],
                                   in_=PH(bT, 0, 512).rearrange("p (a b) -> p a b", a=4)),
                          r=[r_pb[bT]], w=[r_qTs[p2]])
                    kb.dma("sp", s_qT[m].rearrange("p (a b) -> p a b", a=16)[:, 4 * qc:4 * qc + 4, :], qTs[p2][:],
                           r=[r_qTs[p2]])

            if cut <= 4:
                break
            for c2 in range(2):
                wi_ = load_wbuf(C_QI + c2 * 512)
                for bl in range(4):
                    m = 4 * gi + bl
                    p2 = it % 2
                    it += 1
                    bQ = p2
                    bT = 2 + p2
                    for kc in range(16):
                        kb.op("pe", L("matmul", out=PF(bQ), lhsT=xT_g[:, kc, bl * 128:(bl + 1) * 128],
                                      rhs=wbuf[wi_][:, kc, :], start=(kc == 0), stop=(kc == 15)),
                              r=[r_xTg[bl], r_wbuf[wi_]], w=[r_pb[bQ]])
                    kb.op("act", L("copy", out=qn[p2][:].rearrange("p a b -> p (a b)"), in_=PF(bQ)),
                          r=[r_pb[bQ]], w=[r_qn[p2]])
                    import os
                    SUB = int(os.environ.get("SUB", "9"))
                    if SUB <= 1:
                        continue
                    pv = qn[p2][:].rearrange("p a b -> p (a b)").rearrange("p (h d) -> p h d", h=8)
                    qbv = qb[p2][:].rearrange("p (h d) -> p h d", h=8)
                    kb.op("act", L("copy", out=qbv[:, :, 16:64], in_=pv[:, :, 16:64]),
                          r=[r_qn[p2]], w=[r_qb[p2]])
                    if SUB <= 2:
                        continue
                    t4 = [t4q[p2][:, i, :, 0:8] for i in range(4)]
                    rope(qbv, pv, 8, cosIo[:, m, :].unsqueeze(1).broadcast_to([128, 8, 8]),
                         sinIo[:, m, :].unsqueeze(1).broadcast_to([128, 8, 8]), t4,
                         r_qn[p2], r_qb[p2], r_ropeIo, r_t4q[p2])
                    if SUB <= 3:
                        continue
                    for h in range(4):
                        kb.op("pe", L("transpose", out=PH(bT, h * 128, (h + 1) * 128),
                                      in_=qb[p2][:, h * 128:(h + 1) * 128], identity=identb[:]),
                              r=[r_qb[p2], r_id], w=[r_pb[bT]])
                    kb.op("act", L("copy", out=qTs[p2][:],
                                   in_=PH(bT, 0, 512).rearrange("p (a b) -> p a b", a=4)),
                          r=[r_pb[bT]], w=[r_qTs[p2]])
                    if SUB <= 4:
                        continue
                    kb.dma("sp", s_qiT[m].rearrange("p (a b) -> p a b", a=8)[:, 4 * c2:4 * c2 + 4, :], qTs[p2][:],
                           r=[r_qTs[p2]])
            if cut <= 5:
                break
            for bl in range(4):
                m = 4 * gi + bl
                bQ = 4 + bl % 2
                for kc in range(16):
                    kb.op("pe", L("matmul", out=PF(bQ, 0, 16), lhsT=xT_g[:, kc, bl * 128:(bl + 1) * 128],
                                  rhs=wwi[:, kc, :], start=(kc == 0), stop=(kc == 15)),
                          r=[r_xTg[bl], r_wwi], w=[r_pb[bQ]])
                kb.op("act", L("copy", out=wis[:, bl, :], in_=PF(bQ, 0, 16)), r=[r_pb[bQ]], w=[r_wis])
                kb.dma("sp", s_wi[m], wis[:, bl, :], r=[r_wis])
        kb.barrier()

    if stage in ("B1", "B1s") or stage.startswith("B1c"):
        kb.finish()
        kb.emit()
        print("instructions:", kb.nins)
        return

    kvs = ExitStack()
    with kvs:
        ksb = mk_sb(kvs)
        KT = ksb("KT", [128, SEQ], BF16)
        Vt = ksb("Vt", [128, NT, 129], BF16)
        KIT = ksb("KIT", [128, SEQ], BF16)
        r_KT = [Reg(f"KT{n}") for n in range(NT)]
        r_V = [Reg(f"V{n}") for n in range(NT)]
        r_KIT = [Reg(f"KIT{n}") for n in range(NT)]
        r_vone = Reg("vone")
        kb.op("pool", L("memset", Vt[:, :, 128:129], 1.0), w=[r_vone])

        ntile_a = NT if stage not in ("smoke", "B1s", "B2s", "B3s", "Ms") else (2 if stage not in ("B2s", "B3s", "Ms") else 16)
        with ExitStack() as la:
            lsb = mk_sb(la)
            kb.dma("sp", gbuf[:], gA_d, w=[r_gbuf])
            posi = lsb("posi", [128, NT], I32)
            posf = lsb("posf", [128, NT])
            r_pos = Reg("pos")
            kb.dma("sp", posi[:], pos_t, w=[r_pos])
            kb.op("dve", L("tensor_copy", out=posf[:], in_=posi[:]), r=[r_pos], w=[r_pos])
            cosA, sinA = lsb("ra_cos", [128, NT, 16]), lsb("ra_sin", [128, NT, 16])
            cosI, sinI = lsb("ri_cos", [128, NT, 8]), lsb("ri_sin", [128, NT, 8])
            with ExitStack() as tmps:
                _, _, r_ropeA = rope_tables(cosA, sinA, "ra", posf[:], NT, invA, 16, r_pos, tmps)
                _, _, r_ropeI = rope_tables(cosI, sinI, "ri", posf[:], NT, invI, 8, r_pos, tmps)
                kb.barrier()
            wkv = lsb("wkv", [128, 16, 320], BF16)
            r_wkv = Reg("wkv")
            for (c0, c1, o0) in ((C_K, C_K + 256, 0), (C_KI, C_KI + 64, 256)):
                kb.dma("pool", wkv[:, :, o0:o0 + (c1 - c0)], w_in_v[:, :, c0:c1], w=[r_wkv])
            xin = [lsb(f"xin{i}", [128, D]) for i in range(2)]
            r_xin = [Reg(f"xin{i}") for i in range(2)]
            junk = lsb("junk", [128, D], BF16)
            r_junk = Reg("junk")
            xs = [lsb(f"xs{i}", [128, D], BF16) for i in range(2)]
            r_xs = [Reg(f"xs{i}") for i in range(2)]
            xT = [lsb(f"xT{i}", [128, 16, 128], BF16) for i in range(2)]
            r_xT = [Reg(f"xT{i}") for i in range(2)]
            st = [lsb(f"st{i}", [128, 8]) for i in range(2)]
            r_st = [Reg(f"st{i}") for i in range(2)]
            kn = [lsb(f"kn{i}", [128, 192]) for i in range(2)]
            kfin = [lsb(f"kfin{i}", [128, 256], BF16) for i in range(2)]
            tmp = [lsb(f"tmp{i}", [128, 4, 16]) for i in range(2)]
            r_kn = [Reg(f"kn{i}") for i in range(2)]
            r_kf = [Reg(f"kf{i}") for i in range(2)]
            r_tmp = [Reg(f"tmp{i}") for i in range(2)]

            for n in range(ntile_a):
                b2 = n % 2
                bT = 2 * b2
                bZ = 4 + b2
                bK = 6 + b2
                kb.dma("sp", xin[b2][:], xb[n * 128:(n + 1) * 128, :], w=[r_xin[b2]])
                kb.op("act", L("activation", out=junk[:], in_=xin[b2][:], func=AF.Square, accum_out=st[b2][:, 0:1]),
                      r=[r_xin[b2]], w=[r_junk, r_st[b2]])
                rstd_from_ss(st[b2], 0, D, r_st[b2])
                kb.op("dve", L("scalar_tensor_tensor", out=xs[b2][:], in0=xin[b2][:], scalar=st[b2][:, 2:3],
                               in1=gbuf[:], op0=ALU.mult, op1=ALU.mult),
                      r=[r_xin[b2], r_st[b2], r_gbuf], w=[r_xs[b2]])
                for kc in range(16):
                    kb.op("pe", L("transpose", out=PH(bT, kc * 128, (kc + 1) * 128),
                                  in_=xs[b2][:, kc * 128:(kc + 1) * 128], identity=identb[:]),
                          r=[r_xs[b2], r_id], w=[r_pb[bT + kc // 8]])
                kb.op("act", L("copy", out=xT[b2][:].rearrange("p a b -> p (a b)"), in_=PH(bT, 0, 2048)),
                      r=[r_pb[bT], r_pb[bT + 1]], w=[r_xT[b2]])
                for kc in range(16):
                    kb.op("pe", L("matmul", out=PF(bZ, 0, 320), lhsT=xT[b2][:, kc, :], rhs=wkv[:, kc, :],
                                  start=(kc == 0), stop=(kc == 15)),
                          r=[r_xT[b2], r_wkv], w=[r_pb[bZ]])
                kb.op("act", L("copy", out=Vt[:, n, 0:128], in_=PF(bZ, 128, 256)), r=[r_pb[bZ]], w=[r_V[n]])
                kb.op("act", L("activation", out=kn[b2][:, 0:128], in_=PF(bZ, 0, 128), func=AF.Square,
                               accum_out=st[b2][:, 3:4]), r=[r_pb[bZ]], w=[r_kn[b2], r_st[b2]])
                rstd_from_ss(st[b2], 3, 128, r_st[b2])
                kb.op("dve", L("scalar_tensor_tensor", out=kn[b2][:, 0:128], in0=PF(bZ, 0, 128),
                               scalar=st[b2][:, 5:6], in1=gk, op0=ALU.mult, op1=ALU.mult),
                      r=[r_pb[bZ], r_st[b2], r_cst], w=[r_kn[b2]])
                kb.op("dve", L("tensor_copy", out=kn[b2][:, 128:192], in_=PF(bZ, 256, 320)),
                      r=[r_pb[bZ]], w=[r_kn[b2]])
                kb.op("dve", L("tensor_copy", out=kfin[b2][:, 32:128], in_=kn[b2][:, 32:128]),
                      r=[r_kn[b2]], w=[r_kf[b2]])
                kb.op("dve", L("tensor_copy", out=kfin[b2][:, 144:192], in_=kn[b2][:, 144:192]),
                      r=[r_kn[b2]], w=[r_kf[b2]])
                t4a = [tmp[b2][:, i, 0:16] for i in range(4)]
                t4i = [tmp[b2][:, i, 0:8] for i in range(4)]
                rope(kfin[b2][:, 0:128], kn[b2][:, 0:128], 16, cosA[:, n, :], sinA[:, n, :], t4a,
                     r_kn[b2], r_kf[b2], r_ropeA, r_tmp[b2])
                rope(kfin[b2][:, 128:192], kn[b2][:, 128:192], 8, cosI[:, n, :], sinI[:, n, :], t4i,
                     r_kn[b2], r_kf[b2], r_ropeI, r_tmp[b2])
                kb.op("dve", L("tensor_copy", out=kfin[b2][:, 192:256], in_=kfin[b2][:, 128:192]),
                      r=[r_kf[b2]], w=[r_kf[b2]])
                kb.op("pe", L("transpose", out=PH(bK, 0, 128), in_=kfin[b2][:, 0:128], identity=identb[:]),
                      r=[r_kf[b2], r_id], w=[r_pb[bK]])
                kb.op("pe", L("transpose", out=PH(bK, 128, 256), in_=kfin[b2][:, 128:256], identity=identb[:]),
                      r=[r_kf[b2], r_id], w=[r_pb[bK]])
                kb.op("act", L("copy", out=KT[:, n * 128:(n + 1) * 128], in_=PH(bK, 0, 128)),
                      r=[r_pb[bK]], w=[r_KT[n]])
                kb.op("act", L("copy", out=KIT[:, n * 128:(n + 1) * 128], in_=PH(bK, 128, 256)),
                      r=[r_pb[bK]], w=[r_KIT[n]])
            kb.barrier()

        if stage in ("smoke", "A"):
            o_kt = dram("o_kt", [128, SEQ], BF16, kind="ExternalOutput")
            o_v = dram("o_v", [128, NT * 129], BF16, kind="ExternalOutput")
            o_kit = dram("o_kit", [128, SEQ], BF16, kind="ExternalOutput")
            regs = r_KT[:ntile_a] + r_V[:ntile_a] + r_KIT[:ntile_a] + [r_vone]
            ro = Reg("out")
            T_ = ntile_a * 128
            kb.dma("sp", o_kt[:, 0:T_], KT[:, 0:T_], r=regs, w=[ro])
            kb.dma("sp", o_v[:, 0:ntile_a * 129], Vt[:, 0:ntile_a, :].rearrange("p a b -> p (a b)"), r=regs, w=[ro])
            kb.dma("sp", o_kit[:, 0:T_], KIT[:, 0:T_], r=regs, w=[ro])
            kb.finish()
            kb.emit()
            print("instructions:", kb.nins)
            return


        blocks_b2 = list(range(NB))
        if stage == "B2s":
            blocks_b2 = [0, 1]
        if stage in ("B3s", "Ms"):
            blocks_b2 = [0, 1, 2, 3]
        with ExitStack() as l2:
            lsb = mk_sb(l2)
            cmask = lsb("cmask_sb", [128, 512])
            r_cmask = Reg("cmask")
            kb.dma("sp", cmask[:], cmask_d, w=[r_cmask])
            sc = lsb("sc", [128, SEQ])
            r_sc = [Reg(f"sc{i}") for i in range(16)]
            selb = lsb("selb", [128, SEQ], BF16)
            r_sel = Reg("sel")
            selT = lsb("selT", [128, NT, 128], BF16)
            r_selT = [Reg(f"selT{i}") for i in range(4)]
            qTb = [lsb(f"qTb{i}", [128, 16, 128], BF16) for i in range(2)]
            r_qTb = [Reg(f"qTb{i}") for i in range(2)]
            qiTb = [lsb(f"qiTb{i}", [128, 8, 128], BF16) for i in range(2)]
            r_qiTb = [Reg(f"qiTb{i}") for i in range(2)]
            wib = [lsb(f"wib{i}", [128, 16]) for i in range(2)]
            r_wib = [Reg(f"wib{i}") for i in range(2)]
            rl = [lsb(f"rl{i}", [128, 512]) for i in range(2)]
            r_rl = [Reg(f"rl{i}") for i in range(2)]
            Eb = [lsb(f"Eb{i}", [128, 512], BF16) for i in range(2)]
            r_Eb = [Reg(f"Eb{i}") for i in range(2)]
            Pb = [lsb(f"Pb{i}", [128, 4, 128], BF16) for i in range(2)]
            r_Pb = [Reg(f"Pb{i}") for i in range(2)]
            bis = lsb("bis", [128, 8])
            r_bis = Reg("bis")
            bisA = lsb("bisA", [128, 2])
            r_mid, r_cnt, r_cnta = Reg("mid"), Reg("cnt"), Reg("cnta")
            r_selD, r_selA = Reg("selD"), Reg("selA")
            rden = lsb("rden", [128, 16])
            r_rden = Reg("rden")
            Ab = lsb("Ab", [128, 2048], BF16)
            r_Ab = Reg("Ab")
            ATb = lsb("ATb", [128, 2048], BF16)
            r_ATb = Reg("ATb")
            import os
            NJUNK = int(os.environ.get('NJUNK', '1'))
            NIDXB = 3 if NJUNK == 0 else 2
            cnt_ib = [0]
            cnt_ia = [0]

            def load_q(bi, m):
                q2 = bi % 2
                kb.dma("sp", qTb[q2][:].rearrange("p a b -> p (a b)"), s_qT[m], w=[r_qTb[q2]])
                kb.dma("sp", qiTb[q2][:].rearrange("p a b -> p (a b)"), s_qiT[m], w=[r_qiTb[q2]])
                kb.dma("sp", wib[q2][:], s_wi[m], w=[r_wib[q2]])

            def gen_indexer(bi, m):
                q2 = bi % 2
                NCH = m + 1
                steps = [(ch, h) for ch in range(NCH) for h in range(16)]
                ib0 = cnt_ib[0]
                cnt_ib[0] += len(steps)

                def pe_part(k):
                    ch, h = steps[k]
                    bank = 5 + (ib0 + k) % NIDXB
                    hf = h % 2
                    kb.op("pe", L("matmul", out=PF(bank), lhsT=qiTb[q2][hf * 64:(hf + 1) * 64, h // 2, :],
                                  rhs=KIT[hf * 64:(hf + 1) * 64, ch * 512:(ch + 1) * 512], start=True, stop=True),
                          r=[r_qiTb[q2]] + r_KIT[4 * ch:4 * ch + 4], w=[r_pb[bank]])

                def rest_part(k):
                    ch, h = steps[k]
                    bank = 5 + (ib0 + k) % NIDXB
                    i2 = (ib0 + k) % 2
                    scc = sc[:, ch * 512:(ch + 1) * 512]
                    kb.op("act", L("activation", out=rl[i2][:], in_=PF(bank), func=AF.Relu),
                          r=[r_pb[bank]], w=[r_rl[i2]])
                    if h == 0:
                        kb.op("dve", L("tensor_scalar", out=scc, in0=rl[i2][:], scalar1=wib[q2][:, 0:1],
                                       scalar2=None, op0=ALU.mult), r=[r_rl[i2], r_wib[q2]], w=[r_sc[ch]])
                    else:
                        kb.op("dve", L("scalar_tensor_tensor", out=scc, in0=rl[i2][:],
                                       scalar=wib[q2][:, h:h + 1], in1=scc, op0=ALU.mult, op1=ALU.add),
                              r=[r_rl[i2], r_wib[q2], r_sc[ch]], w=[r_sc[ch]])

                pe_part(0)
                for k in range(len(steps)):
                    if k + 1 < len(steps):
                        pe_part(k + 1)
                    rest_part(k)
                    yield

            def topk_and_mask(bi, m):
                NCH = m + 1
                S = 512 * NCH
                NKT = 4 * NCH
                rs = r_sc[0:NCH]
                kb.op("dve", L("tensor_reduce", out=bis[:, 0:1], in_=sc[:, 0:S], axis=AX.X, op=ALU.min),
                      r=rs, w=[r_bis])
                lc = sc[:, S - 512:S]
                kb.op("dve", L("tensor_tensor", out=lc, in0=lc, in1=cmask[:], op=ALU.add),
                      r=[r_sc[NCH - 1], r_cmask], w=[r_sc[NCH - 1]])
                kb.op("dve", L("tensor_reduce", out=bis[:, 1:2], in_=sc[:, 0:S], axis=AX.X, op=ALU.max),
                      r=rs, w=[r_bis])
                kb.op("dve", L("tensor_tensor", out=bis[:, 2:3], in0=bis[:, 1:2], in1=bis[:, 0:1], op=ALU.subtract),
                      r=[r_bis], w=[r_bis])
                kb.op("dve", L("tensor_scalar", out=bis[:, 2:3], in0=bis[:, 2:3], scalar1=1.0001, scalar2=1e-6,
                               op0=ALU.mult, op1=ALU.add), r=[r_bis], w=[r_bis])
                kb.op("dve", L("tensor_copy", out=bis[:, 3:4], in_=bis[:, 0:1]), r=[r_bis], w=[r_bis])
                nd = max(1, int(round(0.45 * NCH)))
                Sd = 512 * nd
                n_act = S - Sd
                rsd = r_sc[0:nd]
                rsa = r_sc[nd:NCH]
                for itb in range(1, NBIS + 1):
                    f = 2.0 ** (-itb)
                    kb.op("dve", L("scalar_tensor_tensor", out=bis[:, 4:5], in0=bis[:, 2:3], scalar=f,
                                   in1=bis[:, 3:4], op0=ALU.mult, op1=ALU.add), r=[r_bis], w=[r_mid])
                    if n_act > 0:
                        kb.op("act", L("activation", out=selb[:, Sd:S], in_=sc[:, Sd:S], func=AF.Sign, scale=-1.0,
                                       bias=bis[:, 4:5], accum_out=bisA[:, 0:1]),
                              r=rsa + [r_mid], w=[r_selA, r_cnta])
                    kb.op("dve", L("tensor_scalar", out=selb[:, 0:Sd], in0=sc[:, 0:Sd], scalar1=bis[:, 4:5],
                                   scalar2=0.0, op0=ALU.is_ge, op1=ALU.add, accum_out=bis[:, 5:6]),
                          r=rsd + [r_mid], w=[r_selD, r_cnt])
                    if n_act > 0:
                        kb.op("dve", L("scalar_tensor_tensor", out=bis[:, 5:6], in0=bisA[:, 0:1], scalar=-0.5,
                                       in1=bis[:, 5:6], op0=ALU.mult, op1=ALU.add), r=[r_cnta, r_cnt], w=[r_cnt])
                    kb.op("dve", L("tensor_scalar", out=bis[:, 6:7], in0=bis[:, 5:6],
                                   scalar1=NSEL - 0.5 - 0.5 * n_act, scalar2=f, op0=ALU.is_ge, op1=ALU.mult),
                          r=[r_cnt], w=[r_bis])
                    kb.op("dve", L("scalar_tensor_tensor", out=bis[:, 3:4], in0=bis[:, 6:7], scalar=bis[:, 2:3],
                                   in1=bis[:, 3:4], op0=ALU.mult, op1=ALU.add), r=[r_bis], w=[r_bis])
                kb.op("dve", L("tensor_scalar", out=selb[:, 0:S], in0=sc[:, 0:S], scalar1=bis[:, 3:4],
                               scalar2=None, op0=ALU.is_ge), r=rs + [r_bis], w=[r_selD, r_selA])
                for g in range((NKT + 15) // 16):
                    n_in = min(16, NKT - 16 * g)
                    bT = 2 * (g % 2)
                    for k2 in range(n_in):
                        kt = 16 * g + k2
                        kb.op("pe", L("transpose", out=PH(bT, k2 * 128, (k2 + 1) * 128),
                                      in_=selb[:, kt * 128:(kt + 1) * 128], identity=identb[:]),
                              r=[r_selD, r_selA, r_id], w=[r_pb[bT + k2 // 8]])
                    kb.op("act", L("copy", out=selT[:, 16 * g:16 * g + n_in, :],
                                   in_=PH(bT, 0, n_in * 128).rearrange("p (a b) -> p a b", a=n_in)),
                          r=[r_pb[bT], r_pb[bT + 1]], w=[r_selT[g]])

            def gen_attention(bi, m):
                q2 = bi % 2
                NCH = m + 1
                NKT = 4 * NCH
                its = [(ps_, kt, hgl) for ps_ in range(2) for kt in range(NKT) for hgl in range(2)]
                ia0 = cnt_ia[0]
                cnt_ia[0] += len(its)

                def issue_qk(idx):
                    ps_, kt, hgl = its[idx]
                    hg = 2 * ps_ + hgl
                    lb = (ia0 + idx) % 2
                    kb.op("pe", L("matmul", out=PF(lb), lhsT=KT[:, kt * 128:(kt + 1) * 128],
                                  rhs=qTb[q2][:, 4 * hg:4 * hg + 4, :], start=True, stop=True),
                          r=[r_KT[kt], r_qTb[q2]], w=[r_pb[lb]])

                def issue_mid(idx):
                    ps_, kt, hgl = its[idx]
                    lb = (ia0 + idx) % 2
                    for _ in range(NJUNK):
                        kb.op("pe", L("matmul", out=PF(7), lhsT=identb[:], rhs=KT[:, 0:512], start=True, stop=True))
                    kb.op("act", L("activation", out=Eb[lb][:], in_=PF(lb), func=AF.Exp, scale=ATTN_SCALE),
                          r=[r_pb[lb]], w=[r_Eb[lb]])
                    kb.op("dve", L("tensor_tensor", out=Pb[lb][:],
                                   in0=Eb[lb][:].rearrange("p (h t) -> p h t", h=4),
                                   in1=selT[:, kt, :].unsqueeze(1).broadcast_to([128, 4, 128]), op=ALU.mult),
                          r=[r_Eb[lb], r_selT[kt // 16]], w=[r_Pb[lb]])

                def issue_pv(idx):
                    ps_, kt, hgl = its[idx]
                    lb = (ia0 + idx) % 2
                    for h4 in range(4):
                        h8 = 4 * hgl + h4
                        pbk = 2 + h8 // 3
                        o0 = (h8 % 3) * 129
                        kb.op("pe", L("matmul", out=PF(pbk, o0, o0 + 129), lhsT=Pb[lb][:, h4, :],
                                      rhs=Vt[:, kt, :], start=(kt == 0 and h8 % 3 == 0),
                                      stop=(kt == NKT - 1 and (h8 % 3 == 2 or h8 == 7))),
                              r=[r_Pb[lb], r_V[kt], r_vone], w=[r_pb[pbk]])

                def normalise(ps_):
                    for pbk in range(2, 5):
                        h0 = 3 * (pbk - 2)
                        nh = min(3, 8 - h0)
                        hh = 8 * ps_ + h0
                        pv3 = PF(pbk, 0, nh * 129).rearrange("p (h c) -> p h c", c=129)
                        kb.op("dve", L("reciprocal", out=rden[:, hh:hh + nh].unsqueeze(2), in_=pv3[:, :, 128:129]),
                              r=[r_pb[pbk]], w=[r_rden])
                        kb.op("dve", L("tensor_tensor",
                                       out=Ab[:, hh * 128:(hh + nh) * 128].rearrange("p (h d) -> p h d", h=nh),
                                       in0=pv3[:, :, 0:128],
                                       in1=rden[:, hh:hh + nh].unsqueeze(2).broadcast_to([128, nh, 128]), op=ALU.mult),
                              r=[r_pb[pbk], r_rden], w=[r_Ab])

                issue_qk(0)
                for idx in range(len(its)):
                    if idx + 1 < len(its) and its[idx + 1][0] == its[idx][0]:
                        issue_qk(idx + 1)
                    issue_mid(idx)
                    yield "mid"
                    issue_pv(idx)
                    if idx + 1 < len(its) and its[idx + 1][0] != its[idx][0]:
                        normalise(0)
                        issue_qk(idx + 1)
                    yield "end"
                normalise(1)
                for h in range(16):
                    kb.op("pe", L("transpose", out=PH(0, h * 128, (h + 1) * 128), in_=Ab[:, h * 128:(h + 1) * 128],
                                  identity=identb[:]), r=[r_Ab, r_id], w=[r_pb[h // 8]])
                kb.op("act", L("copy", out=ATb[:], in_=PH(0, 0, 2048)), r=[r_pb[0], r_pb[1]], w=[r_ATb])
                kb.dma("sp", s_AT[m], ATb[:], r=[r_ATb])

            def run_interleaved(gens):
                att, idxg = gens[0], (gens[1] if len(gens) > 1 else None)
                while att is not None:
                    try:
                        tag = next(att)
                    except StopIteration:
                        att = None
                        break
                    if tag == "mid" and idxg is not None:
                        try:
                            next(idxg)
                        except StopIteration:
                            idxg = None
                if idxg is not None:
                    for _ in idxg:
                        pass

            nb2 = len(blocks_b2)
            load_q(0, blocks_b2[0])
            run_interleaved([None, gen_indexer(0, blocks_b2[0])])
            topk_and_mask(0, blocks_b2[0])
            for bi, m in enumerate(blocks_b2):
                nxt_g = None
                if bi + 1 < nb2:
                    load_q(bi + 1, blocks_b2[bi + 1])
                    nxt_g = gen_indexer(bi + 1, blocks_b2[bi + 1])
                run_interleaved([gen_attention(bi, m), nxt_g])
                if bi + 1 < nb2:
                    topk_and_mask(bi + 1, blocks_b2[bi + 1])
            kb.barrier()

    if stage in ("B2", "B2s"):
        kb.finish()
        kb.emit()
        print("instructions:", kb.nins)
        return

    comb_all = sb("comb_all", [128, NB, NE])
    r_comb = Reg("comb")
    kb.op("pool", L("memset", comb_all[:], 0.0), w=[r_comb])
    ng3 = 4 if stage not in ("B3s", "Ms") else 1
    with ExitStack() as l3:
        lsb = mk_sb(l3)
        kb.dma("sp", gbuf[:], g2_d, w=[r_gbuf])
        wrf = lsb("wrf", [128, 16, 36])
        r_wrf = Reg("wrf")
        kb.dma("sp", wrf[:], wr_d.rearrange("(kc p) n -> p kc n", p=128), w=[r_wrf])
        xTm = [lsb(f"xTm{i}", [128, 16, 128], BF16) for i in range(4)]
        r_xTm = [Reg(f"xTm{i}") for i in range(4)]
        sTm = [lsb(f"sTm{i}", [128, 8, 128], BF16) for i in range(4)]
        r_sTm = [Reg(f"sTm{i}") for i in range(4)]
        ATm = [lsb(f"ATm{i}", [128, 16, 128], BF16) for i in range(4)]
        r_ATm = [Reg(f"ATm{i}") for i in range(4)]
        xh = [lsb(f"xh{i}", [128, D]) for i in range(4)]
        r_xh = [Reg(f"xh{i}") for i in range(4)]
        wb3 = [lsb(f"wb3{i}", [128, 16, 512], BF16) for i in range(2)]
        r_wb3 = [Reg(f"wb3{i}") for i in range(2)]
        sgc = [lsb(f"sgc{i}", [128, 512], BF16) for i in range(4)]
        r_sgc = [Reg(f"sgc{i}") for i in range(4)]
        sga = [lsb(f"sga{i}", [128, 512], BF16) for i in range(4)]
        r_sga = [Reg(f"sga{i}") for i in range(4)]
        t1 = [lsb(f"t1{i}", [128, 512]) for i in range(4)]
        r_t1 = [Reg(f"t1{i}") for i in range(4)]
        t2 = [lsb(f"t2{i}", [128, 512]) for i in range(2)]
        r_t2 = [Reg(f"t2{i}") for i in range(2)]
        mix = [lsb(f"mix{i}", [128, D], BF16) for i in range(4)]
        r_mix = [Reg(f"mix{i}") for i in range(4)]
        mixT = [lsb(f"mixT{i}", [128, 16, 128], BF16) for i in range(4)]
        r_mixT = [Reg(f"mixT{i}") for i in range(4)]
        hn2f = lsb("hn2f", [128, D])
        r_hn2f = Reg("hn2f")
        hTf = lsb("hTf", [128, 16, 128])
        r_hTf = Reg("hTf")
        hTb = lsb("hTb", [128, 16, 128], BF16)
        r_hTb = Reg("hTb")
        st3 = lsb("st3", [128, 8])
        r_st3 = Reg("st3")
        lg = lsb("lg", [128, 36])
        rt = lsb("rt", [128, 96])
        r_rt = Reg("rt")
        w3n = [0]
        pq = [0]

        def load_w3(src_ap, nk):
            i = w3n[0] % 2
            w3n[0] += 1
            kb.dma("pool", wb3[i][:, 0:nk, :], src_ap, w=[r_wb3[i]])
            return i

        w_pw_v = w_pw.rearrange("(kc p) n -> p kc n", p=128)
        w_ao_v = w_ao.rearrange("(kc p) n -> p kc n", p=128)
        w_o_v = w_o.rearrange("(kc p) n -> p kc n", p=128)

        def proj(bl, lhs_fn, nk, wi_, regs):
            bank = pq[0] % 4
            pq[0] += 1
            for kc in range(nk):
                kb.op("pe", L("matmul", out=PF(bank), lhsT=lhs_fn(kc), rhs=wb3[wi_][:, kc, :],
                              start=(kc == 0), stop=(kc == nk - 1)), r=regs + [r_wb3[wi_]], w=[r_pb[bank]])
            return bank

        import os
        CUT3 = int(os.environ.get("CUT3", "99"))
        for gi in range(ng3):
            for bl in range(4):
                m = 4 * gi + bl
                kb.dma("sp", xTm[bl][:].rearrange("p a b -> p (a b)"), s_xT[m], w=[r_xTm[bl]])
                kb.dma("sp", sTm[bl][:].rearrange("p a b -> p (a b)"), s_sT[m], w=[r_sTm[bl]])
                kb.dma("sp", ATm[bl][:].rearrange("p a b -> p (a b)"), s_AT[m], w=[r_ATm[bl]])
                kb.dma("sp", xh[bl][:], xo[m, 32:160, :], w=[r_xh[bl]])
            for c in range(4):
                cs = slice(c * 512, (c + 1) * 512)
                wi_ = load_w3(w_in_v[:, :, C_GC + c * 512:C_GC + (c + 1) * 512], 16)
                for bl in range(4):
                    bank = proj(bl, lambda kc: xTm[bl][:, kc, :], 16, wi_, [r_xTm[bl]])
                    kb.op("act", L("activation", out=sgc[bl][:], in_=PF(bank), func=AF.Sigmoid),
                          r=[r_pb[bank]], w=[r_sgc[bl]])
                wi_ = load_w3(w_pw_v[:, :, cs], 8)
                for bl in range(4):
                    bank = proj(bl, lambda kc: sTm[bl][:, kc, :], 8, wi_, [r_sTm[bl]])
                    kb.op("dve", L("tensor_tensor", out=t1[bl][:], in0=PF(bank), in1=sgc[bl][:], op=ALU.mult),
                          r=[r_pb[bank], r_sgc[bl]], w=[r_t1[bl]])
                wi_ = load_w3(w_in_v[:, :, C_GA + c * 512:C_GA + (c + 1) * 512], 16)
                for bl in range(4):
                    bank = proj(bl, lambda kc: xTm[bl][:, kc, :], 16, wi_, [r_xTm[bl]])
                    kb.op("act", L("activation", out=sga[bl][:], in_=PF(bank), func=AF.Sigmoid),
                          r=[r_pb[bank]], w=[r_sga[bl]])
                wi_ = load_w3(w_ao_v[:, :, cs], 16)
                for bl in range(4):
                    bank = proj(bl, lambda kc: ATm[bl][:, kc, :], 16, wi_, [r_ATm[bl]])
                    kb.op("dve", L("tensor_tensor", out=t2[bl % 2][:], in0=PF(bank), in1=sga[bl][:], op=ALU.mult),
                          r=[r_pb[bank], r_sga[bl]], w=[r_t2[bl % 2]])
                    kb.op("dve", L("tensor_tensor", out=mix[bl][:, cs], in0=t1[bl][:], in1=t2[bl % 2][:], op=ALU.add),
                          r=[r_t1[bl], r_t2[bl % 2]], w=[r_mix[bl]])
            if CUT3 <= 1:
                break
            for bl in range(4):
                bT = 4 + 2 * (bl % 2)
                for kc in range(16):
                    kb.op("pe", L("transpose", out=PH(bT, kc * 128, (kc + 1) * 128),
                                  in_=mix[bl][:, kc * 128:(kc + 1) * 128], identity=identb[:]),
                          r=[r_mix[bl], r_id], w=[r_pb[bT + kc // 8]])
                kb.op("act", L("copy", out=mixT[bl][:].rearrange("p a b -> p (a b)"), in_=PH(bT, 0, 2048)),
                      r=[r_pb[bT], r_pb[bT + 1]], w=[r_mixT[bl]])
            for c in range(4):
                cs = slice(c * 512, (c + 1) * 512)
                wi_ = load_w3(w_o_v[:, :, cs], 16)
                for bl in range(4):
                    bank = proj(bl, lambda kc: mixT[bl][:, kc, :], 16, wi_, [r_mixT[bl]])
                    kb.op("dve", L("tensor_tensor", out=xh[bl][:, cs], in0=PF(bank), in1=xh[bl][:, cs], op=ALU.add),
                          r=[r_pb[bank], r_xh[bl]], w=[r_xh[bl]])
            if CUT3 <= 2:
                break
            for bl in range(4):
                m = 4 * gi + bl
                kb.dma("sp", out_d[m], xh[bl][:], r=[r_xh[bl]])
                kb.op("act", L("activation", out=hn2f[:], in_=xh[bl][:], func=AF.Square, accum_out=st3[:, 0:1]),
                      r=[r_xh[bl]], w=[r_hn2f, r_st3])
                rstd_from_ss(st3, 0, D, r_st3)
                kb.op("dve", L("scalar_tensor_tensor", out=hn2f[:], in0=xh[bl][:], scalar=st3[:, 2:3], in1=gbuf[:],
                               op0=ALU.mult, op1=ALU.mult), r=[r_xh[bl], r_st3, r_gbuf], w=[r_hn2f])
                if CUT3 <= 3:
                    continue
                for kc in range(16):
                    kb.op("pe", L("matmul", out=PF(4 + kc // 4, (kc % 4) * 128, (kc % 4 + 1) * 128),
                                  lhsT=hn2f[:, kc * 128:(kc + 1) * 128], rhs=identf[:], start=True, stop=True),
                          r=[r_hn2f, r_id], w=[r_pb[4 + kc // 4]])
                SUB3 = int(os.environ.get("SUB3", "9"))
                if SUB3 <= 1:
                    continue
                for q4 in range(4):
                    kb.op("act", L("copy", out=hTb[:, 4 * q4:4 * q4 + 4, :].rearrange("p a b -> p (a b)"),
                                   in_=PF(4 + q4)), r=[r_pb[4 + q4]], w=[r_hTb])
                    if SUB3 <= 2:
                        continue
                    kb.op("dve", L("tensor_copy", out=hTf[:, 4 * q4:4 * q4 + 4, :].rearrange("p a b -> p (a b)"),
                                   in_=PF(4 + q4)), r=[r_pb[4 + q4]], w=[r_hTf])
                if SUB3 <= 3:
                    continue
                kb.dma("sp", s_hT[m], hTb[:].rearrange("p a b -> p (a b)"), r=[r_hTb])
                if CUT3 <= 4:
                    continue
                bank = pq[0] % 4
                pq[0] += 1
                for kc in range(16):
                    kb.op("pe", L("matmul", out=PF(bank, 0, 36), lhsT=hTf[:, kc, :], rhs=wrf[:, kc, :],
                                  start=(kc == 0), stop=(kc == 15)), r=[r_hTf, r_wrf], w=[r_pb[bank]])
                if CUT3 <= 5:
                    continue
                R = [r_rt]
                kb.op("dve", L("tensor_tensor", out=lg[:], in0=PF(bank, 0, 36), in1=brt, op=ALU.add),
                      r=[r_pb[bank], r_cst], w=R)
                gl = lg[:, 0:4]
                el = lg[:, 4:36].rearrange("p (g e) -> p g e", g=4)
                gmax, ngmax, sumg, pg = rt[:, 0:1], rt[:, 1:2], rt[:, 2:3], rt[:, 3:4]
                ohg, eg = rt[:, 4:8], rt[:, 8:12]
                tmp48 = rt[:, 12:44].rearrange("p (g e) -> p g e", g=4)
                e_in, mx8, oh1, oh2 = rt[:, 44:52], rt[:, 52:60], rt[:, 60:68], rt[:, 68:76]
                dd, ed, w1, w2 = rt[:, 76:77], rt[:, 77:78], rt[:, 78:79], rt[:, 79:80]
                wi1, wpg = rt[:, 80:88], rt[:, 88:96]
                kb.op("dve", L("tensor_reduce", out=gmax, in_=gl, axis=AX.X, op=ALU.max), r=R, w=R)
                kb.op("dve", L("tensor_scalar", out=ohg, in0=gl, scalar1=gmax, scalar2=None, op0=ALU.is_equal),
                      r=R, w=R)
                kb.op("dve", L("tensor_scalar", out=ngmax, in0=gmax, scalar1=-1.0, scalar2=None, op0=ALU.mult),
                      r=R, w=R)
                kb.op("act", L("activation", out=eg, in_=gl, func=AF.Exp, bias=ngmax, accum_out=sumg), r=R, w=R)
                kb.op("dve", L("reciprocal", out=pg, in_=sumg), r=R, w=R)
                kb.op("dve", L("tensor_tensor", out=tmp48, in0=el, in1=ohg.unsqueeze(2).broadcast_to([128, 4, 8]),
                               op=ALU.mult), r=R, w=R)
                kb.op("dve", L("tensor_reduce", out=e_in, in_=tmp48.rearrange("p g e -> p e g"), axis=AX.X,
                               op=ALU.add), r=R, w=R)
                if CUT3 <= 6:
                    continue
                kb.op("dve", L("max", out=mx8, in_=e_in), r=R, w=R)
                if CUT3 <= 7:
                    continue
                kb.op("dve", L("tensor_scalar", out=oh1, in0=e_in, scalar1=mx8[:, 0:1], scalar2=None,
                               op0=ALU.is_equal), r=R, w=R)
                kb.op("dve", L("tensor_scalar", out=oh2, in0=e_in, scalar1=mx8[:, 1:2], scalar2=None,
                               op0=ALU.is_equal), r=R, w=R)
                kb.op("dve", L("tensor_tensor", out=dd, in0=mx8[:, 1:2], in1=mx8[:, 0:1], op=ALU.subtract), r=R, w=R)
                kb.op("act", L("activation", out=ed, in_=dd, func=AF.Exp), r=R, w=R)
                kb.op("dve", L("tensor_scalar", out=w1, in0=ed, scalar1=1.0, scalar2=None, op0=ALU.add), r=R, w=R)
                kb.op("dve", L("reciprocal", out=w1, in_=w1), r=R, w=R)
                kb.op("dve", L("tensor_tensor", out=w2, in0=ed, in1=w1, op=ALU.mult), r=R, w=R)
                kb.op("dve", L("tensor_scalar", out=wi1, in0=oh1, scalar1=w1, scalar2=None, op0=ALU.mult), r=R, w=R)
                kb.op("dve", L("scalar_tensor_tensor", out=wi1, in0=oh2, scalar=w2, in1=wi1, op0=ALU.mult,
                               op1=ALU.add), r=R, w=R)
                kb.op("dve", L("tensor_scalar", out=wpg, in0=wi1, scalar1=pg, scalar2=None, op0=ALU.mult), r=R, w=R)
                kb.op("dve", L("tensor_tensor", out=comb_all[:, m, :].rearrange("p (g e) -> p g e", g=4),
                               in0=ohg.unsqueeze(2).broadcast_to([128, 4, 8]),
                               in1=wpg.unsqueeze(1).broadcast_to([128, 4, 8]), op=ALU.mult), r=R, w=[r_comb])
        if stage in ("B3", "B3s"):
            kb.dma("sp", s_comb, comb_all[:].rearrange("p a b -> p (a b)"), r=[r_comb])
        kb.barrier()

    if stage in ("B3", "B3s"):
        kb.finish()
        kb.emit()
        print("instructions:", kb.nins)
        return

    with ExitStack() as l4:
        lsb = mk_sb(l4)
        hT = lsb("hT", [128, 16, 1024], BF16)
        r_hT = [Reg(f"hT{i}") for i in range(8)]
        acc = lsb("acc", [128, 8, D])
        r_acc = [[Reg(f"acc{i}_{k}") for k in range(2)] for i in range(8)]
        wg = [lsb(f"wg{i}", [128, 16, 256], BF16) for i in range(2)]
        r_wg = [Reg(f"wg{i}") for i in range(2)]
        wu_ = [lsb(f"wup{i}", [128, 16, 256], BF16) for i in range(2)]
        r_wu_ = [Reg(f"wup{i}") for i in range(2)]
        wd = [lsb(f"wd{i}", [128, 4, D], BF16) for i in range(2)]
        r_wd = [Reg(f"wd{i}") for i in range(2)]
        actT = lsb("actT", [128, 4, 1024], BF16)
        r_actT = [Reg(f"actT{i}") for i in range(4)]
        sgm = [lsb(f"sgm{i}", [128, 512], BF16) for i in range(2)]
        r_sgm = [Reg(f"sgm{i}") for i in range(2)]
        npass = 2
        nblk = 8
        ne_run = NE
        if stage == "Ms":
            npass, nblk, ne_run = 1, 4, 2
        ntch = nblk // 4
        hw_n = 0
        gq_n = 0
        dq_n = 0
        for p in range(npass):
            for blk in range(nblk):
                m = 8 * p + blk
                kb.dma("sp", hT[:, :, blk * 128:(blk + 1) * 128], s_hT[m].rearrange("p (a b) -> p a b", a=16),
                       w=[r_hT[blk]])
                kb.dma("sp", acc[:, blk, :], out_d[m], w=r_acc[blk])
            for e in range(ne_run):
                d2 = e % 2
                kb.dma("pool", wd[d2][:], w_ed[e].rearrange("(fc p) n -> p fc n", p=128), w=[r_wd[d2]])
                for half in range(2):
                    h2 = hw_n % 2
                    hw_n += 1
                    kb.dma("pool", wg[h2][:], w_eg[e].rearrange("(kc p) f -> p kc f", p=128)[:, :, half * 256:(half + 1) * 256],
                           w=[r_wg[h2]])
                    kb.dma("pool", wu_[h2][:], w_eu[e].rearrange("(kc p) f -> p kc f", p=128)[:, :, half * 256:(half + 1) * 256],
                           w=[r_wu_[h2]])
                    for fcl in range(2):
                        fc = 2 * half + fcl
                        for tch in range(ntch):
                            g2_ = gq_n % 2
                            gq_n += 1
                            bG, bU = g2_, 2 + g2_
                            for kc in range(16):
                                kb.op("pe", L("matmul", out=PF(bG), lhsT=wg[h2][:, kc, fcl * 128:(fcl + 1) * 128],
                                              rhs=hT[:, kc, tch * 512:(tch + 1) * 512], start=(kc == 0), stop=(kc == 15)),
                                      r=[r_wg[h2]] + r_hT[4 * tch:4 * tch + 4], w=[r_pb[bG]])
                            for kc in range(16):
                                kb.op("pe", L("matmul", out=PF(bU), lhsT=wu_[h2][:, kc, fcl * 128:(fcl + 1) * 128],
                                              rhs=hT[:, kc, tch * 512:(tch + 1) * 512], start=(kc == 0), stop=(kc == 15)),
                                      r=[r_wu_[h2]] + r_hT[4 * tch:4 * tch + 4], w=[r_pb[bU]])
                            kb.op("act", L("activation", out=sgm[g2_][:], in_=PF(bG), func=AF.Silu),
                                  r=[r_pb[bG]], w=[r_sgm[g2_]])
                            kb.op("dve", L("tensor_tensor", out=actT[:, fc, tch * 512:(tch + 1) * 512], in0=PF(bU),
                                           in1=sgm[g2_][:], op=ALU.mult), r=[r_pb[bU], r_sgm[g2_]], w=[r_actT[fc]])
                for blk in range(nblk):
                    m = 8 * p + blk
                    for nh in range(2):
                        bD = 4 + 2 * (dq_n % 2)
                        dq_n += 1
                        for sub in range(2):
                            c0 = nh * 1024 + sub * 512
                            for fc in range(4):
                                kb.op("pe", L("matmul", out=PF(bD + sub), lhsT=actT[:, fc, blk * 128:(blk + 1) * 128],
                                              rhs=wd[d2][:, fc, c0:c0 + 512], start=(fc == 0), stop=(fc == 3)),
                                      r=[r_actT[fc], r_wd[d2]], w=[r_pb[bD + sub]])
                        av = acc[:, blk, nh * 1024:(nh + 1) * 1024]
                        kb.op("dve", L("scalar_tensor_tensor", out=av, in0=PF(bD, 0, 1024),
                                       scalar=comb_all[:, m, e:e + 1], in1=av, op0=ALU.mult, op1=ALU.add),
                              r=[r_pb[bD], r_pb[bD + 1], r_comb, r_acc[blk][nh]], w=[r_acc[blk][nh]])
            for blk in range(nblk):
                m = 8 * p + blk
                kb.dma("sp", out_d[m], acc[:, blk, :], r=r_acc[blk])
        kb.barrier()
    kb.finish()
    kb.emit()
    print("instructions:", kb.nins)
    return

    raise NotImplementedError


def host_consts(inputs):
    theta = 500000.0
    invA = theta ** (-np.arange(0, 32, 2, dtype=np.float32) / np.float32(32))
    invI = theta ** (-np.arange(0, 16, 2, dtype=np.float32) / np.float32(16))
    c = np.zeros((128, 512), np.float32)
    c[:, 0:128] = np.asarray(inputs["q_norm_g"]).reshape(1, 128)
    c[:, 128:256] = np.asarray(inputs["k_norm_g"]).reshape(1, 128)
    c[:, 256:272] = invA.astype(np.float32)[None, :]
    c[:, 272:280] = invI.astype(np.float32)[None, :]
    c[:, 288:292] = np.asarray(inputs["b_router_group"]).reshape(1, 4)
    c[:, 292:324] = np.asarray(inputs["b_router_expert"]).reshape(1, 32)
    return c


def make_in_maps(inputs, stage="full"):
    x = np.asarray(inputs["x"])
    pos = np.asarray(inputs["positions"])
    consts = host_consts(inputs)
    ident = np.eye(128, dtype=np.float32)
    w_in = np.ascontiguousarray(np.asarray(inputs["w_in"])[0])
    gA = np.ascontiguousarray(np.broadcast_to(np.asarray(inputs["attn_norm_g"]).reshape(1, D), (128, D)))
    g2 = np.ascontiguousarray(np.broadcast_to(np.asarray(inputs["ffn_norm_g"]).reshape(1, D), (128, D)))
    cv = np.zeros((128, 8, 34), np.float32)
    dw = np.asarray(inputs["conv_dw_w"])[0]
    cv[:, :, 0:31] = dw.T.reshape(8, 128, 31).transpose(1, 0, 2)
    cv[:, :, 31] = np.asarray(inputs["conv_dw_b"])[0].reshape(8, 128).T
    cv[:, :, 32] = np.asarray(inputs["conv_ln_g"])[0].reshape(8, 128).T
    cv[:, :, 33] = np.asarray(inputs["conv_ln_b"])[0].reshape(8, 128).T
    wr = np.ascontiguousarray(np.concatenate([np.asarray(inputs["w_router_group"])[0],
                                              np.asarray(inputs["w_router_expert"])[0]], axis=1))
    shared = {
        "consts": consts, "gA": gA, "g2": g2, "w_in": w_in, "ident": ident,
        "cvec": np.ascontiguousarray(cv.reshape(128, 8 * 34)),
        "w_conv_out": np.ascontiguousarray(np.asarray(inputs["w_conv_out"])[0]),
        "w_attn_o": np.ascontiguousarray(np.asarray(inputs["w_attn_o"])[0]),
        "w_out": np.ascontiguousarray(np.asarray(inputs["w_out"])[0]),
        "wr": wr,
        "w_exp_gate": np.ascontiguousarray(np.asarray(inputs["w_exp_gate"])[0]),
        "w_exp_up": np.ascontiguousarray(np.asarray(inputs["w_exp_up"])[0]),
        "w_exp_down": np.ascontiguousarray(np.asarray(inputs["w_exp_down"])[0]),
    }
    maps = []
    for c in range(8):
        b, j = c // 4, c % 4
        xpad = np.concatenate([np.zeros((32, D), np.float32), x[b]], axis=0)
        xo_ = np.stack([xpad[128 * (4 * m + j):128 * (4 * m + j) + 160] for m in range(NB)])
        pos_o = np.stack([pos[b, 128 * (4 * m + j):128 * (4 * m + j) + 128] for m in range(NB)], axis=1)
        cidx = np.arange(512)[None, :]
        prow = np.arange(128)[:, None]
        cmask = np.where(cidx <= 128 * j + prow, 0.0, NEG).astype(np.float32)
        m_ = dict(shared)
        m_.update({
            "xb": np.ascontiguousarray(x[b]),
            "pos_t": np.ascontiguousarray(pos[b].reshape(NT, 128).T),
            "pos_o": np.ascontiguousarray(pos_o.astype(np.int32)),
            "xo": np.ascontiguousarray(xo_),
            "cmask": cmask,
        })
        maps.append(m_)
    return maps


def kernel(**inputs):
    nc = build_nc("full")
    maps = make_in_maps(inputs)
    res = run_bass_kernel_spmd(nc, maps, core_ids=list(range(8)))
    out = np.zeros((2, SEQ, D), np.float32)
    for c in range(8):
        b, j = c // 4, c % 4
        o = np.asarray(res.results[c]["out"])
        for m in range(NB):
            i = 4 * m + j
            out[b, 128 * i:128 * (i + 1)] = o[m]
    return out
```

```python
import math
from contextlib import ExitStack

import numpy as np
import concourse.bass as bass
import concourse.mybir as mybir
from concourse.bass_utils import run_bass_kernel_spmd

F32 = mybir.dt.float32
BF16 = mybir.dt.bfloat16
I32 = mybir.dt.int32
AF = mybir.ActivationFunctionType
ALU = mybir.AluOpType
AX = mybir.AxisListType

D = 2048
SEQ = 8192
NT = SEQ // 128
NB = 16
EPS = 1e-6
IN_TOTAL = 9552
C_U, C_Q, C_K, C_V, C_QI, C_KI, C_WI, C_GC, C_GA = 0, 2048, 4096, 4224, 4352, 5376, 5440, 5456, 7504
NEG = -1.0e30
EPOCH = 30000
NSLOT = 8
NSEL = 256
NBIS = 16
ATTN_SCALE = 128 ** -0.5
NE = 32
FF = 512


def L(name, *a, **k):
    return lambda e: getattr(e, name)(*a, **k)


class Reg:
    __slots__ = ("w", "r", "name", "psum")

    def __init__(self, name="", psum=False):
        self.w = None
        self.r = []
        self.name = name
        self.psum = psum


class KB:
    def __init__(self, nc, es):
        self.nc = nc
        self.es = es
        self.names = ("pe", "act", "dve", "pool", "sp")
        self.sem = {}
        self.cnt = {e: 0 for e in ("pe", "act", "dve", "pool")}
        self.seen = {e: {} for e in self.names}
        self.dn = {q: 0 for q in ("sp", "act", "pool")}
        for q in self.dn:
            for i in range(NSLOT):
                self.sem[("d", q, i)] = es.enter_context(nc.semaphore(f"d_{q}{i}"))
        self.nins = 0
        self.prog = {e: [] for e in self.names}

    def emit(self):
        with self.nc.Block() as block:
            def run(en):
                def body(e):
                    for it in self.prog[en]:
                        if it[0] == "w":
                            e.wait_ge(it[1], it[2])
                        elif it[0] == "i":
                            it[1](e).then_inc(it[2], 1)
                        else:
                            e.dma_start(out=it[1], in_=it[2], **it[4]).then_inc(it[3], 16)
                return body
            block.sync(run("sp"))
            block.tensor(run("pe"))
            block.scalar(run("act"))
            block.vector(run("dve"))
            block.gpsimd(run("pool"))

    def _semfor(self, key):
        if key not in self.sem:
            self.sem[key] = self.es.enter_context(self.nc.semaphore(f"s_{key[0]}{key[1]}"))
        return self.sem[key]

    def _wait(self, en, evs):
        best = {}
        for k, v in evs:
            if best.get(k, 0) < v:
                best[k] = v
        for k, v in best.items():
            if self.seen[en].get(k, 0) < v:
                self.prog[en].append(("w", self._semfor(k), v))
                self.seen[en][k] = v

    def _deps(self, en, r, w, is_dma=False):
        evs = []
        for t in r:
            if t.w is not None:
                if not (en == "pe" and t.w[0][0] == "pe"):
                    evs.append(t.w)
            if t.psum:
                for ev in t.r:
                    if ev[0][0] != en:
                        evs.append(ev)
        for t in w:
            if t.w is not None:
                if not (en == "pe" and t.w[0][0] == "pe"):
                    evs.append(t.w)
            for ev in t.r:
                if en == "pe" and ev[0][0] == "pe":
                    continue
                evs.append(ev)
        return evs

    def _commit(self, ev, r, w):
        for t in r:
            t.r.append(ev)
            if len(t.r) > 48:
                best = {}
                for k, v in t.r:
                    if best.get(k, 0) < v:
                        best[k] = v
                t.r = list(best.items())
        for t in w:
            t.w = ev
            t.r = []

    def op(self, en, fn, r=(), w=()):
        self._wait(en, self._deps(en, r, w))
        c = self.cnt[en]
        key = (en, c // EPOCH)
        val = c % EPOCH + 1
        self.prog[en].append(("i", fn, self._semfor(key)))
        self.cnt[en] = c + 1
        self._commit((key, val), r, w)
        self.nins += 1

    def dma(self, q, out, in_, r=(), w=(), **kw):
        i = self.dn[q]
        slot, use = i % NSLOT, i // NSLOT
        key = ("d", q, slot)
        evs = self._deps(q, r, w, is_dma=True)
        if use > 0:
            evs.append((key, 16 * use))
        self._wait(q, evs)
        self.prog[q].append(("d", out, in_, self.sem[key], kw))
        self.dn[q] = i + 1
        self._commit((key, 16 * (use + 1)), r, w)
        self.nins += 1

    def all_events(self):
        evs = []
        for en, c in self.cnt.items():
            if c > 0:
                evs.append(((en, (c - 1) // EPOCH), (c - 1) % EPOCH + 1))
        for q, i in self.dn.items():
            for back in range(1, min(i, NSLOT) + 1):
                ii = i - back
                evs.append((("d", q, ii % NSLOT), 16 * (ii // NSLOT + 1)))
        return evs

    def barrier(self):
        evs = self.all_events()
        for en in self.names:
            self._wait(en, evs)

    def finish(self):
        self._wait("sp", self.all_events())


def build_nc(stage="full"):
    nc = bass.Bass("TRN2", target_bir_lowering=False)
    es = ExitStack()
    with es:
        _build(nc, es, stage)
    return nc


def _build(nc, es, stage):
    kb = KB(nc, es)
    dbg = stage != "full"

    def dram(name, shape, dt=F32, kind="ExternalInput"):
        return nc.dram_tensor(name, list(shape), dt, kind=kind).ap()

    def scratch(name, shape, dt, want_dbg):
        kind = "ExternalOutput" if (dbg and want_dbg) else "Internal"
        return nc.dram_tensor(name, list(shape), dt, kind=kind).ap()

    def mk_sb(stack):
        def sb(name, shape, dt=F32):
            return stack.enter_context(nc.sbuf_tensor(name, list(shape), dt))
        return sb

    sb = mk_sb(es)

    xb = dram("xb", [SEQ, D])
    pos_t = dram("pos_t", [128, NT], I32)
    pos_o = dram("pos_o", [128, NB], I32)
    consts = dram("consts", [128, 512])
    gA_d = dram("gA", [128, D])
    g2_d = dram("g2", [128, D])
    w_in = dram("w_in", [D, IN_TOTAL])
    ident_d = dram("ident", [128, 128])
    xo = dram("xo", [NB, 160, D])
    cmask_d = dram("cmask", [128, 512])
    cvec_d = dram("cvec", [128, 8 * 34])
    w_pw = dram("w_conv_out", [1024, D])
    w_ao = dram("w_attn_o", [D, D])
    w_o = dram("w_out", [D, D])
    wr_d = dram("wr", [D, 36])
    w_eg = dram("w_exp_gate", [NE, D, FF])
    w_eu = dram("w_exp_up", [NE, D, FF])
    w_ed = dram("w_exp_down", [NE, FF, D])
    out_d = dram("out", [NB, 128, D], kind="ExternalOutput")

    s_qT = scratch("s_qT", [NB, 128, 2048], BF16, stage.startswith("B1"))
    s_qiT = scratch("s_qiT", [NB, 128, 1024], BF16, stage.startswith("B1"))
    s_wi = scratch("s_wi", [NB, 128, 16], F32, stage.startswith("B1"))
    s_sT = scratch("s_sT", [NB, 128, 1024], BF16, stage.startswith("B1"))
    s_xT = scratch("s_xT", [NB, 128, 2048], BF16, stage.startswith("B1"))
    s_AT = scratch("s_AT", [NB, 128, 2048], BF16, stage in ("B2", "B2s"))
    s_hT = scratch("s_hT", [NB, 128, 2048], BF16, stage in ("B3", "B3s"))
    s_comb = scratch("s_comb", [128, NB * NE], F32, stage in ("B3", "B3s"))

    w_in_v = w_in.rearrange("(kc p) n -> p kc n", p=128)

    pall = es.enter_context(nc.psum_tensor("pall", [128, 4096], F32))
    pallh = pall.bitcast(BF16)
    r_pb = [Reg(f"pb{i}", psum=True) for i in range(8)]

    def PF(bank, c0=0, c1=512):
        return pall[:, bank * 512 + c0: bank * 512 + c1]

    def PH(bank, c0=0, c1=1024):
        return pallh[:, bank * 1024 + c0: bank * 1024 + c1]

    cst = sb("cst", [128, 512])
    r_cst = Reg("cst")
    kb.dma("sp", cst[:], consts, w=[r_cst])
    gqs = cst[:, 0:128]
    gk = cst[:, 128:256]
    invA = cst[:, 256:272]
    invI = cst[:, 272:280]
    brt = cst[:, 288:324]
    identf = sb("identf", [128, 128])
    identb = sb("identb", [128, 128], BF16)
    r_id = Reg("ident")
    kb.dma("sp", identf[:], ident_d, w=[r_id])
    kb.op("dve", L("tensor_copy", out=identb[:], in_=identf[:]), r=[r_id], w=[r_id])
    epst = sb("epst", [128, 1])
    r_eps = Reg("eps")
    kb.op("dve", L("memset", epst[:], EPS), w=[r_eps])
    gbuf = sb("gbuf", [128, D])
    r_gbuf = Reg("gbuf")

    def rope_tables(cos_t, sin_t, name, posf_ap, ncol, inv_ap, half, r_in, tmp_stack):
        tsb = mk_sb(tmp_stack)
        u = tsb(name + "_u", [128, ncol, half])
        ki = tsb(name + "_ki", [128, ncol, half], I32)
        kf = tsb(name + "_kf", [128, ncol, half])
        d = tsb(name + "_d", [128, ncol, half])
        m1 = tsb(name + "_m1", [128, ncol, half])
        rr = Reg(name)
        kb.op("dve", L("tensor_tensor", out=u[:], in0=posf_ap.unsqueeze(2).broadcast_to([128, ncol, half]),
                       in1=inv_ap.unsqueeze(1).broadcast_to([128, ncol, half]), op=ALU.mult),
              r=[r_in, r_cst], w=[rr])
        kb.op("dve", L("tensor_scalar", out=u[:], in0=u[:], scalar1=1.0 / (2 * math.pi), scalar2=None,
                       op0=ALU.mult), r=[rr], w=[rr])
        for shift, dst in ((0.0, sin_t), (0.25, cos_t)):
            src = u
            if shift != 0.0:
                kb.op("dve", L("tensor_scalar", out=d[:], in0=u[:], scalar1=shift, scalar2=None, op0=ALU.add),
                      r=[rr], w=[rr])
                src = d
            kb.op("dve", L("tensor_copy", out=ki[:], in_=src[:]), r=[rr], w=[rr])
            kb.op("dve", L("tensor_copy", out=kf[:], in_=ki[:]), r=[rr], w=[rr])
            kb.op("dve", L("tensor_tensor", out=d[:], in0=src[:], in1=kf[:], op=ALU.subtract), r=[rr], w=[rr])
            kb.op("dve", L("tensor_scalar", out=m1[:], in0=d[:], scalar1=0.5, scalar2=None, op0=ALU.is_gt),
                  r=[rr], w=[rr])
            kb.op("dve", L("tensor_tensor", out=d[:], in0=d[:], in1=m1[:], op=ALU.subtract), r=[rr], w=[rr])
            kb.op("dve", L("tensor_scalar", out=m1[:], in0=d[:], scalar1=-0.5, scalar2=None, op0=ALU.is_lt),
                  r=[rr], w=[rr])
            kb.op("dve", L("tensor_tensor", out=d[:], in0=d[:], in1=m1[:], op=ALU.add), r=[rr], w=[rr])
            kb.op("act", L("activation", out=dst[:], in_=d[:], func=AF.Sin, scale=2 * math.pi * (1 - 1e-6)),
                  r=[rr], w=[rr])
        return cos_t, sin_t, rr

    def rope(dst, src, half, cos_ap, sin_ap, t4, r_src, r_dst, r_tab, r_t4):
        def sl(ap, a, b):
            return ap[:, a:b] if len(ap.shape) == 2 else ap[:, :, a:b]
        x1, x2 = sl(src, 0, half), sl(src, half, 2 * half)
        ts = [t4[i] for i in range(4)]
        kb.op("dve", L("tensor_tensor", out=ts[0], in0=x1, in1=cos_ap, op=ALU.mult), r=[r_src, r_tab], w=[r_t4])
        kb.op("dve", L("tensor_tensor", out=ts[1], in0=x2, in1=sin_ap, op=ALU.mult), r=[r_src, r_tab], w=[r_t4])
        kb.op("dve", L("tensor_tensor", out=ts[2], in0=x1, in1=sin_ap, op=ALU.mult), r=[r_src, r_tab], w=[r_t4])
        kb.op("dve", L("tensor_tensor", out=ts[3], in0=x2, in1=cos_ap, op=ALU.mult), r=[r_src, r_tab], w=[r_t4])
        kb.op("dve", L("tensor_tensor", out=sl(dst, 0, half), in0=ts[0], in1=ts[1], op=ALU.subtract),
              r=[r_t4], w=[r_dst])
        kb.op("dve", L("tensor_tensor", out=sl(dst, half, 2 * half), in0=ts[2], in1=ts[3], op=ALU.add),
              r=[r_t4], w=[r_dst])

    def rstd_from_ss(st, c_ss, n, r_st):
        kb.op("act", L("activation", out=st[:, c_ss + 1:c_ss + 2], in_=st[:, c_ss:c_ss + 1], func=AF.Sqrt,
                       scale=1.0 / n, bias=epst[0:st.shape[0], :]), r=[r_st, r_eps], w=[r_st])
        kb.op("dve", L("reciprocal", out=st[:, c_ss + 2:c_ss + 3], in_=st[:, c_ss + 1:c_ss + 2]),
              r=[r_st], w=[r_st])

    posoi = sb("posoi", [128, NB], I32)
    posof = sb("posof", [128, NB])
    r_poso = Reg("poso")
    kb.dma("sp", posoi[:], pos_o, w=[r_poso])
    kb.op("dve", L("tensor_copy", out=posof[:], in_=posoi[:]), r=[r_poso], w=[r_poso])
    cosAo, sinAo = sb("rao_cos", [128, NB, 16]), sb("rao_sin", [128, NB, 16])
    cosIo, sinIo = sb("rio_cos", [128, NB, 8]), sb("rio_sin", [128, NB, 8])
    with ExitStack() as tmps:
        _, _, r_ropeAo = rope_tables(cosAo, sinAo, "rao", posof[:], NB, invA, 16, r_poso, tmps)
        _, _, r_ropeIo = rope_tables(cosIo, sinIo, "rio", posof[:], NB, invI, 8, r_poso, tmps)
        kb.barrier()

    ngroups = 4 if stage not in ("B1s", "B2s", "B3s", "Ms") and not stage.startswith("B1c") else 1
    cut = int(stage[3:]) if stage.startswith("B1c") else 99
    if stage in ("smoke", "A"):
        ngroups = 0
    with ExitStack() as lb:
        lsb = mk_sb(lb)
        kb.dma("sp", gbuf[:], gA_d, w=[r_gbuf])
        cw = lsb("cw", [128, 8, 34])
        r_cw = Reg("cw")
        kb.dma("sp", cw[:].rearrange("p a b -> p (a b)"), cvec_d, w=[r_cw])
        onesf = lsb("onesf", [128, 128])
        r_ones = Reg("ones")
        kb.op("dve", L("memset", onesf[:], 1.0 / 1024.0), w=[r_ones])
        xT_g = lsb("xT_g", [128, 16, 640], BF16)
        r_xTg = [Reg(f"xTg{i}") for i in range(4)]
        xm = [lsb(f"xm{i}", [128, D]) for i in range(2)]
        r_xm = [Reg(f"xm{i}") for i in range(2)]
        xhl1 = lsb("xhl", [32, D])
        xhl = [xhl1, xhl1]
        r_xhl1 = Reg("xhl")
        r_xhl = [r_xhl1, r_xhl1]
        xs = [lsb(f"xsb{i}", [128, D], BF16) for i in range(2)]
        r_xs = [Reg(f"xsb{i}") for i in range(2)]
        xsh1 = lsb("xsh", [32, D], BF16)
        xsh = [xsh1, xsh1]
        r_xsh1 = Reg("xsh")
        r_xsh = [r_xsh1, r_xsh1]
        st = [lsb(f"stb{i}", [128, 8]) for i in range(2)]
        r_st = [Reg(f"stb{i}") for i in range(2)]
        yT = lsb("yT", [128, 8, 640])
        r_yT = [Reg(f"yT{i}") for i in range(8)]
        cacc = lsb("cacc", [128, 8, 512])
        r_cacc = [Reg(f"cacc{i}") for i in range(8)]
        sT = lsb("sT", [128, 8, 512], BF16)
        r_sT = Reg("sT")
        wu = [lsb(f"wu{i}", [128, 16, 256], BF16) for i in range(2)]
        r_wu = [Reg(f"wu{i}") for i in range(2)]
        wbuf = [lsb(f"wbuf{i}", [128, 16, 512], BF16) for i in range(2)]
        r_wbuf = [Reg(f"wbuf{i}") for i in range(2)]
        sg = [lsb(f"sg{i}", [128, 640]) for i in range(2)]
        r_sg = [Reg(f"sg{i}") for i in range(2)]
        lnm = lsb("lnm", [128, 3, 512])
        r_lnm = Reg("lnm")
        qsq = lsb("qsq", [128, 512])
        r_qsq = Reg("qsq")
        stq = [lsb(f"stq{i}", [128, 12]) for i in range(2)]
        r_stq = [Reg(f"stq{i}") for i in range(2)]
        qn = [lsb(f"qn{i}", [128, 4, 128]) for i in range(2)]
        r_qn = [Reg(f"qn{i}") for i in range(2)]
        qb = [lsb(f"qb{i}", [128, 512], BF16) for i in range(2)]
        r_qb = [Reg(f"qb{i}") for i in range(2)]
        t4q = [lsb(f"t4q{i}", [128, 4, 8, 16]) for i in range(2)]
        r_t4q = [Reg(f"t4q{i}") for i in range(2)]
        qTs = [lsb(f"qTs{i}", [128, 4, 128], BF16) for i in range(2)]
        r_qTs = [Reg(f"qTs{i}") for i in range(2)]
        wis = lsb("wis", [128, 4, 16])
        r_wis = Reg("wis")
        wwi = lsb("wwi", [128, 16, 16], BF16)
        r_wwi = Reg("wwi")
        kb.dma("pool", wwi[:], w_in_v[:, :, C_WI:C_WI + 16], w=[r_wwi])

        wu_n = [0]
        wb_n = [0]

        def load_wbuf(c0):
            i = wb_n[0] % 2
            wb_n[0] += 1
            kb.dma("pool", wbuf[i][:], w_in_v[:, :, c0:c0 + 512], w=[r_wbuf[i]])
            return i

        for gi in range(ngroups):
            for bl in range(4):
                m = 4 * gi + bl
                b2 = bl % 2
                bT = 2 * b2
                kb.dma("sp", xm[b2][:], xo[m, 32:160, :], w=[r_xm[b2]])
                kb.dma("sp", xhl[b2][:], xo[m, 0:32, :], w=[r_xhl[b2]])
                kb.op("act", L("activation", out=xs[b2][:], in_=xm[b2][:], func=AF.Square,
                               accum_out=st[b2][:, 0:1]), r=[r_xm[b2]], w=[r_xs[b2], r_st[b2]])
                rstd_from_ss(st[b2], 0, D, r_st[b2])
                kb.op("dve", L("scalar_tensor_tensor", out=xs[b2][:], in0=xm[b2][:], scalar=st[b2][:, 2:3],
                               in1=gbuf[:], op0=ALU.mult, op1=ALU.mult),
                      r=[r_xm[b2], r_st[b2], r_gbuf], w=[r_xs[b2]])
                for kc in range(16):
                    kb.op("pe", L("transpose", out=PH(bT, kc * 128, (kc + 1) * 128),
                                  in_=xs[b2][:, kc * 128:(kc + 1) * 128], identity=identb[:]),
                          r=[r_xs[b2], r_id], w=[r_pb[bT + kc // 8]])
                kb.op("act", L("copy", out=xT_g[:, :, bl * 128:(bl + 1) * 128],
                               in_=PH(bT, 0, 2048).rearrange("p (a b) -> p a b", a=16)),
                      r=[r_pb[bT], r_pb[bT + 1]], w=[r_xTg[bl]])
                kb.dma("sp", s_xT[m].rearrange("p (a b) -> p a b", a=16), xT_g[:, :, bl * 128:(bl + 1) * 128],
                       r=[r_xTg[bl]])
                bH = 4 + b2
                kb.op("act", L("activation", out=xsh[b2][:], in_=xhl[b2][:], func=AF.Square,
                               accum_out=st[b2][0:32, 3:4]), r=[r_xhl[b2]], w=[r_xsh[b2], r_st[b2]])
                rstd_from_ss(st[b2][0:32, :], 3, D, r_st[b2])
                kb.op("dve", L("scalar_tensor_tensor", out=xsh[b2][:], in0=xhl[b2][:], scalar=st[b2][0:32, 5:6],
                               in1=gbuf[0:32, :], op0=ALU.mult, op1=ALU.mult),
                      r=[r_xhl[b2], r_st[b2], r_gbuf], w=[r_xsh[b2]])
                for kc in range(16):
                    kb.op("pe", L("transpose", out=PH(bH, kc * 32, (kc + 1) * 32),
                                  in_=xsh[b2][:, kc * 128:(kc + 1) * 128], identity=identb[0:32, 0:32]),
                          r=[r_xsh[b2], r_id], w=[r_pb[bH]])
                kb.op("act", L("copy", out=xT_g[:, :, 512 + bl * 32:512 + (bl + 1) * 32],
                               in_=PH(bH, 0, 512).rearrange("p (a b) -> p a b", a=16)),
                      r=[r_pb[bH]], w=[r_xTg[bl]])

            if cut <= 1:
                break
            for cc in range(8):
                i = wu_n[0] % 2
                wu_n[0] += 1
                kb.dma("pool", wu[i][:, :, 0:128], w_in_v[:, :, C_U + cc * 128:C_U + (cc + 1) * 128], w=[r_wu[i]])
                kb.dma("pool", wu[i][:, :, 128:256], w_in_v[:, :, C_U + 1024 + cc * 128:C_U + 1024 + (cc + 1) * 128],
                       w=[r_wu[i]])
                b3 = 3 * (cc % 2)
                bA, bG, bHh = b3, b3 + 1, b3 + 2
                for (bank, c0, c1, wc, x0, x1) in ((bA, 0, 512, 0, 0, 512), (bG, 0, 512, 128, 0, 512),
                                                   (bHh, 0, 128, 0, 512, 640), (bHh, 128, 256, 128, 512, 640)):
                    for kc in range(16):
                        kb.op("pe", L("matmul", out=PF(bank, c0, c1), lhsT=wu[i][:, kc, wc:wc + 128],
                                      rhs=xT_g[:, kc, x0:x1], start=(kc == 0), stop=(kc == 15)),
                              r=[r_wu[i]] + r_xTg, w=[r_pb[bank]])
                s2 = cc % 2
                kb.op("act", L("activation", out=sg[s2][:, 0:512], in_=PF(bG), func=AF.Sigmoid),
                      r=[r_pb[bG]], w=[r_sg[s2]])
                kb.op("act", L("activation", out=sg[s2][:, 512:640], in_=PF(bHh, 128, 256), func=AF.Sigmoid),
                      r=[r_pb[bHh]], w=[r_sg[s2]])
                yv = yT[:, cc, :].rearrange("p (b t) -> p b t", b=4)
                kb.op("dve", L("tensor_tensor", out=yv[:, :, 32:160],
                               in0=PF(bA).rearrange("p (b t) -> p b t", b=4),
                               in1=sg[s2][:, 0:512].rearrange("p (b t) -> p b t", b=4), op=ALU.mult),
                      r=[r_pb[bA], r_sg[s2]], w=[r_yT[cc]])
                kb.op("dve", L("tensor_tensor", out=yv[:, :, 0:32],
                               in0=PF(bHh, 0, 128).rearrange("p (b t) -> p b t", b=4),
                               in1=sg[s2][:, 512:640].rearrange("p (b t) -> p b t", b=4), op=ALU.mult),
                      r=[r_pb[bHh], r_sg[s2]], w=[r_yT[cc]])

            if cut <= 2:
                break
            def gen_conv():
                for cc in range(8):
                    yv = yT[:, cc, :].rearrange("p (b t) -> p b t", b=4)
                    av = cacc[:, cc, :].rearrange("p (b t) -> p b t", b=4)
                    kb.op("dve", L("tensor_scalar", out=av, in0=yv[:, :, 2:130], scalar1=cw[:, cc, 0:1],
                                   scalar2=cw[:, cc, 31:32], op0=ALU.mult, op1=ALU.add),
                          r=[r_yT[cc], r_cw], w=[r_cacc[cc]])
                    yield
                    for k in range(1, 31):
                        kb.op("dve", L("scalar_tensor_tensor", out=av, in0=yv[:, :, 2 + k:130 + k],
                                       scalar=cw[:, cc, k:k + 1], in1=av, op0=ALU.mult, op1=ALU.add),
                              r=[r_yT[cc], r_cw, r_cacc[cc]], w=[r_cacc[cc]])
                        yield
                bM, bS = 6, 7
                for cc in range(8):
                    kb.op("pe", L("matmul", out=PF(bM), lhsT=onesf[:], rhs=cacc[:, cc, :], start=(cc == 0),
                                  stop=(cc == 7)), r=[r_ones, r_cacc[cc]], w=[r_pb[bM]])
                sqv = yT[:].rearrange("p a b -> p (a b)")[:, 0:4096].rearrange("p (a b) -> p a b", a=8)
                kb.op("act", L("activation", out=sqv, in_=cacc[:], func=AF.Square), r=r_cacc, w=r_yT)
                for cc in range(8):
                    kb.op("pe", L("matmul", out=PF(bS), lhsT=onesf[:], rhs=sqv[:, cc, :], start=(cc == 0),
                                  stop=(cc == 7)), r=[r_ones] + r_yT, w=[r_pb[bS]])
                kb.op("act", L("copy", out=lnm[:, 0, :], in_=PF(bM)), r=[r_pb[bM]], w=[r_lnm])
                kb.op("dve", L("tensor_tensor", out=lnm[:, 1, :], in0=lnm[:, 0, :], in1=lnm[:, 0, :], op=ALU.mult),
                      r=[r_lnm], w=[r_lnm])
                kb.op("dve", L("tensor_tensor", out=lnm[:, 1, :], in0=PF(bS), in1=lnm[:, 1, :], op=ALU.subtract),
                      r=[r_pb[bS], r_lnm], w=[r_lnm])
                kb.op("act", L("activation", out=lnm[:, 2, :], in_=lnm[:, 1, :], func=AF.Sqrt, bias=epst[:]),
                      r=[r_lnm, r_eps], w=[r_lnm])
                kb.op("dve", L("reciprocal", out=lnm[:, 1, :], in_=lnm[:, 2, :]), r=[r_lnm], w=[r_lnm])
                yield
                for cc in range(8):
                    kb.op("dve", L("tensor_tensor", out=cacc[:, cc, :], in0=cacc[:, cc, :], in1=lnm[:, 0, :],
                                   op=ALU.subtract), r=[r_cacc[cc], r_lnm], w=[r_cacc[cc]])
                    kb.op("dve", L("tensor_tensor", out=cacc[:, cc, :], in0=cacc[:, cc, :], in1=lnm[:, 1, :],
                                   op=ALU.mult), r=[r_cacc[cc], r_lnm], w=[r_cacc[cc]])
                    kb.op("act", L("activation", out=sT[:, cc, :], in_=cacc[:, cc, :], func=AF.Silu,
                                   scale=cw[:, cc, 32:33], bias=cw[:, cc, 33:34]),
                          r=[r_cacc[cc], r_cw], w=[r_sT])
                    yield
                for bl in range(4):
                    m = 4 * gi + bl
                    kb.dma("sp", s_sT[m].rearrange("p (a b) -> p a b", a=8), sT[:, :, bl * 128:(bl + 1) * 128],
                           r=[r_sT])

            def gen_q():
                iters = [("q", qc, bl) for qc in range(4) for bl in range(4)] + \
                        [("qi", c2, bl) for c2 in range(2) for bl in range(4)]
                wsel = {}

                def phase_a(i):
                    kind, c, bl = iters[i]
                    if bl == 0:
                        wsel[(kind, c)] = load_wbuf((C_Q if kind == "q" else C_QI) + c * 512)
                    wi_ = wsel[(kind, c)]
                    bQ = i % 2
                    for kc in range(16):
                        kb.op("pe", L("matmul", out=PF(bQ), lhsT=xT_g[:, kc, bl * 128:(bl + 1) * 128],
                                      rhs=wbuf[wi_][:, kc, :], start=(kc == 0), stop=(kc == 15)),
                              r=[r_xTg[bl], r_wbuf[wi_]], w=[r_pb[bQ]])

                def phase_b(i):
                    kind, c, bl = iters[i]
                    m = 4 * gi + bl
                    p2 = i % 2
                    bQ = p2
                    bT = 2 + p2
                    if kind == "q":
                        kb.op("act", L("activation", out=qsq[:], in_=PF(bQ), func=AF.Square),
                              r=[r_pb[bQ]], w=[r_qsq])
                        kb.op("dve", L("tensor_reduce", out=stq[p2][:, 0:4],
                                       in_=qsq[:].rearrange("p (h d) -> p h d", h=4), axis=AX.X, op=ALU.add),
                              r=[r_qsq], w=[r_stq[p2]])
                        kb.op("act", L("activation", out=stq[p2][:, 4:8], in_=stq[p2][:, 0:4], func=AF.Sqrt,
                                       scale=1.0 / 128, bias=epst[:]), r=[r_stq[p2], r_eps], w=[r_stq[p2]])
                        kb.op("dve", L("reciprocal", out=stq[p2][:, 8:12], in_=stq[p2][:, 4:8]),
                              r=[r_stq[p2]], w=[r_stq[p2]])
                        kb.op("dve", L("tensor_tensor", out=qn[p2][:],
                                       in0=PF(bQ).rearrange("p (h d) -> p h d", h=4),
                                       in1=stq[p2][:, 8:12].unsqueeze(2).broadcast_to([128, 4, 128]), op=ALU.mult),
                              r=[r_pb[bQ], r_stq[p2]], w=[r_qn[p2]])
                        kb.op("dve", L("tensor_tensor", out=qn[p2][:], in0=qn[p2][:],
                                       in1=gqs.unsqueeze(1).broadcast_to([128, 4, 128]), op=ALU.mult),
                              r=[r_qn[p2], r_cst], w=[r_qn[p2]])
                        qbv = qb[p2][:].rearrange("p (h d) -> p h d", h=4)
                        kb.op("act", L("copy", out=qbv[:, :, 32:128], in_=qn[p2][:, :, 32:128]),
                              r=[r_qn[p2]], w=[r_qb[p2]])
                        t4 = [t4q[p2][:, j4, 0:4, :] for j4 in range(4)]
                        rope(qbv, qn[p2][:], 16, cosAo[:, m, :].unsqueeze(1).broadcast_to([128, 4, 16]),
                             sinAo[:, m, :].unsqueeze(1).broadcast_to([128, 4, 16]), t4,
                             r_qn[p2], r_qb[p2], r_ropeAo, r_t4q[p2])
                        dst = s_qT[m].rearrange("p (a b) -> p a b", a=16)[:, 4 * c:4 * c + 4, :]
                    else:
                        kb.op("act", L("copy", out=qn[p2][:].rearrange("p a b -> p (a b)"), in_=PF(bQ)),
                              r=[r_pb[bQ]], w=[r_qn[p2]])
                        pv = qn[p2][:].rearrange("p a b -> p (a b)").rearrange("p (h d) -> p h d", h=8)
                        qbv = qb[p2][:].rearrange("p (h d) -> p h d", h=8)
                        kb.op("act", L("copy", out=qbv[:, :, 16:64], in_=pv[:, :, 16:64]),
                              r=[r_qn[p2]], w=[r_qb[p2]])
                        t4 = [t4q[p2][:, j4, :, 0:8] for j4 in range(4)]
                        rope(qbv, pv, 8, cosIo[:, m, :].unsqueeze(1).broadcast_to([128, 8, 8]),
                             sinIo[:, m, :].unsqueeze(1).broadcast_to([128, 8, 8]), t4,
                             r_qn[p2], r_qb[p2], r_ropeIo, r_t4q[p2])
                        dst = s_qiT[m].rearrange("p (a b) -> p a b", a=8)[:, 4 * c:4 * c + 4, :]
                    yield
                    for h in range(4):
                        kb.op("pe", L("transpose", out=PH(bT, h * 128, (h + 1) * 128),
                                      in_=qb[p2][:, h * 128:(h + 1) * 128], identity=identb[:]),
                              r=[r_qb[p2], r_id], w=[r_pb[bT]])
                    kb.op("act", L("copy", out=qTs[p2][:],
                                   in_=PH(bT, 0, 512).rearrange("p (a b) -> p a b", a=4)),
                          r=[r_pb[bT]], w=[r_qTs[p2]])
                    kb.dma("sp", dst, qTs[p2][:], r=[r_qTs[p2]])

                phase_a(0)
                for i in range(len(iters)):
                    if i + 1 < len(iters):
                        phase_a(i + 1)
                    yield
                    yield from phase_b(i)
                    yield

            gc_, gq_ = gen_conv(), gen_q()
            alive_c, alive_q = True, True
            while alive_c or alive_q:
                if alive_q:
                    try:
                        next(gq_)
                    except StopIteration:
                        alive_q = False
                for _ in range(4):
                    if alive_c:
                        try:
                            next(gc_)
                        except StopIteration:
                            alive_c = False
            if cut <= 5:
                break
            for bl in range(4):
                m = 4 * gi + bl
                bQ = 4 + bl % 2
                for kc in range(16):
                    kb.op("pe", L("matmul", out=PF(bQ, 0, 16), lhsT=xT_g[:, kc, bl * 128:(bl + 1) * 128],
                                  rhs=wwi[:, kc, :], start=(kc == 0), stop=(kc == 15)),
                          r=[r_xTg[bl], r_wwi], w=[r_pb[bQ]])
                kb.op("act", L("copy", out=wis[:, bl, :], in_=PF(bQ, 0, 16)), r=[r_pb[bQ]], w=[r_wis])
                kb.dma("sp", s_wi[m], wis[:, bl, :], r=[r_wis])
        kb.barrier()

    if stage in ("B1", "B1s") or stage.startswith("B1c"):
        kb.finish()
        kb.emit()
        print("instructions:", kb.nins)
        return

    kvs = ExitStack()
    with kvs:
        ksb = mk_sb(kvs)
        KT = ksb("KT", [128, SEQ], BF16)
        Vt = ksb("Vt", [128, NT, 129], BF16)
        KIT = ksb("KIT", [128, SEQ], BF16)
        r_KT = [Reg(f"KT{n}") for n in range(NT)]
        r_V = [Reg(f"V{n}") for n in range(NT)]
        r_KIT = [Reg(f"KIT{n}") for n in range(NT)]
        r_vone = Reg("vone")
        kb.op("pool", L("memset", Vt[:, :, 128:129], 1.0), w=[r_vone])

        ntile_a = NT if stage not in ("smoke", "B1s", "B2s", "B3s", "Ms") else (2 if stage not in ("B2s", "B3s", "Ms") else 16)
        with ExitStack() as la:
            lsb = mk_sb(la)
            kb.dma("sp", gbuf[:], gA_d, w=[r_gbuf])
            posi = lsb("posi", [128, NT], I32)
            posf = lsb("posf", [128, NT])
            r_pos = Reg("pos")
            kb.dma("sp", posi[:], pos_t, w=[r_pos])
            kb.op("dve", L("tensor_copy", out=posf[:], in_=posi[:]), r=[r_pos], w=[r_pos])
            cosA, sinA = lsb("ra_cos", [128, NT, 16]), lsb("ra_sin", [128, NT, 16])
            cosI, sinI = lsb("ri_cos", [128, NT, 8]), lsb("ri_sin", [128, NT, 8])
            with ExitStack() as tmps:
                _, _, r_ropeA = rope_tables(cosA, sinA, "ra", posf[:], NT, invA, 16, r_pos, tmps)
                _, _, r_ropeI = rope_tables(cosI, sinI, "ri", posf[:], NT, invI, 8, r_pos, tmps)
                kb.barrier()
            wkv = lsb("wkv", [128, 16, 320], BF16)
            r_wkv = Reg("wkv")
            for (c0, c1, o0) in ((C_K, C_K + 256, 0), (C_KI, C_KI + 64, 256)):
                kb.dma("pool", wkv[:, :, o0:o0 + (c1 - c0)], w_in_v[:, :, c0:c1], w=[r_wkv])
            xin = [lsb(f"xin{i}", [128, D]) for i in range(2)]
            r_xin = [Reg(f"xin{i}") for i in range(2)]
            junk = lsb("junk", [128, D], BF16)
            r_junk = Reg("junk")
            xs = [lsb(f"xs{i}", [128, D], BF16) for i in range(2)]
            r_xs = [Reg(f"xs{i}") for i in range(2)]
            xT = [lsb(f"xT{i}", [128, 16, 128], BF16) for i in range(2)]
            r_xT = [Reg(f"xT{i}") for i in range(2)]
            st = [lsb(f"st{i}", [128, 8]) for i in range(2)]
            r_st = [Reg(f"st{i}") for i in range(2)]
            kn = [lsb(f"kn{i}", [128, 192]) for i in range(2)]
            kfin = [lsb(f"kfin{i}", [128, 256], BF16) for i in range(2)]
            tmp = [lsb(f"tmp{i}", [128, 4, 16]) for i in range(2)]
            r_kn = [Reg(f"kn{i}") for i in range(2)]
            r_kf = [Reg(f"kf{i}") for i in range(2)]
            r_tmp = [Reg(f"tmp{i}") for i in range(2)]

            def a_s1(n):
                b2 = n % 2
                bT = 2 * b2
                bZ = 4 + b2
                bK = 6 + b2
                kb.dma("sp", xin[b2][:], xb[n * 128:(n + 1) * 128, :], w=[r_xin[b2]])
                kb.op("act", L("activation", out=junk[:], in_=xin[b2][:], func=AF.Square, accum_out=st[b2][:, 0:1]),
                      r=[r_xin[b2]], w=[r_junk, r_st[b2]])
                rstd_from_ss(st[b2], 0, D, r_st[b2])
                kb.op("dve", L("scalar_tensor_tensor", out=xs[b2][:], in0=xin[b2][:], scalar=st[b2][:, 2:3],
                               in1=gbuf[:], op0=ALU.mult, op1=ALU.mult),
                      r=[r_xin[b2], r_st[b2], r_gbuf], w=[r_xs[b2]])
                for kc in range(16):
                    kb.op("pe", L("transpose", out=PH(bT, kc * 128, (kc + 1) * 128),
                                  in_=xs[b2][:, kc * 128:(kc + 1) * 128], identity=identb[:]),
                          r=[r_xs[b2], r_id], w=[r_pb[bT + kc // 8]])
                kb.op("act", L("copy", out=xT[b2][:].rearrange("p a b -> p (a b)"), in_=PH(bT, 0, 2048)),
                      r=[r_pb[bT], r_pb[bT + 1]], w=[r_xT[b2]])

            def a_s2(n):
                b2 = n % 2
                bT = 2 * b2
                bZ = 4 + b2
                bK = 6 + b2
                for kc in range(16):
                    kb.op("pe", L("matmul", out=PF(bZ, 0, 320), lhsT=xT[b2][:, kc, :], rhs=wkv[:, kc, :],
                                  start=(kc == 0), stop=(kc == 15)),
                          r=[r_xT[b2], r_wkv], w=[r_pb[bZ]])
                kb.op("act", L("copy", out=Vt[:, n, 0:128], in_=PF(bZ, 128, 256)), r=[r_pb[bZ]], w=[r_V[n]])
                kb.op("act", L("activation", out=kn[b2][:, 0:128], in_=PF(bZ, 0, 128), func=AF.Square,
                               accum_out=st[b2][:, 3:4]), r=[r_pb[bZ]], w=[r_kn[b2], r_st[b2]])
                rstd_from_ss(st[b2], 3, 128, r_st[b2])
                kb.op("dve", L("scalar_tensor_tensor", out=kn[b2][:, 0:128], in0=PF(bZ, 0, 128),
                               scalar=st[b2][:, 5:6], in1=gk, op0=ALU.mult, op1=ALU.mult),
                      r=[r_pb[bZ], r_st[b2], r_cst], w=[r_kn[b2]])
                kb.op("dve", L("tensor_copy", out=kn[b2][:, 128:192], in_=PF(bZ, 256, 320)),
                      r=[r_pb[bZ]], w=[r_kn[b2]])
                kb.op("dve", L("tensor_copy", out=kfin[b2][:, 32:128], in_=kn[b2][:, 32:128]),
                      r=[r_kn[b2]], w=[r_kf[b2]])
                kb.op("dve", L("tensor_copy", out=kfin[b2][:, 144:192], in_=kn[b2][:, 144:192]),
                      r=[r_kn[b2]], w=[r_kf[b2]])
                t4a = [tmp[b2][:, i, 0:16] for i in range(4)]
                t4i = [tmp[b2][:, i, 0:8] for i in range(4)]
                rope(kfin[b2][:, 0:128], kn[b2][:, 0:128], 16, cosA[:, n, :], sinA[:, n, :], t4a,
                     r_kn[b2], r_kf[b2], r_ropeA, r_tmp[b2])
                rope(kfin[b2][:, 128:192], kn[b2][:, 128:192], 8, cosI[:, n, :], sinI[:, n, :], t4i,
                     r_kn[b2], r_kf[b2], r_ropeI, r_tmp[b2])
                kb.op("dve", L("tensor_copy", out=kfin[b2][:, 192:256], in_=kfin[b2][:, 128:192]),
                      r=[r_kf[b2]], w=[r_kf[b2]])

            def a_s3(n):
                b2 = n % 2
                bT = 2 * b2
                bZ = 4 + b2
                bK = 6 + b2
                kb.op("pe", L("transpose", out=PH(bK, 0, 128), in_=kfin[b2][:, 0:128], identity=identb[:]),
                      r=[r_kf[b2], r_id], w=[r_pb[bK]])
                kb.op("pe", L("transpose", out=PH(bK, 128, 256), in_=kfin[b2][:, 128:256], identity=identb[:]),
                      r=[r_kf[b2], r_id], w=[r_pb[bK]])
                kb.op("act", L("copy", out=KT[:, n * 128:(n + 1) * 128], in_=PH(bK, 0, 128)),
                      r=[r_pb[bK]], w=[r_KT[n]])
                kb.op("act", L("copy", out=KIT[:, n * 128:(n + 1) * 128], in_=PH(bK, 128, 256)),
                      r=[r_pb[bK]], w=[r_KIT[n]])

            for n0 in range(min(2, ntile_a)):
                a_s1(n0)
            if ntile_a > 0:
                a_s2(0)
            for n in range(ntile_a):
                if n + 2 < ntile_a:
                    a_s1(n + 2)
                if n + 1 < ntile_a:
                    a_s2(n + 1)
                a_s3(n)
            kb.barrier()

        if stage in ("smoke", "A"):
            o_kt = dram("o_kt", [128, SEQ], BF16, kind="ExternalOutput")
            o_v = dram("o_v", [128, NT * 129], BF16, kind="ExternalOutput")
            o_kit = dram("o_kit", [128, SEQ], BF16, kind="ExternalOutput")
            regs = r_KT[:ntile_a] + r_V[:ntile_a] + r_KIT[:ntile_a] + [r_vone]
            ro = Reg("out")
            T_ = ntile_a * 128
            kb.dma("sp", o_kt[:, 0:T_], KT[:, 0:T_], r=regs, w=[ro])
            kb.dma("sp", o_v[:, 0:ntile_a * 129], Vt[:, 0:ntile_a, :].rearrange("p a b -> p (a b)"), r=regs, w=[ro])
            kb.dma("sp", o_kit[:, 0:T_], KIT[:, 0:T_], r=regs, w=[ro])
            kb.finish()
            kb.emit()
            print("instructions:", kb.nins)
            return


        blocks_b2 = list(range(NB))
        if stage == "B2s":
            blocks_b2 = [0, 1]
        if stage in ("B3s", "Ms"):
            blocks_b2 = [0, 1, 2, 3]
        with ExitStack() as l2:
            lsb = mk_sb(l2)
            cmask = lsb("cmask_sb", [128, 512])
            r_cmask = Reg("cmask")
            kb.dma("sp", cmask[:], cmask_d, w=[r_cmask])
            sc = lsb("sc", [128, SEQ])
            r_sc = [Reg(f"sc{i}") for i in range(16)]
            selb = lsb("selb", [128, SEQ], BF16)
            r_sel = Reg("sel")
            selT = lsb("selT", [128, NT, 128], BF16)
            r_selT = [Reg(f"selT{i}") for i in range(4)]
            qTb = [lsb(f"qTb{i}", [128, 16, 128], BF16) for i in range(2)]
            r_qTb = [Reg(f"qTb{i}") for i in range(2)]
            qiTb = [lsb(f"qiTb{i}", [128, 8, 128], BF16) for i in range(2)]
            r_qiTb = [Reg(f"qiTb{i}") for i in range(2)]
            wib = [lsb(f"wib{i}", [128, 16]) for i in range(2)]
            r_wib = [Reg(f"wib{i}") for i in range(2)]
            rl = [lsb(f"rl{i}", [128, 512]) for i in range(2)]
            r_rl = [Reg(f"rl{i}") for i in range(2)]
            Eb = [lsb(f"Eb{i}", [128, 512], BF16) for i in range(2)]
            r_Eb = [Reg(f"Eb{i}") for i in range(2)]
            Pb = [lsb(f"Pb{i}", [128, 4, 128], BF16) for i in range(2)]
            r_Pb = [Reg(f"Pb{i}") for i in range(2)]
            bis = lsb("bis", [128, 8])
            r_bis = Reg("bis")
            bisA = lsb("bisA", [128, 2])
            r_mid, r_cnt, r_cnta = Reg("mid"), Reg("cnt"), Reg("cnta")
            r_selD, r_selA = Reg("selD"), Reg("selA")
            rden = lsb("rden", [128, 16])
            r_rden = Reg("rden")
            Ab = lsb("Ab", [128, 2048], BF16)
            r_Ab = Reg("Ab")
            ATb = lsb("ATb", [128, 2048], BF16)
            r_ATb = Reg("ATb")
            import os
            NJUNK = int(os.environ.get('NJUNK', '1'))
            NIDXB = 3 if NJUNK == 0 else 2
            MASK_ENG = os.environ.get('MASK_ENG', 'dve')
            cnt_ib = [0]
            cnt_ia = [0]

            def load_q(bi, m):
                q2 = bi % 2
                kb.dma("sp", qTb[q2][:].rearrange("p a b -> p (a b)"), s_qT[m], w=[r_qTb[q2]])
                kb.dma("sp", qiTb[q2][:].rearrange("p a b -> p (a b)"), s_qiT[m], w=[r_qiTb[q2]])
                kb.dma("sp", wib[q2][:], s_wi[m], w=[r_wib[q2]])

            def gen_indexer(bi, m):
                q2 = bi % 2
                NCH = m + 1
                steps = [(ch, h) for ch in range(NCH) for h in range(16)]
                ib0 = cnt_ib[0]
                cnt_ib[0] += len(steps)

                def pe_part(k):
                    ch, h = steps[k]
                    bank = 5 + (ib0 + k) % NIDXB
                    hf = h % 2
                    kb.op("pe", L("matmul", out=PF(bank), lhsT=qiTb[q2][hf * 64:(hf + 1) * 64, h // 2, :],
                                  rhs=KIT[hf * 64:(hf + 1) * 64, ch * 512:(ch + 1) * 512], start=True, stop=True),
                          r=[r_qiTb[q2]] + r_KIT[4 * ch:4 * ch + 4], w=[r_pb[bank]])

                def rest_part(k):
                    ch, h = steps[k]
                    bank = 5 + (ib0 + k) % NIDXB
                    i2 = (ib0 + k) % 2
                    scc = sc[:, ch * 512:(ch + 1) * 512]
                    kb.op("act", L("activation", out=rl[i2][:], in_=PF(bank), func=AF.Relu),
                          r=[r_pb[bank]], w=[r_rl[i2]])
                    if h == 0:
                        kb.op("dve", L("tensor_scalar", out=scc, in0=rl[i2][:], scalar1=wib[q2][:, 0:1],
                                       scalar2=None, op0=ALU.mult), r=[r_rl[i2], r_wib[q2]], w=[r_sc[ch]])
                    else:
                        kb.op("dve", L("scalar_tensor_tensor", out=scc, in0=rl[i2][:],
                                       scalar=wib[q2][:, h:h + 1], in1=scc, op0=ALU.mult, op1=ALU.add),
                              r=[r_rl[i2], r_wib[q2], r_sc[ch]], w=[r_sc[ch]])

                pe_part(0)
                for k in range(len(steps)):
                    if k + 1 < len(steps):
                        pe_part(k + 1)
                    rest_part(k)
                    yield

            def topk_and_mask(bi, m):
                NCH = m + 1
                S = 512 * NCH
                NKT = 4 * NCH
                rs = r_sc[0:NCH]
                kb.op("dve", L("tensor_reduce", out=bis[:, 0:1], in_=sc[:, 0:S], axis=AX.X, op=ALU.min),
                      r=rs, w=[r_bis])
                lc = sc[:, S - 512:S]
                kb.op("dve", L("tensor_tensor", out=lc, in0=lc, in1=cmask[:], op=ALU.add),
                      r=[r_sc[NCH - 1], r_cmask], w=[r_sc[NCH - 1]])
                kb.op("dve", L("tensor_reduce", out=bis[:, 1:2], in_=sc[:, 0:S], axis=AX.X, op=ALU.max),
                      r=rs, w=[r_bis])
                kb.op("dve", L("tensor_tensor", out=bis[:, 2:3], in0=bis[:, 1:2], in1=bis[:, 0:1], op=ALU.subtract),
                      r=[r_bis], w=[r_bis])
                kb.op("dve", L("tensor_scalar", out=bis[:, 2:3], in0=bis[:, 2:3], scalar1=1.0001, scalar2=1e-6,
                               op0=ALU.mult, op1=ALU.add), r=[r_bis], w=[r_bis])
                kb.op("dve", L("tensor_copy", out=bis[:, 3:4], in_=bis[:, 0:1]), r=[r_bis], w=[r_bis])
                nd = max(1, int(round(0.45 * NCH)))
                Sd = 512 * nd
                n_act = S - Sd
                rsd = r_sc[0:nd]
                rsa = r_sc[nd:NCH]
                for itb in range(1, NBIS + 1):
                    f = 2.0 ** (-itb)
                    kb.op("dve", L("scalar_tensor_tensor", out=bis[:, 4:5], in0=bis[:, 2:3], scalar=f,
                                   in1=bis[:, 3:4], op0=ALU.mult, op1=ALU.add), r=[r_bis], w=[r_mid])
                    if n_act > 0:
                        kb.op("act", L("activation", out=selb[:, Sd:S], in_=sc[:, Sd:S], func=AF.Sign, scale=-1.0,
                                       bias=bis[:, 4:5], accum_out=bisA[:, 0:1]),
                              r=rsa + [r_mid], w=[r_selA, r_cnta])
                    kb.op("dve", L("tensor_scalar", out=selb[:, 0:Sd], in0=sc[:, 0:Sd], scalar1=bis[:, 4:5],
                                   scalar2=0.0, op0=ALU.is_ge, op1=ALU.add, accum_out=bis[:, 5:6]),
                          r=rsd + [r_mid], w=[r_selD, r_cnt])
                    if n_act > 0:
                        kb.op("dve", L("scalar_tensor_tensor", out=bis[:, 5:6], in0=bisA[:, 0:1], scalar=-0.5,
                                       in1=bis[:, 5:6], op0=ALU.mult, op1=ALU.add), r=[r_cnta, r_cnt], w=[r_cnt])
                    kb.op("dve", L("tensor_scalar", out=bis[:, 6:7], in0=bis[:, 5:6],
                                   scalar1=NSEL - 0.5 - 0.5 * n_act, scalar2=f, op0=ALU.is_ge, op1=ALU.mult),
                          r=[r_cnt], w=[r_bis])
                    kb.op("dve", L("scalar_tensor_tensor", out=bis[:, 3:4], in0=bis[:, 6:7], scalar=bis[:, 2:3],
                                   in1=bis[:, 3:4], op0=ALU.mult, op1=ALU.add), r=[r_bis], w=[r_bis])
                kb.op("dve", L("tensor_scalar", out=selb[:, 0:S], in0=sc[:, 0:S], scalar1=bis[:, 3:4],
                               scalar2=None, op0=ALU.is_ge), r=rs + [r_bis], w=[r_selD, r_selA])
                for g in range((NKT + 15) // 16):
                    n_in = min(16, NKT - 16 * g)
                    bT = 2 * (g % 2)
                    for k2 in range(n_in):
                        kt = 16 * g + k2
                        kb.op("pe", L("transpose", out=PH(bT, k2 * 128, (k2 + 1) * 128),
                                      in_=selb[:, kt * 128:(kt + 1) * 128], identity=identb[:]),
                              r=[r_selD, r_selA, r_id], w=[r_pb[bT + k2 // 8]])
                    kb.op("act", L("copy", out=selT[:, 16 * g:16 * g + n_in, :],
                                   in_=PH(bT, 0, n_in * 128).rearrange("p (a b) -> p a b", a=n_in)),
                          r=[r_pb[bT], r_pb[bT + 1]], w=[r_selT[g]])

            def gen_attention(bi, m):
                q2 = bi % 2
                NCH = m + 1
                NKT = 4 * NCH
                its = [(ps_, kt, hgl) for ps_ in range(2) for kt in range(NKT) for hgl in range(2)]
                ia0 = cnt_ia[0]
                cnt_ia[0] += len(its)

                def issue_qk(idx):
                    ps_, kt, hgl = its[idx]
                    hg = 2 * ps_ + hgl
                    lb = (ia0 + idx) % 2
                    kb.op("pe", L("matmul", out=PF(lb), lhsT=KT[:, kt * 128:(kt + 1) * 128],
                                  rhs=qTb[q2][:, 4 * hg:4 * hg + 4, :], start=True, stop=True),
                          r=[r_KT[kt], r_qTb[q2]], w=[r_pb[lb]])

                def issue_mid(idx):
                    ps_, kt, hgl = its[idx]
                    lb = (ia0 + idx) % 2
                    for _ in range(NJUNK):
                        kb.op("pe", L("matmul", out=PF(7), lhsT=identb[:], rhs=KT[:, 0:512], start=True, stop=True))
                    kb.op("act", L("activation", out=Eb[lb][:], in_=PF(lb), func=AF.Exp, scale=ATTN_SCALE),
                          r=[r_pb[lb]], w=[r_Eb[lb]])
                    kb.op(MASK_ENG, L("tensor_tensor", out=Pb[lb][:],
                                      in0=Eb[lb][:].rearrange("p (h t) -> p h t", h=4),
                                      in1=selT[:, kt, :].unsqueeze(1).broadcast_to([128, 4, 128]), op=ALU.mult),
                          r=[r_Eb[lb], r_selT[kt // 16]], w=[r_Pb[lb]])

                def issue_pv(idx):
                    ps_, kt, hgl = its[idx]
                    lb = (ia0 + idx) % 2
                    for h4 in range(4):
                        h8 = 4 * hgl + h4
                        pbk = 2 + h8 // 3
                        o0 = (h8 % 3) * 129
                        kb.op("pe", L("matmul", out=PF(pbk, o0, o0 + 129), lhsT=Pb[lb][:, h4, :],
                                      rhs=Vt[:, kt, :], start=(kt == 0 and h8 % 3 == 0),
                                      stop=(kt == NKT - 1 and (h8 % 3 == 2 or h8 == 7))),
                              r=[r_Pb[lb], r_V[kt], r_vone], w=[r_pb[pbk]])

                def normalise(ps_):
                    for pbk in range(2, 5):
                        h0 = 3 * (pbk - 2)
                        nh = min(3, 8 - h0)
                        hh = 8 * ps_ + h0
                        pv3 = PF(pbk, 0, nh * 129).rearrange("p (h c) -> p h c", c=129)
                        kb.op("dve", L("reciprocal", out=rden[:, hh:hh + nh].unsqueeze(2), in_=pv3[:, :, 128:129]),
                              r=[r_pb[pbk]], w=[r_rden])
                        kb.op("dve", L("tensor_tensor",
                                       out=Ab[:, hh * 128:(hh + nh) * 128].rearrange("p (h d) -> p h d", h=nh),
                                       in0=pv3[:, :, 0:128],
                                       in1=rden[:, hh:hh + nh].unsqueeze(2).broadcast_to([128, nh, 128]), op=ALU.mult),
                              r=[r_pb[pbk], r_rden], w=[r_Ab])

                issue_qk(0)
                for idx in range(len(its)):
                    if idx + 1 < len(its) and its[idx + 1][0] == its[idx][0]:
                        issue_qk(idx + 1)
                    issue_mid(idx)
                    yield "mid"
                    issue_pv(idx)
                    if idx + 1 < len(its) and its[idx + 1][0] != its[idx][0]:
                        normalise(0)
                        issue_qk(idx + 1)
                    yield "end"
                normalise(1)
                for h in range(16):
                    kb.op("pe", L("transpose", out=PH(0, h * 128, (h + 1) * 128), in_=Ab[:, h * 128:(h + 1) * 128],
                                  identity=identb[:]), r=[r_Ab, r_id], w=[r_pb[h // 8]])
                kb.op("act", L("copy", out=ATb[:], in_=PH(0, 0, 2048)), r=[r_pb[0], r_pb[1]], w=[r_ATb])
                kb.dma("sp", s_AT[m], ATb[:], r=[r_ATb])

            def run_interleaved(gens):
                att, idxg = gens[0], (gens[1] if len(gens) > 1 else None)
                while att is not None:
                    try:
                        tag = next(att)
                    except StopIteration:
                        att = None
                        break
                    if tag == "mid" and idxg is not None:
                        try:
                            next(idxg)
                        except StopIteration:
                            idxg = None
                if idxg is not None:
                    for _ in idxg:
                        pass

            nb2 = len(blocks_b2)
            load_q(0, blocks_b2[0])
            run_interleaved([None, gen_indexer(0, blocks_b2[0])])
            topk_and_mask(0, blocks_b2[0])
            for bi, m in enumerate(blocks_b2):
                nxt_g = None
                if bi + 1 < nb2:
                    load_q(bi + 1, blocks_b2[bi + 1])
                    nxt_g = gen_indexer(bi + 1, blocks_b2[bi + 1])
                run_interleaved([gen_attention(bi, m), nxt_g])
                if bi + 1 < nb2:
                    topk_and_mask(bi + 1, blocks_b2[bi + 1])
            kb.barrier()

    if stage in ("B2", "B2s"):
        kb.finish()
        kb.emit()
        print("instructions:", kb.nins)
        return

    comb_all = sb("comb_all", [128, NB, NE])
    r_comb = Reg("comb")
    kb.op("pool", L("memset", comb_all[:], 0.0), w=[r_comb])
    ng3 = 4 if stage not in ("B3s", "Ms") else 1
    with ExitStack() as l3:
        lsb = mk_sb(l3)
        kb.dma("sp", gbuf[:], g2_d, w=[r_gbuf])
        wrf = lsb("wrf", [128, 16, 36])
        r_wrf = Reg("wrf")
        kb.dma("sp", wrf[:], wr_d.rearrange("(kc p) n -> p kc n", p=128), w=[r_wrf])
        xTm = [lsb(f"xTm{i}", [128, 16, 128], BF16) for i in range(4)]
        r_xTm = [Reg(f"xTm{i}") for i in range(4)]
        sTm = [lsb(f"sTm{i}", [128, 8, 128], BF16) for i in range(4)]
        r_sTm = [Reg(f"sTm{i}") for i in range(4)]
        ATm = [lsb(f"ATm{i}", [128, 16, 128], BF16) for i in range(4)]
        r_ATm = [Reg(f"ATm{i}") for i in range(4)]
        xh = [lsb(f"xh{i}", [128, D]) for i in range(4)]
        r_xh = [Reg(f"xh{i}") for i in range(4)]
        wb3 = [lsb(f"wb3{i}", [128, 16, 512], BF16) for i in range(2)]
        r_wb3 = [Reg(f"wb3{i}") for i in range(2)]
        sgc = [lsb(f"sgc{i}", [128, 512], BF16) for i in range(4)]
        r_sgc = [Reg(f"sgc{i}") for i in range(4)]
        sga = [lsb(f"sga{i}", [128, 512], BF16) for i in range(4)]
        r_sga = [Reg(f"sga{i}") for i in range(4)]
        t1 = [lsb(f"t1{i}", [128, 512]) for i in range(4)]
        r_t1 = [Reg(f"t1{i}") for i in range(4)]
        t2 = [lsb(f"t2{i}", [128, 512]) for i in range(2)]
        r_t2 = [Reg(f"t2{i}") for i in range(2)]
        mix = [lsb(f"mix{i}", [128, D], BF16) for i in range(4)]
        r_mix = [Reg(f"mix{i}") for i in range(4)]
        mixT = [lsb(f"mixT{i}", [128, 16, 128], BF16) for i in range(4)]
        r_mixT = [Reg(f"mixT{i}") for i in range(4)]
        hn2f = lsb("hn2f", [128, D])
        r_hn2f = Reg("hn2f")
        hTf = lsb("hTf", [128, 16, 128])
        r_hTf = Reg("hTf")
        hTb = lsb("hTb", [128, 16, 128], BF16)
        r_hTb = Reg("hTb")
        st3 = lsb("st3", [128, 8])
        r_st3 = Reg("st3")
        lg = lsb("lg", [128, 36])
        rt = lsb("rt", [128, 96])
        r_rt = Reg("rt")
        w3n = [0]
        pq = [0]

        def load_w3(src_ap, nk):
            i = w3n[0] % 2
            w3n[0] += 1
            kb.dma("pool", wb3[i][:, 0:nk, :], src_ap, w=[r_wb3[i]])
            return i

        w_pw_v = w_pw.rearrange("(kc p) n -> p kc n", p=128)
        w_ao_v = w_ao.rearrange("(kc p) n -> p kc n", p=128)
        w_o_v = w_o.rearrange("(kc p) n -> p kc n", p=128)

        def proj(bl, lhs_fn, nk, wi_, regs):
            bank = pq[0] % 4
            pq[0] += 1
            for kc in range(nk):
                kb.op("pe", L("matmul", out=PF(bank), lhsT=lhs_fn(kc), rhs=wb3[wi_][:, kc, :],
                              start=(kc == 0), stop=(kc == nk - 1)), r=regs + [r_wb3[wi_]], w=[r_pb[bank]])
            return bank

        import os
        CUT3 = int(os.environ.get("CUT3", "99"))
        for gi in range(ng3):
            for bl in range(4):
                m = 4 * gi + bl
                kb.dma("sp", xTm[bl][:].rearrange("p a b -> p (a b)"), s_xT[m], w=[r_xTm[bl]])
                kb.dma("sp", sTm[bl][:].rearrange("p a b -> p (a b)"), s_sT[m], w=[r_sTm[bl]])
                kb.dma("sp", ATm[bl][:].rearrange("p a b -> p (a b)"), s_AT[m], w=[r_ATm[bl]])
                kb.dma("sp", xh[bl][:], xo[m, 32:160, :], w=[r_xh[bl]])
            for c in range(4):
                cs = slice(c * 512, (c + 1) * 512)
                wi_ = load_w3(w_in_v[:, :, C_GC + c * 512:C_GC + (c + 1) * 512], 16)
                for bl in range(4):
                    bank = proj(bl, lambda kc: xTm[bl][:, kc, :], 16, wi_, [r_xTm[bl]])
                    kb.op("act", L("activation", out=sgc[bl][:], in_=PF(bank), func=AF.Sigmoid),
                          r=[r_pb[bank]], w=[r_sgc[bl]])
                wi_ = load_w3(w_pw_v[:, :, cs], 8)
                for bl in range(4):
                    bank = proj(bl, lambda kc: sTm[bl][:, kc, :], 8, wi_, [r_sTm[bl]])
                    kb.op("dve", L("tensor_tensor", out=t1[bl][:], in0=PF(bank), in1=sgc[bl][:], op=ALU.mult),
                          r=[r_pb[bank], r_sgc[bl]], w=[r_t1[bl]])
                wi_ = load_w3(w_in_v[:, :, C_GA + c * 512:C_GA + (c + 1) * 512], 16)
                for bl in range(4):
                    bank = proj(bl, lambda kc: xTm[bl][:, kc, :], 16, wi_, [r_xTm[bl]])
                    kb.op("act", L("activation", out=sga[bl][:], in_=PF(bank), func=AF.Sigmoid),
                          r=[r_pb[bank]], w=[r_sga[bl]])
                wi_ = load_w3(w_ao_v[:, :, cs], 16)
                for bl in range(4):
                    bank = proj(bl, lambda kc: ATm[bl][:, kc, :], 16, wi_, [r_ATm[bl]])
                    kb.op("dve", L("tensor_tensor", out=t2[bl % 2][:], in0=PF(bank), in1=sga[bl][:], op=ALU.mult),
                          r=[r_pb[bank], r_sga[bl]], w=[r_t2[bl % 2]])
                    kb.op("dve", L("tensor_tensor", out=mix[bl][:, cs], in0=t1[bl][:], in1=t2[bl % 2][:], op=ALU.add),
                          r=[r_t1[bl], r_t2[bl % 2]], w=[r_mix[bl]])
            if CUT3 <= 1:
                break
            for bl in range(4):
                bT = 4 + 2 * (bl % 2)
                for kc in range(16):
                    kb.op("pe", L("transpose", out=PH(bT, kc * 128, (kc + 1) * 128),
                                  in_=mix[bl][:, kc * 128:(kc + 1) * 128], identity=identb[:]),
                          r=[r_mix[bl], r_id], w=[r_pb[bT + kc // 8]])
                kb.op("act", L("copy", out=mixT[bl][:].rearrange("p a b -> p (a b)"), in_=PH(bT, 0, 2048)),
                      r=[r_pb[bT], r_pb[bT + 1]], w=[r_mixT[bl]])
            for c in range(4):
                cs = slice(c * 512, (c + 1) * 512)
                wi_ = load_w3(w_o_v[:, :, cs], 16)
                for bl in range(4):
                    bank = proj(bl, lambda kc: mixT[bl][:, kc, :], 16, wi_, [r_mixT[bl]])
                    kb.op("dve", L("tensor_tensor", out=xh[bl][:, cs], in0=PF(bank), in1=xh[bl][:, cs], op=ALU.add),
                          r=[r_pb[bank], r_xh[bl]], w=[r_xh[bl]])
            if CUT3 <= 2:
                break
            for bl in range(4):
                m = 4 * gi + bl
                kb.dma("sp", out_d[m], xh[bl][:], r=[r_xh[bl]])
                kb.op("act", L("activation", out=hn2f[:], in_=xh[bl][:], func=AF.Square, accum_out=st3[:, 0:1]),
                      r=[r_xh[bl]], w=[r_hn2f, r_st3])
                rstd_from_ss(st3, 0, D, r_st3)
                kb.op("dve", L("scalar_tensor_tensor", out=hn2f[:], in0=xh[bl][:], scalar=st3[:, 2:3], in1=gbuf[:],
                               op0=ALU.mult, op1=ALU.mult), r=[r_xh[bl], r_st3, r_gbuf], w=[r_hn2f])
                if CUT3 <= 3:
                    continue
                for kc in range(16):
                    kb.op("pe", L("matmul", out=PF(4 + kc // 4, (kc % 4) * 128, (kc % 4 + 1) * 128),
                                  lhsT=hn2f[:, kc * 128:(kc + 1) * 128], rhs=identf[:], start=True, stop=True),
                          r=[r_hn2f, r_id], w=[r_pb[4 + kc // 4]])
                SUB3 = int(os.environ.get("SUB3", "9"))
                if SUB3 <= 1:
                    continue
                for q4 in range(4):
                    kb.op("act", L("copy", out=hTb[:, 4 * q4:4 * q4 + 4, :].rearrange("p a b -> p (a b)"),
                                   in_=PF(4 + q4)), r=[r_pb[4 + q4]], w=[r_hTb])
                    if SUB3 <= 2:
                        continue
                    kb.op("dve", L("tensor_copy", out=hTf[:, 4 * q4:4 * q4 + 4, :].rearrange("p a b -> p (a b)"),
                                   in_=PF(4 + q4)), r=[r_pb[4 + q4]], w=[r_hTf])
                if SUB3 <= 3:
                    continue
                kb.dma("sp", s_hT[m], hTb[:].rearrange("p a b -> p (a b)"), r=[r_hTb])
                if CUT3 <= 4:
                    continue
                bank = pq[0] % 4
                pq[0] += 1
                for kc in range(16):
                    kb.op("pe", L("matmul", out=PF(bank, 0, 36), lhsT=hTf[:, kc, :], rhs=wrf[:, kc, :],
                                  start=(kc == 0), stop=(kc == 15)), r=[r_hTf, r_wrf], w=[r_pb[bank]])
                if CUT3 <= 5:
                    continue
                R = [r_rt]
                kb.op("dve", L("tensor_tensor", out=lg[:], in0=PF(bank, 0, 36), in1=brt, op=ALU.add),
                      r=[r_pb[bank], r_cst], w=R)
                gl = lg[:, 0:4]
                el = lg[:, 4:36].rearrange("p (g e) -> p g e", g=4)
                gmax, ngmax, sumg, pg = rt[:, 0:1], rt[:, 1:2], rt[:, 2:3], rt[:, 3:4]
                ohg, eg = rt[:, 4:8], rt[:, 8:12]
                tmp48 = rt[:, 12:44].rearrange("p (g e) -> p g e", g=4)
                e_in, mx8, oh1, oh2 = rt[:, 44:52], rt[:, 52:60], rt[:, 60:68], rt[:, 68:76]
                dd, ed, w1, w2 = rt[:, 76:77], rt[:, 77:78], rt[:, 78:79], rt[:, 79:80]
                wi1, wpg = rt[:, 80:88], rt[:, 88:96]
                kb.op("dve", L("tensor_reduce", out=gmax, in_=gl, axis=AX.X, op=ALU.max), r=R, w=R)
                kb.op("dve", L("tensor_scalar", out=ohg, in0=gl, scalar1=gmax, scalar2=None, op0=ALU.is_equal),
                      r=R, w=R)
                kb.op("dve", L("tensor_scalar", out=ngmax, in0=gmax, scalar1=-1.0, scalar2=None, op0=ALU.mult),
                      r=R, w=R)
                kb.op("act", L("activation", out=eg, in_=gl, func=AF.Exp, bias=ngmax, accum_out=sumg), r=R, w=R)
                kb.op("dve", L("reciprocal", out=pg, in_=sumg), r=R, w=R)
                kb.op("dve", L("tensor_tensor", out=tmp48, in0=el, in1=ohg.unsqueeze(2).broadcast_to([128, 4, 8]),
                               op=ALU.mult), r=R, w=R)
                kb.op("dve", L("tensor_reduce", out=e_in, in_=tmp48.rearrange("p g e -> p e g"), axis=AX.X,
                               op=ALU.add), r=R, w=R)
                if CUT3 <= 6:
                    continue
                kb.op("dve", L("max", out=mx8, in_=e_in), r=R, w=R)
                if CUT3 <= 7:
                    continue
                kb.op("dve", L("tensor_scalar", out=oh1, in0=e_in, scalar1=mx8[:, 0:1], scalar2=None,
                               op0=ALU.is_equal), r=R, w=R)
                kb.op("dve", L("tensor_scalar", out=oh2, in0=e_in, scalar1=mx8[:, 1:2], scalar2=None,
                               op0=ALU.is_equal), r=R, w=R)
                kb.op("dve", L("tensor_tensor", out=dd, in0=mx8[:, 1:2], in1=mx8[:, 0:1], op=ALU.subtract), r=R, w=R)
                kb.op("act", L("activation", out=ed, in_=dd, func=AF.Exp), r=R, w=R)
                kb.op("dve", L("tensor_scalar", out=w1, in0=ed, scalar1=1.0, scalar2=None, op0=ALU.add), r=R, w=R)
                kb.op("dve", L("reciprocal", out=w1, in_=w1), r=R, w=R)
                kb.op("dve", L("tensor_tensor", out=w2, in0=ed, in1=w1, op=ALU.mult), r=R, w=R)
                kb.op("dve", L("tensor_scalar", out=wi1, in0=oh1, scalar1=w1, scalar2=None, op0=ALU.mult), r=R, w=R)
                kb.op("dve", L("scalar_tensor_tensor", out=wi1, in0=oh2, scalar=w2, in1=wi1, op0=ALU.mult,
                               op1=ALU.add), r=R, w=R)
                kb.op("dve", L("tensor_scalar", out=wpg, in0=wi1, scalar1=pg, scalar2=None, op0=ALU.mult), r=R, w=R)
                kb.op("dve", L("tensor_tensor", out=comb_all[:, m, :].rearrange("p (g e) -> p g e", g=4),
                               in0=ohg.unsqueeze(2).broadcast_to([128, 4, 8]),
                               in1=wpg.unsqueeze(1).broadcast_to([128, 4, 8]), op=ALU.mult), r=R, w=[r_comb])
        if stage in ("B3", "B3s"):
            kb.dma("sp", s_comb, comb_all[:].rearrange("p a b -> p (a b)"), r=[r_comb])
        kb.barrier()

    if stage in ("B3", "B3s"):
        kb.finish()
        kb.emit()
        print("instructions:", kb.nins)
        return

    with ExitStack() as l4:
        lsb = mk_sb(l4)
        hT = lsb("hT", [128, 16, 1024], BF16)
        r_hT = [Reg(f"hT{i}") for i in range(8)]
        acc = lsb("acc", [128, 8, D])
        r_acc = [[Reg(f"acc{i}_{k}") for k in range(2)] for i in range(8)]
        wg = [lsb(f"wg{i}", [128, 16, 256], BF16) for i in range(2)]
        r_wg = [Reg(f"wg{i}") for i in range(2)]
        wu_ = [lsb(f"wup{i}", [128, 16, 256], BF16) for i in range(2)]
        r_wu_ = [Reg(f"wup{i}") for i in range(2)]
        wd = [lsb(f"wd{i}", [128, 4, D], BF16) for i in range(2)]
        r_wd = [Reg(f"wd{i}") for i in range(2)]
        actT = lsb("actT", [128, 4, 1024], BF16)
        r_actT = [Reg(f"actT{i}") for i in range(4)]
        sgm = [lsb(f"sgm{i}", [128, 512], BF16) for i in range(2)]
        r_sgm = [Reg(f"sgm{i}") for i in range(2)]
        npass = 2
        nblk = 8
        ne_run = NE
        if stage == "Ms":
            npass, nblk, ne_run = 1, 4, 2
        ntch = nblk // 4
        hw_n = 0
        gq_n = 0
        dq_n = 0
        for p in range(npass):
            for blk in range(nblk):
                m = 8 * p + blk
                kb.dma("sp", hT[:, :, blk * 128:(blk + 1) * 128], s_hT[m].rearrange("p (a b) -> p a b", a=16),
                       w=[r_hT[blk]])
                kb.dma("sp", acc[:, blk, :], out_d[m], w=r_acc[blk])
            for e in range(ne_run):
                d2 = e % 2
                kb.dma("pool", wd[d2][:], w_ed[e].rearrange("(fc p) n -> p fc n", p=128), w=[r_wd[d2]])
                for half in range(2):
                    h2 = hw_n % 2
                    hw_n += 1
                    kb.dma("pool", wg[h2][:], w_eg[e].rearrange("(kc p) f -> p kc f", p=128)[:, :, half * 256:(half + 1) * 256],
                           w=[r_wg[h2]])
                    kb.dma("pool", wu_[h2][:], w_eu[e].rearrange("(kc p) f -> p kc f", p=128)[:, :, half * 256:(half + 1) * 256],
                           w=[r_wu_[h2]])
                    for fcl in range(2):
                        fc = 2 * half + fcl
                        for tch in range(ntch):
                            g2_ = gq_n % 2
                            gq_n += 1
                            bG, bU = g2_, 2 + g2_
                            for kc in range(16):
                                kb.op("pe", L("matmul", out=PF(bG), lhsT=wg[h2][:, kc, fcl * 128:(fcl + 1) * 128],
                                              rhs=hT[:, kc, tch * 512:(tch + 1) * 512], start=(kc == 0), stop=(kc == 15)),
                                      r=[r_wg[h2]] + r_hT[4 * tch:4 * tch + 4], w=[r_pb[bG]])
                            for kc in range(16):
                                kb.op("pe", L("matmul", out=PF(bU), lhsT=wu_[h2][:, kc, fcl * 128:(fcl + 1) * 128],
                                              rhs=hT[:, kc, tch * 512:(tch + 1) * 512], start=(kc == 0), stop=(kc == 15)),
                                      r=[r_wu_[h2]] + r_hT[4 * tch:4 * tch + 4], w=[r_pb[bU]])
                            kb.op("act", L("activation", out=sgm[g2_][:], in_=PF(bG), func=AF.Silu),
                                  r=[r_pb[bG]], w=[r_sgm[g2_]])
                            kb.op("dve", L("tensor_tensor", out=actT[:, fc, tch * 512:(tch + 1) * 512], in0=PF(bU),
                                           in1=sgm[g2_][:], op=ALU.mult), r=[r_pb[bU], r_sgm[g2_]], w=[r_actT[fc]])
                for blk in range(nblk):
                    m = 8 * p + blk
                    for nh in range(2):
                        bD = 4 + 2 * (dq_n % 2)
                        dq_n += 1
                        for sub in range(2):
                            c0 = nh * 1024 + sub * 512
                            for fc in range(4):
                                kb.op("pe", L("matmul", out=PF(bD + sub), lhsT=actT[:, fc, blk * 128:(blk + 1) * 128],
                                              rhs=wd[d2][:, fc, c0:c0 + 512], start=(fc == 0), stop=(fc == 3)),
                                      r=[r_actT[fc], r_wd[d2]], w=[r_pb[bD + sub]])
                        av = acc[:, blk, nh * 1024:(nh + 1) * 1024]
                        kb.op("dve", L("scalar_tensor_tensor", out=av, in0=PF(bD, 0, 1024),
                                       scalar=comb_all[:, m, e:e + 1], in1=av, op0=ALU.mult, op1=ALU.add),
                              r=[r_pb[bD], r_pb[bD + 1], r_comb, r_acc[blk][nh]], w=[r_acc[blk][nh]])
            for blk in range(nblk):
                m = 8 * p + blk
                kb.dma("sp", out_d[m], acc[:, blk, :], r=r_acc[blk])
        kb.barrier()
    kb.finish()
    kb.emit()
    print("instructions:", kb.nins)
    return

    raise NotImplementedError


def host_consts(inputs):
    theta = 500000.0
    invA = theta ** (-np.arange(0, 32, 2, dtype=np.float32) / np.float32(32))
    invI = theta ** (-np.arange(0, 16, 2, dtype=np.float32) / np.float32(16))
    c = np.zeros((128, 512), np.float32)
    c[:, 0:128] = np.asarray(inputs["q_norm_g"]).reshape(1, 128)
    c[:, 128:256] = np.asarray(inputs["k_norm_g"]).reshape(1, 128)
    c[:, 256:272] = invA.astype(np.float32)[None, :]
    c[:, 272:280] = invI.astype(np.float32)[None, :]
    c[:, 288:292] = np.asarray(inputs["b_router_group"]).reshape(1, 4)
    c[:, 292:324] = np.asarray(inputs["b_router_expert"]).reshape(1, 32)
    return c


def make_in_maps(inputs, stage="full"):
    x = np.asarray(inputs["x"])
    pos = np.asarray(inputs["positions"])
    consts = host_consts(inputs)
    ident = np.eye(128, dtype=np.float32)
    w_in = np.ascontiguousarray(np.asarray(inputs["w_in"])[0])
    gA = np.ascontiguousarray(np.broadcast_to(np.asarray(inputs["attn_norm_g"]).reshape(1, D), (128, D)))
    g2 = np.ascontiguousarray(np.broadcast_to(np.asarray(inputs["ffn_norm_g"]).reshape(1, D), (128, D)))
    cv = np.zeros((128, 8, 34), np.float32)
    dw = np.asarray(inputs["conv_dw_w"])[0]
    cv[:, :, 0:31] = dw.T.reshape(8, 128, 31).transpose(1, 0, 2)
    cv[:, :, 31] = np.asarray(inputs["conv_dw_b"])[0].reshape(8, 128).T
    cv[:, :, 32] = np.asarray(inputs["conv_ln_g"])[0].reshape(8, 128).T
    cv[:, :, 33] = np.asarray(inputs["conv_ln_b"])[0].reshape(8, 128).T
    wr = np.ascontiguousarray(np.concatenate([np.asarray(inputs["w_router_group"])[0],
                                              np.asarray(inputs["w_router_expert"])[0]], axis=1))
    shared = {
        "consts": consts, "gA": gA, "g2": g2, "w_in": w_in, "ident": ident,
        "cvec": np.ascontiguousarray(cv.reshape(128, 8 * 34)),
        "w_conv_out": np.ascontiguousarray(np.asarray(inputs["w_conv_out"])[0]),
        "w_attn_o": np.ascontiguousarray(np.asarray(inputs["w_attn_o"])[0]),
        "w_out": np.ascontiguousarray(np.asarray(inputs["w_out"])[0]),
        "wr": wr,
        "w_exp_gate": np.ascontiguousarray(np.asarray(inputs["w_exp_gate"])[0]),
        "w_exp_up": np.ascontiguousarray(np.asarray(inputs["w_exp_up"])[0]),
        "w_exp_down": np.ascontiguousarray(np.asarray(inputs["w_exp_down"])[0]),
    }
    maps = []
    for c in range(8):
        b, j = c // 4, c % 4
        xpad = np.concatenate([np.zeros((32, D), np.float32), x[b]], axis=0)
        xo_ = np.stack([xpad[128 * (4 * m + j):128 * (4 * m + j) + 160] for m in range(NB)])
        pos_o = np.stack([pos[b, 128 * (4 * m + j):128 * (4 * m + j) + 128] for m in range(NB)], axis=1)
        cidx = np.arange(512)[None, :]
        prow = np.arange(128)[:, None]
        cmask = np.where(cidx <= 128 * j + prow, 0.0, NEG).astype(np.float32)
        m_ = dict(shared)
        m_.update({
            "xb": np.ascontiguousarray(x[b]),
            "pos_t": np.ascontiguousarray(pos[b].reshape(NT, 128).T),
            "pos_o": np.ascontiguousarray(pos_o.astype(np.int32)),
            "xo": np.ascontiguousarray(xo_),
            "cmask": cmask,
        })
        maps.append(m_)
    return maps


def kernel(**inputs):
    nc = build_nc("full")
    maps = make_in_maps(inputs)
    res = run_bass_kernel_spmd(nc, maps, core_ids=list(range(8)))
    out = np.zeros((2, SEQ, D), np.float32)
    for c in range(8):
        b, j = c // 4, c % 4
        o = np.asarray(res.results[c]["out"])
        for m in range(NB):
            i = 4 * m + j
            out[b, 128 * i:128 * (i + 1)] = o[m]
    return out
```

```python
import math
from contextlib import ExitStack

import numpy as np
import concourse.bass as bass
import concourse.mybir as mybir
from concourse.bass_utils import run_bass_kernel_spmd

F32 = mybir.dt.float32
BF16 = mybir.dt.bfloat16
I32 = mybir.dt.int32
AF = mybir.ActivationFunctionType
ALU = mybir.AluOpType
AX = mybir.AxisListType

D = 2048
SEQ = 8192
NT = SEQ // 128
NB = 16
EPS = 1e-6
IN_TOTAL = 9552
C_U, C_Q, C_K, C_V, C_QI, C_KI, C_WI, C_GC, C_GA = 0, 2048, 4096, 4224, 4352, 5376, 5440, 5456, 7504
NEG = -1.0e30
EPOCH = 30000
NSLOT = 8
NSEL = 256
NBIS = 16
ATTN_SCALE = 128 ** -0.5
NE = 32
FF = 512


def L(name, *a, **k):
    return lambda e: getattr(e, name)(*a, **k)


class Reg:
    __slots__ = ("w", "r", "name", "psum")

    def __init__(self, name="", psum=False):
        self.w = None
        self.r = []
        self.name = name
        self.psum = psum


class KB:
    def __init__(self, nc, es):
        self.nc = nc
        self.es = es
        self.names = ("pe", "act", "dve", "pool", "sp")
        self.sem = {}
        self.cnt = {e: 0 for e in ("pe", "act", "dve", "pool")}
        self.seen = {e: {} for e in self.names}
        self.dn = {q: 0 for q in ("sp", "act", "pool")}
        for q in self.dn:
            for i in range(NSLOT):
                self.sem[("d", q, i)] = es.enter_context(nc.semaphore(f"d_{q}{i}"))
        self.nins = 0
        self.prog = {e: [] for e in self.names}

    def emit(self):
        with self.nc.Block() as block:
            def run(en):
                def body(e):
                    for it in self.prog[en]:
                        if it[0] == "w":
                            e.wait_ge(it[1], it[2])
                        elif it[0] == "i":
                            it[1](e).then_inc(it[2], 1)
                        else:
                            e.dma_start(out=it[1], in_=it[2], **it[4]).then_inc(it[3], 16)
                return body
            block.sync(run("sp"))
            block.tensor(run("pe"))
            block.scalar(run("act"))
            block.vector(run("dve"))
            block.gpsimd(run("pool"))

    def _semfor(self, key):
        if key not in self.sem:
            self.sem[key] = self.es.enter_context(self.nc.semaphore(f"s_{key[0]}{key[1]}"))
        return self.sem[key]

    def _wait(self, en, evs):
        best = {}
        for k, v in evs:
            if best.get(k, 0) < v:
                best[k] = v
        for k, v in best.items():
            if self.seen[en].get(k, 0) < v:
                self.prog[en].append(("w", self._semfor(k), v))
                self.seen[en][k] = v

    def _deps(self, en, r, w, is_dma=False):
        evs = []
        for t in r:
            if t.w is not None:
                if not (en == "pe" and t.w[0][0] == "pe"):
                    evs.append(t.w)
            if t.psum:
                for ev in t.r:
                    if ev[0][0] != en:
                        evs.append(ev)
        for t in w:
            if t.w is not None:
                if not (en == "pe" and t.w[0][0] == "pe"):
                    evs.append(t.w)
            for ev in t.r:
                if en == "pe" and ev[0][0] == "pe":
                    continue
                evs.append(ev)
        return evs

    def _commit(self, ev, r, w):
        for t in r:
            t.r.append(ev)
            if len(t.r) > 48:
                best = {}
                for k, v in t.r:
                    if best.get(k, 0) < v:
                        best[k] = v
                t.r = list(best.items())
        for t in w:
            t.w = ev
            t.r = []

    def op(self, en, fn, r=(), w=()):
        self._wait(en, self._deps(en, r, w))
        c = self.cnt[en]
        key = (en, c // EPOCH)
        val = c % EPOCH + 1
        self.prog[en].append(("i", fn, self._semfor(key)))
        self.cnt[en] = c + 1
        self._commit((key, val), r, w)
        self.nins += 1

    def dma(self, q, out, in_, r=(), w=(), **kw):
        i = self.dn[q]
        slot, use = i % NSLOT, i // NSLOT
        key = ("d", q, slot)
        evs = self._deps(q, r, w, is_dma=True)
        if use > 0:
            evs.append((key, 16 * use))
        self._wait(q, evs)
        self.prog[q].append(("d", out, in_, self.sem[key], kw))
        self.dn[q] = i + 1
        self._commit((key, 16 * (use + 1)), r, w)
        self.nins += 1

    def all_events(self):
        evs = []
        for en, c in self.cnt.items():
            if c > 0:
                evs.append(((en, (c - 1) // EPOCH), (c - 1) % EPOCH + 1))
        for q, i in self.dn.items():
            for back in range(1, min(i, NSLOT) + 1):
                ii = i - back
                evs.append((("d", q, ii % NSLOT), 16 * (ii // NSLOT + 1)))
        return evs

    def barrier(self):
        evs = self.all_events()
        for en in self.names:
            self._wait(en, evs)

    def finish(self):
        self._wait("sp", self.all_events())


def build_nc(stage="full"):
    nc = bass.Bass("TRN2", target_bir_lowering=False)
    es = ExitStack()
    with es:
        _build(nc, es, stage)
    return nc


def _build(nc, es, stage):
    kb = KB(nc, es)
    dbg = stage != "full"

    def dram(name, shape, dt=F32, kind="ExternalInput"):
        return nc.dram_tensor(name, list(shape), dt, kind=kind).ap()

    def scratch(name, shape, dt, want_dbg):
        kind = "ExternalOutput" if (dbg and want_dbg) else "Internal"
        return nc.dram_tensor(name, list(shape), dt, kind=kind).ap()

    def mk_sb(stack):
        def sb(name, shape, dt=F32):
            return stack.enter_context(nc.sbuf_tensor(name, list(shape), dt))
        return sb

    sb = mk_sb(es)

    xb = dram("xb", [SEQ, D])
    pos_t = dram("pos_t", [128, NT], I32)
    pos_o = dram("pos_o", [128, NB], I32)
    consts = dram("consts", [128, 512])
    gA_d = dram("gA", [128, D])
    g2_d = dram("g2", [128, D])
    w_in = dram("w_in", [D, IN_TOTAL])
    ident_d = dram("ident", [128, 128])
    xo = dram("xo", [NB, 160, D])
    cmask_d = dram("cmask", [128, 512])
    cvec_d = dram("cvec", [128, 8 * 34])
    w_pw = dram("w_conv_out", [1024, D])
    w_ao = dram("w_attn_o", [D, D])
    w_o = dram("w_out", [D, D])
    wr_d = dram("wr", [D, 36])
    w_eg = dram("w_exp_gate", [NE, D, FF])
    w_eu = dram("w_exp_up", [NE, D, FF])
    w_ed = dram("w_exp_down", [NE, FF, D])
    out_d = dram("out", [NB, 128, D], kind="ExternalOutput")

    s_qT = scratch("s_qT", [NB, 128, 2048], BF16, stage.startswith("B1"))
    s_qiT = scratch("s_qiT", [NB, 128, 1024], BF16, stage.startswith("B1"))
    s_wi = scratch("s_wi", [NB, 128, 16], F32, stage.startswith("B1"))
    s_sT = scratch("s_sT", [NB, 128, 1024], BF16, stage.startswith("B1"))
    s_xT = scratch("s_xT", [NB, 128, 2048], BF16, stage.startswith("B1"))
    s_AT = scratch("s_AT", [NB, 128, 2048], BF16, stage in ("B2", "B2s"))
    s_hT = scratch("s_hT", [NB, 128, 2048], BF16, stage in ("B3", "B3s"))
    s_comb = scratch("s_comb", [128, NB * NE], F32, stage in ("B3", "B3s"))

    w_in_v = w_in.rearrange("(kc p) n -> p kc n", p=128)

    pall = es.enter_context(nc.psum_tensor("pall", [128, 4096], F32))
    pallh = pall.bitcast(BF16)
    r_pb = [Reg(f"pb{i}", psum=True) for i in range(8)]

    def PF(bank, c0=0, c1=512):
        return pall[:, bank * 512 + c0: bank * 512 + c1]

    def PH(bank, c0=0, c1=1024):
        return pallh[:, bank * 1024 + c0: bank * 1024 + c1]

    cst = sb("cst", [128, 512])
    r_cst = Reg("cst")
    kb.dma("sp", cst[:], consts, w=[r_cst])
    gqs = cst[:, 0:128]
    gk = cst[:, 128:256]
    invA = cst[:, 256:272]
    invI = cst[:, 272:280]
    brt = cst[:, 288:324]
    identf = sb("identf", [128, 128])
    identb = sb("identb", [128, 128], BF16)
    r_id = Reg("ident")
    kb.dma("sp", identf[:], ident_d, w=[r_id])
    kb.op("dve", L("tensor_copy", out=identb[:], in_=identf[:]), r=[r_id], w=[r_id])
    epst = sb("epst", [128, 1])
    r_eps = Reg("eps")
    kb.op("dve", L("memset", epst[:], EPS), w=[r_eps])
    gbuf = sb("gbuf", [128, D])
    r_gbuf = Reg("gbuf")

    def rope_tables(cos_t, sin_t, name, posf_ap, ncol, inv_ap, half, r_in, tmp_stack):
        tsb = mk_sb(tmp_stack)
        u = tsb(name + "_u", [128, ncol, half])
        ki = tsb(name + "_ki", [128, ncol, half], I32)
        kf = tsb(name + "_kf", [128, ncol, half])
        d = tsb(name + "_d", [128, ncol, half])
        m1 = tsb(name + "_m1", [128, ncol, half])
        rr = Reg(name)
        kb.op("dve", L("tensor_tensor", out=u[:], in0=posf_ap.unsqueeze(2).broadcast_to([128, ncol, half]),
                       in1=inv_ap.unsqueeze(1).broadcast_to([128, ncol, half]), op=ALU.mult),
              r=[r_in, r_cst], w=[rr])
        kb.op("dve", L("tensor_scalar", out=u[:], in0=u[:], scalar1=1.0 / (2 * math.pi), scalar2=None,
                       op0=ALU.mult), r=[rr], w=[rr])
        for shift, dst in ((0.0, sin_t), (0.25, cos_t)):
            src = u
            if shift != 0.0:
                kb.op("dve", L("tensor_scalar", out=d[:], in0=u[:], scalar1=shift, scalar2=None, op0=ALU.add),
                      r=[rr], w=[rr])
                src = d
            kb.op("dve", L("tensor_copy", out=ki[:], in_=src[:]), r=[rr], w=[rr])
            kb.op("dve", L("tensor_copy", out=kf[:], in_=ki[:]), r=[rr], w=[rr])
            kb.op("dve", L("tensor_tensor", out=d[:], in0=src[:], in1=kf[:], op=ALU.subtract), r=[rr], w=[rr])
            kb.op("dve", L("tensor_scalar", out=m1[:], in0=d[:], scalar1=0.5, scalar2=None, op0=ALU.is_gt),
                  r=[rr], w=[rr])
            kb.op("dve", L("tensor_tensor", out=d[:], in0=d[:], in1=m1[:], op=ALU.subtract), r=[rr], w=[rr])
            kb.op("dve", L("tensor_scalar", out=m1[:], in0=d[:], scalar1=-0.5, scalar2=None, op0=ALU.is_lt),
                  r=[rr], w=[rr])
            kb.op("dve", L("tensor_tensor", out=d[:], in0=d[:], in1=m1[:], op=ALU.add), r=[rr], w=[rr])
            kb.op("act", L("activation", out=dst[:], in_=d[:], func=AF.Sin, scale=2 * math.pi * (1 - 1e-6)),
                  r=[rr], w=[rr])
        return cos_t, sin_t, rr

    def rope(dst, src, half, cos_ap, sin_ap, t4, r_src, r_dst, r_tab, r_t4):
        def sl(ap, a, b):
            return ap[:, a:b] if len(ap.shape) == 2 else ap[:, :, a:b]
        x1, x2 = sl(src, 0, half), sl(src, half, 2 * half)
        ts = [t4[i] for i in range(4)]
        kb.op("dve", L("tensor_tensor", out=ts[0], in0=x1, in1=cos_ap, op=ALU.mult), r=[r_src, r_tab], w=[r_t4])
        kb.op("dve", L("tensor_tensor", out=ts[1], in0=x2, in1=sin_ap, op=ALU.mult), r=[r_src, r_tab], w=[r_t4])
        kb.op("dve", L("tensor_tensor", out=ts[2], in0=x1, in1=sin_ap, op=ALU.mult), r=[r_src, r_tab], w=[r_t4])
        kb.op("dve", L("tensor_tensor", out=ts[3], in0=x2, in1=cos_ap, op=ALU.mult), r=[r_src, r_tab], w=[r_t4])
        kb.op("dve", L("tensor_tensor", out=sl(dst, 0, half), in0=ts[0], in1=ts[1], op=ALU.subtract),
              r=[r_t4], w=[r_dst])
        kb.op("dve", L("tensor_tensor", out=sl(dst, half, 2 * half), in0=ts[2], in1=ts[3], op=ALU.add),
              r=[r_t4], w=[r_dst])

    def rstd_from_ss(st, c_ss, n, r_st):
        kb.op("act", L("activation", out=st[:, c_ss + 1:c_ss + 2], in_=st[:, c_ss:c_ss + 1], func=AF.Sqrt,
                       scale=1.0 / n, bias=epst[0:st.shape[0], :]), r=[r_st, r_eps], w=[r_st])
        kb.op("dve", L("reciprocal", out=st[:, c_ss + 2:c_ss + 3], in_=st[:, c_ss + 1:c_ss + 2]),
              r=[r_st], w=[r_st])

    posoi = sb("posoi", [128, NB], I32)
    posof = sb("posof", [128, NB])
    r_poso = Reg("poso")
    kb.dma("sp", posoi[:], pos_o, w=[r_poso])
    kb.op("dve", L("tensor_copy", out=posof[:], in_=posoi[:]), r=[r_poso], w=[r_poso])
    cosAo, sinAo = sb("rao_cos", [128, NB, 16]), sb("rao_sin", [128, NB, 16])
    cosIo, sinIo = sb("rio_cos", [128, NB, 8]), sb("rio_sin", [128, NB, 8])
    with ExitStack() as tmps:
        _, _, r_ropeAo = rope_tables(cosAo, sinAo, "rao", posof[:], NB, invA, 16, r_poso, tmps)
        _, _, r_ropeIo = rope_tables(cosIo, sinIo, "rio", posof[:], NB, invI, 8, r_poso, tmps)
        kb.barrier()

    ngroups = 4 if stage not in ("B1s", "B2s", "B3s", "Ms") and not stage.startswith("B1c") else 1
    cut = int(stage[3:]) if stage.startswith("B1c") else 99
    if stage in ("smoke", "A"):
        ngroups = 0
    with ExitStack() as lb:
        lsb = mk_sb(lb)
        kb.dma("sp", gbuf[:], gA_d, w=[r_gbuf])
        cw = lsb("cw", [128, 8, 34])
        r_cw = Reg("cw")
        kb.dma("sp", cw[:].rearrange("p a b -> p (a b)"), cvec_d, w=[r_cw])
        onesf = lsb("onesf", [128, 128])
        r_ones = Reg("ones")
        kb.op("dve", L("memset", onesf[:], 1.0 / 1024.0), w=[r_ones])
        xT_g = lsb("xT_g", [128, 16, 640], BF16)
        r_xTg = [Reg(f"xTg{i}") for i in range(4)]
        xm = [lsb(f"xm{i}", [128, D]) for i in range(2)]
        r_xm = [Reg(f"xm{i}") for i in range(2)]
        xhl1 = lsb("xhl", [32, D])
        xhl = [xhl1, xhl1]
        r_xhl1 = Reg("xhl")
        r_xhl = [r_xhl1, r_xhl1]
        xs = [lsb(f"xsb{i}", [128, D], BF16) for i in range(2)]
        r_xs = [Reg(f"xsb{i}") for i in range(2)]
        xsh1 = lsb("xsh", [32, D], BF16)
        xsh = [xsh1, xsh1]
        r_xsh1 = Reg("xsh")
        r_xsh = [r_xsh1, r_xsh1]
        st = [lsb(f"stb{i}", [128, 8]) for i in range(2)]
        r_st = [Reg(f"stb{i}") for i in range(2)]
        yT = lsb("yT", [128, 8, 640], BF16)
        r_yT = [Reg(f"yT{i}") for i in range(8)]
        ysq = lsb("ysq", [128, 8, 512])
        r_ysq = Reg("ysq")
        dg = [lsb(f"dg{i}", [128, 128], BF16) for i in range(4)]
        r_dg = [Reg(f"dg{i}") for i in range(4)]
        cacc = lsb("cacc", [128, 8, 512])
        r_cacc = [Reg(f"cacc{i}") for i in range(8)]
        sT = lsb("sT", [128, 8, 512], BF16)
        r_sT = Reg("sT")
        wu = [lsb(f"wu{i}", [128, 16, 256], BF16) for i in range(2)]
        r_wu = [Reg(f"wu{i}") for i in range(2)]
        wbuf = [lsb(f"wbuf{i}", [128, 16, 512], BF16) for i in range(2)]
        r_wbuf = [Reg(f"wbuf{i}") for i in range(2)]
        sg = [lsb(f"sg{i}", [128, 640]) for i in range(2)]
        r_sg = [Reg(f"sg{i}") for i in range(2)]
        lnm = lsb("lnm", [128, 3, 512])
        r_lnm = Reg("lnm")
        qsq = lsb("qsq", [128, 512])
        r_qsq = Reg("qsq")
        stq = [lsb(f"stq{i}", [128, 12]) for i in range(2)]
        r_stq = [Reg(f"stq{i}") for i in range(2)]
        qn = [lsb(f"qn{i}", [128, 4, 128]) for i in range(2)]
        r_qn = [Reg(f"qn{i}") for i in range(2)]
        qb = [lsb(f"qb{i}", [128, 512], BF16) for i in range(2)]
        r_qb = [Reg(f"qb{i}") for i in range(2)]
        t4q = [lsb(f"t4q{i}", [128, 4, 8, 16]) for i in range(2)]
        r_t4q = [Reg(f"t4q{i}") for i in range(2)]
        qTs = [lsb(f"qTs{i}", [128, 4, 128], BF16) for i in range(2)]
        r_qTs = [Reg(f"qTs{i}") for i in range(2)]
        wis = lsb("wis", [128, 4, 16])
        r_wis = Reg("wis")
        wwi = lsb("wwi", [128, 16, 16], BF16)
        r_wwi = Reg("wwi")
        kb.dma("pool", wwi[:], w_in_v[:, :, C_WI:C_WI + 16], w=[r_wwi])

        wu_n = [0]
        wb_n = [0]

        def load_wbuf(c0):
            i = wb_n[0] % 2
            wb_n[0] += 1
            kb.dma("pool", wbuf[i][:], w_in_v[:, :, c0:c0 + 512], w=[r_wbuf[i]])
            return i

        for gi in range(ngroups):
            for bl in range(4):
                m = 4 * gi + bl
                b2 = bl % 2
                bT = 2 * b2
                kb.dma("sp", xm[b2][:], xo[m, 32:160, :], w=[r_xm[b2]])
                kb.dma("sp", xhl[b2][:], xo[m, 0:32, :], w=[r_xhl[b2]])
                kb.op("act", L("activation", out=xs[b2][:], in_=xm[b2][:], func=AF.Square,
                               accum_out=st[b2][:, 0:1]), r=[r_xm[b2]], w=[r_xs[b2], r_st[b2]])
                rstd_from_ss(st[b2], 0, D, r_st[b2])
                kb.op("dve", L("scalar_tensor_tensor", out=xs[b2][:], in0=xm[b2][:], scalar=st[b2][:, 2:3],
                               in1=gbuf[:], op0=ALU.mult, op1=ALU.mult),
                      r=[r_xm[b2], r_st[b2], r_gbuf], w=[r_xs[b2]])
                for kc in range(16):
                    kb.op("pe", L("transpose", out=PH(bT, kc * 128, (kc + 1) * 128),
                                  in_=xs[b2][:, kc * 128:(kc + 1) * 128], identity=identb[:]),
                          r=[r_xs[b2], r_id], w=[r_pb[bT + kc // 8]])
                kb.op("act", L("copy", out=xT_g[:, :, bl * 128:(bl + 1) * 128],
                               in_=PH(bT, 0, 2048).rearrange("p (a b) -> p a b", a=16)),
                      r=[r_pb[bT], r_pb[bT + 1]], w=[r_xTg[bl]])
                kb.dma("sp", s_xT[m].rearrange("p (a b) -> p a b", a=16), xT_g[:, :, bl * 128:(bl + 1) * 128],
                       r=[r_xTg[bl]])
                bH = 4 + b2
                kb.op("act", L("activation", out=xsh[b2][:], in_=xhl[b2][:], func=AF.Square,
                               accum_out=st[b2][0:32, 3:4]), r=[r_xhl[b2]], w=[r_xsh[b2], r_st[b2]])
                rstd_from_ss(st[b2][0:32, :], 3, D, r_st[b2])
                kb.op("dve", L("scalar_tensor_tensor", out=xsh[b2][:], in0=xhl[b2][:], scalar=st[b2][0:32, 5:6],
                               in1=gbuf[0:32, :], op0=ALU.mult, op1=ALU.mult),
                      r=[r_xhl[b2], r_st[b2], r_gbuf], w=[r_xsh[b2]])
                for kc in range(16):
                    kb.op("pe", L("transpose", out=PH(bH, kc * 32, (kc + 1) * 32),
                                  in_=xsh[b2][:, kc * 128:(kc + 1) * 128], identity=identb[0:32, 0:32]),
                          r=[r_xsh[b2], r_id], w=[r_pb[bH]])
                kb.op("act", L("copy", out=xT_g[:, :, 512 + bl * 32:512 + (bl + 1) * 32],
                               in_=PH(bH, 0, 512).rearrange("p (a b) -> p a b", a=16)),
                      r=[r_pb[bH]], w=[r_xTg[bl]])

            if cut <= 1:
                break
            for cc in range(8):
                i = wu_n[0] % 2
                wu_n[0] += 1
                kb.dma("pool", wu[i][:, :, 0:128], w_in_v[:, :, C_U + cc * 128:C_U + (cc + 1) * 128], w=[r_wu[i]])
                kb.dma("pool", wu[i][:, :, 128:256], w_in_v[:, :, C_U + 1024 + cc * 128:C_U + 1024 + (cc + 1) * 128],
                       w=[r_wu[i]])
                b3 = 3 * (cc % 2)
                bA, bG, bHh = b3, b3 + 1, b3 + 2
                for (bank, c0, c1, wc, x0, x1) in ((bA, 0, 512, 0, 0, 512), (bG, 0, 512, 128, 0, 512),
                                                   (bHh, 0, 128, 0, 512, 640), (bHh, 128, 256, 128, 512, 640)):
                    for kc in range(16):
                        kb.op("pe", L("matmul", out=PF(bank, c0, c1), lhsT=wu[i][:, kc, wc:wc + 128],
                                      rhs=xT_g[:, kc, x0:x1], start=(kc == 0), stop=(kc == 15)),
                              r=[r_wu[i]] + r_xTg, w=[r_pb[bank]])
                s2 = cc % 2
                kb.op("act", L("activation", out=sg[s2][:, 0:512], in_=PF(bG), func=AF.Sigmoid),
                      r=[r_pb[bG]], w=[r_sg[s2]])
                kb.op("act", L("activation", out=sg[s2][:, 512:640], in_=PF(bHh, 128, 256), func=AF.Sigmoid),
                      r=[r_pb[bHh]], w=[r_sg[s2]])
                yv = yT[:, cc, :].rearrange("p (b t) -> p b t", b=4)
                kb.op("dve", L("tensor_tensor", out=yv[:, :, 32:160],
                               in0=PF(bA).rearrange("p (b t) -> p b t", b=4),
                               in1=sg[s2][:, 0:512].rearrange("p (b t) -> p b t", b=4), op=ALU.mult),
                      r=[r_pb[bA], r_sg[s2]], w=[r_yT[cc]])
                kb.op("dve", L("tensor_tensor", out=yv[:, :, 0:32],
                               in0=PF(bHh, 0, 128).rearrange("p (b t) -> p b t", b=4),
                               in1=sg[s2][:, 512:640].rearrange("p (b t) -> p b t", b=4), op=ALU.mult),
                      r=[r_pb[bHh], r_sg[s2]], w=[r_yT[cc]])

            if cut <= 2:
                break
            def gen_conv():
                ndg = 0
                for cc in range(8):
                    yv = yT[:, cc, :].rearrange("p (b t) -> p b t", b=4)
                    bC = 4 + cc % 2
                    for k in range(31):
                        di = ndg % 4
                        ndg += 1
                        kb.op("dve", L("tensor_scalar", out=dg[di][:], in0=identb[:], scalar1=cw[:, cc, k:k + 1],
                                        scalar2=None, op0=ALU.mult), r=[r_id, r_cw], w=[r_dg[di]])
                        kb.op("pe", L("matmul", out=PF(bC), lhsT=dg[di][:], rhs=yv[:, :, 2 + k:130 + k],
                                      start=(k == 0), stop=(k == 30)), r=[r_dg[di], r_yT[cc]], w=[r_pb[bC]])
                        if k % 4 == 3:
                            yield
                    kb.op("act", L("activation", out=cacc[:, cc, :], in_=PF(bC), func=AF.Identity,
                                   bias=cw[:, cc, 31:32]), r=[r_pb[bC], r_cw], w=[r_cacc[cc]])
                    yield
                bM, bS = 6, 7
                for cc in range(8):
                    kb.op("pe", L("matmul", out=PF(bM), lhsT=onesf[:], rhs=cacc[:, cc, :], start=(cc == 0),
                                  stop=(cc == 7)), r=[r_ones, r_cacc[cc]], w=[r_pb[bM]])
                sqv = ysq
                kb.op("act", L("activation", out=ysq[:], in_=cacc[:], func=AF.Square), r=r_cacc, w=[r_ysq])
                for cc in range(8):
                    kb.op("pe", L("matmul", out=PF(bS), lhsT=onesf[:], rhs=ysq[:, cc, :], start=(cc == 0),
                                  stop=(cc == 7)), r=[r_ones, r_ysq], w=[r_pb[bS]])
                kb.op("act", L("copy", out=lnm[:, 0, :], in_=PF(bM)), r=[r_pb[bM]], w=[r_lnm])
                kb.op("dve", L("tensor_tensor", out=lnm[:, 1, :], in0=lnm[:, 0, :], in1=lnm[:, 0, :], op=ALU.mult),
                      r=[r_lnm], w=[r_lnm])
                kb.op("dve", L("tensor_tensor", out=lnm[:, 1, :], in0=PF(bS), in1=lnm[:, 1, :], op=ALU.subtract),
                      r=[r_pb[bS], r_lnm], w=[r_lnm])
                kb.op("act", L("activation", out=lnm[:, 2, :], in_=lnm[:, 1, :], func=AF.Sqrt, bias=epst[:]),
                      r=[r_lnm, r_eps], w=[r_lnm])
                kb.op("dve", L("reciprocal", out=lnm[:, 1, :], in_=lnm[:, 2, :]), r=[r_lnm], w=[r_lnm])
                yield
                for cc in range(8):
                    kb.op("dve", L("tensor_tensor", out=cacc[:, cc, :], in0=cacc[:, cc, :], in1=lnm[:, 0, :],
                                   op=ALU.subtract), r=[r_cacc[cc], r_lnm], w=[r_cacc[cc]])
                    kb.op("dve", L("tensor_tensor", out=cacc[:, cc, :], in0=cacc[:, cc, :], in1=lnm[:, 1, :],
                                   op=ALU.mult), r=[r_cacc[cc], r_lnm], w=[r_cacc[cc]])
                    kb.op("act", L("activation", out=sT[:, cc, :], in_=cacc[:, cc, :], func=AF.Silu,
                                   scale=cw[:, cc, 32:33], bias=cw[:, cc, 33:34]),
                          r=[r_cacc[cc], r_cw], w=[r_sT])
                    yield
                for bl in range(4):
                    m = 4 * gi + bl
                    kb.dma("sp", s_sT[m].rearrange("p (a b) -> p a b", a=8), sT[:, :, bl * 128:(bl + 1) * 128],
                           r=[r_sT])

            def gen_q():
                iters = [("q", qc, bl) for qc in range(4) for bl in range(4)] + \
                        [("qi", c2, bl) for c2 in range(2) for bl in range(4)]
                wsel = {}

                def phase_a(i):
                    kind, c, bl = iters[i]
                    if bl == 0:
                        wsel[(kind, c)] = load_wbuf((C_Q if kind == "q" else C_QI) + c * 512)
                    wi_ = wsel[(kind, c)]
                    bQ = i % 2
                    for kc in range(16):
                        kb.op("pe", L("matmul", out=PF(bQ), lhsT=xT_g[:, kc, bl * 128:(bl + 1) * 128],
                                      rhs=wbuf[wi_][:, kc, :], start=(kc == 0), stop=(kc == 15)),
                              r=[r_xTg[bl], r_wbuf[wi_]], w=[r_pb[bQ]])

                def phase_b(i):
                    kind, c, bl = iters[i]
                    m = 4 * gi + bl
                    p2 = i % 2
                    bQ = p2
                    bT = 2 + p2
                    if kind == "q":
                        kb.op("act", L("activation", out=qsq[:], in_=PF(bQ), func=AF.Square),
                              r=[r_pb[bQ]], w=[r_qsq])
                        kb.op("dve", L("tensor_reduce", out=stq[p2][:, 0:4],
                                       in_=qsq[:].rearrange("p (h d) -> p h d", h=4), axis=AX.X, op=ALU.add),
                              r=[r_qsq], w=[r_stq[p2]])
                        kb.op("act", L("activation", out=stq[p2][:, 4:8], in_=stq[p2][:, 0:4], func=AF.Sqrt,
                                       scale=1.0 / 128, bias=epst[:]), r=[r_stq[p2], r_eps], w=[r_stq[p2]])
                        kb.op("dve", L("reciprocal", out=stq[p2][:, 8:12], in_=stq[p2][:, 4:8]),
                              r=[r_stq[p2]], w=[r_stq[p2]])
                        kb.op("dve", L("tensor_tensor", out=qn[p2][:],
                                       in0=PF(bQ).rearrange("p (h d) -> p h d", h=4),
                                       in1=stq[p2][:, 8:12].unsqueeze(2).broadcast_to([128, 4, 128]), op=ALU.mult),
                              r=[r_pb[bQ], r_stq[p2]], w=[r_qn[p2]])
                        kb.op("dve", L("tensor_tensor", out=qn[p2][:], in0=qn[p2][:],
                                       in1=gqs.unsqueeze(1).broadcast_to([128, 4, 128]), op=ALU.mult),
                              r=[r_qn[p2], r_cst], w=[r_qn[p2]])
                        qbv = qb[p2][:].rearrange("p (h d) -> p h d", h=4)
                        kb.op("act", L("copy", out=qbv[:, :, 32:128], in_=qn[p2][:, :, 32:128]),
                              r=[r_qn[p2]], w=[r_qb[p2]])
                        t4 = [t4q[p2][:, j4, 0:4, :] for j4 in range(4)]
                        rope(qbv, qn[p2][:], 16, cosAo[:, m, :].unsqueeze(1).broadcast_to([128, 4, 16]),
                             sinAo[:, m, :].unsqueeze(1).broadcast_to([128, 4, 16]), t4,
                             r_qn[p2], r_qb[p2], r_ropeAo, r_t4q[p2])
                        dst = s_qT[m].rearrange("p (a b) -> p a b", a=16)[:, 4 * c:4 * c + 4, :]
                    else:
                        kb.op("act", L("copy", out=qn[p2][:].rearrange("p a b -> p (a b)"), in_=PF(bQ)),
                              r=[r_pb[bQ]], w=[r_qn[p2]])
                        pv = qn[p2][:].rearrange("p a b -> p (a b)").rearrange("p (h d) -> p h d", h=8)
                        qbv = qb[p2][:].rearrange("p (h d) -> p h d", h=8)
                        kb.op("act", L("copy", out=qbv[:, :, 16:64], in_=pv[:, :, 16:64]),
                              r=[r_qn[p2]], w=[r_qb[p2]])
                        t4 = [t4q[p2][:, j4, :, 0:8] for j4 in range(4)]
                        rope(qbv, pv, 8, cosIo[:, m, :].unsqueeze(1).broadcast_to([128, 8, 8]),
                             sinIo[:, m, :].unsqueeze(1).broadcast_to([128, 8, 8]), t4,
                             r_qn[p2], r_qb[p2], r_ropeIo, r_t4q[p2])
                        dst = s_qiT[m].rearrange("p (a b) -> p a b", a=8)[:, 4 * c:4 * c + 4, :]
                    yield
                    for h in range(4):
                        kb.op("pe", L("transpose", out=PH(bT, h * 128, (h + 1) * 128),
                                      in_=qb[p2][:, h * 128:(h + 1) * 128], identity=identb[:]),
                              r=[r_qb[p2], r_id], w=[r_pb[bT]])
                    kb.op("act", L("copy", out=qTs[p2][:],
                                   in_=PH(bT, 0, 512).rearrange("p (a b) -> p a b", a=4)),
                          r=[r_pb[bT]], w=[r_qTs[p2]])
                    kb.dma("sp", dst, qTs[p2][:], r=[r_qTs[p2]])

                phase_a(0)
                for i in range(len(iters)):
                    if i + 1 < len(iters):
                        phase_a(i + 1)
                    yield
                    yield from phase_b(i)
                    yield

            gc_, gq_ = gen_conv(), gen_q()
            alive_c, alive_q = True, True
            while alive_c or alive_q:
                if alive_q:
                    try:
                        next(gq_)
                    except StopIteration:
                        alive_q = False
                for _ in range(4):
                    if alive_c:
                        try:
                            next(gc_)
                        except StopIteration:
                            alive_c = False
            if cut <= 5:
                break
            for bl in range(4):
                m = 4 * gi + bl
                bQ = 4 + bl % 2
                for kc in range(16):
                    kb.op("pe", L("matmul", out=PF(bQ, 0, 16), lhsT=xT_g[:, kc, bl * 128:(bl + 1) * 128],
                                  rhs=wwi[:, kc, :], start=(kc == 0), stop=(kc == 15)),
                          r=[r_xTg[bl], r_wwi], w=[r_pb[bQ]])
                kb.op("act", L("copy", out=wis[:, bl, :], in_=PF(bQ, 0, 16)), r=[r_pb[bQ]], w=[r_wis])
                kb.dma("sp", s_wi[m], wis[:, bl, :], r=[r_wis])
        kb.barrier()

    if stage in ("B1", "B1s") or stage.startswith("B1c"):
        kb.finish()
        kb.emit()
        print("instructions:", kb.nins)
        return

    kvs = ExitStack()
    with kvs:
        ksb = mk_sb(kvs)
        KT = ksb("KT", [128, SEQ], BF16)
        Vt = ksb("Vt", [128, NT, 129], BF16)
        KIT = ksb("KIT", [128, SEQ], BF16)
        r_KT = [Reg(f"KT{n}") for n in range(NT)]
        r_V = [Reg(f"V{n}") for n in range(NT)]
        r_KIT = [Reg(f"KIT{n}") for n in range(NT)]
        r_vone = Reg("vone")
        kb.op("pool", L("memset", Vt[:, :, 128:129], 1.0), w=[r_vone])

        ntile_a = NT if stage not in ("smoke", "B1s", "B2s", "B3s", "Ms") else (2 if stage not in ("B2s", "B3s", "Ms") else 16)
        with ExitStack() as la:
            lsb = mk_sb(la)
            kb.dma("sp", gbuf[:], gA_d, w=[r_gbuf])
            posi = lsb("posi", [128, NT], I32)
            posf = lsb("posf", [128, NT])
            r_pos = Reg("pos")
            kb.dma("sp", posi[:], pos_t, w=[r_pos])
            kb.op("dve", L("tensor_copy", out=posf[:], in_=posi[:]), r=[r_pos], w=[r_pos])
            cosA, sinA = lsb("ra_cos", [128, NT, 16]), lsb("ra_sin", [128, NT, 16])
            cosI, sinI = lsb("ri_cos", [128, NT, 8]), lsb("ri_sin", [128, NT, 8])
            with ExitStack() as tmps:
                _, _, r_ropeA = rope_tables(cosA, sinA, "ra", posf[:], NT, invA, 16, r_pos, tmps)
                _, _, r_ropeI = rope_tables(cosI, sinI, "ri", posf[:], NT, invI, 8, r_pos, tmps)
                kb.barrier()
            wkv = lsb("wkv", [128, 16, 320], BF16)
            r_wkv = Reg("wkv")
            for (c0, c1, o0) in ((C_K, C_K + 256, 0), (C_KI, C_KI + 64, 256)):
                kb.dma("pool", wkv[:, :, o0:o0 + (c1 - c0)], w_in_v[:, :, c0:c1], w=[r_wkv])
            xin = [lsb(f"xin{i}", [128, D]) for i in range(2)]
            r_xin = [Reg(f"xin{i}") for i in range(2)]
            junk = lsb("junk", [128, D], BF16)
            r_junk = Reg("junk")
            xs = [lsb(f"xs{i}", [128, D], BF16) for i in range(2)]
            r_xs = [Reg(f"xs{i}") for i in range(2)]
            xT = [lsb(f"xT{i}", [128, 16, 128], BF16) for i in range(2)]
            r_xT = [Reg(f"xT{i}") for i in range(2)]
            st = [lsb(f"st{i}", [128, 8]) for i in range(2)]
            r_st = [Reg(f"st{i}") for i in range(2)]
            kn = [lsb(f"kn{i}", [128, 192]) for i in range(2)]
            kfin = [lsb(f"kfin{i}", [128, 256], BF16) for i in range(2)]
            tmp = [lsb(f"tmp{i}", [128, 4, 16]) for i in range(2)]
            r_kn = [Reg(f"kn{i}") for i in range(2)]
            r_kf = [Reg(f"kf{i}") for i in range(2)]
            r_tmp = [Reg(f"tmp{i}") for i in range(2)]

            def a_s1(n):
                b2 = n % 2
                bT = 2 * b2
                bZ = 4 + b2
                bK = 6 + b2
                kb.dma("sp", xin[b2][:], xb[n * 128:(n + 1) * 128, :], w=[r_xin[b2]])
                kb.op("act", L("activation", out=junk[:], in_=xin[b2][:], func=AF.Square, accum_out=st[b2][:, 0:1]),
                      r=[r_xin[b2]], w=[r_junk, r_st[b2]])
                rstd_from_ss(st[b2], 0, D, r_st[b2])
                kb.op("dve", L("scalar_tensor_tensor", out=xs[b2][:], in0=xin[b2][:], scalar=st[b2][:, 2:3],
                               in1=gbuf[:], op0=ALU.mult, op1=ALU.mult),
                      r=[r_xin[b2], r_st[b2], r_gbuf], w=[r_xs[b2]])
                for kc in range(16):
                    kb.op("pe", L("transpose", out=PH(bT, kc * 128, (kc + 1) * 128),
                                  in_=xs[b2][:, kc * 128:(kc + 1) * 128], identity=identb[:]),
                          r=[r_xs[b2], r_id], w=[r_pb[bT + kc // 8]])
                kb.op("act", L("copy", out=xT[b2][:].rearrange("p a b -> p (a b)"), in_=PH(bT, 0, 2048)),
                      r=[r_pb[bT], r_pb[bT + 1]], w=[r_xT[b2]])

            def a_s2(n):
                b2 = n % 2
                bT = 2 * b2
                bZ = 4 + b2
                bK = 6 + b2
                for kc in range(16):
                    kb.op("pe", L("matmul", out=PF(bZ, 0, 320), lhsT=xT[b2][:, kc, :], rhs=wkv[:, kc, :],
                                  start=(kc == 0), stop=(kc == 15)),
                          r=[r_xT[b2], r_wkv], w=[r_pb[bZ]])
                kb.op("act", L("copy", out=Vt[:, n, 0:128], in_=PF(bZ, 128, 256)), r=[r_pb[bZ]], w=[r_V[n]])
                kb.op("act", L("activation", out=kn[b2][:, 0:128], in_=PF(bZ, 0, 128), func=AF.Square,
                               accum_out=st[b2][:, 3:4]), r=[r_pb[bZ]], w=[r_kn[b2], r_st[b2]])
                rstd_from_ss(st[b2], 3, 128, r_st[b2])
                kb.op("dve", L("scalar_tensor_tensor", out=kn[b2][:, 0:128], in0=PF(bZ, 0, 128),
                               scalar=st[b2][:, 5:6], in1=gk, op0=ALU.mult, op1=ALU.mult),
                      r=[r_pb[bZ], r_st[b2], r_cst], w=[r_kn[b2]])
                kb.op("dve", L("tensor_copy", out=kn[b2][:, 128:192], in_=PF(bZ, 256, 320)),
                      r=[r_pb[bZ]], w=[r_kn[b2]])
                kb.op("dve", L("tensor_copy", out=kfin[b2][:, 32:128], in_=kn[b2][:, 32:128]),
                      r=[r_kn[b2]], w=[r_kf[b2]])
                kb.op("dve", L("tensor_copy", out=kfin[b2][:, 144:192], in_=kn[b2][:, 144:192]),
                      r=[r_kn[b2]], w=[r_kf[b2]])
                t4a = [tmp[b2][:, i, 0:16] for i in range(4)]
                t4i = [tmp[b2][:, i, 0:8] for i in range(4)]
                rope(kfin[b2][:, 0:128], kn[b2][:, 0:128], 16, cosA[:, n, :], sinA[:, n, :], t4a,
                     r_kn[b2], r_kf[b2], r_ropeA, r_tmp[b2])
                rope(kfin[b2][:, 128:192], kn[b2][:, 128:192], 8, cosI[:, n, :], sinI[:, n, :], t4i,
                     r_kn[b2], r_kf[b2], r_ropeI, r_tmp[b2])
                kb.op("dve", L("tensor_copy", out=kfin[b2][:, 192:256], in_=kfin[b2][:, 128:192]),
                      r=[r_kf[b2]], w=[r_kf[b2]])

            def a_s3(n):
                b2 = n % 2
                bT = 2 * b2
                bZ = 4 + b2
                bK = 6 + b2
                kb.op("pe", L("transpose", out=PH(bK, 0, 128), in_=kfin[b2][:, 0:128], identity=identb[:]),
                      r=[r_kf[b2], r_id], w=[r_pb[bK]])
                kb.op("pe", L("transpose", out=PH(bK, 128, 256), in_=kfin[b2][:, 128:256], identity=identb[:]),
                      r=[r_kf[b2], r_id], w=[r_pb[bK]])
                kb.op("act", L("copy", out=KT[:, n * 128:(n + 1) * 128], in_=PH(bK, 0, 128)),
                      r=[r_pb[bK]], w=[r_KT[n]])
                kb.op("act", L("copy", out=KIT[:, n * 128:(n + 1) * 128], in_=PH(bK, 128, 256)),
                      r=[r_pb[bK]], w=[r_KIT[n]])

            for n0 in range(min(2, ntile_a)):
                a_s1(n0)
            if ntile_a > 0:
                a_s2(0)
            for n in range(ntile_a):
                if n + 2 < ntile_a:
                    a_s1(n + 2)
                if n + 1 < ntile_a:
                    a_s2(n + 1)
                a_s3(n)
            kb.barrier()

        if stage in ("smoke", "A"):
            o_kt = dram("o_kt", [128, SEQ], BF16, kind="ExternalOutput")
            o_v = dram("o_v", [128, NT * 129], BF16, kind="ExternalOutput")
            o_kit = dram("o_kit", [128, SEQ], BF16, kind="ExternalOutput")
            regs = r_KT[:ntile_a] + r_V[:ntile_a] + r_KIT[:ntile_a] + [r_vone]
            ro = Reg("out")
            T_ = ntile_a * 128
            kb.dma("sp", o_kt[:, 0:T_], KT[:, 0:T_], r=regs, w=[ro])
            kb.dma("sp", o_v[:, 0:ntile_a * 129], Vt[:, 0:ntile_a, :].rearrange("p a b -> p (a b)"), r=regs, w=[ro])
            kb.dma("sp", o_kit[:, 0:T_], KIT[:, 0:T_], r=regs, w=[ro])
            kb.finish()
            kb.emit()
            print("instructions:", kb.nins)
            return


        blocks_b2 = list(range(NB))
        if stage == "B2s":
            blocks_b2 = [0, 1]
        if stage in ("B3s", "Ms"):
            blocks_b2 = [0, 1, 2, 3]
        with ExitStack() as l2:
            lsb = mk_sb(l2)
            cmask = lsb("cmask_sb", [128, 512])
            r_cmask = Reg("cmask")
            kb.dma("sp", cmask[:], cmask_d, w=[r_cmask])
            sc = lsb("sc", [128, SEQ])
            r_sc = [Reg(f"sc{i}") for i in range(16)]
            selb = lsb("selb", [128, SEQ], BF16)
            r_sel = Reg("sel")
            selT = lsb("selT", [128, NT, 128], BF16)
            r_selT = [Reg(f"selT{i}") for i in range(4)]
            qTb = [lsb(f"qTb{i}", [128, 16, 128], BF16) for i in range(2)]
            r_qTb = [Reg(f"qTb{i}") for i in range(2)]
            qiTb = [lsb(f"qiTb{i}", [128, 8, 128], BF16) for i in range(2)]
            r_qiTb = [Reg(f"qiTb{i}") for i in range(2)]
            wib = [lsb(f"wib{i}", [128, 16]) for i in range(2)]
            r_wib = [Reg(f"wib{i}") for i in range(2)]
            rl = [lsb(f"rl{i}", [128, 512]) for i in range(2)]
            r_rl = [Reg(f"rl{i}") for i in range(2)]
            Eb = [lsb(f"Eb{i}", [128, 512], BF16) for i in range(2)]
            r_Eb = [Reg(f"Eb{i}") for i in range(2)]
            Pb = [lsb(f"Pb{i}", [128, 4, 128], BF16) for i in range(2)]
            r_Pb = [Reg(f"Pb{i}") for i in range(2)]
            bis = lsb("bis", [128, 8])
            r_bis = Reg("bis")
            bisA = lsb("bisA", [128, 2])
            r_mid, r_cnt, r_cnta = Reg("mid"), Reg("cnt"), Reg("cnta")
            r_selD, r_selA = Reg("selD"), Reg("selA")
            rden = lsb("rden", [128, 16])
            r_rden = Reg("rden")
            Ab = lsb("Ab", [128, 2048], BF16)
            r_Ab = Reg("Ab")
            ATb = lsb("ATb", [128, 2048], BF16)
            r_ATb = Reg("ATb")
            import os
            NJUNK = int(os.environ.get('NJUNK', '1'))
            NIDXB = 3 if NJUNK == 0 else 2
            MASK_ENG = os.environ.get('MASK_ENG', 'dve')
            cnt_ib = [0]
            cnt_ia = [0]

            def load_q(bi, m):
                q2 = bi % 2
                kb.dma("sp", qTb[q2][:].rearrange("p a b -> p (a b)"), s_qT[m], w=[r_qTb[q2]])
                kb.dma("sp", qiTb[q2][:].rearrange("p a b -> p (a b)"), s_qiT[m], w=[r_qiTb[q2]])
                kb.dma("sp", wib[q2][:], s_wi[m], w=[r_wib[q2]])

            def gen_indexer(bi, m):
                q2 = bi % 2
                NCH = m + 1
                steps = [(ch, h) for ch in range(NCH) for h in range(16)]
                ib0 = cnt_ib[0]
                cnt_ib[0] += len(steps)

                def pe_part(k):
                    ch, h = steps[k]
                    bank = 5 + (ib0 + k) % NIDXB
                    hf = h % 2
                    kb.op("pe", L("matmul", out=PF(bank), lhsT=qiTb[q2][hf * 64:(hf + 1) * 64, h // 2, :],
                                  rhs=KIT[hf * 64:(hf + 1) * 64, ch * 512:(ch + 1) * 512], start=True, stop=True),
                          r=[r_qiTb[q2]] + r_KIT[4 * ch:4 * ch + 4], w=[r_pb[bank]])

                def rest_part(k):
                    ch, h = steps[k]
                    bank = 5 + (ib0 + k) % NIDXB
                    i2 = (ib0 + k) % 2
                    scc = sc[:, ch * 512:(ch + 1) * 512]
                    kb.op("act", L("activation", out=rl[i2][:], in_=PF(bank), func=AF.Relu),
                          r=[r_pb[bank]], w=[r_rl[i2]])
                    if h == 0:
                        kb.op("dve", L("tensor_scalar", out=scc, in0=rl[i2][:], scalar1=wib[q2][:, 0:1],
                                       scalar2=None, op0=ALU.mult), r=[r_rl[i2], r_wib[q2]], w=[r_sc[ch]])
                    else:
                        kb.op("dve", L("scalar_tensor_tensor", out=scc, in0=rl[i2][:],
                                       scalar=wib[q2][:, h:h + 1], in1=scc, op0=ALU.mult, op1=ALU.add),
                              r=[r_rl[i2], r_wib[q2], r_sc[ch]], w=[r_sc[ch]])

                pe_part(0)
                for k in range(len(steps)):
                    if k + 1 < len(steps):
                        pe_part(k + 1)
                    rest_part(k)
                    yield

            def topk_and_mask(bi, m):
                NCH = m + 1
                S = 512 * NCH
                NKT = 4 * NCH
                rs = r_sc[0:NCH]
                kb.op("dve", L("tensor_reduce", out=bis[:, 0:1], in_=sc[:, 0:S], axis=AX.X, op=ALU.min),
                      r=rs, w=[r_bis])
                lc = sc[:, S - 512:S]
                kb.op("dve", L("tensor_tensor", out=lc, in0=lc, in1=cmask[:], op=ALU.add),
                      r=[r_sc[NCH - 1], r_cmask], w=[r_sc[NCH - 1]])
                kb.op("dve", L("tensor_reduce", out=bis[:, 1:2], in_=sc[:, 0:S], axis=AX.X, op=ALU.max),
                      r=rs, w=[r_bis])
                kb.op("dve", L("tensor_tensor", out=bis[:, 2:3], in0=bis[:, 1:2], in1=bis[:, 0:1], op=ALU.subtract),
                      r=[r_bis], w=[r_bis])
                kb.op("dve", L("tensor_scalar", out=bis[:, 2:3], in0=bis[:, 2:3], scalar1=1.0001, scalar2=1e-6,
                               op0=ALU.mult, op1=ALU.add), r=[r_bis], w=[r_bis])
                kb.op("dve", L("tensor_copy", out=bis[:, 3:4], in_=bis[:, 0:1]), r=[r_bis], w=[r_bis])
                nd = max(1, int(round(0.45 * NCH)))
                Sd = 512 * nd
                n_act = S - Sd
                rsd = r_sc[0:nd]
                rsa = r_sc[nd:NCH]
                for itb in range(1, NBIS + 1):
                    f = 2.0 ** (-itb)
                    kb.op("dve", L("scalar_tensor_tensor", out=bis[:, 4:5], in0=bis[:, 2:3], scalar=f,
                                   in1=bis[:, 3:4], op0=ALU.mult, op1=ALU.add), r=[r_bis], w=[r_mid])
                    if n_act > 0:
                        kb.op("act", L("activation", out=selb[:, Sd:S], in_=sc[:, Sd:S], func=AF.Sign, scale=-1.0,
                                       bias=bis[:, 4:5], accum_out=bisA[:, 0:1]),
                              r=rsa + [r_mid], w=[r_selA, r_cnta])
                    kb.op("dve", L("tensor_scalar", out=selb[:, 0:Sd], in0=sc[:, 0:Sd], scalar1=bis[:, 4:5],
                                   scalar2=0.0, op0=ALU.is_ge, op1=ALU.add, accum_out=bis[:, 5:6]),
                          r=rsd + [r_mid], w=[r_selD, r_cnt])
                    if n_act > 0:
                        kb.op("dve", L("scalar_tensor_tensor", out=bis[:, 5:6], in0=bisA[:, 0:1], scalar=-0.5,
                                       in1=bis[:, 5:6], op0=ALU.mult, op1=ALU.add), r=[r_cnta, r_cnt], w=[r_cnt])
                    kb.op("dve", L("tensor_scalar", out=bis[:, 6:7], in0=bis[:, 5:6],
                                   scalar1=NSEL - 0.5 - 0.5 * n_act, scalar2=f, op0=ALU.is_ge, op1=ALU.mult),
                          r=[r_cnt], w=[r_bis])
                    kb.op("dve", L("scalar_tensor_tensor", out=bis[:, 3:4], in0=bis[:, 6:7], scalar=bis[:, 2:3],
                                   in1=bis[:, 3:4], op0=ALU.mult, op1=ALU.add), r=[r_bis], w=[r_bis])
                kb.op("dve", L("tensor_scalar", out=selb[:, 0:S], in0=sc[:, 0:S], scalar1=bis[:, 3:4],
                               scalar2=None, op0=ALU.is_ge), r=rs + [r_bis], w=[r_selD, r_selA])
                for g in range((NKT + 15) // 16):
                    n_in = min(16, NKT - 16 * g)
                    bT = 2 * (g % 2)
                    for k2 in range(n_in):
                        kt = 16 * g + k2
                        kb.op("pe", L("transpose", out=PH(bT, k2 * 128, (k2 + 1) * 128),
                                      in_=selb[:, kt * 128:(kt + 1) * 128], identity=identb[:]),
                              r=[r_selD, r_selA, r_id], w=[r_pb[bT + k2 // 8]])
                    kb.op("act", L("copy", out=selT[:, 16 * g:16 * g + n_in, :],
                                   in_=PH(bT, 0, n_in * 128).rearrange("p (a b) -> p a b", a=n_in)),
                          r=[r_pb[bT], r_pb[bT + 1]], w=[r_selT[g]])

            def gen_attention(bi, m):
                q2 = bi % 2
                NCH = m + 1
                NKT = 4 * NCH
                its = [(ps_, kt, hgl) for ps_ in range(2) for kt in range(NKT) for hgl in range(2)]
                ia0 = cnt_ia[0]
                cnt_ia[0] += len(its)

                def issue_qk(idx):
                    ps_, kt, hgl = its[idx]
                    hg = 2 * ps_ + hgl
                    lb = (ia0 + idx) % 2
                    kb.op("pe", L("matmul", out=PF(lb), lhsT=KT[:, kt * 128:(kt + 1) * 128],
                                  rhs=qTb[q2][:, 4 * hg:4 * hg + 4, :], start=True, stop=True),
                          r=[r_KT[kt], r_qTb[q2]], w=[r_pb[lb]])

                def issue_mid(idx):
                    ps_, kt, hgl = its[idx]
                    lb = (ia0 + idx) % 2
                    for _ in range(NJUNK):
                        kb.op("pe", L("matmul", out=PF(7), lhsT=identb[:], rhs=KT[:, 0:512], start=True, stop=True))
                    kb.op("act", L("activation", out=Eb[lb][:], in_=PF(lb), func=AF.Exp, scale=ATTN_SCALE),
                          r=[r_pb[lb]], w=[r_Eb[lb]])
                    kb.op(MASK_ENG, L("tensor_tensor", out=Pb[lb][:],
                                      in0=Eb[lb][:].rearrange("p (h t) -> p h t", h=4),
                                      in1=selT[:, kt, :].unsqueeze(1).broadcast_to([128, 4, 128]), op=ALU.mult),
                          r=[r_Eb[lb], r_selT[kt // 16]], w=[r_Pb[lb]])

                def issue_pv(idx):
                    ps_, kt, hgl = its[idx]
                    lb = (ia0 + idx) % 2
                    for h4 in range(4):
                        h8 = 4 * hgl + h4
                        pbk = 2 + h8 // 3
                        o0 = (h8 % 3) * 129
                        kb.op("pe", L("matmul", out=PF(pbk, o0, o0 + 129), lhsT=Pb[lb][:, h4, :],
                                      rhs=Vt[:, kt, :], start=(kt == 0 and h8 % 3 == 0),
                                      stop=(kt == NKT - 1 and (h8 % 3 == 2 or h8 == 7))),
                              r=[r_Pb[lb], r_V[kt], r_vone], w=[r_pb[pbk]])

                def normalise(ps_):
                    for pbk in range(2, 5):
                        h0 = 3 * (pbk - 2)
                        nh = min(3, 8 - h0)
                        hh = 8 * ps_ + h0
                        pv3 = PF(pbk, 0, nh * 129).rearrange("p (h c) -> p h c", c=129)
                        kb.op("dve", L("reciprocal", out=rden[:, hh:hh + nh].unsqueeze(2), in_=pv3[:, :, 128:129]),
                              r=[r_pb[pbk]], w=[r_rden])
                        kb.op("dve", L("tensor_tensor",
                                       out=Ab[:, hh * 128:(hh + nh) * 128].rearrange("p (h d) -> p h d", h=nh),
                                       in0=pv3[:, :, 0:128],
                                       in1=rden[:, hh:hh + nh].unsqueeze(2).broadcast_to([128, nh, 128]), op=ALU.mult),
                              r=[r_pb[pbk], r_rden], w=[r_Ab])

                issue_qk(0)
                for idx in range(len(its)):
                    if idx + 1 < len(its) and its[idx + 1][0] == its[idx][0]:
                        issue_qk(idx + 1)
                    issue_mid(idx)
                    yield "mid"
                    issue_pv(idx)
                    if idx + 1 < len(its) and its[idx + 1][0] != its[idx][0]:
                        normalise(0)
                        issue_qk(idx + 1)
                    yield "end"
                normalise(1)
                for h in range(16):
                    kb.op("pe", L("transpose", out=PH(0, h * 128, (h + 1) * 128), in_=Ab[:, h * 128:(h + 1) * 128],
                                  identity=identb[:]), r=[r_Ab, r_id], w=[r_pb[h // 8]])
                kb.op("act", L("copy", out=ATb[:], in_=PH(0, 0, 2048)), r=[r_pb[0], r_pb[1]], w=[r_ATb])
                kb.dma("sp", s_AT[m], ATb[:], r=[r_ATb])

            def run_interleaved(gens):
                att, idxg = gens[0], (gens[1] if len(gens) > 1 else None)
                while att is not None:
                    try:
                        tag = next(att)
                    except StopIteration:
                        att = None
                        break
                    if tag == "mid" and idxg is not None:
                        try:
                            next(idxg)
                        except StopIteration:
                            idxg = None
                if idxg is not None:
                    for _ in idxg:
                        pass

            nb2 = len(blocks_b2)
            load_q(0, blocks_b2[0])
            run_interleaved([None, gen_indexer(0, blocks_b2[0])])
            topk_and_mask(0, blocks_b2[0])
            for bi, m in enumerate(blocks_b2):
                nxt_g = None
                if bi + 1 < nb2:
                    load_q(bi + 1, blocks_b2[bi + 1])
                    nxt_g = gen_indexer(bi + 1, blocks_b2[bi + 1])
                run_interleaved([gen_attention(bi, m), nxt_g])
                if bi + 1 < nb2:
                    topk_and_mask(bi + 1, blocks_b2[bi + 1])
            kb.barrier()

    if stage in ("B2", "B2s"):
        kb.finish()
        kb.emit()
        print("instructions:", kb.nins)
        return

    comb_all = sb("comb_all", [128, NB, NE])
    r_comb = Reg("comb")
    kb.op("pool", L("memset", comb_all[:], 0.0), w=[r_comb])
    ng3 = 4 if stage not in ("B3s", "Ms") else 1
    with ExitStack() as l3:
        lsb = mk_sb(l3)
        kb.dma("sp", gbuf[:], g2_d, w=[r_gbuf])
        wrf = lsb("wrf", [128, 16, 36])
        r_wrf = Reg("wrf")
        kb.dma("sp", wrf[:], wr_d.rearrange("(kc p) n -> p kc n", p=128), w=[r_wrf])
        xTm = [lsb(f"xTm{i}", [128, 16, 128], BF16) for i in range(4)]
        r_xTm = [Reg(f"xTm{i}") for i in range(4)]
        sTm = [lsb(f"sTm{i}", [128, 8, 128], BF16) for i in range(4)]
        r_sTm = [Reg(f"sTm{i}") for i in range(4)]
        ATm = [lsb(f"ATm{i}", [128, 16, 128], BF16) for i in range(4)]
        r_ATm = [Reg(f"ATm{i}") for i in range(4)]
        xh = [lsb(f"xh{i}", [128, D]) for i in range(4)]
        r_xh = [Reg(f"xh{i}") for i in range(4)]
        wb3 = [lsb(f"wb3{i}", [128, 16, 512], BF16) for i in range(2)]
        r_wb3 = [Reg(f"wb3{i}") for i in range(2)]
        sgc = [lsb(f"sgc{i}", [128, 512], BF16) for i in range(4)]
        r_sgc = [Reg(f"sgc{i}") for i in range(4)]
        sga = [lsb(f"sga{i}", [128, 512], BF16) for i in range(4)]
        r_sga = [Reg(f"sga{i}") for i in range(4)]
        t1 = [lsb(f"t1{i}", [128, 512]) for i in range(4)]
        r_t1 = [Reg(f"t1{i}") for i in range(4)]
        t2 = [lsb(f"t2{i}", [128, 512]) for i in range(2)]
        r_t2 = [Reg(f"t2{i}") for i in range(2)]
        mix = [lsb(f"mix{i}", [128, D], BF16) for i in range(4)]
        r_mix = [Reg(f"mix{i}") for i in range(4)]
        mixT = [lsb(f"mixT{i}", [128, 16, 128], BF16) for i in range(4)]
        r_mixT = [Reg(f"mixT{i}") for i in range(4)]
        hn2f = lsb("hn2f", [128, D])
        r_hn2f = Reg("hn2f")
        hTf = lsb("hTf", [128, 16, 128])
        r_hTf = Reg("hTf")
        hTb = lsb("hTb", [128, 16, 128], BF16)
        r_hTb = Reg("hTb")
        st3 = lsb("st3", [128, 8])
        r_st3 = Reg("st3")
        lg = lsb("lg", [128, 36])
        rt = lsb("rt", [128, 96])
        r_rt = Reg("rt")
        w3n = [0]
        pq = [0]

        def load_w3(src_ap, nk):
            i = w3n[0] % 2
            w3n[0] += 1
            kb.dma("pool", wb3[i][:, 0:nk, :], src_ap, w=[r_wb3[i]])
            return i

        w_pw_v = w_pw.rearrange("(kc p) n -> p kc n", p=128)
        w_ao_v = w_ao.rearrange("(kc p) n -> p kc n", p=128)
        w_o_v = w_o.rearrange("(kc p) n -> p kc n", p=128)

        def proj(bl, lhs_fn, nk, wi_, regs):
            bank = pq[0] % 4
            pq[0] += 1
            for kc in range(nk):
                kb.op("pe", L("matmul", out=PF(bank), lhsT=lhs_fn(kc), rhs=wb3[wi_][:, kc, :],
                              start=(kc == 0), stop=(kc == nk - 1)), r=regs + [r_wb3[wi_]], w=[r_pb[bank]])
            return bank

        import os
        CUT3 = int(os.environ.get("CUT3", "99"))
        for gi in range(ng3):
            for bl in range(4):
                m = 4 * gi + bl
                kb.dma("sp", xTm[bl][:].rearrange("p a b -> p (a b)"), s_xT[m], w=[r_xTm[bl]])
                kb.dma("sp", sTm[bl][:].rearrange("p a b -> p (a b)"), s_sT[m], w=[r_sTm[bl]])
                kb.dma("sp", ATm[bl][:].rearrange("p a b -> p (a b)"), s_AT[m], w=[r_ATm[bl]])
                kb.dma("sp", xh[bl][:], xo[m, 32:160, :], w=[r_xh[bl]])
            for c in range(4):
                cs = slice(c * 512, (c + 1) * 512)
                wi_ = load_w3(w_in_v[:, :, C_GC + c * 512:C_GC + (c + 1) * 512], 16)
                for bl in range(4):
                    bank = proj(bl, lambda kc: xTm[bl][:, kc, :], 16, wi_, [r_xTm[bl]])
                    kb.op("act", L("activation", out=sgc[bl][:], in_=PF(bank), func=AF.Sigmoid),
                          r=[r_pb[bank]], w=[r_sgc[bl]])
                wi_ = load_w3(w_pw_v[:, :, cs], 8)
                for bl in range(4):
                    bank = proj(bl, lambda kc: sTm[bl][:, kc, :], 8, wi_, [r_sTm[bl]])
                    kb.op("dve", L("tensor_tensor", out=t1[bl][:], in0=PF(bank), in1=sgc[bl][:], op=ALU.mult),
                          r=[r_pb[bank], r_sgc[bl]], w=[r_t1[bl]])
                wi_ = load_w3(w_in_v[:, :, C_GA + c * 512:C_GA + (c + 1) * 512], 16)
                for bl in range(4):
                    bank = proj(bl, lambda kc: xTm[bl][:, kc, :], 16, wi_, [r_xTm[bl]])
                    kb.op("act", L("activation", out=sga[bl][:], in_=PF(bank), func=AF.Sigmoid),
                          r=[r_pb[bank]], w=[r_sga[bl]])
                wi_ = load_w3(w_ao_v[:, :, cs], 16)
                for bl in range(4):
                    bank = proj(bl, lambda kc: ATm[bl][:, kc, :], 16, wi_, [r_ATm[bl]])
                    kb.op("dve", L("tensor_tensor", out=t2[bl % 2][:], in0=PF(bank), in1=sga[bl][:], op=ALU.mult),
                          r=[r_pb[bank], r_sga[bl]], w=[r_t2[bl % 2]])
                    kb.op("dve", L("tensor_tensor", out=mix[bl][:, cs], in0=t1[bl][:], in1=t2[bl % 2][:], op=ALU.add),
                          r=[r_t1[bl], r_t2[bl % 2]], w=[r_mix[bl]])
            if CUT3 <= 1:
                break
            for bl in range(4):
                bT = 4 + 2 * (bl % 2)
                for kc in range(16):
                    kb.op("pe", L("transpose", out=PH(bT, kc * 128, (kc + 1) * 128),
                                  in_=mix[bl][:, kc * 128:(kc + 1) * 128], identity=identb[:]),
                          r=[r_mix[bl], r_id], w=[r_pb[bT + kc // 8]])
                kb.op("act", L("copy", out=mixT[bl][:].rearrange("p a b -> p (a b)"), in_=PH(bT, 0, 2048)),
                      r=[r_pb[bT], r_pb[bT + 1]], w=[r_mixT[bl]])
            for c in range(4):
                cs = slice(c * 512, (c + 1) * 512)
                wi_ = load_w3(w_o_v[:, :, cs], 16)
                for bl in range(4):
                    bank = proj(bl, lambda kc: mixT[bl][:, kc, :], 16, wi_, [r_mixT[bl]])
                    kb.op("dve", L("tensor_tensor", out=xh[bl][:, cs], in0=PF(bank), in1=xh[bl][:, cs], op=ALU.add),
                          r=[r_pb[bank], r_xh[bl]], w=[r_xh[bl]])
            if CUT3 <= 2:
                break
            for bl in range(4):
                m = 4 * gi + bl
                kb.dma("sp", out_d[m], xh[bl][:], r=[r_xh[bl]])
                kb.op("act", L("activation", out=hn2f[:], in_=xh[bl][:], func=AF.Square, accum_out=st3[:, 0:1]),
                      r=[r_xh[bl]], w=[r_hn2f, r_st3])
                rstd_from_ss(st3, 0, D, r_st3)
                kb.op("dve", L("scalar_tensor_tensor", out=hn2f[:], in0=xh[bl][:], scalar=st3[:, 2:3], in1=gbuf[:],
                               op0=ALU.mult, op1=ALU.mult), r=[r_xh[bl], r_st3, r_gbuf], w=[r_hn2f])
                if CUT3 <= 3:
                    continue
                for kc in range(16):
                    kb.op("pe", L("matmul", out=PF(4 + kc // 4, (kc % 4) * 128, (kc % 4 + 1) * 128),
                                  lhsT=hn2f[:, kc * 128:(kc + 1) * 128], rhs=identf[:], start=True, stop=True),
                          r=[r_hn2f, r_id], w=[r_pb[4 + kc // 4]])
                SUB3 = int(os.environ.get("SUB3", "9"))
                if SUB3 <= 1:
                    continue
                for q4 in range(4):
                    kb.op("act", L("copy", out=hTb[:, 4 * q4:4 * q4 + 4, :].rearrange("p a b -> p (a b)"),
                                   in_=PF(4 + q4)), r=[r_pb[4 + q4]], w=[r_hTb])
                    if SUB3 <= 2:
                        continue
                    kb.op("dve", L("tensor_copy", out=hTf[:, 4 * q4:4 * q4 + 4, :].rearrange("p a b -> p (a b)"),
                                   in_=PF(4 + q4)), r=[r_pb[4 + q4]], w=[r_hTf])
                if SUB3 <= 3:
                    continue
                kb.dma("sp", s_hT[m], hTb[:].rearrange("p a b -> p (a b)"), r=[r_hTb])
                if CUT3 <= 4:
                    continue
                bank = pq[0] % 4
                pq[0] += 1
                for kc in range(16):
                    kb.op("pe", L("matmul", out=PF(bank, 0, 36), lhsT=hTf[:, kc, :], rhs=wrf[:, kc, :],
                                  start=(kc == 0), stop=(kc == 15)), r=[r_hTf, r_wrf], w=[r_pb[bank]])
                if CUT3 <= 5:
                    continue
                R = [r_rt]
                kb.op("dve", L("tensor_tensor", out=lg[:], in0=PF(bank, 0, 36), in1=brt, op=ALU.add),
                      r=[r_pb[bank], r_cst], w=R)
                gl = lg[:, 0:4]
                el = lg[:, 4:36].rearrange("p (g e) -> p g e", g=4)
                gmax, ngmax, sumg, pg = rt[:, 0:1], rt[:, 1:2], rt[:, 2:3], rt[:, 3:4]
                ohg, eg = rt[:, 4:8], rt[:, 8:12]
                tmp48 = rt[:, 12:44].rearrange("p (g e) -> p g e", g=4)
                e_in, mx8, oh1, oh2 = rt[:, 44:52], rt[:, 52:60], rt[:, 60:68], rt[:, 68:76]
                dd, ed, w1, w2 = rt[:, 76:77], rt[:, 77:78], rt[:, 78:79], rt[:, 79:80]
                wi1, wpg = rt[:, 80:88], rt[:, 88:96]
                kb.op("dve", L("tensor_reduce", out=gmax, in_=gl, axis=AX.X, op=ALU.max), r=R, w=R)
                kb.op("dve", L("tensor_scalar", out=ohg, in0=gl, scalar1=gmax, scalar2=None, op0=ALU.is_equal),
                      r=R, w=R)
                kb.op("dve", L("tensor_scalar", out=ngmax, in0=gmax, scalar1=-1.0, scalar2=None, op0=ALU.mult),
                      r=R, w=R)
                kb.op("act", L("activation", out=eg, in_=gl, func=AF.Exp, bias=ngmax, accum_out=sumg), r=R, w=R)
                kb.op("dve", L("reciprocal", out=pg, in_=sumg), r=R, w=R)
                kb.op("dve", L("tensor_tensor", out=tmp48, in0=el, in1=ohg.unsqueeze(2).broadcast_to([128, 4, 8]),
                               op=ALU.mult), r=R, w=R)
                kb.op("dve", L("tensor_reduce", out=e_in, in_=tmp48.rearrange("p g e -> p e g"), axis=AX.X,
                               op=ALU.add), r=R, w=R)
                if CUT3 <= 6:
                    continue
                kb.op("dve", L("max", out=mx8, in_=e_in), r=R, w=R)
                if CUT3 <= 7:
                    continue
                kb.op("dve", L("tensor_scalar", out=oh1, in0=e_in, scalar1=mx8[:, 0:1], scalar2=None,
                               op0=ALU.is_equal), r=R, w=R)
                kb.op("dve", L("tensor_scalar", out=oh2, in0=e_in, scalar1=mx8[:, 1:2], scalar2=None,
                               op0=ALU.is_equal), r=R, w=R)
                kb.op("dve", L("tensor_tensor", out=dd, in0=mx8[:, 1:2], in1=mx8[:, 0:1], op=ALU.subtract), r=R, w=R)
                kb.op("act", L("activation", out=ed, in_=dd, func=AF.Exp), r=R, w=R)
                kb.op("dve", L("tensor_scalar", out=w1, in0=ed, scalar1=1.0, scalar2=None, op0=ALU.add), r=R, w=R)
                kb.op("dve", L("reciprocal", out=w1, in_=w1), r=R, w=R)
                kb.op("dve", L("tensor_tensor", out=w2, in0=ed, in1=w1, op=ALU.mult), r=R, w=R)
                kb.op("dve", L("tensor_scalar", out=wi1, in0=oh1, scalar1=w1, scalar2=None, op0=ALU.mult), r=R, w=R)
                kb.op("dve", L("scalar_tensor_tensor", out=wi1, in0=oh2, scalar=w2, in1=wi1, op0=ALU.mult,
                               op1=ALU.add), r=R, w=R)
                kb.op("dve", L("tensor_scalar", out=wpg, in0=wi1, scalar1=pg, scalar2=None, op0=ALU.mult), r=R, w=R)
                kb.op("dve", L("tensor_tensor", out=comb_all[:, m, :].rearrange("p (g e) -> p g e", g=4),
                               in0=ohg.unsqueeze(2).broadcast_to([128, 4, 8]),
                               in1=wpg.unsqueeze(1).broadcast_to([128, 4, 8]), op=ALU.mult), r=R, w=[r_comb])
        if stage in ("B3", "B3s"):
            kb.dma("sp", s_comb, comb_all[:].rearrange("p a b -> p (a b)"), r=[r_comb])
        kb.barrier()

    if stage in ("B3", "B3s"):
        kb.finish()
        kb.emit()
        print("instructions:", kb.nins)
        return

    with ExitStack() as l4:
        lsb = mk_sb(l4)
        hT = lsb("hT", [128, 16, 1024], BF16)
        r_hT = [Reg(f"hT{i}") for i in range(8)]
        acc = lsb("acc", [128, 8, D])
        r_acc = [[Reg(f"acc{i}_{k}") for k in range(2)] for i in range(8)]
        wg = [lsb(f"wg{i}", [128, 16, 256], BF16) for i in range(2)]
        r_wg = [Reg(f"wg{i}") for i in range(2)]
        wu_ = [lsb(f"wup{i}", [128, 16, 256], BF16) for i in range(2)]
        r_wu_ = [Reg(f"wup{i}") for i in range(2)]
        wd = [lsb(f"wd{i}", [128, 4, D], BF16) for i in range(2)]
        r_wd = [Reg(f"wd{i}") for i in range(2)]
        actT = lsb("actT", [128, 4, 1024], BF16)
        r_actT = [Reg(f"actT{i}") for i in range(4)]
        sgm = [lsb(f"sgm{i}", [128, 512], BF16) for i in range(2)]
        r_sgm = [Reg(f"sgm{i}") for i in range(2)]
        npass = 2
        nblk = 8
        ne_run = NE
        if stage == "Ms":
            npass, nblk, ne_run = 1, 4, 2
        ntch = nblk // 4
        hw_n = 0
        gq_n = 0
        dq_n = 0
        for p in range(npass):
            for blk in range(nblk):
                m = 8 * p + blk
                kb.dma("sp", hT[:, :, blk * 128:(blk + 1) * 128], s_hT[m].rearrange("p (a b) -> p a b", a=16),
                       w=[r_hT[blk]])
                kb.dma("sp", acc[:, blk, :], out_d[m], w=r_acc[blk])
            for e in range(ne_run):
                d2 = e % 2
                kb.dma("pool", wd[d2][:], w_ed[e].rearrange("(fc p) n -> p fc n", p=128), w=[r_wd[d2]])
                for half in range(2):
                    h2 = hw_n % 2
                    hw_n += 1
                    kb.dma("pool", wg[h2][:], w_eg[e].rearrange("(kc p) f -> p kc f", p=128)[:, :, half * 256:(half + 1) * 256],
                           w=[r_wg[h2]])
                    kb.dma("pool", wu_[h2][:], w_eu[e].rearrange("(kc p) f -> p kc f", p=128)[:, :, half * 256:(half + 1) * 256],
                           w=[r_wu_[h2]])
                    for fcl in range(2):
                        fc = 2 * half + fcl
                        for tch in range(ntch):
                            g2_ = gq_n % 2
                            gq_n += 1
                            bG, bU = g2_, 2 + g2_
                            for kc in range(16):
                                kb.op("pe", L("matmul", out=PF(bG), lhsT=wg[h2][:, kc, fcl * 128:(fcl + 1) * 128],
                                              rhs=hT[:, kc, tch * 512:(tch + 1) * 512], start=(kc == 0), stop=(kc == 15)),
                                      r=[r_wg[h2]] + r_hT[4 * tch:4 * tch + 4], w=[r_pb[bG]])
                            for kc in range(16):
                                kb.op("pe", L("matmul", out=PF(bU), lhsT=wu_[h2][:, kc, fcl * 128:(fcl + 1) * 128],
                                              rhs=hT[:, kc, tch * 512:(tch + 1) * 512], start=(kc == 0), stop=(kc == 15)),
                                      r=[r_wu_[h2]] + r_hT[4 * tch:4 * tch + 4], w=[r_pb[bU]])
                            kb.op("act", L("activation", out=sgm[g2_][:], in_=PF(bG), func=AF.Silu),
                                  r=[r_pb[bG]], w=[r_sgm[g2_]])
                            kb.op("dve", L("tensor_tensor", out=actT[:, fc, tch * 512:(tch + 1) * 512], in0=PF(bU),
                                           in1=sgm[g2_][:], op=ALU.mult), r=[r_pb[bU], r_sgm[g2_]], w=[r_actT[fc]])
                for blk in range(nblk):
                    m = 8 * p + blk
                    for nh in range(2):
                        bD = 4 + 2 * (dq_n % 2)
                        dq_n += 1
                        for sub in range(2):
                            c0 = nh * 1024 + sub * 512
                            for fc in range(4):
                                kb.op("pe", L("matmul", out=PF(bD + sub), lhsT=actT[:, fc, blk * 128:(blk + 1) * 128],
                                              rhs=wd[d2][:, fc, c0:c0 + 512], start=(fc == 0), stop=(fc == 3)),
                                      r=[r_actT[fc], r_wd[d2]], w=[r_pb[bD + sub]])
                        av = acc[:, blk, nh * 1024:(nh + 1) * 1024]
                        kb.op("dve", L("scalar_tensor_tensor", out=av, in0=PF(bD, 0, 1024),
                                       scalar=comb_all[:, m, e:e + 1], in1=av, op0=ALU.mult, op1=ALU.add),
                              r=[r_pb[bD], r_pb[bD + 1], r_comb, r_acc[blk][nh]], w=[r_acc[blk][nh]])
            for blk in range(nblk):
                m = 8 * p + blk
                kb.dma("sp", out_d[m], acc[:, blk, :], r=r_acc[blk])
        kb.barrier()
    kb.finish()
    kb.emit()
    print("instructions:", kb.nins)
    return

    raise NotImplementedError


def host_consts(inputs):
    theta = 500000.0
    invA = theta ** (-np.arange(0, 32, 2, dtype=np.float32) / np.float32(32))
    invI = theta ** (-np.arange(0, 16, 2, dtype=np.float32) / np.float32(16))
    c = np.zeros((128, 512), np.float32)
    c[:, 0:128] = np.asarray(inputs["q_norm_g"]).reshape(1, 128)
    c[:, 128:256] = np.asarray(inputs["k_norm_g"]).reshape(1, 128)
    c[:, 256:272] = invA.astype(np.float32)[None, :]
    c[:, 272:280] = invI.astype(np.float32)[None, :]
    c[:, 288:292] = np.asarray(inputs["b_router_group"]).reshape(1, 4)
    c[:, 292:324] = np.asarray(inputs["b_router_expert"]).reshape(1, 32)
    return c


def make_in_maps(inputs, stage="full"):
    x = np.asarray(inputs["x"])
    pos = np.asarray(inputs["positions"])
    consts = host_consts(inputs)
    ident = np.eye(128, dtype=np.float32)
    w_in = np.ascontiguousarray(np.asarray(inputs["w_in"])[0])
    gA = np.ascontiguousarray(np.broadcast_to(np.asarray(inputs["attn_norm_g"]).reshape(1, D), (128, D)))
    g2 = np.ascontiguousarray(np.broadcast_to(np.asarray(inputs["ffn_norm_g"]).reshape(1, D), (128, D)))
    cv = np.zeros((128, 8, 34), np.float32)
    dw = np.asarray(inputs["conv_dw_w"])[0]
    cv[:, :, 0:31] = dw.T.reshape(8, 128, 31).transpose(1, 0, 2)
    cv[:, :, 31] = np.asarray(inputs["conv_dw_b"])[0].reshape(8, 128).T
    cv[:, :, 32] = np.asarray(inputs["conv_ln_g"])[0].reshape(8, 128).T
    cv[:, :, 33] = np.asarray(inputs["conv_ln_b"])[0].reshape(8, 128).T
    wr = np.ascontiguousarray(np.concatenate([np.asarray(inputs["w_router_group"])[0],
                                              np.asarray(inputs["w_router_expert"])[0]], axis=1))
    shared = {
        "consts": consts, "gA": gA, "g2": g2, "w_in": w_in, "ident": ident,
        "cvec": np.ascontiguousarray(cv.reshape(128, 8 * 34)),
        "w_conv_out": np.ascontiguousarray(np.asarray(inputs["w_conv_out"])[0]),
        "w_attn_o": np.ascontiguousarray(np.asarray(inputs["w_attn_o"])[0]),
        "w_out": np.ascontiguousarray(np.asarray(inputs["w_out"])[0]),
        "wr": wr,
        "w_exp_gate": np.ascontiguousarray(np.asarray(inputs["w_exp_gate"])[0]),
        "w_exp_up": np.ascontiguousarray(np.asarray(inputs["w_exp_up"])[0]),
        "w_exp_down": np.ascontiguousarray(np.asarray(inputs["w_exp_down"])[0]),
    }
    maps = []
    for c in range(8):
        b, j = c // 4, c % 4
        xpad = np.concatenate([np.zeros((32, D), np.float32), x[b]], axis=0)
        xo_ = np.stack([xpad[128 * (4 * m + j):128 * (4 * m + j) + 160] for m in range(NB)])
        pos_o = np.stack([pos[b, 128 * (4 * m + j):128 * (4 * m + j) + 128] for m in range(NB)], axis=1)
        cidx = np.arange(512)[None, :]
        prow = np.arange(128)[:, None]
        cmask = np.where(cidx <= 128 * j + prow, 0.0, NEG).astype(np.float32)
        m_ = dict(shared)
        m_.update({
            "xb": np.ascontiguousarray(x[b]),
            "pos_t": np.ascontiguousarray(pos[b].reshape(NT, 128).T),
            "pos_o": np.ascontiguousarray(pos_o.astype(np.int32)),
            "xo": np.ascontiguousarray(xo_),
            "cmask": cmask,
        })
        maps.append(m_)
    return maps


def kernel(**inputs):
    nc = build_nc("full")
    maps = make_in_maps(inputs)
    res = run_bass_kernel_spmd(nc, maps, core_ids=list(range(8)))
    out = np.zeros((2, SEQ, D), np.float32)
    for c in range(8):
        b, j = c // 4, c % 4
        o = np.asarray(res.results[c]["out"])
        for m in range(NB):
            i = 4 * m + j
            out[b, 128 * i:128 * (i + 1)] = o[m]
    return out
```
